# Optimizing a Trainium2 kernel written in Bass

```python
import jax
import jax.numpy as jnp
from jax import lax
import numpy as np

D_MODEL = 2048
BATCH = 4
SEQ = 4096
DEPTH = 1

GRID_W = 64
CTX_LEN = 256
HEAD_DIM = 128
MIX_WIDTH = D_MODEL
A_HEADS = MIX_WIDTH // (2 * HEAD_DIM)
A_KV_HEADS = A_HEADS // 4
A_GROUP = A_HEADS // A_KV_HEADS
A_WIDTH = A_HEADS * HEAD_DIM
WINDOW = 128
A_BLOCK = 128
ROPE_THETA = 10000.0
B_WIDTH = MIX_WIDTH - A_WIDTH
B_HEADS = 4
B_DV = B_WIDTH // B_HEADS
B_DK = B_DV // 2
B_KEY_WIDTH = B_HEADS * B_DK
GATE_RANK = 16
GATE_TAU = 16.0
GLA_CHUNK = 64
IN_SPLITS = (A_WIDTH, A_KV_HEADS * HEAD_DIM, A_KV_HEADS * HEAD_DIM, B_KEY_WIDTH, B_KEY_WIDTH, B_WIDTH, B_WIDTH, 2 * GATE_RANK)
IN_WIDTH = sum(IN_SPLITS)
N_GROUPS = 4
EXPERTS_PER_GROUP = 8
N_EXPERTS = N_GROUPS * EXPERTS_PER_GROUP
TOP_K = 2
EXPERT_HIDDEN = D_MODEL // 2
MOE_BLOCK = 128
ALPHA = (2.0 * DEPTH) ** 0.25
BETA = (8.0 * DEPTH) ** -0.25
LN_EPS = 1e-6

kernel_name = 'hymba_style_gqa_gla_hmoe_dit_layer'


def layer_norm(x, gain=None, bias=None):
    xf = x.astype(jnp.float32)
    mu = jnp.mean(xf, axis=-1, keepdims=True)
    var = jnp.mean(jnp.square(xf - mu), axis=-1, keepdims=True)
    y = (xf - mu) * lax.rsqrt(var + LN_EPS)
    if gain is not None:
        y = y * gain.astype(jnp.float32) + bias.astype(jnp.float32)
    return y.astype(x.dtype)


def modulate(x, shift, scale):
    return layer_norm(x) * (1 + scale) + shift


def heads(t, n):
    return t.reshape(t.shape[:-1] + (n, t.shape[-1] // n))


def flip(t):
    return t[:, ::-1]


def axial_rope(rows, dtype):
    n_freq = HEAD_DIM // 4
    inv_freq = ROPE_THETA ** (-jnp.arange(n_freq, dtype=jnp.float32) / n_freq)
    row = jnp.repeat(jnp.arange(rows, dtype=jnp.float32), GRID_W)
    col = jnp.tile(jnp.arange(GRID_W, dtype=jnp.float32), rows)
    ang = jnp.stack([row[:, None] * inv_freq, col[:, None] * inv_freq], axis=1)
    return jnp.cos(ang)[:, None].astype(dtype), jnp.sin(ang)[:, None].astype(dtype)


def apply_rope(t, cos, sin):
    tr = t.reshape(t.shape[:-1] + (2, 2, HEAD_DIM // 4))
    t1, t2 = tr[..., 0, :], tr[..., 1, :]
    return jnp.stack([t1 * cos - t2 * sin, t2 * cos + t1 * sin], axis=-2).reshape(t.shape)


def windowed_gqa(q, k, v, k_ctx, v_ctx, sink):
    B, S = q.shape[:2]
    L = k_ctx.shape[1]
    nb = S // A_BLOCK
    scale = HEAD_DIM ** -0.5
    qb = q.reshape(B, nb, A_BLOCK, A_KV_HEADS, A_GROUP, HEAD_DIM)
    pad = ((0, 0), (A_BLOCK, A_BLOCK), (0, 0), (0, 0))
    kp = jnp.pad(k, pad).reshape(B, nb + 2, A_BLOCK, A_KV_HEADS, HEAD_DIM)
    vp = jnp.pad(v, pad).reshape(B, nb + 2, A_BLOCK, A_KV_HEADS, HEAD_DIM)
    band = lambda t: jnp.concatenate([t[:, :-2], t[:, 1:-1], t[:, 2:]], axis=2)
    kb, vb = band(kp), band(vp)
    s_band = jnp.einsum('bnqhgd,bnkhd->bnhgqk', qb, kb).astype(jnp.float32) * scale
    blk = jnp.arange(nb)[:, None] * A_BLOCK
    qpos = blk + jnp.arange(A_BLOCK)[None, :]
    kpos = blk - A_BLOCK + jnp.arange(3 * A_BLOCK)[None, :]
    rel = kpos[:, None, :] - qpos[:, :, None]
    valid = (jnp.abs(rel) <= WINDOW) & (kpos[:, None, :] >= 0) & (kpos[:, None, :] < S)
    s_band = jnp.where(valid[None, :, None, None], s_band, -jnp.inf)
    s_ctx = jnp.einsum('bnqhgd,blhd->bnhgql', qb, k_ctx).astype(jnp.float32) * scale
    sink_l = jnp.broadcast_to(sink.astype(jnp.float32).reshape(A_KV_HEADS, A_GROUP)[None, None, :, :, None, None], (B, nb, A_KV_HEADS, A_GROUP, A_BLOCK, 1))
    p = jax.nn.softmax(jnp.concatenate([sink_l, s_ctx, s_band], axis=-1), axis=-1).astype(v.dtype)
    out = jnp.einsum('bnhgql,blhd->bnqhgd', p[..., 1:1 + L], v_ctx) + jnp.einsum('bnhgqk,bnkhd->bnqhgd', p[..., 1 + L:], vb)
    return out.reshape(B, S, A_WIDTH)


def context_attention(q_ctx, k_ctx, v_ctx, sink):
    B, L = q_ctx.shape[:2]
    qg = q_ctx.reshape(B, L, A_KV_HEADS, A_GROUP, HEAD_DIM)
    s = jnp.einsum('blhgd,bmhd->bhglm', qg, k_ctx).astype(jnp.float32) * HEAD_DIM ** -0.5
    sink_l = jnp.broadcast_to(sink.astype(jnp.float32).reshape(A_KV_HEADS, A_GROUP)[None, :, :, None, None], (B, A_KV_HEADS, A_GROUP, L, 1))
    p = jax.nn.softmax(jnp.concatenate([sink_l, s], axis=-1), axis=-1).astype(v_ctx.dtype)
    out = jnp.einsum('bhglm,bmhd->blhgd', p[..., 1:], v_ctx)
    return out.reshape(B, L, A_WIDTH)


def gla_log_decay(g_low, w_up, b_up):
    z = (g_low @ w_up + b_up).astype(jnp.float32)
    return heads(jax.nn.log_sigmoid(z) / GATE_TAU, B_HEADS)


def gla_chunked(q, k, v, log_a, state0):
    B, N, H, dk = q.shape
    nc = N // GLA_CHUNK
    chunks = lambda t: t.astype(jnp.float32).reshape(B, nc, GLA_CHUNK, H, t.shape[-1]).transpose(0, 1, 3, 2, 4)
    qc, kc, vc, gc = chunks(q) * dk ** -0.5, chunks(k), chunks(v), chunks(log_a)
    b = jnp.cumsum(gc, axis=3)
    b_last = b[:, :, :, -1:]
    b_mid = b[:, :, :, GLA_CHUNK // 2 - 1:GLA_CHUNK // 2]
    qm = qc * jnp.exp(b - b_mid)
    km = kc * jnp.exp(b_mid - b)
    tri = jnp.tril(jnp.ones((GLA_CHUNK, GLA_CHUNK), dtype=bool))
    a_intra = jnp.where(tri, jnp.einsum('bnhtd,bnhsd->bnhts', qm, km), 0.0)
    o_intra = jnp.einsum('bnhts,bnhsv->bnhtv', a_intra, vc)
    kv = jnp.einsum('bnhsd,bnhsv->bnhdv', kc * jnp.exp(b_last - b), vc)
    decay = jnp.exp(b_last[:, :, :, 0])

    def step(s, inp):
        d, kv_c = inp
        return d[..., None] * s + kv_c, s

    s_final, s_prev = lax.scan(step, state0, (decay.transpose(1, 0, 2, 3), kv.transpose(1, 0, 2, 3, 4)))
    s_prev = s_prev.transpose(1, 0, 2, 3, 4)
    o_inter = jnp.einsum('bnhtd,bnhdv->bnhtv', qc * jnp.exp(b), s_prev)
    o = (o_intra + o_inter).transpose(0, 1, 3, 2, 4).reshape(B, N, H, v.shape[-1])
    return o.astype(v.dtype), s_final


def gla_output(o, r, norm_w):
    of = o.astype(jnp.float32)
    of = of * lax.rsqrt(jnp.mean(jnp.square(of), axis=-1, keepdims=True) + LN_EPS)
    of = of.reshape(o.shape[:2] + (B_WIDTH,)) * norm_w.astype(jnp.float32)
    return of.astype(r.dtype) * jax.nn.silu(r)


def mixing_sublayer(h, h_ctx, cos, sin, w_in, w_gate_up, b_gate, sink, norm_w, w_out, need_ctx):
    split_at = [int(s) for s in np.cumsum(IN_SPLITS)[:-1]]
    qa, ka, va, qb, kb, vb, rb, gb = jnp.split(h @ w_in, split_at, axis=-1)
    qa_c, ka_c, va_c, qb_c, kb_c, vb_c, rb_c, gb_c = jnp.split(h_ctx @ w_in, split_at, axis=-1)
    qa = apply_rope(heads(qa, A_HEADS), cos, sin)
    ka = apply_rope(heads(ka, A_KV_HEADS), cos, sin)
    va = heads(va, A_KV_HEADS)
    ka_c, va_c = heads(ka_c, A_KV_HEADS), heads(va_c, A_KV_HEADS)
    out_a = windowed_gqa(qa, ka, va, ka_c, va_c, sink)
    qb, kb, vb = heads(qb, B_HEADS), heads(kb, B_HEADS), heads(vb, B_HEADS)
    qb_c, kb_c, vb_c = heads(qb_c, B_HEADS), heads(kb_c, B_HEADS), heads(vb_c, B_HEADS)
    la_f = gla_log_decay(gb[..., :GATE_RANK], w_gate_up[0], b_gate[0])
    la_b = gla_log_decay(gb[..., GATE_RANK:], w_gate_up[1], b_gate[1])
    lac_f = gla_log_decay(gb_c[..., :GATE_RANK], w_gate_up[0], b_gate[0])
    lac_b = gla_log_decay(gb_c[..., GATE_RANK:], w_gate_up[1], b_gate[1])
    s0 = jnp.zeros((h.shape[0], B_HEADS, B_DK, B_DV), jnp.float32)
    oc_f, sc_f = gla_chunked(qb_c, kb_c, vb_c, lac_f, s0)
    oc_b, sc_b = gla_chunked(flip(qb_c), flip(kb_c), flip(vb_c), flip(lac_b), s0)
    o_f, _ = gla_chunked(qb, kb, vb, la_f, sc_f)
    o_b, _ = gla_chunked(flip(qb), flip(kb), flip(vb), flip(la_b), sc_b)
    out_b = gla_output(o_f + flip(o_b), rb, norm_w)
    y = jnp.concatenate([out_a, out_b], axis=-1) @ w_out
    if not need_ctx:
        return y, None
    out_a_c = context_attention(heads(qa_c, A_HEADS), ka_c, va_c, sink)
    out_b_c = gla_output(oc_f + flip(oc_b), rb_c, norm_w)
    y_ctx = jnp.concatenate([out_a_c, out_b_c], axis=-1) @ w_out
    return y, y_ctx


def hier_moe(h, w_rg, b_rg, w_re, b_re, w_gate, w_up, w_down):
    T, D = h.shape
    pg = jax.nn.softmax((h @ w_rg).astype(jnp.float32) + b_rg.astype(jnp.float32), axis=-1)
    pg_top, grp = lax.top_k(pg, 1)
    el = ((h @ w_re).astype(jnp.float32) + b_re.astype(jnp.float32)).reshape(T, N_GROUPS, EXPERTS_PER_GROUP)
    el = jnp.take_along_axis(el, grp[:, :, None], axis=1)[:, 0]
    top_p, top_i = lax.top_k(jax.nn.softmax(el, axis=-1), TOP_K)
    weights = pg_top * top_p / jnp.sum(top_p, axis=-1, keepdims=True)
    expert_id = (grp * EXPERTS_PER_GROUP + top_i).reshape(-1)
    M = T * TOP_K
    token = jnp.arange(M) // TOP_K
    order = jnp.argsort(expert_id, stable=True)
    e_sorted = expert_id[order]
    counts = jnp.zeros((N_EXPERTS,), jnp.int32).at[expert_id].add(1)
    start = jnp.cumsum(counts) - counts
    padded = (counts + MOE_BLOCK - 1) // MOE_BLOCK * MOE_BLOCK
    pad_end = jnp.cumsum(padded)
    pad_start = pad_end - padded
    dest = pad_start[e_sorted] + jnp.arange(M) - start[e_sorted]
    n_blocks = -(-M // MOE_BLOCK) + N_EXPERTS
    buf = jnp.zeros((n_blocks * MOE_BLOCK, D), h.dtype).at[dest].set(h[token[order]])
    block_e = jnp.minimum(jnp.searchsorted(pad_end, jnp.arange(n_blocks) * MOE_BLOCK, side='right'), N_EXPERTS - 1)

    def expert_block(args):
        xb, e = args
        return (jax.nn.silu(xb @ w_gate[e]) * (xb @ w_up[e])) @ w_down[e]

    out = lax.map(expert_block, (buf.reshape(n_blocks, MOE_BLOCK, D), block_e)).reshape(-1, D)
    w_sorted = weights.reshape(-1)[order].astype(h.dtype)
    return jax.ops.segment_sum(out[dest] * w_sorted[:, None], token[order], num_segments=T)


def setup_inputs(seed: int = 0) -> dict:
    key = jax.random.key(seed)
    ks = jax.random.split(key, 24)
    n = lambda k, shape, s: jax.random.normal(k, shape, jnp.float32) * s
    D = D_MODEL
    return {
        'x': n(ks[0], (BATCH, SEQ, D), 1.0),
        'c': n(ks[1], (BATCH, D), 1.0),
        'ctx': n(ks[2], (BATCH, CTX_LEN, D), 1.0),
        'c_ctx': n(ks[3], (D,), 1.0),
        'w_ada': n(ks[4], (DEPTH, D, 6 * D), D ** -0.5),
        'b_ada': n(ks[5], (DEPTH, 6 * D), 0.02),
        'w_in': n(ks[6], (DEPTH, D, IN_WIDTH), D ** -0.5),
        'w_gate_up': n(ks[7], (DEPTH, 2, GATE_RANK, B_KEY_WIDTH), GATE_RANK ** -0.5),
        'b_gate': n(ks[8], (DEPTH, 2, B_KEY_WIDTH), 0.1),
        'attn_sink': n(ks[9], (DEPTH, A_HEADS), 1.0),
        'gla_norm_w': 1.0 + n(ks[10], (DEPTH, B_WIDTH), 0.02),
        'w_out': n(ks[11], (DEPTH, MIX_WIDTH, D), MIX_WIDTH ** -0.5 * BETA),
        'ln1_g': 1.0 + n(ks[12], (DEPTH, D), 0.02),
        'ln1_b': n(ks[13], (DEPTH, D), 0.02),
        'w_router_group': n(ks[14], (DEPTH, D, N_GROUPS), D ** -0.5),
        'b_router_group': n(ks[15], (DEPTH, N_GROUPS), 0.01),
        'w_router_expert': n(ks[16], (DEPTH, D, N_EXPERTS), D ** -0.5),
        'b_router_expert': n(ks[17], (DEPTH, N_EXPERTS), 0.01),
        'w_exp_gate': n(ks[18], (DEPTH, N_EXPERTS, D, EXPERT_HIDDEN), D ** -0.5),
        'w_exp_up': n(ks[19], (DEPTH, N_EXPERTS, D, EXPERT_HIDDEN), D ** -0.5),
        'w_exp_down': n(ks[20], (DEPTH, N_EXPERTS, EXPERT_HIDDEN, D), EXPERT_HIDDEN ** -0.5 * BETA),
        'ln2_g': 1.0 + n(ks[21], (DEPTH, D), 0.02),
        'ln2_b': n(ks[22], (DEPTH, D), 0.02),
    }


def reference(x, c, ctx, c_ctx, w_ada, b_ada, w_in, w_gate_up, b_gate, attn_sink, gla_norm_w, w_out, ln1_g, ln1_b, w_router_group, b_router_group, w_router_expert, b_router_expert, w_exp_gate, w_exp_up, w_exp_down, ln2_g, ln2_b):
    B, S, D = x.shape
    L = ctx.shape[1]
    rows = S // GRID_W
    cos, sin = axial_rope(rows, x.dtype)
    for layer in range(DEPTH):
        need_ctx = layer + 1 < DEPTH
        mod = jax.nn.silu(c) @ w_ada[layer] + b_ada[layer]
        mod_c = jax.nn.silu(c_ctx) @ w_ada[layer] + b_ada[layer]
        sh1, sc1, g1, sh2, sc2, g2 = jnp.split(mod[:, None, :], 6, axis=-1)
        csh1, csc1, cg1, csh2, csc2, cg2 = jnp.split(mod_c, 6, axis=-1)
        h = modulate(x, sh1, sc1)
        h_c = modulate(ctx, csh1, csc1)
        y, y_c = mixing_sublayer(h, h_c, cos, sin, w_in[layer], w_gate_up[layer], b_gate[layer], attn_sink[layer], gla_norm_w[layer], w_out[layer], need_ctx)
        x = layer_norm(ALPHA * x + g1 * y, ln1_g[layer], ln1_b[layer])
        h = modulate(x, sh2, sc2).reshape(B * S, D)
        moe_args = (w_router_group[layer], b_router_group[layer], w_router_expert[layer], b_router_expert[layer], w_exp_gate[layer], w_exp_up[layer], w_exp_down[layer])
        if need_ctx:
            ctx = layer_norm(ALPHA * ctx + cg1 * y_c, ln1_g[layer], ln1_b[layer])
            h_c = modulate(ctx, csh2, csc2).reshape(B * L, D)
            f = hier_moe(jnp.concatenate([h_c, h], axis=0), *moe_args)
            ctx = layer_norm(ALPHA * ctx + cg2 * f[:B * L].reshape(B, L, D), ln2_g[layer], ln2_b[layer])
            f_x = f[B * L:]
        else:
            f_x = hier_moe(h, *moe_args)
        x = layer_norm(ALPHA * x + g2 * f_x.reshape(B, S, D), ln2_g[layer], ln2_b[layer])
    return x
```

```python
import numpy as np
from contextlib import ExitStack
import concourse.bass as bass
import concourse.mybir as mybir
from concourse.bass_utils import run_bass_kernel_spmd

F32 = mybir.dt.float32
BF16 = mybir.dt.bfloat16
I32 = mybir.dt.int32
AF = mybir.ActivationFunctionType
ALU = mybir.AluOpType

D = 2048
KC = 16
SEQ = 4096
NOWN = 2048
NT = 16
P = 128
LN_EPS = 1e-6
ALPHA = 2.0 ** 0.25
HD = 128
N_EXP = 32
HID = 1024
CAPB = 8
CAPS = CAPB * 128

FM_QA, FM_KA, FM_QB, FM_KB, FM_RB = 0, 8, 10, 14, 18
N_FM = 26
FM_COLS = N_FM * 128
GB_OFF = FM_COLS
TM_OFF = GB_OFF + 64
TM_VA, TM_KB, TM_VB = 0, 256, 768
TM_COLS = 1792
WIN_COLS = TM_OFF + TM_COLS


class Buf:
    __slots__ = ("name", "w", "r")

    def __init__(self, name):
        self.name = name
        self.w = None
        self.r = []


class Sched:
    ENG = ("pe", "act", "dve", "pool", "sp")

    def __init__(self, nc, es):
        self.nc = nc
        self.es = es
        self.eng = {"pe": nc.tensor, "act": nc.scalar, "dve": nc.vector, "pool": nc.gpsimd, "sp": nc.sync}
        self.items = {e: [] for e in self.ENG}
        self.sems = {}
        self.cnt = {}
        self.waited = {e: {} for e in self.ENG}
        for e in self.ENG:
            self._sem("E_" + e)
        self.in_if = None
        self.rt = {}

    def _sem(self, key):
        if key not in self.sems:
            self.sems[key] = self.es.enter_context(self.nc.semaphore("s_" + key))
            self.cnt[key] = 0
        return self.sems[key]

    def _need(self, engine, tok):
        if tok is None:
            return
        key, val = tok
        if self.waited[engine].get(key, 0) >= val:
            return
        self.waited[engine][key] = val
        self.items[engine].append(("wait", key, val))

    def _deps(self, engine, reads, writes):
        for b in reads:
            self._need(engine, b.w)
        for b in writes:
            self._need(engine, b.w)
            for t in b.r:
                self._need(engine, t)

    def _commit(self, tok, reads, writes):
        for b in reads:
            b.r.append(tok)
        for b in writes:
            b.w = tok
            b.r = []

    def op(self, engine, fns, reads=(), writes=()):
        if not isinstance(fns, (list, tuple)):
            fns = [fns]
        self._deps(engine, reads, writes)
        key = "E_" + engine
        self.cnt[key] += 1
        tok = (key, self.cnt[key])
        self.items[engine].append(("ops", list(fns), key, 1))
        if engine == "pe":
            self.waited[engine][key] = tok[1]
        self._commit(tok, reads, writes)
        if self.in_if is not None:
            self.in_if["incs"].setdefault(engine, {}).setdefault(key, 0)
            self.in_if["incs"][engine][key] += 1
        return tok

    def dma(self, engine, fn, key, reads=(), writes=()):
        key = "D_" + key + "_" + engine
        self._sem(key)
        self._deps(engine, reads, writes)
        self.cnt[key] += 16
        tok = (key, self.cnt[key])
        self.items[engine].append(("ops", [fn], key, 16))
        self._commit(tok, reads, writes)
        if self.in_if is not None:
            self.in_if["incs"].setdefault(engine, {}).setdefault(key, 0)
            self.in_if["incs"][engine][key] += 16
        return tok

    IF_ENG = ("pe", "act", "dve")

    def reg_load(self, regname, ap, buf):
        for e in self.IF_ENG:
            self._need(e, buf.w)
            self.items[e].append(("regload", regname, ap))

    def begin_if(self, regname, thr):
        assert self.in_if is None
        self.in_if = {"incs": {}, "start": {e: len(self.items[e]) for e in self.ENG},
                      "waited": {e: dict(self.waited[e]) for e in self.ENG},
                      "cnt0": dict(self.cnt)}
        for e in self.IF_ENG:
            self.items[e].append(("if", regname, thr))

    def end_if(self, dummies):
        st = self.in_if
        self.in_if = None
        for e in self.ENG:
            incs = st["incs"].get(e, {})
            if e not in self.IF_ENG:
                assert not incs, f"engine {e} cannot be used inside a dynamic section"
                del self.items[e][st["start"][e]:]
            else:
                wk = set("E_" + x for x in self.IF_ENG) | set(incs.keys())
                self.items[e].append(("else", dict(incs), dummies[e], {k: st["cnt0"].get(k, 0) for k in wk}))
                self.items[e].append(("endif",))
            self.waited[e] = st["waited"][e]

    def barrier(self):
        for e in self.ENG:
            for key, c in self.cnt.items():
                if c > 0:
                    self._need(e, (key, c))

    def final_wait(self, engine, toks):
        for t in toks:
            self._need(engine, t)

    def emit(self, block):
        nc = self.nc
        deco = {"pe": block.tensor, "act": block.scalar, "dve": block.vector, "pool": block.gpsimd, "sp": block.sync}
        for e in self.ENG:
            items = self.items[e]

            def body(eng, items=items, e=e):
                regs = {}
                stack = []
                with ExitStack() as rs:
                    if e == "pool":
                        self.rt["bnd"] = rs.enter_context(eng.register("bnd_reg"))
                        eng.reg_mov(self.rt["bnd"], N_EXP * CAPS - 1)
                    for it in items:
                        k = it[0]
                        if k == "wait":
                            eng.wait_ge(self.sems[it[1]], it[2])
                        elif k == "ops":
                            ins = None
                            for fn in it[1]:
                                ins = fn(eng)
                            ins.then_inc(self.sems[it[2]], it[3])
                        elif k == "regload":
                            if it[1] not in regs:
                                regs[it[1]] = rs.enter_context(eng.register(it[1] + "_" + e))
                            eng.reg_load(regs[it[1]], it[2])
                        elif k == "if":
                            g = eng.If_cmp(regs[it[1]], it[2], "IS_GT")
                            g.__enter__()
                            stack.append(g)
                        elif k == "else":
                            g = stack.pop()
                            g.__exit__(None, None, None)
                            g2 = eng.Else()
                            g2.__enter__()
                            for key, val in it[3].items():
                                if val > 0:
                                    eng.wait_ge(self.sems[key], val)
                            for kk, (key, inc) in enumerate(it[1].items()):
                                it[2](eng, kk).then_inc(self.sems[key], inc)
                            stack.append(g2)
                        elif k == "endif":
                            g = stack.pop()
                            g.__exit__(None, None, None)
            deco[e](body)


def _rope_tables(half):
    n_freq = HD // 4
    inv_freq = (10000.0 ** (-np.arange(n_freq, dtype=np.float32) / n_freq)).astype(np.float32)
    j = np.arange(NOWN + 128)
    g = j if half == 0 else (SEQ - 1 - j)
    row = (g // 64).astype(np.float32)
    col = (g % 64).astype(np.float32)
    cos = np.zeros((HD, NOWN + 128), np.float32)
    sin = np.zeros((HD, NOWN + 128), np.float32)
    for d in range(HD):
        seg, f = d // 32, d % 32
        ang = (row if seg < 2 else col) * inv_freq[f]
        cos[d] = np.cos(ang.astype(np.float32))
        s = np.sin(ang.astype(np.float32))
        sin[d] = -s if seg in (0, 2) else s
    return cos, sin


def _consts():
    c = {}
    c["ident"] = np.eye(P, dtype=np.float32)
    rm = np.zeros((P, P), np.float32)
    for m in range(P):
        seg = m // 32
        partner = m + 32 if seg in (0, 2) else m - 32
        rm[partner, m] = 1.0
    c["rotm"] = rm
    s = np.arange(P)[:, None]
    t = np.arange(P)[None, :]
    le = (s <= t).astype(np.float32)
    ge = (s >= t).astype(np.float32)
    m1f = -(le - (s <= 63).astype(np.float32)) / 16.0
    m2f = -le / 16.0
    m1r = -(ge - (s >= 64).astype(np.float32)) / 16.0
    m2r = -ge / 16.0
    c["m12f"] = np.concatenate([m1f, m2f], axis=1).astype(np.float32)
    c["m12r"] = np.concatenate([m1r, m2r], axis=1).astype(np.float32)
    c["m3f"] = (-(s > t).astype(np.float32) / 16.0)
    c["m3r"] = (-(s < t).astype(np.float32) / 16.0)
    c["gmaskf"] = np.tile(le, (1, 4))
    c["gmaskr"] = np.tile(ge, (1, 4))
    c["amprev"] = np.tile(ge, (1, 4))
    c["amnext"] = np.tile(le, (1, 4))
    c["ones"] = np.ones((P, P), np.float32)
    c["n16"] = np.full((P, 1), -1.0 / 16.0, np.float32)
    c["lstrict"] = (s < t).astype(np.float32)
    c["slotbase"] = np.tile((np.arange(N_EXP, dtype=np.float32) * CAPS)[None, :], (P, 1))
    c["iotap"] = np.arange(P, dtype=np.float32)[:, None].copy()
    return c


CONST_SHAPES = {"ident": (P, P), "rotm": (P, P), "m12f": (P, 256), "m12r": (P, 256), "m3f": (P, P), "m3r": (P, P),
                "gmaskf": (P, 512), "gmaskr": (P, 512), "amprev": (P, 512), "amnext": (P, 512), "ones": (P, P),
                "n16": (P, 1), "lstrict": (P, P), "slotbase": (P, N_EXP), "iotap": (P, 1)}


class Builder:
    def __init__(self, debug=()):
        self.debug = set(debug)
        self.nc = bass.Bass("TRN2", target_bir_lowering=False)
        self.es = ExitStack()
        self.S = Sched(self.nc, self.es)
        self.bufs = {}
        self.outs = []
        self.final_toks = []
        self.scopes = []
        self.nalloc = 0
        self.alloc_log = []

    def sb(self, name, shape, dt):
        stack = self.scopes[-1] if self.scopes else self.es
        self.nalloc += 1
        nb = int(np.prod(shape[1:])) * (4 if dt in (F32, I32) else 2)
        self.alloc_log.append((name, nb, len(self.scopes)))
        return stack.enter_context(self.nc.sbuf_tensor(f"{name}_{self.nalloc}", list(shape), dt))

    def push_scope(self):
        self.scopes.append(ExitStack())

    def pop_scope(self):
        self.S.barrier()
        self.scopes.pop().close()

    def din(self, name, shape, dt=F32):
        return self.nc.dram_tensor(name, list(shape), dt, kind="ExternalInput").ap()

    def dout(self, name, shape, dt=F32):
        self.outs.append(name)
        return self.nc.dram_tensor(name, list(shape), dt, kind="ExternalOutput").ap()

    def dscr(self, name, shape, dt):
        return self.nc.dram_tensor(name, list(shape), dt, kind="Internal").ap()

    def B(self, name):
        if name not in self.bufs:
            self.bufs[name] = Buf(name)
        return self.bufs[name]

    def dbg_out(self, name, src_ap, shape, dt, reads, eng="sp"):
        if name not in self.debug:
            return
        o = self.dout("dbg_" + name, shape, dt)
        tok = self.S.dma(eng, lambda q, o=o, s=src_ap: q.dma_start(out=o, in_=s), "dbg", reads=reads)
        self.final_toks.append(tok)

    def declare_inputs(self, with_experts=True):
        self.x_own = self.din("x_own", [NOWN, D])
        self.x_oth = self.din("x_oth", [NOWN, D])
        self.ctxl = self.din("ctxl", [256, D])
        self.cc = self.din("cc", [P, 32])
        self.w_ada = self.din("w_ada", [D, 6 * D])
        self.b_ada = self.din("b_ada", [6 * D])
        self.w_in = self.din("w_in", [D, WIN_COLS])
        self.wgu = self.din("wgu", [2, 64, 512])
        self.bgate = self.din("bgate", [2, 512])
        self.sink = self.din("sink", [8])
        self.normw = self.din("normw", [P, 8])
        self.w_out = self.din("w_out", [D, D])
        self.ln1g = self.din("ln1g", [D])
        self.ln1b = self.din("ln1b", [D])
        self.ln2g = self.din("ln2g", [D])
        self.ln2b = self.din("ln2b", [D])
        self.w_r = self.din("w_r", [D, 36])
        self.b_r = self.din("b_r", [36])
        if with_experts:
            self.weg = self.din("weg", [N_EXP, D, HID])
            self.weu = self.din("weu", [N_EXP, D, HID])
            self.wed = self.din("wed", [N_EXP, HID, D])
        self.cosT = self.din("cosT", [HD, NOWN + 128])
        self.sinT = self.din("sinT", [HD, NOWN + 128])
        self.cin = {k: self.din("c_" + k, list(s)) for k, s in CONST_SHAPES.items()}
        self.y = self.dout("y", [NOWN, D])

    def load_consts(self):
        S = self.S
        self.c32 = {}
        self.c16 = {}
        cb = self.B("consts")
        for k in ("m12f", "m12r", "m3f", "m3r", "ones", "n16", "ident", "lstrict", "slotbase", "iotap"):
            t = self.sb("c32_" + k, CONST_SHAPES[k], F32)
            S.dma("sp", lambda q, t=t, k=k: q.dma_start(out=t[:], in_=self.cin[k]), "consts", writes=[cb])
            self.c32[k] = t
        for k in ("ident", "rotm", "gmaskf", "gmaskr", "amprev", "amnext", "ones"):
            t = self.sb("c16_" + k, CONST_SHAPES[k], BF16)
            S.dma("pool", lambda q, t=t, k=k: q.dma_start(out=t[:], in_=self.cin[k]), "consts", writes=[cb])
            self.c16[k] = t
        self.psum = [self.es.enter_context(self.nc.psum_tensor(f"pb{i}", [P, 512], F32)) for i in range(8)]
        self.pbuf = [self.B(f"psum{i}") for i in range(8)]
        self.dummy = self.sb("dummy_t", [P, 8], F32)
        self.eps_t = self.sb("eps_t", [P, 1], F32)
        S.op("dve", lambda v: v.memset(self.eps_t[:], LN_EPS), writes=[cb])
        self.one_t = self.sb("one_t", [P, 1], F32)
        S.op("dve", lambda v: v.memset(self.one_t[:], 1.0), writes=[cb])
        S.op("pool", lambda g: g.memset(self.dummy[:], 0.0), writes=[self.B("dummy")])

    def phase0(self):
        S, nc = self.S, self.nc
        self.mod_d = self.dscr("mod_d", [2, 6 * D], F32)
        b_modd = self.B("mod_d")
        self.vecs = self.sb("vecs", [P, 6, KC], F32)
        b_vecs = self.B("vecs")
        self.push_scope()
        cc_sb = self.sb("cc_sb", [P, 32], F32)
        sc_bf = self.sb("sc_bf", [P, 32], BF16)
        b_cc, b_sc = self.B("cc"), self.B("sc_bf")
        S.dma("sp", lambda q: q.dma_start(out=cc_sb[:], in_=self.cc), "p0a", writes=[b_cc])
        S.op("act", lambda a: a.activation(out=sc_bf[:], in_=cc_sb[:], func=AF.Silu), reads=[b_cc], writes=[b_sc])
        wv = self.w_ada.rearrange("(k p) n -> p k n", p=P)
        NB = 3
        wbufs = [self.sb(f"wada{i}", [P, KC, 1024], BF16) for i in range(NB)]
        wb = [self.B(f"wada{i}") for i in range(NB)]
        bad = [self.sb(f"bada{i}", [2, 1024], F32) for i in range(2)]
        badb = [self.B(f"bada{i}") for i in range(2)]
        mods = [self.sb(f"mods{i}", [2, 1024], F32) for i in range(2)]
        modb = [self.B(f"mods{i}") for i in range(2)]
        for cbk in range(12):
            i = cbk % NB
            j = cbk % 2
            S.dma("pool", lambda q, i=i, cbk=cbk: q.dma_start(out=wbufs[i][:], in_=wv[:, :, cbk * 1024:(cbk + 1) * 1024]),
                  f"wada{i}", writes=[wb[i]])
            S.dma("sp", lambda q, j=j, cbk=cbk: q.dma_start(
                out=bad[j][:], in_=self.b_ada[cbk * 1024:(cbk + 1) * 1024].partition_broadcast(2)),
                f"bada{j}", writes=[badb[j]])
            for n in range(2):
                pi = n
                ps = self.psum[pi]
                S.op("pe", [lambda t, k=k, i=i, n=n, ps=ps: t.matmul(ps[0:2, :], lhsT=sc_bf[:, 2 * k:2 * k + 2],
                                                                    rhs=wbufs[i][:, k, n * 512:(n + 1) * 512],
                                                                    start=(k == 0), stop=(k == KC - 1)) for k in range(KC)],
                     reads=[b_sc, wb[i]], writes=[self.pbuf[pi]])
                S.op("dve", lambda v, ps=ps, n=n, j=j: v.tensor_tensor(out=mods[j][0:2, n * 512:(n + 1) * 512], in0=ps[0:2, :],
                                                                     in1=bad[j][0:2, n * 512:(n + 1) * 512], op=ALU.add),
                     reads=[self.pbuf[pi], badb[j]], writes=[modb[j]])
            S.dma("sp", lambda q, j=j, cbk=cbk: q.dma_start(out=self.mod_d[:, cbk * 1024:(cbk + 1) * 1024], in_=mods[j][:]),
                  "p0b", reads=[modb[j]], writes=[b_modd])
        srcs = [(0, 0), (0, D), (0, 3 * D), (0, 4 * D), (1, 0), (1, D)]
        for i, (r, off) in enumerate(srcs):
            S.dma("sp", lambda q, i=i, r=r, off=off: q.dma_start(
                out=self.vecs[:, i, :], in_=self.mod_d[r, off:off + D].rearrange("(k p) -> p k", p=P),
                allow_slow_non_contiguous=True), "p0c", reads=[b_modd], writes=[b_vecs])
        for i in (1, 3, 5):
            S.op("dve", lambda v, i=i: v.tensor_scalar(out=self.vecs[:, i, :], in0=self.vecs[:, i, :], scalar1=1.0,
                                                       scalar2=None, op0=ALU.add), reads=[b_vecs], writes=[b_vecs])
        self.dbg_out("vecs", self.vecs[:], [P, 6, KC], F32, [b_vecs])
        if "mod" in self.debug:
            o = self.dout("dbg_mod", [2, 6 * D], F32)
            self.final_toks.append(S.dma("sp", lambda q: q.dma_start(out=o, in_=self.mod_d), "dbg", reads=[b_modd]))
        self.pop_scope()

    def mix_setup(self):
        S = self.S
        self.xin = [self.sb(f"xin{i}", [P, D], F32) for i in range(2)]
        self.xinb = [self.B(f"xin{i}") for i in range(2)]
        self.xn = [self.sb(f"xn{i}", [P, D], BF16) for i in range(2)]
        self.xnb = [self.B(f"xn{i}") for i in range(2)]
        self.stats = self.sb("stats", [P, 4, 6], F32)
        self.mv = self.sb("mv", [P, 2], F32)
        self.rstd = self.sb("rstd", [P, 1], F32)
        self.b_stats, self.b_mv, self.b_rstd = self.B("stats"), self.B("mv"), self.B("rstd")
        self.ln_i = 0

    def ln_norm(self, src_ap, out_bf, b_out, src_buf=None, load=True, xin_idx=None):
        S = self.S
        i = self.ln_i % 2 if xin_idx is None else xin_idx
        self.ln_i += 1
        xin, xb = self.xin[i], self.xinb[i]
        if load:
            S.dma("sp", lambda q: q.dma_start(out=xin[:], in_=src_ap), f"xin{i}", writes=[xb])
        S.op("dve", [lambda v, c=c: v.bn_stats(out=self.stats[:, c, :], in_=xin[:, c * 512:(c + 1) * 512]) for c in range(4)],
             reads=[xb], writes=[self.b_stats])
        S.op("dve", lambda v: v.bn_aggr(out=self.mv[:], in_=self.stats[:].rearrange("p a b -> p (a b)")),
             reads=[self.b_stats, xb], writes=[self.b_mv])
        S.op("act", lambda a: a.activation(out=self.rstd[:], in_=self.mv[:, 1:2], func=AF.Ln, bias=self.eps_t[:, 0:1]),
             reads=[self.b_mv], writes=[self.b_rstd])
        S.op("act", lambda a: a.activation(out=self.rstd[:], in_=self.rstd[:], func=AF.Exp, scale=-0.5),
             reads=[self.b_rstd], writes=[self.b_rstd])
        S.op("dve", lambda v: v.tensor_scalar(out=out_bf, in0=xin[:], scalar1=self.mv[:, 0:1], scalar2=self.rstd[:, 0:1],
                                              op0=ALU.subtract, op1=ALU.mult),
             reads=[xb, self.b_mv, self.b_rstd], writes=[b_out])
        return i

    def transpose_mod(self, xn_bf, b_xn, hT, b_hT, vi_shift, vi_scale, banks=(0, 1)):
        S = self.S
        idn = self.c16["ident"]
        for half in range(2):
            pb = self.psum[banks[half]][:].bitcast(BF16)
            S.op("pe", [lambda t, k=k, pb=pb: t.transpose(out=pb[:, (k % 8) * 128:(k % 8 + 1) * 128],
                                                        in_=xn_bf[:, k * 128:(k + 1) * 128], identity=idn[:])
                        for k in range(half * 8, half * 8 + 8)],
                 reads=[b_xn, self.B("consts")], writes=[self.pbuf[banks[half]]])
            S.op("act", [lambda a, k=k, pb=pb: a.activation(out=hT[:, k, :], in_=pb[:, (k % 8) * 128:(k % 8 + 1) * 128],
                                                          func=AF.Identity, scale=self.vecs[:, vi_scale, k:k + 1],
                                                          bias=self.vecs[:, vi_shift, k:k + 1])
                         for k in range(half * 8, half * 8 + 8)],
                 reads=[self.pbuf[banks[half]], self.B("vecs")], writes=[b_hT])

    def chain_setup(self):
        S = self.S
        cb = self.B("consts")
        self.Sst = [self.sb(f"Sst{d}", [P, 1024], F32) for d in range(2)]
        self.Sb = [self.B(f"Sst{d}") for d in range(2)]
        for d in range(2):
            S.op("dve", lambda v, d=d: v.memset(self.Sst[d][:], 0.0), writes=[self.Sb[d]])
        self.wgu_sb = [self.sb(f"wgu{d}", [64, 512], F32) for d in range(2)]
        self.bg_sb = [self.sb(f"bg{d}", [1, 512], F32) for d in range(2)]
        for d in range(2):
            S.dma("sp", lambda q, d=d: q.dma_start(out=self.wgu_sb[d][:], in_=self.wgu[d]), "consts", writes=[cb])
            S.dma("sp", lambda q, d=d: q.dma_start(out=self.bg_sb[d][:], in_=self.bgate[d:d + 1, :]), "consts", writes=[cb])
        self.ktok = [self.sb(f"ktok{i}", [P, 512], BF16) for i in range(2)]
        self.vtok = [self.sb(f"vtok{i}", [P, 1024], BF16) for i in range(2)]
        self.glT = [self.sb(f"glT{i}", [64, P], F32) for i in range(2)]
        self.gp = [[self.sb(f"gp{i}_{d}", [P, 512], F32) for d in range(2)] for i in range(2)]
        self.b_ktok = [self.B(f"ktok{i}") for i in range(2)]
        self.b_vtok = [self.B(f"vtok{i}") for i in range(2)]
        self.b_glT = [self.B(f"glT{i}") for i in range(2)]
        self.b_gp = [[self.B(f"gp{i}_{d}") for d in range(2)] for i in range(2)]
        self.etmp = self.sb("etmp", [P, 512], F32)
        self.b_etmp = self.B("etmp")
        self.e3 = self.sb("e3", [P, 512], F32)
        self.b_e3 = self.B("e3")
        self.k3 = self.sb("k3", [P, 512], BF16)
        self.b_k3 = self.B("k3")
        self.dec = self.sb("dec", [P, 4], F32)
        self.b_dec = self.B("dec")

    def gate_gp(self, i, d, zbank=4):
        S = self.S
        cb = self.B("consts")
        ps = self.psum[zbank]
        S.op("pe", [lambda t: t.matmul(ps[:, :], lhsT=self.glT[i][:, :], rhs=self.wgu_sb[d][:, :], start=True, stop=False),
                    lambda t: t.matmul(ps[:, :], lhsT=self.c32["ones"][0:1, :], rhs=self.bg_sb[d][0:1, :], start=False, stop=True)],
             reads=[self.b_glT[i], cb], writes=[self.pbuf[zbank]])
        S.op("act", lambda a: a.activation(out=self.etmp[:], in_=ps[:, :], func=AF.Exp, scale=-1.0),
             reads=[self.pbuf[zbank]], writes=[self.b_etmp])
        S.op("act", lambda a: a.activation(out=self.gp[i][d][:], in_=self.etmp[:], func=AF.Ln, bias=self.one_t[:, 0:1]),
             reads=[self.b_etmp], writes=[self.b_gp[i][d]])

    def chain_update(self, i, d, banks=(4, 5, 6, 7)):
        S = self.S
        cb = self.B("consts")
        m3 = self.c32["m3f" if d == 0 else "m3r"]
        r3b, totb, kvb0, kvb1 = banks
        ps = self.psum[r3b]
        S.op("pe", lambda t: t.matmul(ps[:, :], lhsT=m3[:, :], rhs=self.gp[i][d][:, :], start=True, stop=True),
             reads=[self.b_gp[i][d], cb], writes=[self.pbuf[r3b]])
        S.op("act", lambda a: a.activation(out=self.e3[:], in_=ps[:, :], func=AF.Exp), reads=[self.pbuf[r3b]], writes=[self.b_e3])
        S.op("dve", lambda v: v.tensor_tensor(out=self.k3[:], in0=self.ktok[i][:], in1=self.e3[:], op=ALU.mult),
             reads=[self.b_ktok[i], self.b_e3], writes=[self.b_k3])
        pt = self.psum[totb]
        S.op("pe", [lambda t, h=h: t.matmul(pt[:, h:h + 1], lhsT=self.gp[i][d][:, h * 128:(h + 1) * 128], rhs=self.c32["n16"][:, 0:1],
                                             start=True, stop=True) for h in range(4)],
             reads=[self.b_gp[i][d], cb], writes=[self.pbuf[totb]])
        S.op("act", lambda a: a.activation(out=self.dec[:], in_=pt[:, 0:4], func=AF.Exp), reads=[self.pbuf[totb]], writes=[self.b_dec])
        for hh in range(2):
            pk = self.psum[(kvb0, kvb1)[hh]]
            S.op("pe", [lambda t, h=h, pk=pk: t.matmul(pk[:, (h % 2) * 256:(h % 2 + 1) * 256], lhsT=self.k3[:, h * 128:(h + 1) * 128],
                                                      rhs=self.vtok[i][:, h * 256:(h + 1) * 256], start=True, stop=True)
                        for h in (2 * hh, 2 * hh + 1)],
                 reads=[self.b_k3, self.b_vtok[i]], writes=[self.pbuf[(kvb0, kvb1)[hh]]])
            for h in (2 * hh, 2 * hh + 1):
                S.op("dve", lambda v, h=h, pk=pk: v.scalar_tensor_tensor(
                    out=self.Sst[d][:, h * 256:(h + 1) * 256], in0=self.Sst[d][:, h * 256:(h + 1) * 256],
                    scalar=self.dec[:, h:h + 1], in1=pk[:, (h % 2) * 256:(h % 2 + 1) * 256], op0=ALU.mult, op1=ALU.add),
                    reads=[self.pbuf[(kvb0, kvb1)[hh]], self.b_dec, self.Sb[d]], writes=[self.Sb[d]])

    def proj_tile_B(self, i, hT, b_hT, want_kv=True, kaT=None, b_kaT=None, va=None, b_va=None, rope_cols=None):
        S = self.S
        wgt, wka, bw = self.wgt, self.wka, self.B("W_B")
        ps = self.psum[2]
        S.op("pe", [lambda t, k=k: t.matmul(ps[0:64, 0:128], lhsT=wgt[:, k, 0:64], rhs=hT[:, k, :], start=(k == 0), stop=(k == KC - 1))
                    for k in range(KC)], reads=[b_hT, bw], writes=[self.pbuf[2]])
        S.op("dve", lambda v: v.tensor_copy(out=self.glT[i][:], in_=ps[0:64, 0:128]), reads=[self.pbuf[2]], writes=[self.b_glT[i]])
        col0 = 64 + 256
        for n in range(3):
            pb_i = 3 if n % 2 == 0 else 2
            ps2 = self.psum[pb_i]
            S.op("pe", [lambda t, k=k, n=n, ps2=ps2: t.matmul(ps2[:, :], lhsT=hT[:, k, :], rhs=wgt[:, k, col0 + n * 512:col0 + (n + 1) * 512],
                                                           start=(k == 0), stop=(k == KC - 1)) for k in range(KC)],
                 reads=[b_hT, bw], writes=[self.pbuf[pb_i]])
            if n == 0:
                S.op("act", lambda a, ps2=ps2: a.copy(out=self.ktok[i][:], in_=ps2[:, :]), reads=[self.pbuf[pb_i]], writes=[self.b_ktok[i]])
            else:
                S.op("act", lambda a, ps2=ps2, n=n: a.copy(out=self.vtok[i][:, (n - 1) * 512:n * 512], in_=ps2[:, :]),
                     reads=[self.pbuf[pb_i]], writes=[self.b_vtok[i]])
        if va is not None:
            ps3 = self.psum[3]
            S.op("pe", [lambda t, k=k: t.matmul(ps3[:, 0:256], lhsT=hT[:, k, :], rhs=wgt[:, k, 64:64 + 256], start=(k == 0), stop=(k == KC - 1))
                        for k in range(KC)], reads=[b_hT, bw], writes=[self.pbuf[3]])
            S.op("act", lambda a: a.copy(out=va, in_=ps3[:, 0:256]), reads=[self.pbuf[3]], writes=[b_va])
        if kaT is not None:
            for blk in range(2):
                ps4 = self.psum[2]
                S.op("pe", [lambda t, k=k, blk=blk: t.matmul(ps4[:, 0:128], lhsT=wka[:, k, blk * 128:(blk + 1) * 128], rhs=hT[:, k, :],
                                                             start=(k == 0), stop=(k == KC - 1)) for k in range(KC)],
                     reads=[b_hT, bw], writes=[self.pbuf[2]])
                if rope_cols is None:
                    S.op("act", lambda a, blk=blk: a.copy(out=kaT[:, blk, :], in_=ps4[:, 0:128]), reads=[self.pbuf[2]], writes=[b_kaT])
                else:
                    self.rope(ps4[:, 0:128], self.pbuf[2], kaT[:, blk, :], b_kaT, rope_cols, 128, rotbank=3)

    def rope(self, src_ps, b_src, out_bf, b_out, col0, n, rotbank):
        S = self.S
        cb = self.B("consts")
        qs, t1, t2 = self.rp_qs, self.rp_t1, self.rp_t2
        S.op("act", lambda a: a.copy(out=qs[:, 0:n], in_=src_ps), reads=[b_src], writes=[self.B("rp_qs")])
        S.op("act", lambda a: a.copy(out=t1[:, 0:n], in_=src_ps), reads=[b_src], writes=[self.B("rp_t1")])
        pr = self.psum[rotbank]
        S.op("pe", lambda t: t.matmul(pr[:, 0:n], lhsT=self.c16["rotm"][:, :], rhs=qs[:, 0:n], start=True, stop=True),
             reads=[self.B("rp_qs"), cb], writes=[self.pbuf[rotbank]])
        S.op("act", lambda a: a.copy(out=t2[:, 0:n], in_=pr[:, 0:n]), reads=[self.pbuf[rotbank]], writes=[self.B("rp_t2")])
        S.op("dve", lambda v: v.tensor_tensor(out=t1[:, 0:n], in0=t1[:, 0:n], in1=self.cos_sb[:, col0:col0 + n], op=ALU.mult),
             reads=[self.B("rp_t1"), cb], writes=[self.B("rp_t1")])
        S.op("dve", lambda v: v.tensor_tensor(out=t2[:, 0:n], in0=t2[:, 0:n], in1=self.sin_sb[:, col0:col0 + n], op=ALU.mult),
             reads=[self.B("rp_t2"), cb], writes=[self.B("rp_t2")])
        S.op("dve", lambda g: g.tensor_tensor(out=out_bf, in0=t1[:, 0:n], in1=t2[:, 0:n], op=ALU.add),
             reads=[self.B("rp_t1"), self.B("rp_t2")], writes=[b_out])

    def phase1(self):
        S = self.S
        cb = self.B("consts")
        self.mix_setup()
        self.chain_setup()
        self.kaT_c = self.sb("kaT_c", [P, 2, 256], BF16)
        self.va_c = self.sb("va_c", [P, 2, 256], BF16)
        self.kaT_h = self.sb("kaT_h", [P, 2, P], BF16)
        self.va_h = self.sb("va_h", [P, 256], BF16)
        self.push_scope()
        self.cos_sb = self.sb("cos_sb", [HD, NOWN + 128], F32)
        self.sin_sb = self.sb("sin_sb", [HD, NOWN + 128], F32)
        S.dma("sp", lambda q: q.dma_start(out=self.cos_sb[:], in_=self.cosT), "consts", writes=[cb])
        S.dma("sp", lambda q: q.dma_start(out=self.sin_sb[:], in_=self.sinT), "consts", writes=[cb])
        self.rp_qs = self.sb("rp_qs", [P, 512], BF16)
        self.rp_t1 = self.sb("rp_t1", [P, 512], F32)
        self.rp_t2 = self.sb("rp_t2", [P, 512], F32)
        self.push_scope()
        wv = self.w_in.rearrange("(k p) n -> p k n", p=P)
        self.wgt = self.sb("wgt", [P, KC, 1856], BF16)
        self.wka = self.sb("wka", [P, KC, 256], BF16)
        bw = self.B("W_B")
        S.dma("pool", lambda q: q.dma_start(out=self.wka[:], in_=wv[:, :, FM_KA * 128:FM_KA * 128 + 256]), "W_B", writes=[bw])
        for c in range(4):
            S.dma("pool", lambda q, c=c: q.dma_start(out=self.wgt[:, :, c * 464:(c + 1) * 464],
                                                      in_=wv[:, :, GB_OFF + c * 464:GB_OFF + (c + 1) * 464]), "W_B", writes=[bw])
        hT = [self.sb(f"hT{i}", [P, KC, P], BF16) for i in range(2)]
        b_hT = [self.B(f"hT{i}") for i in range(2)]
        kaT_tmp = self.sb("kaT_tmp", [P, 2, P], BF16)
        for ti in range(2):
            self.ln_norm(self.ctxl[ti * P:(ti + 1) * P, :], self.xn[ti][:], self.xnb[ti])
            self.transpose_mod(self.xn[ti][:], self.xnb[ti], hT[ti], b_hT[ti], 4, 5)
            self.proj_tile_B(ti, hT[ti], b_hT[ti], kaT=kaT_tmp, b_kaT=self.B("kaT_tmp"),
                             va=self.va_c[:, ti, :], b_va=self.B("va_c"))
            for blk in range(2):
                S.op("pool", lambda g, blk=blk, ti=ti: g.tensor_copy(out=self.kaT_c[:, blk, ti * P:(ti + 1) * P], in_=kaT_tmp[:, blk, :]),
                     reads=[self.B("kaT_tmp")], writes=[self.B("kaT_c")])
            for d in range(2):
                self.gate_gp(ti, d)
        for ti in (0, 1):
            self.chain_update(ti, 0)
        for ti in (1, 0):
            self.chain_update(ti, 1)
        self.dbg_out("S_ctx_F", self.Sst[0][:], [P, 1024], F32, [self.Sb[0]])
        self.dbg_out("S_ctx_R", self.Sst[1][:], [P, 1024], F32, [self.Sb[1]])
        self.dbg_out("kaT_c", self.kaT_c[:], [P, 2, 256], BF16, [self.B("kaT_c")])
        if self.n_other > 0:
            for idx, ti in enumerate(range(NT - 1, NT - 1 - self.n_other, -1)):
                i = idx % 2
                self.ln_norm(self.x_oth[ti * P:(ti + 1) * P, :], self.xn[i][:], self.xnb[i])
                self.transpose_mod(self.xn[i][:], self.xnb[i], hT[i], b_hT[i], 0, 1)
                if ti == 0:
                    self.proj_tile_B(i, hT[i], b_hT[i], kaT=self.kaT_h, b_kaT=self.B("kaT_h"), va=self.va_h[:], b_va=self.B("va_h"),
                                     rope_cols=NOWN)
                else:
                    self.proj_tile_B(i, hT[i], b_hT[i])
                self.gate_gp(i, 1)
                self.chain_update(i, 1)
        self.dbg_out("S_bnd_R", self.Sst[1][:], [P, 1024], F32, [self.Sb[1]])
        self.dbg_out("kaT_h", self.kaT_h[:], [P, 2, P], BF16, [self.B("kaT_h")])
        self.pop_scope()

    def phase2(self):
        S = self.S
        cb = self.B("consts")
        self.pfm_d = self.dscr("pfm_d", [N_FM, P, NOWN], BF16)
        self.pgl_d = self.dscr("pgl_d", [64, NOWN], F32)
        self.ptm_d = self.dscr("ptm_d", [NOWN, TM_COLS], BF16)
        b_pfm, b_pgl, b_ptm = self.B("pfm_d"), self.B("pgl_d"), self.B("ptm_d")
        self.push_scope()
        hT = self.sb("hT_own", [P, KC, NOWN], BF16)
        b_hT = self.B("hT_own")
        for ti in range(NT):
            i = ti % 2
            self.ln_norm(self.x_own[ti * P:(ti + 1) * P, :], self.xn[i][:], self.xnb[i])
            self.transpose_mod(self.xn[i][:], self.xnb[i], hT[:, :, ti * P:(ti + 1) * P], b_hT, 0, 1)
        import os
        stop = os.environ.get("P2_STOP", "")
        if stop == "ln":
            self.dbg_out("hT", hT[:, 0, :], [P, NOWN], BF16, [b_hT])
            self.pop_scope()
            return
        wv = self.w_in.rearrange("(k p) n -> p k n", p=P)
        NB = 2
        wbuf = [self.sb(f"wst{i}", [P, KC, 512], BF16) for i in range(NB)]
        wbb = [self.B(f"wst{i}") for i in range(NB)]
        stg = [self.sb(f"stg{i}", [P, NOWN], BF16) for i in range(2)]
        stgb = [self.B(f"stg{i}") for i in range(2)]
        stg32 = [self.sb(f"stg32_{i}", [64, 512], F32) for i in range(2)]
        stt = [self.sb(f"stt{i}", [P, 512], BF16) for i in range(3)]
        sttb = [self.B(f"stt{i}") for i in range(3)]
        gi = 0
        ev = 0
        pbank = 0
        nstg = 0
        fm_groups = [(g * 512, 512) for g in range(6)] + [(3072, 320)]
        if stop.startswith("fm"):
            fm_groups = fm_groups[int(stop[2:4]):int(stop[4:6])]
        if stop.startswith("tm"):
            fm_groups = []
        for (c0, ncol) in fm_groups:
            w = gi % NB
            gi += 1
            for hh in range(2):
                h0, h1 = hh * (ncol // 2), (hh + 1) * (ncol // 2)
                S.dma("pool", lambda q, w=w, c0=c0, h0=h0, h1=h1: q.dma_start(out=wbuf[w][:, :, h0:h1], in_=wv[:, :, c0 + h0:c0 + h1]),
                      f"wst{w}", writes=[wbb[w]])
            nblk = (ncol + 127) // 128
            for bl in range(nblk):
                blk = c0 // 128 + bl
                M = min(128, ncol - bl * 128)
                is_gb = (M == 64)
                si = nstg % 2
                if not is_gb:
                    nstg += 1
                for ch in range(4):
                    pb_i = pbank % 4
                    pbank += 1
                    ps = self.psum[pb_i]
                    S.op("pe", [lambda t, k=k, w=w, bl=bl, M=M, ch=ch, ps=ps: t.matmul(
                        ps[0:M, :], lhsT=wbuf[w][:, k, bl * 128:bl * 128 + M], rhs=hT[:, k, ch * 512:(ch + 1) * 512],
                        start=(k == 0), stop=(k == KC - 1)) for k in range(KC)],
                        reads=[b_hT, wbb[w]], writes=[self.pbuf[pb_i]])
                    if is_gb:
                        S.op("dve", lambda v, ps=ps, ch=ch: v.tensor_copy(out=stg32[ch % 2][:, :], in_=ps[0:64, :]),
                             reads=[self.pbuf[pb_i]], writes=[self.B(f"stg32_{ch % 2}")])
                        S.dma("sp", lambda q, ch=ch: q.dma_start(out=self.pgl_d[:, ch * 512:(ch + 1) * 512], in_=stg32[ch % 2][:, :]),
                              f"pgl{ch % 2}", reads=[self.B(f"stg32_{ch % 2}")], writes=[b_pgl])
                    elif blk < FM_QB:
                        self.rope(ps[:, :], self.pbuf[pb_i], stg[si][:, ch * 512:(ch + 1) * 512], stgb[si], ch * 512, 512,
                                  rotbank=4 + (ch % 2))
                    else:
                        eng = "act" if ev % 2 == 0 else "dve"
                        ev += 1
                        if eng == "act":
                            S.op("act", lambda a, ps=ps, ch=ch, si=si: a.copy(out=stg[si][:, ch * 512:(ch + 1) * 512], in_=ps[:, :]),
                                 reads=[self.pbuf[pb_i]], writes=[stgb[si]])
                        else:
                            S.op("dve", lambda v, ps=ps, ch=ch, si=si: v.tensor_copy(out=stg[si][:, ch * 512:(ch + 1) * 512], in_=ps[:, :]),
                                 reads=[self.pbuf[pb_i]], writes=[stgb[si]])
                if not is_gb:
                    S.dma("sp", lambda q, blk=blk, si=si: q.dma_start(out=self.pfm_d[blk], in_=stg[si][:]), f"stg{si}",
                          reads=[stgb[si]], writes=[b_pfm])
        tm_groups = [(0, 512), (512, 512), (1024, 512), (1536, 256)]
        if stop.startswith("fm"):
            tm_groups = []
        if stop.startswith("tm"):
            tm_groups = tm_groups[int(stop[2:4]):int(stop[4:6])]
        nt_ = 0
        for (c0, ncol) in tm_groups:
            w = gi % NB
            gi += 1
            for hh in range(2):
                h0, h1 = hh * (ncol // 2), (hh + 1) * (ncol // 2)
                S.dma("pool", lambda q, w=w, c0=c0, h0=h0, h1=h1: q.dma_start(out=wbuf[w][:, :, h0:h1], in_=wv[:, :, TM_OFF + c0 + h0:TM_OFF + c0 + h1]),
                      f"wst{w}", writes=[wbb[w]])
            for ti in range(NT):
                pb_i = pbank % 4
                pbank += 1
                ps = self.psum[pb_i]
                S.op("pe", [lambda t, k=k, w=w, ti=ti, ncol=ncol, ps=ps: t.matmul(
                    ps[:, 0:ncol], lhsT=hT[:, k, ti * P:(ti + 1) * P], rhs=wbuf[w][:, k, 0:ncol],
                    start=(k == 0), stop=(k == KC - 1)) for k in range(KC)],
                    reads=[b_hT, wbb[w]], writes=[self.pbuf[pb_i]])
                j = nt_ % 3
                nt_ += 1
                eng = "act" if ev % 2 == 0 else "dve"
                ev += 1
                if eng == "act":
                    S.op("act", lambda a, ps=ps, j=j, ncol=ncol: a.copy(out=stt[j][:, 0:ncol], in_=ps[:, 0:ncol]),
                         reads=[self.pbuf[pb_i]], writes=[sttb[j]])
                else:
                    S.op("dve", lambda v, ps=ps, j=j, ncol=ncol: v.tensor_copy(out=stt[j][:, 0:ncol], in_=ps[:, 0:ncol]),
                         reads=[self.pbuf[pb_i]], writes=[sttb[j]])
                S.dma("sp", lambda q, ti=ti, c0=c0, ncol=ncol, j=j: q.dma_start(
                    out=self.ptm_d[ti * P:(ti + 1) * P, c0:c0 + ncol], in_=stt[j][:, 0:ncol]), f"stt{j}",
                    reads=[sttb[j]], writes=[b_ptm])
        if "pfm" in self.debug:
            o = self.dout("dbg_pfm", [N_FM, P, NOWN], BF16)
            for blk in range(N_FM):
                self.final_toks.append(S.dma("sp", lambda q, o=o, blk=blk: q.dma_start(out=o[blk], in_=self.pfm_d[blk]), "dbg", reads=[b_pfm]))
        if "pgl" in self.debug:
            o2 = self.dout("dbg_pgl", [64, NOWN], F32)
            self.final_toks.append(S.dma("sp", lambda q: q.dma_start(out=o2, in_=self.pgl_d), "dbg", reads=[b_pgl]))
        if "ptm" in self.debug:
            o3 = self.dout("dbg_ptm", [NOWN, TM_COLS], BF16)
            for ti in range(NT):
                self.final_toks.append(S.dma("sp", lambda q, ti=ti: q.dma_start(out=o3[ti * P:(ti + 1) * P, :], in_=self.ptm_d[ti * P:(ti + 1) * P, :]),
                                             "dbg", reads=[b_ptm]))
        self.pop_scope()
        self.pop_scope()

    def load_chain_tile(self, i, ti):
        S = self.S
        S.dma("sp", lambda q: q.dma_start(out=self.ktok[i][:], in_=self.ptm_d[ti * P:(ti + 1) * P, TM_KB:TM_KB + 512]), f"ktok{i}",
              reads=[self.B("ptm_d")], writes=[self.b_ktok[i]])
        S.dma("sp", lambda q: q.dma_start(out=self.vtok[i][:], in_=self.ptm_d[ti * P:(ti + 1) * P, TM_VB:TM_VB + 1024]), f"vtok{i}",
              reads=[self.B("ptm_d")], writes=[self.b_vtok[i]])
        S.dma("sp", lambda q: q.dma_start(out=self.glT[i][:], in_=self.pgl_d[:, ti * P:(ti + 1) * P]), f"glT{i}",
              reads=[self.B("pgl_d")], writes=[self.b_glT[i]])

    def phase3(self):
        S = self.S
        self.SR_d = self.dscr("SR_d", [NT, P, 1024], BF16)
        b_SR = self.B("SR_d")
        self.push_scope()
        sbf = [self.sb(f"sbf{i}", [P, 1024], BF16) for i in range(2)]
        sbfb = [self.B(f"sbf{i}") for i in range(2)]
        for idx, ti in enumerate(range(NT - 1, -1, -1)):
            i = idx % 2
            self.load_chain_tile(i, ti)
            self.gate_gp(i, 1)
            S.op("act", lambda a, i=i: a.copy(out=sbf[i][:], in_=self.Sst[1][:]), reads=[self.Sb[1]], writes=[sbfb[i]])
            S.dma("sp", lambda q, i=i, ti=ti: q.dma_start(out=self.SR_d[ti], in_=sbf[i][:]), f"sbf{i}", reads=[sbfb[i]], writes=[b_SR])
            self.chain_update(i, 1)
        self.pop_scope()

    def phase4(self):
        S = self.S
        cb = self.B("consts")
        self.cat_d = self.dscr("cat_d", [NT, P, KC * P], BF16)
        b_cat = self.B("cat_d")
        b_pfm, b_ptm = self.B("pfm_d"), self.B("ptm_d")
        self.push_scope()
        SCALE = float(HD) ** -0.5
        kaT = self.sb("kaT_all", [P, 2, NOWN], BF16)
        va = self.sb("va_all", [P, NT, 256], BF16)
        b_ka, b_va = self.B("kaT_all"), self.B("va_all")
        for h in range(2):
            S.dma("sp", lambda q, h=h: q.dma_start(out=kaT[:, h, :], in_=self.pfm_d[FM_KA + h]), "kaT_all", reads=[b_pfm], writes=[b_ka])
        for t4 in range(4):
            S.dma("sp", lambda q, t4=t4: q.dma_start(out=va[:, t4 * 4:(t4 + 1) * 4, :],
                                                    in_=self.ptm_d[t4 * 512:(t4 + 1) * 512, TM_VA:TM_VA + 256].rearrange("(t p) n -> p t n", p=P)),
                  "va_all", reads=[b_ptm], writes=[b_va])
        esink = self.sb("esink", [P, 8], F32)
        b_es = self.B("esink")
        S.dma("sp", lambda q: q.dma_start(out=esink[:], in_=self.sink.partition_broadcast(P)), "esink", writes=[b_es])
        S.op("act", lambda a: a.activation(out=esink[:], in_=esink[:], func=AF.Exp), reads=[b_es], writes=[b_es])
        normw = self.sb("normw_sb", [P, 8], F32)
        S.dma("sp", lambda q: q.dma_start(out=normw[:], in_=self.normw), "normw", writes=[self.B("normw")])
        qaT = self.sb("qaT", [P, 8, P], BF16)
        qbT = self.sb("qbT", [P, 4, P], BF16)
        kbT = self.sb("kbT", [P, 4, P], BF16)
        rbT = self.sb("rbT", [P, 8, P], BF16)
        SRb = self.sb("SRb", [P, 1024], BF16)
        SFb = self.sb("SFb", [P, 1024], BF16)
        pT = [self.sb(f"pT{i}", [P, 512], BF16) for i in range(2)]
        pTb = [self.B(f"pT{i}") for i in range(2)]
        dsb = self.sb("dsb", [P, 512], F32)
        osb = self.sb("osb", [P, 512], F32)
        catT = self.sb("catT", [P, KC, P], BF16)
        b_catT = self.B("catT")
        E1 = self.sb("E1", [P, 4, P], F32)
        E2 = self.sb("E2", [P, 4, P], F32)
        EB = self.sb("EB", [P, 4, P], F32)
        qm = [self.sb(f"qm{d}", [P, 4, P], BF16) for d in range(2)]
        km = [self.sb(f"km{d}", [P, 4, P], BF16) for d in range(2)]
        qe = [self.sb(f"qe{d}", [P, 4, P], BF16) for d in range(2)]
        ATm = [self.sb(f"ATm{d}", [P, 512], BF16) for d in range(2)]
        attmp = self.sb("attmp", [P, 512], F32)
        osq = self.sb("osq", [P, 1024], F32)
        of = self.sb("of", [P, 1024], F32)
        rinv = self.sb("rinv", [P, 512], F32)
        srb = self.sb("srb", [P, 8, P], F32)
        otmp = self.sb("otmp", [P, P], F32)
        Bn = self.B
        nps = 0
        for ti in range(NT):
            c0 = ti * P
            S.dma("sp", lambda q, c0=c0: q.dma_start(out=qaT[:], in_=self.pfm_d[FM_QA:FM_QA + 8, :, c0:c0 + P].rearrange("b p n -> p b n")),
                  "qaT", reads=[b_pfm], writes=[Bn("qaT")])
            S.dma("sp", lambda q, c0=c0: q.dma_start(out=qbT[:], in_=self.pfm_d[FM_QB:FM_QB + 4, :, c0:c0 + P].rearrange("b p n -> p b n")),
                  "qbT", reads=[b_pfm], writes=[Bn("qbT")])
            S.dma("sp", lambda q, c0=c0: q.dma_start(out=kbT[:], in_=self.pfm_d[FM_KB:FM_KB + 4, :, c0:c0 + P].rearrange("b p n -> p b n")),
                  "kbT", reads=[b_pfm], writes=[Bn("kbT")])
            S.dma("sp", lambda q, c0=c0: q.dma_start(out=rbT[:], in_=self.pfm_d[FM_RB:FM_RB + 8, :, c0:c0 + P].rearrange("b p n -> p b n")),
                  "rbT", reads=[b_pfm], writes=[Bn("rbT")])
            S.dma("sp", lambda q, ti=ti: q.dma_start(out=SRb[:], in_=self.SR_d[ti]), "SRb", reads=[Bn("SR_d")], writes=[Bn("SRb")])
            i = ti % 2
            self.load_chain_tile(i, ti)
            for h in range(2):
                blocks = [("c", 0), ("c", 1)]
                if ti > 0:
                    blocks.append(("p", ti - 1))
                blocks.append(("o", ti))
                blocks.append(("n", ti + 1))
                nb = len(blocks)
                for bi, (kind, idx) in enumerate(blocks):
                    if kind == "c":
                        kT, rk = self.kaT_c[:, h, idx * P:(idx + 1) * P], Bn("kaT_c")
                        vv, rv = self.va_c[:, idx, h * P:(h + 1) * P], Bn("va_c")
                    elif kind == "n" and idx == NT:
                        kT, rk = self.kaT_h[:, h, :], Bn("kaT_h")
                        vv, rv = self.va_h[:, h * P:(h + 1) * P], Bn("va_h")
                    else:
                        kT, rk = kaT[:, h, idx * P:(idx + 1) * P], b_ka
                        vv, rv = va[:, idx, h * P:(h + 1) * P], b_va
                    sb_i = nps % 2
                    nps += 1
                    ps = self.psum[sb_i]
                    S.op("pe", lambda t, kT=kT, h=h, ps=ps: t.matmul(ps[:, :], lhsT=kT, rhs=qaT[:, 4 * h:4 * h + 4, :].rearrange("p a b -> p (a b)"),
                                                                     start=True, stop=True),
                         reads=[rk, Bn("qaT")], writes=[self.pbuf[sb_i]])
                    S.op("act", lambda a, ps=ps, sb_i=sb_i: a.activation(out=pT[sb_i][:], in_=ps[:, :], func=AF.Exp, scale=SCALE),
                         reads=[self.pbuf[sb_i]], writes=[pTb[sb_i]])
                    if kind in ("p", "n"):
                        mk = self.c16["amprev" if kind == "p" else "amnext"]
                        S.op("dve", lambda v, sb_i=sb_i, mk=mk: v.tensor_tensor(out=pT[sb_i][:], in0=pT[sb_i][:], in1=mk[:, :], op=ALU.mult),
                             reads=[pTb[sb_i], cb], writes=[pTb[sb_i]])
                    S.op("pe", [lambda t, vv=vv, sb_i=sb_i, bi=bi, nb=nb: t.matmul(self.psum[2][:, :], lhsT=vv, rhs=pT[sb_i][:], start=(bi == 0), stop=(bi == nb - 1)),
                                lambda t, sb_i=sb_i, bi=bi, nb=nb: t.matmul(self.psum[3][:, :], lhsT=self.c16["ones"][:, :], rhs=pT[sb_i][:], start=(bi == 0), stop=(bi == nb - 1))],
                         reads=[rv, pTb[sb_i], cb], writes=[self.pbuf[2], self.pbuf[3]])
                S.op("act", lambda a: a.copy(out=dsb[:], in_=self.psum[3][:, :]), reads=[self.pbuf[3]], writes=[Bn("dsb")])
                S.op("act", lambda a: a.copy(out=osb[:], in_=self.psum[2][:, :]), reads=[self.pbuf[2]], writes=[Bn("osb")])
                S.op("dve", [lambda v, g=g, h=h: v.tensor_scalar(out=dsb[:, g * P:(g + 1) * P], in0=dsb[:, g * P:(g + 1) * P],
                                                                 scalar1=esink[:, 4 * h + g:4 * h + g + 1], scalar2=None, op0=ALU.add)
                             for g in range(4)], reads=[Bn("dsb"), b_es], writes=[Bn("dsb")])
                S.op("dve", lambda v: v.reciprocal(out=dsb[:], in_=dsb[:]), reads=[Bn("dsb")], writes=[Bn("dsb")])
                S.op("dve", lambda v, h=h: v.tensor_tensor(out=catT[:, 4 * h:4 * h + 4, :].rearrange("p a b -> p (a b)"), in0=osb[:], in1=dsb[:], op=ALU.mult),
                     reads=[Bn("osb"), Bn("dsb")], writes=[b_catT])
            for d in range(2):
                self.gate_gp(i, d)
            S.op("act", lambda a: a.copy(out=SFb[:], in_=self.Sst[0][:]), reads=[self.Sb[0]], writes=[Bn("SFb")])
            for d in range(2):
                m12 = self.c32["m12f" if d == 0 else "m12r"]
                for hb in range(2):
                    S.op("pe", [lambda t, h=h, hb=hb, d=d, m12=m12, i=i: t.matmul(self.psum[6 + hb][:, (h % 2) * 256:(h % 2 + 1) * 256],
                                                                           lhsT=self.gp[i][d][:, h * P:(h + 1) * P], rhs=m12[:, :], start=True, stop=True)
                                for h in (2 * hb, 2 * hb + 1)], reads=[self.b_gp[i][d], cb], writes=[self.pbuf[6 + hb]])
                    src = self.psum[6 + hb][:, :].rearrange("p (h c) -> p h c", h=2)
                    S.op("act", [lambda a, hb=hb, src=src: a.activation(out=E1[:, 2 * hb:2 * hb + 2, :], in_=src[:, :, 0:P], func=AF.Exp),
                                 lambda a, hb=hb, src=src: a.activation(out=E2[:, 2 * hb:2 * hb + 2, :], in_=src[:, :, 0:P], func=AF.Exp, scale=-1.0),
                                 lambda a, hb=hb, src=src: a.activation(out=EB[:, 2 * hb:2 * hb + 2, :], in_=src[:, :, P:2 * P], func=AF.Exp)],
                         reads=[self.pbuf[6 + hb]], writes=[Bn("E123")])
                fl = lambda t_: t_[:].rearrange("p a b -> p (a b)")
                S.op("dve", lambda v, d=d: v.scalar_tensor_tensor(out=fl(qm[d]), in0=fl(qbT), scalar=SCALE, in1=fl(E1), op0=ALU.mult, op1=ALU.mult),
                     reads=[Bn("qbT"), Bn("E123")], writes=[Bn(f"qm{d}")])
                S.op("dve", lambda v, d=d: v.tensor_tensor(out=fl(km[d]), in0=fl(kbT), in1=fl(E2), op=ALU.mult),
                     reads=[Bn("kbT"), Bn("E123")], writes=[Bn(f"km{d}")])
                S.op("dve", lambda v, d=d: v.scalar_tensor_tensor(out=fl(qe[d]), in0=fl(qbT), scalar=SCALE, in1=fl(EB), op0=ALU.mult, op1=ALU.mult),
                     reads=[Bn("qbT"), Bn("E123")], writes=[Bn(f"qe{d}")])
                S.op("pe", [lambda t, h=h, d=d: t.matmul(self.psum[4][:, h * P:(h + 1) * P], lhsT=km[d][:, h, :], rhs=qm[d][:, h, :], start=True, stop=True)
                            for h in range(4)], reads=[Bn(f"qm{d}"), Bn(f"km{d}")], writes=[self.pbuf[4]])
                S.op("act", lambda a: a.copy(out=attmp[:], in_=self.psum[4][:, :]), reads=[self.pbuf[4]], writes=[Bn("attmp")])
                gm = self.c16["gmaskf" if d == 0 else "gmaskr"]
                S.op("dve", lambda v, d=d, gm=gm: v.tensor_tensor(out=ATm[d][:], in0=attmp[:], in1=gm[:, :], op=ALU.mult),
                     reads=[Bn("attmp"), cb], writes=[Bn(f"ATm{d}")])
            for hb in range(2):
                fns = []
                for r in range(4 * hb, 4 * hb + 4):
                    h, c = r // 2, r % 2
                    vs = slice(h * 256 + c * P, h * 256 + (c + 1) * P)
                    dst = self.psum[6 + hb][:, (r % 4) * P:(r % 4 + 1) * P]
                    fns += [lambda t, vs=vs, dst=dst, h=h, i=i: t.matmul(dst, lhsT=self.vtok[i][:, vs], rhs=ATm[0][:, h * P:(h + 1) * P], start=True, stop=False),
                            lambda t, vs=vs, dst=dst, h=h, i=i: t.matmul(dst, lhsT=self.vtok[i][:, vs], rhs=ATm[1][:, h * P:(h + 1) * P], start=False, stop=False),
                            lambda t, vs=vs, dst=dst, h=h: t.matmul(dst, lhsT=SFb[:, vs], rhs=qe[0][:, h, :], start=False, stop=False),
                            lambda t, vs=vs, dst=dst, h=h: t.matmul(dst, lhsT=SRb[:, vs], rhs=qe[1][:, h, :], start=False, stop=True)]
                S.op("pe", fns, reads=[self.b_vtok[i], Bn("ATm0"), Bn("ATm1"), Bn("SFb"), Bn("SRb"), Bn("qe0"), Bn("qe1")], writes=[self.pbuf[6 + hb]])
                S.op("act", [lambda a, hb=hb: a.activation(out=osq[:, hb * 512:(hb + 1) * 512], in_=self.psum[6 + hb][:, :], func=AF.Square),
                             lambda a, hb=hb: a.copy(out=of[:, hb * 512:(hb + 1) * 512], in_=self.psum[6 + hb][:, :])],
                     reads=[self.pbuf[6 + hb]], writes=[Bn("osq_of")])
            S.op("pe", [lambda t, h=h, c=c: t.matmul(self.psum[5][:, h * P:(h + 1) * P], lhsT=self.c32["ones"][:, :],
                                                   rhs=osq[:, (2 * h + c) * P:(2 * h + c + 1) * P], start=(c == 0), stop=(c == 1))
                        for h in range(4) for c in range(2)], reads=[Bn("osq_of"), cb], writes=[self.pbuf[5]])
            S.op("act", lambda a: a.activation(out=rinv[:], in_=self.psum[5][:, :], func=AF.Ln, scale=1.0 / 256.0, bias=self.eps_t[:, 0:1]),
                 reads=[self.pbuf[5], cb], writes=[Bn("rinv")])
            S.op("act", lambda a: a.activation(out=rinv[:], in_=rinv[:], func=AF.Exp, scale=-0.5), reads=[Bn("rinv")], writes=[Bn("rinv")])
            S.op("act", lambda a: a.activation(out=srb[:].rearrange("p a b -> p (a b)"), in_=rbT[:].rearrange("p a b -> p (a b)"), func=AF.Silu),
                 reads=[Bn("rbT")], writes=[Bn("srb")])
            for r in range(8):
                h = r // 2
                S.op("dve", lambda v, r=r, h=h: v.tensor_tensor(out=otmp[:], in0=of[:, r * P:(r + 1) * P], in1=rinv[:, h * P:(h + 1) * P], op=ALU.mult),
                     reads=[Bn("osq_of"), Bn("rinv")], writes=[Bn("otmp")])
                S.op("dve", lambda v, r=r: v.scalar_tensor_tensor(out=catT[:, 8 + r, :], in0=otmp[:], scalar=normw[:, r:r + 1], in1=srb[:, r, :],
                                                                  op0=ALU.mult, op1=ALU.mult),
                     reads=[Bn("otmp"), Bn("srb"), Bn("normw")], writes=[b_catT])
            self.chain_update(i, 0)
            S.dma("sp", lambda q, ti=ti: q.dma_start(out=self.cat_d[ti], in_=catT[:].rearrange("p a b -> p (a b)")), "catT",
                  reads=[b_catT], writes=[b_cat])
        if "cat" in self.debug:
            o = self.dout("dbg_cat", [NT, P, KC * P], BF16)
            for ti in range(NT):
                self.final_toks.append(S.dma("sp", lambda q, ti=ti: q.dma_start(out=o[ti], in_=self.cat_d[ti]), "dbg", reads=[b_cat]))
        self.pop_scope()

    def ln_stats(self, src, b_src):
        S = self.S
        S.op("dve", [lambda v, c=c: v.bn_stats(out=self.stats[:, c, :], in_=src[:, c * 512:(c + 1) * 512]) for c in range(4)],
             reads=[b_src], writes=[self.b_stats])
        S.op("dve", lambda v: v.bn_aggr(out=self.mv[:], in_=self.stats[:].rearrange("p a b -> p (a b)")),
             reads=[self.b_stats], writes=[self.b_mv])
        S.op("act", lambda a: a.activation(out=self.rstd[:], in_=self.mv[:, 1:2], func=AF.Ln, bias=self.eps_t[:, 0:1]),
             reads=[self.b_mv], writes=[self.b_rstd])
        S.op("act", lambda a: a.activation(out=self.rstd[:], in_=self.rstd[:], func=AF.Exp, scale=-0.5),
             reads=[self.b_rstd], writes=[self.b_rstd])

    def phase5(self):
        S = self.S
        cb = self.B("consts")
        Bn = self.B
        self.x1_d = self.dscr("x1_d", [NOWN, D], F32)
        self.xbuf_d = self.dscr("xbuf_d", [N_EXP * CAPS, D], BF16)
        b_x1, b_xbuf = Bn("x1_d"), Bn("xbuf_d")
        self.idx_all = self.sb("idx_all", [P, NT, 2], I32)
        self.w_all = self.sb("w_all", [P, NT, 2], F32)
        self.cnt_bc = self.sb("cnt_bc", [P, N_EXP], F32)
        self.cnt_i = self.sb("cnt_i", [1, N_EXP], I32)
        b_idx, b_w, b_cnt = Bn("idx_all"), Bn("w_all"), Bn("cnt_bc")
        S.op("dve", lambda v: v.memset(self.cnt_bc[:], 0.0), writes=[b_cnt])
        self.push_scope()
        wout = self.sb("wout", [P, KC, D], BF16)
        b_wout = Bn("wout")
        wov = self.w_out.rearrange("(k p) n -> p k n", p=P)
        for c in range(8):
            S.dma("pool", lambda q, c=c: q.dma_start(out=wout[:, :, c * 256:(c + 1) * 256], in_=wov[:, :, c * 256:(c + 1) * 256]),
                  "wout", writes=[b_wout])
        wr = self.sb("wr", [P, KC, 36], BF16)
        S.dma("pool", lambda q: q.dma_start(out=wr[:], in_=self.w_r.rearrange("(k p) n -> p k n", p=P)), "wr", writes=[Bn("wr")])
        brb = self.sb("brb", [P, 36], F32)
        S.dma("sp", lambda q: q.dma_start(out=brb[:], in_=self.b_r.partition_broadcast(P)), "brb", writes=[Bn("brb")])
        bc = [self.sb(f"bc{i}", [P, D], F32) for i in range(3)]
        bcb = [Bn(f"bc{i}") for i in range(3)]
        S.dma("sp", lambda q: q.dma_start(out=bc[0][:], in_=self.mod_d[0, 2 * D:3 * D].partition_broadcast(P)), "bc0",
              reads=[Bn("mod_d")], writes=[bcb[0]])
        S.dma("sp", lambda q: q.dma_start(out=bc[1][:], in_=self.ln1g.partition_broadcast(P)), "bc1", writes=[bcb[1]])
        S.dma("sp", lambda q: q.dma_start(out=bc[2][:], in_=self.ln1b.partition_broadcast(P)), "bc2", writes=[bcb[2]])
        catT = [self.sb(f"catT{i}", [P, KC, P], BF16) for i in range(2)]
        catb = [Bn(f"catTb{i}") for i in range(2)]
        ysb = self.sb("ysb", [P, D], F32)
        b_ysb = Bn("ysb")
        h2T = self.sb("h2T", [P, KC, P], BF16)
        b_h2T = Bn("h2T")
        lg = self.sb("lg", [P, 36], F32)
        sm = self.sb("rsm", [P, 16], F32)
        gm4 = self.sb("gm4", [P, 4], F32)
        pen = self.sb("pen", [P, 4], F32)
        ge4 = self.sb("ge4", [P, 4], F32)
        elm = self.sb("elm", [P, N_EXP], F32)
        m8 = self.sb("m8", [P, 8], F32)
        A1 = self.sb("A1", [P, N_EXP], F32)
        A2 = self.sb("A2", [P, N_EXP], F32)
        A12 = self.sb("A12", [P, N_EXP], F32)
        posc = self.sb("posc", [P, 64], F32)
        tpos = self.sb("tpos", [P, N_EXP], F32)
        tsl = self.sb("tsl", [P, N_EXP], F32)
        tov = self.sb("tov", [P, N_EXP], F32)
        idxf = self.sb("idxf", [P, 2], F32)
        b_r_ = Bn("route_tmp")
        AX = mybir.AxisListType.X
        for ti in range(NT):
            i = ti % 2
            xin, xb = self.xin[i], self.xinb[i]
            S.dma("sp", lambda q, i=i, ti=ti: q.dma_start(out=catT[i][:].rearrange("p a b -> p (a b)"), in_=self.cat_d[ti]), f"catT{i}",
                  reads=[Bn("cat_d")], writes=[catb[i]])
            S.dma("sp", lambda q, xin=xin, ti=ti: q.dma_start(out=xin[:], in_=self.x_own[ti * P:(ti + 1) * P, :]), f"xin{i}", writes=[xb])
            for n in range(4):
                S.op("pe", [lambda t, k=k, n=n, i=i: t.matmul(self.psum[n][:, :], lhsT=catT[i][:, k, :], rhs=wout[:, k, n * 512:(n + 1) * 512],
                                                             start=(k == 0), stop=(k == KC - 1)) for k in range(KC)],
                     reads=[catb[i], b_wout], writes=[self.pbuf[n]])
                S.op("act", lambda a, n=n: a.copy(out=ysb[:, n * 512:(n + 1) * 512], in_=self.psum[n][:, :]), reads=[self.pbuf[n]], writes=[b_ysb])
            S.op("dve", lambda v: v.tensor_tensor(out=ysb[:], in0=ysb[:], in1=bc[0][:], op=ALU.mult), reads=[b_ysb, bcb[0]], writes=[b_ysb])
            S.op("dve", lambda v, xin=xin: v.scalar_tensor_tensor(out=xin[:], in0=xin[:], scalar=float(ALPHA), in1=ysb[:], op0=ALU.mult, op1=ALU.add),
                 reads=[xb, b_ysb], writes=[xb])
            self.ln_stats(xin, xb)
            S.op("dve", lambda v, xin=xin: v.tensor_scalar(out=ysb[:], in0=xin[:], scalar1=self.mv[:, 0:1], scalar2=self.rstd[:, 0:1],
                                                           op0=ALU.subtract, op1=ALU.mult), reads=[xb, self.b_mv, self.b_rstd], writes=[b_ysb])
            S.op("dve", lambda v: v.tensor_tensor(out=ysb[:], in0=ysb[:], in1=bc[1][:], op=ALU.mult), reads=[b_ysb, bcb[1]], writes=[b_ysb])
            S.op("pool", lambda g: g.tensor_tensor(out=ysb[:], in0=ysb[:], in1=bc[2][:], op=ALU.add), reads=[b_ysb, bcb[2]], writes=[b_ysb])
            S.dma("sp", lambda q, ti=ti: q.dma_start(out=self.x1_d[ti * P:(ti + 1) * P, :], in_=ysb[:]), "x1st", reads=[b_ysb], writes=[b_x1])
            self.ln_stats(ysb, b_ysb)
            S.op("dve", lambda v, i=i: v.tensor_scalar(out=self.xn[i][:], in0=ysb[:], scalar1=self.mv[:, 0:1], scalar2=self.rstd[:, 0:1],
                                                       op0=ALU.subtract, op1=ALU.mult), reads=[b_ysb, self.b_mv, self.b_rstd], writes=[self.xnb[i]])
            self.transpose_mod(self.xn[i][:], self.xnb[i], h2T, b_h2T, 2, 3, banks=(4, 5))
            S.op("pe", [lambda t, k=k: t.matmul(self.psum[6][:, 0:36], lhsT=h2T[:, k, :], rhs=wr[:, k, :], start=(k == 0), stop=(k == KC - 1))
                        for k in range(KC)], reads=[b_h2T, Bn("wr")], writes=[self.pbuf[6]])
            S.op("act", lambda a: a.copy(out=lg[:], in_=self.psum[6][:, 0:36]), reads=[self.pbuf[6]], writes=[b_r_])
            R = lambda fn, extra_r=(), extra_w=(): S.op("dve", fn, reads=[b_r_] + list(extra_r), writes=[b_r_] + list(extra_w))
            R(lambda v: v.tensor_tensor(out=lg[:], in0=lg[:], in1=brb[:], op=ALU.add), extra_r=[Bn("brb")])
            R(lambda v: v.tensor_reduce(out=sm[:, 0:1], in_=lg[:, 0:4], axis=AX, op=ALU.max))
            R(lambda v: v.tensor_scalar(out=sm[:, 1:2], in0=sm[:, 0:1], scalar1=-1.0, scalar2=None, op0=ALU.mult))
            S.op("act", lambda a: a.activation(out=ge4[:], in_=lg[:, 0:4], func=AF.Exp, bias=sm[:, 1:2], accum_out=sm[:, 2:3]),
                 reads=[b_r_], writes=[b_r_])
            R(lambda v: v.reciprocal(out=sm[:, 3:4], in_=sm[:, 2:3]))
            R(lambda v: v.tensor_scalar(out=gm4[:], in0=lg[:, 0:4], scalar1=sm[:, 0:1], scalar2=None, op0=ALU.is_ge))
            R(lambda v: v.tensor_scalar(out=pen[:], in0=gm4[:], scalar1=1.0, scalar2=1e30, op0=ALU.subtract, op1=ALU.mult))
            R([lambda v, g=g: v.tensor_scalar(out=elm[:, g * 8:(g + 1) * 8], in0=lg[:, 4 + g * 8:4 + (g + 1) * 8], scalar1=pen[:, g:g + 1],
                                              scalar2=None, op0=ALU.add) for g in range(4)])
            R(lambda v: v.max(out=m8[:], in_=elm[:]))
            R(lambda v: v.tensor_scalar(out=A1[:], in0=elm[:], scalar1=m8[:, 0:1], scalar2=None, op0=ALU.is_equal))
            R(lambda v: v.tensor_scalar(out=A2[:], in0=elm[:], scalar1=m8[:, 1:2], scalar2=None, op0=ALU.is_equal))
            R(lambda v: v.tensor_tensor(out=sm[:, 4:5], in0=m8[:, 1:2], in1=m8[:, 0:1], op=ALU.subtract))
            S.op("act", lambda a: a.activation(out=sm[:, 5:6], in_=sm[:, 4:5], func=AF.Exp), reads=[b_r_], writes=[b_r_])
            R(lambda v: v.tensor_scalar(out=sm[:, 5:6], in0=sm[:, 5:6], scalar1=1.0, scalar2=None, op0=ALU.add))
            R(lambda v: v.reciprocal(out=sm[:, 6:7], in_=sm[:, 5:6]))
            R(lambda v, ti=ti: v.tensor_tensor(out=self.w_all[:, ti, 0:1], in0=sm[:, 3:4], in1=sm[:, 6:7], op=ALU.mult), extra_w=[b_w])
            R(lambda v, ti=ti: v.tensor_tensor(out=self.w_all[:, ti, 1:2], in0=sm[:, 3:4], in1=self.w_all[:, ti, 0:1], op=ALU.subtract),
              extra_r=[b_w], extra_w=[b_w])
            R(lambda v: v.tensor_tensor(out=A12[:], in0=A1[:], in1=A2[:], op=ALU.add))
            S.op("pe", [lambda t: t.matmul(self.psum[7][:, 0:32], lhsT=self.c32["lstrict"][:, :], rhs=A12[:], start=True, stop=True),
                        lambda t: t.matmul(self.psum[7][:, 32:64], lhsT=self.c32["ones"][:, :], rhs=A12[:], start=True, stop=True)],
                 reads=[b_r_, cb], writes=[self.pbuf[7]])
            S.op("act", lambda a: a.copy(out=posc[:], in_=self.psum[7][:, 0:64]), reads=[self.pbuf[7]], writes=[b_r_])
            R(lambda v: v.tensor_tensor(out=tpos[:], in0=posc[:, 0:32], in1=self.cnt_bc[:], op=ALU.add), extra_r=[b_cnt])
            R(lambda v: v.tensor_tensor(out=self.cnt_bc[:], in0=self.cnt_bc[:], in1=posc[:, 32:64], op=ALU.add), extra_r=[b_cnt], extra_w=[b_cnt])
            R(lambda v: v.tensor_scalar(out=tov[:], in0=tpos[:], scalar1=float(CAPS), scalar2=40000.0, op0=ALU.is_ge, op1=ALU.mult))
            R(lambda v: v.tensor_tensor(out=tsl[:], in0=tpos[:], in1=self.c32["slotbase"][:], op=ALU.add), extra_r=[cb])
            R(lambda v: v.tensor_tensor(out=tsl[:], in0=tsl[:], in1=tov[:], op=ALU.add))
            R(lambda v: v.tensor_tensor(out=A1[:], in0=A1[:], in1=tsl[:], op=ALU.mult))
            R(lambda v: v.tensor_tensor(out=A2[:], in0=A2[:], in1=tsl[:], op=ALU.mult))
            R(lambda v: v.tensor_reduce(out=idxf[:, 0:1], in_=A1[:], axis=AX, op=ALU.add))
            R(lambda v: v.tensor_reduce(out=idxf[:, 1:2], in_=A2[:], axis=AX, op=ALU.add))
            R(lambda v, ti=ti: v.tensor_copy(out=self.idx_all[:, ti, :], in_=idxf[:]), extra_w=[b_idx])
            for j in range(2):
                S.dma("pool", lambda g, ti=ti, j=j, i=i: g.indirect_dma_start(
                    out=self.xbuf_d[:, :], out_offset=bass.IndirectOffsetOnAxis(ap=self.idx_all[:, ti, j:j + 1], axis=0),
                    in_=self.xn[i][:, :], in_offset=None, bounds_check=S.rt["bnd"], oob_is_err=False),
                    "scat", reads=[self.xnb[i], b_idx], writes=[b_xbuf])
            if ti == 0 and "lg" in self.debug:
                self.dbg_out("lg", lg[:], [P, 36], F32, [b_r_])
        S.op("dve", lambda v: v.tensor_copy(out=self.cnt_i[:], in_=self.cnt_bc[0:1, :]), reads=[b_cnt], writes=[Bn("cnt_i")])
        zt = self.sb("zt", [P, D], BF16)
        S.op("pool", lambda g: g.memset(zt[:], 0.0), writes=[Bn("zt")])
        ci = self.sb("ci", [P, N_EXP], I32)
        rf = self.sb("rf", [P, N_EXP], F32)
        zi = self.sb("zi", [P, N_EXP], I32)
        bz = Bn("ztmp")
        Z = lambda fn, extra_r=(): S.op("dve", fn, reads=[bz, b_cnt, cb] + list(extra_r), writes=[bz])
        Z(lambda v: v.tensor_copy(out=ci[:], in_=self.cnt_bc[:]))
        Z(lambda v: v.tensor_single_scalar(out=ci[:], in_=ci[:], scalar=127, op=ALU.bitwise_and))
        Z(lambda v: v.tensor_copy(out=rf[:], in_=ci[:]))
        Z(lambda v: v.tensor_scalar(out=rf[:], in0=rf[:], scalar1=self.c32["iotap"][:, 0:1], scalar2=128.0, op0=ALU.add, op1=ALU.is_ge))
        Z(lambda v: v.tensor_scalar(out=rf[:], in0=rf[:], scalar1=40000.0, scalar2=self.c32["iotap"][:, 0:1], op0=ALU.mult, op1=ALU.add))
        Z(lambda v: v.tensor_tensor(out=rf[:], in0=rf[:], in1=self.cnt_bc[:], op=ALU.add))
        Z(lambda v: v.tensor_tensor(out=rf[:], in0=rf[:], in1=self.c32["slotbase"][:], op=ALU.add))
        Z(lambda v: v.tensor_copy(out=zi[:], in_=rf[:]))
        import os
        for e in range(int(os.environ.get("NZS", N_EXP))):
            S.dma("pool", lambda g, e=e: g.indirect_dma_start(
                out=self.xbuf_d[:, :], out_offset=bass.IndirectOffsetOnAxis(ap=zi[:, e:e + 1], axis=0),
                in_=zt[:, :], in_offset=None, bounds_check=S.rt["bnd"], oob_is_err=False),
                "scat", reads=[Bn("zt"), bz], writes=[b_xbuf])
        self.dbg_out("zi", zi[:], [P, N_EXP], I32, [bz])
        self.dbg_out("rf", rf[:], [P, N_EXP], F32, [bz])
        for nm, t_, shp, dt, bb in (("idx_all", self.idx_all, [P, NT, 2], I32, b_idx), ("w_all", self.w_all, [P, NT, 2], F32, b_w),
                                    ("cnt", self.cnt_bc, [P, N_EXP], F32, b_cnt)):
            self.dbg_out(nm, t_[:], shp, dt, [bb])
        if "x1" in self.debug:
            o = self.dout("dbg_x1", [NOWN, D], F32)
            for ti in range(NT):
                self.final_toks.append(S.dma("sp", lambda q, ti=ti, o=o: q.dma_start(out=o[ti * P:(ti + 1) * P, :], in_=self.x1_d[ti * P:(ti + 1) * P, :]),
                                             "dbg", reads=[b_x1]))
        self.pop_scope()

    def phase6(self):
        S = self.S
        Bn = self.B
        cb = Bn("consts")
        self.obuf_d = [self.dscr(f"obuf{h}_d", [N_EXP * CAPS, D], F32) for h in range(2)]
        b_ob = [Bn(f"obuf{h}_d") for h in range(2)]
        b_xbuf = Bn("xbuf_d")
        self.push_scope()
        wg = [self.sb(f"wg{i}", [P, KC, 512], BF16) for i in range(2)]
        wu = [self.sb(f"wu{i}", [P, KC, 512], BF16) for i in range(2)]
        wd = [self.sb(f"wd{i}", [P, 4, D], BF16) for i in range(2)]
        wsb = [Bn(f"wset{i}") for i in range(2)]
        G = self.sb("G", [P, D], BF16)
        hxT = self.sb("hxT", [P, KC, P], BF16)
        sg = self.sb("sg", [P, 512], F32)
        su = self.sb("su", [P, 512], F32)
        hid = self.sb("hid", [P, 512], BF16)
        hidT = self.sb("hidT", [P, 4, P], BF16)
        ost = self.sb("ost", [P, D], F32)
        idn = self.c16["ident"]
        dm = self.dummy
        dummies = {
            "pe": lambda t, k: t.matmul(self.psum[0][0:1, k:k + 1], lhsT=idn[:, 0:1], rhs=idn[:, 0:1], start=True, stop=True),
            "act": lambda a, k: a.copy(out=dm[0:1, k:k + 1], in_=dm[0:1, 7:8]),
            "dve": lambda v, k: v.memset(dm[32:33, k:k + 1], 0.0),
        }
        import os
        n_exp = int(os.environ.get("MOE_NEXP", N_EXP))
        for e in range(n_exp):
            gv = self.weg[e].rearrange("(k p) n -> p k n", p=P)
            uv = self.weu[e].rearrange("(k p) n -> p k n", p=P)
            for half in range(2):
                s_ = (2 * e + half) % 2
                hs = slice(half * 512, (half + 1) * 512)
                dv = self.wed[e, half * 512:(half + 1) * 512, :].rearrange("(k p) n -> p k n", p=P)
                for c in range(2):
                    S.dma("pool", lambda q, s_=s_, gv=gv, half=half, c=c: q.dma_start(
                        out=wg[s_][:, :, c * 256:(c + 1) * 256], in_=gv[:, :, half * 512 + c * 256:half * 512 + (c + 1) * 256]),
                        f"wset{s_}", writes=[wsb[s_]])
                    S.dma("pool", lambda q, s_=s_, uv=uv, half=half, c=c: q.dma_start(
                        out=wu[s_][:, :, c * 256:(c + 1) * 256], in_=uv[:, :, half * 512 + c * 256:half * 512 + (c + 1) * 256]),
                        f"wset{s_}", writes=[wsb[s_]])
                for c in range(4):
                    S.dma("pool", lambda q, s_=s_, dv=dv, c=c: q.dma_start(out=wd[s_][:, :, c * 512:(c + 1) * 512], in_=dv[:, :, c * 512:(c + 1) * 512]),
                          f"wset{s_}", writes=[wsb[s_]])
                S.reg_load("cnt", self.cnt_i[0:1, e:e + 1], Bn("cnt_i"))
                for j in range(CAPB):
                    r0 = e * CAPS + j * P
                    S.begin_if("cnt", j * P)
                    S.dma("act", lambda q, r0=r0: q.dma_start(out=G[:], in_=self.xbuf_d[r0:r0 + P, :]), "G", reads=[b_xbuf], writes=[Bn("G")])
                    self.transpose_mod(G[:], Bn("G"), hxT, Bn("hxT"), 2, 3, banks=(0, 1))
                    S.op("pe", [lambda t, k=k, s_=s_: t.matmul(self.psum[2][:, :], lhsT=hxT[:, k, :], rhs=wg[s_][:, k, :], start=(k == 0), stop=(k == KC - 1))
                                for k in range(KC)] +
                               [lambda t, k=k, s_=s_: t.matmul(self.psum[3][:, :], lhsT=hxT[:, k, :], rhs=wu[s_][:, k, :], start=(k == 0), stop=(k == KC - 1))
                                for k in range(KC)], reads=[Bn("hxT"), wsb[s_]], writes=[self.pbuf[2], self.pbuf[3]])
                    S.op("act", [lambda a: a.activation(out=sg[:], in_=self.psum[2][:, :], func=AF.Silu),
                                 lambda a: a.copy(out=su[:], in_=self.psum[3][:, :])], reads=[self.pbuf[2], self.pbuf[3]], writes=[Bn("sgsu")])
                    S.op("dve", lambda v: v.tensor_tensor(out=hid[:], in0=sg[:], in1=su[:], op=ALU.mult), reads=[Bn("sgsu")], writes=[Bn("hid")])
                    pb0 = self.psum[0][:].bitcast(BF16)
                    S.op("pe", [lambda t, c=c, pb0=pb0: t.transpose(out=pb0[:, c * P:(c + 1) * P], in_=hid[:, c * P:(c + 1) * P], identity=idn[:])
                                for c in range(4)], reads=[Bn("hid"), cb], writes=[self.pbuf[0]])
                    S.op("act", lambda a, pb0=pb0: a.copy(out=hidT[:].rearrange("p a b -> p (a b)"), in_=pb0[:, 0:512]), reads=[self.pbuf[0]], writes=[Bn("hidT")])
                    for n in range(4):
                        S.op("pe", [lambda t, c=c, n=n, s_=s_: t.matmul(self.psum[4 + n][:, :], lhsT=hidT[:, c, :], rhs=wd[s_][:, c, n * 512:(n + 1) * 512],
                                                                        start=(c == 0), stop=(c == 3)) for c in range(4)],
                             reads=[Bn("hidT"), wsb[s_]], writes=[self.pbuf[4 + n]])
                        if n % 2 == 0:
                            S.op("act", lambda a, n=n: a.copy(out=ost[:, n * 512:(n + 1) * 512], in_=self.psum[4 + n][:, :]), reads=[self.pbuf[4 + n]], writes=[Bn("ost")])
                        else:
                            S.op("dve", lambda v, n=n: v.tensor_copy(out=ost[:, n * 512:(n + 1) * 512], in_=self.psum[4 + n][:, :]), reads=[self.pbuf[4 + n]], writes=[Bn("ost")])
                    S.dma("act", lambda q, r0=r0, half=half: q.dma_start(out=self.obuf_d[half][r0:r0 + P, :], in_=ost[:]), "ost",
                          reads=[Bn("ost")], writes=[b_ob[half]])
                    S.end_if(dummies)
        self.pop_scope()

    def phase7(self):
        S = self.S
        Bn = self.B
        self.push_scope()
        bc = [self.sb(f"bcf{i}", [P, D], F32) for i in range(3)]
        bcb = [Bn(f"bcf{i}") for i in range(3)]
        S.dma("sp", lambda q: q.dma_start(out=bc[0][:], in_=self.mod_d[0, 5 * D:6 * D].partition_broadcast(P)), "bcf0",
              reads=[Bn("mod_d")], writes=[bcb[0]])
        S.dma("sp", lambda q: q.dma_start(out=bc[1][:], in_=self.ln2g.partition_broadcast(P)), "bcf1", writes=[bcb[1]])
        S.dma("sp", lambda q: q.dma_start(out=bc[2][:], in_=self.ln2b.partition_broadcast(P)), "bcf2", writes=[bcb[2]])
        og = [[self.sb(f"og{h}{j}", [P, D], F32) for j in range(2)] for h in range(2)]
        ogb = [[Bn(f"og{h}{j}") for j in range(2)] for h in range(2)]
        fsb = self.sb("fsb", [P, D], F32)
        b_f = Bn("fsb")
        b_ob = [Bn(f"obuf{h}_d") for h in range(2)]
        for ti in range(NT):
            i = ti % 2
            xin, xb = self.xin[i], self.xinb[i]
            S.dma("sp", lambda q, xin=xin, ti=ti: q.dma_start(out=xin[:], in_=self.x1_d[ti * P:(ti + 1) * P, :]), f"xin{i}",
                  reads=[Bn("x1_d")], writes=[xb])
            for h in range(2):
                for j in range(2):
                    S.dma("pool", lambda g, h=h, j=j, ti=ti: g.indirect_dma_start(
                        out=og[h][j][:, :], out_offset=None, in_=self.obuf_d[h][:, :],
                        in_offset=bass.IndirectOffsetOnAxis(ap=self.idx_all[:, ti, j:j + 1], axis=0),
                        bounds_check=S.rt["bnd"], oob_is_err=False), f"og{h}{j}", reads=[b_ob[h], Bn("idx_all")], writes=[ogb[h][j]])
            for j in range(2):
                S.op("dve" if j == 0 else "pool", lambda v, j=j: v.tensor_tensor(out=og[0][j][:], in0=og[0][j][:], in1=og[1][j][:], op=ALU.add),
                     reads=[ogb[0][j], ogb[1][j]], writes=[ogb[0][j]])
            S.op("dve", lambda v, ti=ti: v.tensor_scalar(out=fsb[:], in0=og[0][0][:], scalar1=self.w_all[:, ti, 0:1], scalar2=None, op0=ALU.mult),
                 reads=[ogb[0][0], Bn("w_all")], writes=[b_f])
            S.op("dve", lambda v, ti=ti: v.scalar_tensor_tensor(out=fsb[:], in0=og[0][1][:], scalar=self.w_all[:, ti, 1:2], in1=fsb[:],
                                                                op0=ALU.mult, op1=ALU.add), reads=[ogb[0][1], Bn("w_all"), b_f], writes=[b_f])
            S.op("dve", lambda v: v.tensor_tensor(out=fsb[:], in0=fsb[:], in1=bc[0][:], op=ALU.mult), reads=[b_f, bcb[0]], writes=[b_f])
            S.op("dve", lambda v, xin=xin: v.scalar_tensor_tensor(out=xin[:], in0=xin[:], scalar=float(ALPHA), in1=fsb[:], op0=ALU.mult, op1=ALU.add),
                 reads=[xb, b_f], writes=[xb])
            self.ln_stats(xin, xb)
            S.op("dve", lambda v, xin=xin: v.tensor_scalar(out=fsb[:], in0=xin[:], scalar1=self.mv[:, 0:1], scalar2=self.rstd[:, 0:1],
                                                           op0=ALU.subtract, op1=ALU.mult), reads=[xb, self.b_mv, self.b_rstd], writes=[b_f])
            S.op("dve", lambda v: v.tensor_tensor(out=fsb[:], in0=fsb[:], in1=bc[1][:], op=ALU.mult), reads=[b_f, bcb[1]], writes=[b_f])
            S.op("pool", lambda g: g.tensor_tensor(out=fsb[:], in0=fsb[:], in1=bc[2][:], op=ALU.add), reads=[b_f, bcb[2]], writes=[b_f])
            self.final_toks.append(S.dma("sp", lambda q, ti=ti: q.dma_start(out=self.y[ti * P:(ti + 1) * P, :], in_=fsb[:]), "yout",
                                         reads=[b_f], writes=[Bn("y")]))
        self.pop_scope()

    def finish(self):
        S = self.S
        S.final_wait("sp", self.final_toks)
        block = self.es.enter_context(self.nc.Block())
        S.emit(block)
        self.es.close()
        return self.nc


def build_program(debug=(), upto="all", n_other=NT):
    b = Builder(debug)
    b.n_other = n_other
    b.declare_inputs(with_experts=(upto in ("all", "moe")))
    b.load_consts()
    b.phase0()
    if upto == "p0":
        return b.finish(), b
    b.phase1()
    if upto == "p1":
        return b.finish(), b
    b.phase2()
    if upto == "p2":
        return b.finish(), b
    b.phase3()
    b.phase4()
    if upto == "p4":
        return b.finish(), b
    b.phase5()
    if upto == "p5":
        return b.finish(), b
    b.phase6()
    b.phase7()
    return b.finish(), b


def _win_layout(w_in, half):
    qa, ka, va, qb, kb, vb, rb, gb = 0, 1024, 1280, 1536, 2048, 2560, 3584, 4608
    out = np.zeros((D, WIN_COLS), np.float32)
    out[:, FM_QA * 128:FM_QA * 128 + 1024] = w_in[:, qa:qa + 1024]
    out[:, FM_KA * 128:FM_KA * 128 + 256] = w_in[:, ka:ka + 256]
    out[:, FM_QB * 128:FM_QB * 128 + 512] = w_in[:, qb:qb + 512]
    out[:, FM_KB * 128:FM_KB * 128 + 512] = w_in[:, kb:kb + 512]
    out[:, FM_RB * 128:FM_RB * 128 + 1024] = w_in[:, rb:rb + 1024]
    gF, gR = (gb, gb + 16) if half == 0 else (gb + 16, gb)
    out[:, GB_OFF:GB_OFF + 16] = w_in[:, gF:gF + 16]
    out[:, GB_OFF + 32:GB_OFF + 48] = w_in[:, gR:gR + 16]
    out[:, TM_OFF + TM_VA:TM_OFF + TM_VA + 256] = w_in[:, va:va + 256]
    out[:, TM_OFF + TM_KB:TM_OFF + TM_KB + 512] = w_in[:, kb:kb + 512]
    out[:, TM_OFF + TM_VB:TM_OFF + TM_VB + 1024] = w_in[:, vb:vb + 1024]
    return out


def prep_inputs(inp, cores=range(8)):
    f = lambda a: np.ascontiguousarray(np.asarray(a, dtype=np.float32))
    x, c, ctx, c_ctx = f(inp["x"]), f(inp["c"]), f(inp["ctx"]), f(inp["c_ctx"])
    w_ada, b_ada = f(inp["w_ada"])[0], f(inp["b_ada"])[0]
    w_in = f(inp["w_in"])[0]
    wgu_in, bg_in = f(inp["w_gate_up"])[0], f(inp["b_gate"])[0]
    consts = _consts()
    shared = {
        "w_ada": w_ada, "b_ada": b_ada, "sink": f(inp["attn_sink"])[0],
        "normw": np.ascontiguousarray(f(inp["gla_norm_w"])[0].reshape(8, P).T),
        "w_out": f(inp["w_out"])[0], "ln1g": f(inp["ln1_g"])[0], "ln1b": f(inp["ln1_b"])[0],
        "ln2g": f(inp["ln2_g"])[0], "ln2b": f(inp["ln2_b"])[0],
        "w_r": np.ascontiguousarray(np.concatenate([f(inp["w_router_group"])[0], f(inp["w_router_expert"])[0]], axis=1)),
        "b_r": np.ascontiguousarray(np.concatenate([f(inp["b_router_group"])[0], f(inp["b_router_expert"])[0]])),
        "weg": f(inp["w_exp_gate"])[0], "weu": f(inp["w_exp_up"])[0], "wed": f(inp["w_exp_down"])[0],
    }
    for k, v in consts.items():
        shared["c_" + k] = v
    per_half = {}
    for half in (0, 1):
        cosT, sinT = _rope_tables(half)
        wgu = np.zeros((2, 64, 512), np.float32)
        sF, sR = (0, 1) if half == 0 else (1, 0)
        wgu[0, 0:16] = wgu_in[sF]
        wgu[1, 32:48] = wgu_in[sR]
        bgate = np.ascontiguousarray(np.stack([bg_in[sF], bg_in[sR]]))
        per_half[half] = {"w_in": _win_layout(w_in, half), "wgu": wgu, "bgate": bgate, "cosT": cosT, "sinT": sinT}
    maps = []
    for core in cores:
        b, half = core // 2, core % 2
        xl = x[b] if half == 0 else x[b][::-1]
        cl = ctx[b] if half == 0 else ctx[b][::-1]
        ccv = np.stack([c[b].reshape(KC, P).T, c_ctx.reshape(KC, P).T], axis=2).reshape(P, 32)
        m = {"x_own": np.ascontiguousarray(xl[:NOWN]), "x_oth": np.ascontiguousarray(xl[NOWN:]),
             "ctxl": np.ascontiguousarray(cl), "cc": np.ascontiguousarray(ccv)}
        m.update(per_half[half])
        m.update(shared)
        maps.append(m)
    return maps


def kernel(**inputs):
    nc, b = build_program()
    maps = prep_inputs(inputs)
    res = run_bass_kernel_spmd(nc, maps, core_ids=list(range(8)))
    out = np.zeros((4, SEQ, D), np.float32)
    for core in range(8):
        bb, half = core // 2, core % 2
        yc = np.asarray(res.results[core]["y"])
        if half == 0:
            out[bb, :NOWN] = yc
        else:
            out[bb, NOWN:] = yc[::-1]
    return out
```

```python
import numpy as np
from contextlib import ExitStack
import concourse.bass as bass
import concourse.mybir as mybir
from concourse.bass_utils import run_bass_kernel_spmd

F32 = mybir.dt.float32
BF16 = mybir.dt.bfloat16
I32 = mybir.dt.int32
AF = mybir.ActivationFunctionType
ALU = mybir.AluOpType

D = 2048
KC = 16
SEQ = 4096
NOWN = 2048
NT = 16
P = 128
LN_EPS = 1e-6
ALPHA = 2.0 ** 0.25
HD = 128
N_EXP = 32
HID = 1024
CAPB = 8
CAPS = CAPB * 128
WD_SPLIT = 4

FM_QA, FM_KA, FM_QB, FM_KB, FM_RB = 0, 8, 10, 14, 18
N_FM = 26
FM_COLS = N_FM * 128
GB_OFF = FM_COLS
TM_OFF = GB_OFF + 64
TM_VA, TM_KB, TM_VB = 0, 256, 768
TM_COLS = 1792
WIN_COLS = TM_OFF + TM_COLS


class Buf:
    __slots__ = ("name", "w", "r")

    def __init__(self, name):
        self.name = name
        self.w = None
        self.r = []


class Sched:
    ENG = ("pe", "act", "dve", "pool", "sp")

    def __init__(self, nc, es):
        self.nc = nc
        self.es = es
        self.eng = {"pe": nc.tensor, "act": nc.scalar, "dve": nc.vector, "pool": nc.gpsimd, "sp": nc.sync}
        self.items = {e: [] for e in self.ENG}
        self.sems = {}
        self.cnt = {}
        self.waited = {e: {} for e in self.ENG}
        for e in self.ENG:
            self._sem("E_" + e)
        self.in_if = None
        self.rt = {}
        self.if_keys = {}

    def _sem(self, key):
        if key not in self.sems:
            self.sems[key] = self.es.enter_context(self.nc.semaphore("s_" + key))
            self.cnt[key] = 0
        return self.sems[key]

    def _need(self, engine, tok):
        if tok is None:
            return
        key, val = tok
        if self.waited[engine].get(key, 0) >= val:
            return
        self.waited[engine][key] = val
        self.items[engine].append(("wait", key, val))

    def _need_all(self, engine, toks):
        best = {}
        for t in toks:
            if t is not None and best.get(t[0], 0) < t[1]:
                best[t[0]] = t[1]
        for k, v in best.items():
            self._need(engine, (k, v))

    def _deps(self, engine, reads, writes):
        toks = [b.w for b in reads]
        for b in writes:
            toks.append(b.w)
            toks.extend(b.r)
        self._need_all(engine, toks)

    def _commit(self, tok, reads, writes):
        for b in reads:
            b.r.append(tok)
        for b in writes:
            b.w = tok
            b.r = []

    def op(self, engine, fns, reads=(), writes=()):
        if not isinstance(fns, (list, tuple)):
            fns = [fns]
        self._deps(engine, reads, writes)
        key = "E_" + engine
        self.cnt[key] += 1
        tok = (key, self.cnt[key])
        self.items[engine].append(("ops", list(fns), key, 1))
        if engine == "pe":
            self.waited[engine][key] = tok[1]
        self._commit(tok, reads, writes)
        if self.in_if is not None:
            self.in_if["incs"].setdefault(engine, {}).setdefault(key, 0)
            self.in_if["incs"][engine][key] += 1
        return tok

    def dma(self, engine, fn, key, reads=(), writes=()):
        key = "D_" + key + "_" + engine
        self._sem(key)
        toks = [b.w for b in reads]
        for b in writes:
            if not (b.w is not None and b.w[0] == key):
                toks.append(b.w)
            toks.extend(b.r)
        self._need_all(engine, toks)
        self.cnt[key] += 16
        tok = (key, self.cnt[key])
        self.items[engine].append(("ops", [fn], key, 16))
        self._commit(tok, reads, writes)
        if self.in_if is not None:
            self.in_if["incs"].setdefault(engine, {}).setdefault(key, 0)
            self.in_if["incs"][engine][key] += 16
        return tok

    IF_ENG = ("pe", "act", "dve", "sp")

    def reg_load(self, regname, ap, buf):
        for e in self.IF_ENG:
            self._need(e, buf.w)
            self.items[e].append(("regload", regname, ap))

    def begin_if(self, regname, thr):
        assert self.in_if is None
        self.in_if = {"incs": {}, "start": {e: len(self.items[e]) for e in self.ENG},
                      "waited": {e: dict(self.waited[e]) for e in self.ENG},
                      "cnt0": dict(self.cnt)}
        for e in self.IF_ENG:
            self.items[e].append(("if", regname, thr))

    def end_if(self, dummies):
        st = self.in_if
        self.in_if = None
        for e in self.ENG:
            incs = st["incs"].get(e, {})
            if e not in self.IF_ENG:
                assert not incs, f"engine {e} cannot be used inside a dynamic section"
                del self.items[e][st["start"][e]:]
            else:
                self.if_keys.setdefault(e, set()).update(incs.keys())
                wk = set(self.if_keys[e]) if e == "sp" else (set("E_" + x for x in ("pe", "act", "dve")) | set(self.if_keys[e]))
                self.items[e].append(("else", dict(incs), dummies[e], {k: st["cnt0"].get(k, 0) for k in wk}))
                self.items[e].append(("endif",))
            self.waited[e] = st["waited"][e]

    def barrier(self):
        for e in self.ENG:
            for key, c in self.cnt.items():
                if c > 0:
                    self._need(e, (key, c))

    def final_wait(self, engine, toks):
        for t in toks:
            self._need(engine, t)

    def emit(self, block):
        nc = self.nc
        deco = {"pe": block.tensor, "act": block.scalar, "dve": block.vector, "pool": block.gpsimd, "sp": block.sync}
        for e in self.ENG:
            items = self.items[e]

            def body(eng, items=items, e=e):
                regs = {}
                stack = []
                with ExitStack() as rs:
                    if e == "pool":
                        self.rt["bnd"] = rs.enter_context(eng.register("bnd_reg"))
                        eng.reg_mov(self.rt["bnd"], N_EXP * CAPS - 1)
                    for it in items:
                        k = it[0]
                        if k == "wait":
                            eng.wait_ge(self.sems[it[1]], it[2])
                        elif k == "ops":
                            ins = None
                            for fn in it[1]:
                                ins = fn(eng)
                            ins.then_inc(self.sems[it[2]], it[3])
                        elif k == "regload":
                            if it[1] not in regs:
                                regs[it[1]] = rs.enter_context(eng.register(it[1] + "_" + e))
                            eng.reg_load(regs[it[1]], it[2])
                        elif k == "if":
                            g = eng.If_cmp(regs[it[1]], it[2], "IS_GT")
                            g.__enter__()
                            stack.append(g)
                        elif k == "else":
                            g = stack.pop()
                            g.__exit__(None, None, None)
                            g2 = eng.Else()
                            g2.__enter__()
                            for key, val in it[3].items():
                                if val > 0:
                                    eng.wait_ge(self.sems[key], val)
                            for kk, (key, inc) in enumerate(it[1].items()):
                                it[2](eng, kk).then_inc(self.sems[key], inc)
                            stack.append(g2)
                        elif k == "endif":
                            g = stack.pop()
                            g.__exit__(None, None, None)
            deco[e](body)


def _rope_tables(half):
    n_freq = HD // 4
    inv_freq = (10000.0 ** (-np.arange(n_freq, dtype=np.float32) / n_freq)).astype(np.float32)
    j = np.arange(NOWN + 128)
    g = j if half == 0 else (SEQ - 1 - j)
    row = (g // 64).astype(np.float32)
    col = (g % 64).astype(np.float32)
    cos = np.zeros((HD, NOWN + 128), np.float32)
    sin = np.zeros((HD, NOWN + 128), np.float32)
    for d in range(HD):
        seg, f = d // 32, d % 32
        ang = (row if seg < 2 else col) * inv_freq[f]
        cos[d] = np.cos(ang.astype(np.float32))
        s = np.sin(ang.astype(np.float32))
        sin[d] = -s if seg in (0, 2) else s
    return cos, sin


def _consts():
    c = {}
    c["ident"] = np.eye(P, dtype=np.float32)
    rm = np.zeros((P, P), np.float32)
    for m in range(P):
        seg = m // 32
        partner = m + 32 if seg in (0, 2) else m - 32
        rm[partner, m] = 1.0
    c["rotm"] = rm
    s = np.arange(P)[:, None]
    t = np.arange(P)[None, :]
    le = (s <= t).astype(np.float32)
    ge = (s >= t).astype(np.float32)
    m1f = -(le - (s <= 63).astype(np.float32)) / 16.0
    m2f = -le / 16.0
    m1r = -(ge - (s >= 64).astype(np.float32)) / 16.0
    m2r = -ge / 16.0
    c["m12f"] = np.concatenate([m1f, m2f], axis=1).astype(np.float32)
    c["m12r"] = np.concatenate([m1r, m2r], axis=1).astype(np.float32)
    c["m3f"] = (-(s > t).astype(np.float32) / 16.0)
    c["m3r"] = (-(s < t).astype(np.float32) / 16.0)
    c["gmaskf"] = np.tile(le, (1, 4))
    c["gmaskr"] = np.tile(ge, (1, 4))
    c["amprev"] = np.tile(ge, (1, 4))
    c["amnext"] = np.tile(le, (1, 4))
    c["ones"] = np.ones((P, P), np.float32)
    c["n16"] = np.full((P, 1), -1.0 / 16.0, np.float32)
    c["lstrict"] = (s < t).astype(np.float32)
    c["slotbase"] = np.tile((np.arange(N_EXP, dtype=np.float32) * CAPS)[None, :], (P, 1))
    c["iotap"] = np.arange(P, dtype=np.float32)[:, None].copy()
    return c


CONST_SHAPES = {"ident": (P, P), "rotm": (P, P), "m12f": (P, 256), "m12r": (P, 256), "m3f": (P, P), "m3r": (P, P),
                "gmaskf": (P, 512), "gmaskr": (P, 512), "amprev": (P, 512), "amnext": (P, 512), "ones": (P, P),
                "n16": (P, 1), "lstrict": (P, P), "slotbase": (P, N_EXP), "iotap": (P, 1)}


class Builder:
    def __init__(self, debug=()):
        self.debug = set(debug)
        self.nc = bass.Bass("TRN2", target_bir_lowering=False)
        self.es = ExitStack()
        self.S = Sched(self.nc, self.es)
        self.bufs = {}
        self.outs = []
        self.final_toks = []
        self.scopes = []
        self.nalloc = 0
        self.alloc_log = []

    def sb(self, name, shape, dt):
        stack = self.scopes[-1] if self.scopes else self.es
        self.nalloc += 1
        nb = int(np.prod(shape[1:])) * (4 if dt in (F32, I32) else 2)
        self.alloc_log.append((name, nb, len(self.scopes)))
        return stack.enter_context(self.nc.sbuf_tensor(f"{name}_{self.nalloc}", list(shape), dt))

    def push_scope(self):
        self.scopes.append(ExitStack())

    def pop_scope(self):
        self.S.barrier()
        self.scopes.pop().close()

    def din(self, name, shape, dt=F32):
        return self.nc.dram_tensor(name, list(shape), dt, kind="ExternalInput").ap()

    def dout(self, name, shape, dt=F32):
        self.outs.append(name)
        return self.nc.dram_tensor(name, list(shape), dt, kind="ExternalOutput").ap()

    def dscr(self, name, shape, dt):
        return self.nc.dram_tensor(name, list(shape), dt, kind="Internal").ap()

    def B(self, name):
        if name not in self.bufs:
            self.bufs[name] = Buf(name)
        return self.bufs[name]

    def dbg_out(self, name, src_ap, shape, dt, reads, eng="sp"):
        if name not in self.debug:
            return
        o = self.dout("dbg_" + name, shape, dt)
        tok = self.S.dma(eng, lambda q, o=o, s=src_ap: q.dma_start(out=o, in_=s), "dbg", reads=reads)
        self.final_toks.append(tok)

    def declare_inputs(self, with_experts=True):
        self.x_own = self.din("x_own", [NOWN, D])
        self.x_oth = self.din("x_oth", [NOWN, D])
        self.ctxl = self.din("ctxl", [256, D])
        self.cc = self.din("cc", [P, 32])
        self.w_ada = self.din("w_ada", [D, 6 * D])
        self.b_ada = self.din("b_ada", [6 * D])
        self.w_in = self.din("w_in", [D, WIN_COLS])
        self.wgu = self.din("wgu", [2, 64, 512])
        self.bgate = self.din("bgate", [2, 512])
        self.sink = self.din("sink", [8])
        self.normw = self.din("normw", [P, 8])
        self.w_out = self.din("w_out", [D, D])
        self.ln1g = self.din("ln1g", [D])
        self.ln1b = self.din("ln1b", [D])
        self.ln2g = self.din("ln2g", [D])
        self.ln2b = self.din("ln2b", [D])
        self.w_r = self.din("w_r", [D, 36])
        self.b_r = self.din("b_r", [36])
        if with_experts:
            self.weg = self.din("weg", [N_EXP, D, HID])
            self.weu = self.din("weu", [N_EXP, D, HID])
            self.wed = self.din("wed", [N_EXP, HID, D])
        self.cosT = self.din("cosT", [HD, NOWN + 128])
        self.sinT = self.din("sinT", [HD, NOWN + 128])
        self.cin = {k: self.din("c_" + k, list(s)) for k, s in CONST_SHAPES.items()}
        self.y = self.dout("y", [NOWN, D])

    def load_consts(self):
        S = self.S
        self.c32 = {}
        self.c16 = {}
        cb = self.B("consts")
        for k in ("m12f", "m12r", "m3f", "m3r", "ones", "n16", "ident", "lstrict", "slotbase", "iotap"):
            t = self.sb("c32_" + k, CONST_SHAPES[k], F32)
            S.dma("sp", lambda q, t=t, k=k: q.dma_start(out=t[:], in_=self.cin[k]), "consts", writes=[cb])
            self.c32[k] = t
        for k in ("ident", "rotm", "gmaskf", "gmaskr", "amprev", "amnext", "ones"):
            t = self.sb("c16_" + k, CONST_SHAPES[k], BF16)
            S.dma("pool", lambda q, t=t, k=k: q.dma_start(out=t[:], in_=self.cin[k]), "consts", writes=[cb])
            self.c16[k] = t
        self.psum = [self.es.enter_context(self.nc.psum_tensor(f"pb{i}", [P, 512], F32)) for i in range(8)]
        self.pbuf = [self.B(f"psum{i}") for i in range(8)]
        self.dummy = self.sb("dummy_t", [P, 8], F32)
        self.eps_t = self.sb("eps_t", [P, 1], F32)
        S.op("dve", lambda v: v.memset(self.eps_t[:], LN_EPS), writes=[cb])
        self.one_t = self.sb("one_t", [P, 1], F32)
        S.op("dve", lambda v: v.memset(self.one_t[:], 1.0), writes=[cb])
        S.op("pool", lambda g: g.memset(self.dummy[:], 0.0), writes=[self.B("dummy")])

    def phase0(self):
        S, nc = self.S, self.nc
        self.mod_d = self.dscr("mod_d", [2, 6 * D], F32)
        b_modd = self.B("mod_d")
        self.vecs = self.sb("vecs", [P, 6, KC], F32)
        b_vecs = self.B("vecs")
        self.push_scope()
        cc_sb = self.sb("cc_sb", [P, 32], F32)
        sc_bf = self.sb("sc_bf", [P, 32], BF16)
        b_cc, b_sc = self.B("cc"), self.B("sc_bf")
        S.dma("sp", lambda q: q.dma_start(out=cc_sb[:], in_=self.cc), "p0a", writes=[b_cc])
        S.op("act", lambda a: a.activation(out=sc_bf[:], in_=cc_sb[:], func=AF.Silu), reads=[b_cc], writes=[b_sc])
        wv = self.w_ada.rearrange("(k p) n -> p k n", p=P)
        NB = 3
        wbufs = [self.sb(f"wada{i}", [P, KC, 1024], BF16) for i in range(NB)]
        wb = [self.B(f"wada{i}") for i in range(NB)]
        bad = [self.sb(f"bada{i}", [2, 1024], F32) for i in range(2)]
        badb = [self.B(f"bada{i}") for i in range(2)]
        mods = [self.sb(f"mods{i}", [2, 1024], F32) for i in range(2)]
        modb = [self.B(f"mods{i}") for i in range(2)]
        for cbk in range(12):
            i = cbk % NB
            j = cbk % 2
            S.dma("pool", lambda q, i=i, cbk=cbk: q.dma_start(out=wbufs[i][:], in_=wv[:, :, cbk * 1024:(cbk + 1) * 1024]),
                  f"wada{i}", writes=[wb[i]])
            S.dma("sp", lambda q, j=j, cbk=cbk: q.dma_start(
                out=bad[j][:], in_=self.b_ada[cbk * 1024:(cbk + 1) * 1024].partition_broadcast(2)),
                f"bada{j}", writes=[badb[j]])
            for n in range(2):
                pi = n
                ps = self.psum[pi]
                S.op("pe", [lambda t, k=k, i=i, n=n, ps=ps: t.matmul(ps[0:2, :], lhsT=sc_bf[:, 2 * k:2 * k + 2],
                                                                    rhs=wbufs[i][:, k, n * 512:(n + 1) * 512],
                                                                    start=(k == 0), stop=(k == KC - 1)) for k in range(KC)],
                     reads=[b_sc, wb[i]], writes=[self.pbuf[pi]])
                S.op("dve", lambda v, ps=ps, n=n, j=j: v.tensor_tensor(out=mods[j][0:2, n * 512:(n + 1) * 512], in0=ps[0:2, :],
                                                                     in1=bad[j][0:2, n * 512:(n + 1) * 512], op=ALU.add),
                     reads=[self.pbuf[pi], badb[j]], writes=[modb[j]])
            S.dma("sp", lambda q, j=j, cbk=cbk: q.dma_start(out=self.mod_d[:, cbk * 1024:(cbk + 1) * 1024], in_=mods[j][:]),
                  f"p0b{j}", reads=[modb[j]], writes=[self.B(f"mod_d_st{j}")])
        srcs = [(0, 0), (0, D), (0, 3 * D), (0, 4 * D), (1, 0), (1, D)]
        for i, (r, off) in enumerate(srcs):
            S.dma("sp", lambda q, i=i, r=r, off=off: q.dma_start(
                out=self.vecs[:, i, :], in_=self.mod_d[r, off:off + D].rearrange("(k p) -> p k", p=P),
                allow_slow_non_contiguous=True), "p0c", reads=[self.B("mod_d_st0"), self.B("mod_d_st1")], writes=[b_vecs])
        for i in (1, 3, 5):
            S.op("dve", lambda v, i=i: v.tensor_scalar(out=self.vecs[:, i, :], in0=self.vecs[:, i, :], scalar1=1.0,
                                                       scalar2=None, op0=ALU.add), reads=[b_vecs], writes=[b_vecs])
        self.dbg_out("vecs", self.vecs[:], [P, 6, KC], F32, [b_vecs])
        if "mod" in self.debug:
            o = self.dout("dbg_mod", [2, 6 * D], F32)
            self.final_toks.append(S.dma("sp", lambda q: q.dma_start(out=o, in_=self.mod_d), "dbg", reads=[self.B("mod_d_st0"), self.B("mod_d_st1")]))
        self.pop_scope()

    def mix_setup(self):
        S = self.S
        self.xin = [self.sb(f"xin{i}", [P, D], F32) for i in range(2)]
        self.xinb = [self.B(f"xin{i}") for i in range(2)]
        self.xn = [self.sb(f"xn{i}", [P, D], BF16) for i in range(2)]
        self.xnb = [self.B(f"xn{i}") for i in range(2)]
        self.stats = self.sb("stats", [P, 4, 6], F32)
        self.mv = self.sb("mv", [P, 2], F32)
        self.rstd = self.sb("rstd", [P, 1], F32)
        self.b_stats, self.b_mv, self.b_rstd = self.B("stats"), self.B("mv"), self.B("rstd")
        self.ln_i = 0

    def ln_norm(self, src_ap, out_bf, b_out, src_buf=None, load=True, xin_idx=None):
        S = self.S
        i = self.ln_i % 2 if xin_idx is None else xin_idx
        self.ln_i += 1
        xin, xb = self.xin[i], self.xinb[i]
        if load:
            S.dma("sp", lambda q: q.dma_start(out=xin[:], in_=src_ap), f"xin{i}", writes=[xb])
        S.op("dve", [lambda v, c=c: v.bn_stats(out=self.stats[:, c, :], in_=xin[:, c * 512:(c + 1) * 512]) for c in range(4)],
             reads=[xb], writes=[self.b_stats])
        S.op("dve", lambda v: v.bn_aggr(out=self.mv[:], in_=self.stats[:].rearrange("p a b -> p (a b)")),
             reads=[self.b_stats, xb], writes=[self.b_mv])
        S.op("act", lambda a: a.activation(out=self.rstd[:], in_=self.mv[:, 1:2], func=AF.Ln, bias=self.eps_t[:, 0:1]),
             reads=[self.b_mv], writes=[self.b_rstd])
        S.op("act", lambda a: a.activation(out=self.rstd[:], in_=self.rstd[:], func=AF.Exp, scale=-0.5),
             reads=[self.b_rstd], writes=[self.b_rstd])
        S.op("dve", lambda v: v.tensor_scalar(out=out_bf, in0=xin[:], scalar1=self.mv[:, 0:1], scalar2=self.rstd[:, 0:1],
                                              op0=ALU.subtract, op1=ALU.mult),
             reads=[xb, self.b_mv, self.b_rstd], writes=[b_out])
        return i

    def transpose_mod(self, xn_bf, b_xn, hT, b_hT, vi_shift, vi_scale, banks=(0, 1), plain=False):
        S = self.S
        idn = self.c16["ident"]
        for half in range(2):
            pb = self.psum[banks[half]][:].bitcast(BF16)
            S.op("pe", [lambda t, k=k, pb=pb: t.transpose(out=pb[:, (k % 8) * 128:(k % 8 + 1) * 128],
                                                        in_=xn_bf[:, k * 128:(k + 1) * 128], identity=idn[:])
                        for k in range(half * 8, half * 8 + 8)],
                 reads=[b_xn, self.B("consts")], writes=[self.pbuf[banks[half]]])
            if plain:
                eng = "act" if half == 0 else "dve"
                if eng == "act":
                    S.op("act", lambda a, pb=pb, half=half: a.copy(out=hT[:, half * 8:half * 8 + 8, :].rearrange("p a b -> p (a b)"), in_=pb[:, :]),
                         reads=[self.pbuf[banks[half]]], writes=[b_hT])
                else:
                    S.op("dve", lambda v, pb=pb, half=half: v.tensor_copy(out=hT[:, half * 8:half * 8 + 8, :].rearrange("p a b -> p (a b)"), in_=pb[:, :]),
                         reads=[self.pbuf[banks[half]]], writes=[b_hT])
                continue
            S.op("act", [lambda a, k=k, pb=pb: a.activation(out=hT[:, k, :], in_=pb[:, (k % 8) * 128:(k % 8 + 1) * 128],
                                                          func=AF.Identity, scale=self.vecs[:, vi_scale, k:k + 1],
                                                          bias=self.vecs[:, vi_shift, k:k + 1])
                         for k in range(half * 8, half * 8 + 8)],
                 reads=[self.pbuf[banks[half]], self.B("vecs")], writes=[b_hT])

    def chain_setup(self):
        S = self.S
        cb = self.B("consts")
        self.Sst = [self.sb(f"Sst{d}", [P, 1024], F32) for d in range(2)]
        self.Sb = [self.B(f"Sst{d}") for d in range(2)]
        for d in range(2):
            S.op("dve", lambda v, d=d: v.memset(self.Sst[d][:], 0.0), writes=[self.Sb[d]])
        self.wgu_sb = [self.sb(f"wgu{d}", [64, 512], F32) for d in range(2)]
        self.bg_sb = [self.sb(f"bg{d}", [1, 512], F32) for d in range(2)]
        for d in range(2):
            S.dma("sp", lambda q, d=d: q.dma_start(out=self.wgu_sb[d][:], in_=self.wgu[d]), "cstc", writes=[self.B("cst_chain")])
            S.dma("sp", lambda q, d=d: q.dma_start(out=self.bg_sb[d][:], in_=self.bgate[d:d + 1, :]), "cstc", writes=[self.B("cst_chain")])
        self.ktok = [self.sb(f"ktok{i}", [P, 512], BF16) for i in range(2)]
        self.vtok = [self.sb(f"vtok{i}", [P, 1024], BF16) for i in range(2)]
        self.glT = [self.sb(f"glT{i}", [64, P], F32) for i in range(2)]
        self.gp = [[self.sb(f"gp{i}_{d}", [P, 512], F32) for d in range(2)] for i in range(2)]
        self.b_ktok = [self.B(f"ktok{i}") for i in range(2)]
        self.b_vtok = [self.B(f"vtok{i}") for i in range(2)]
        self.b_glT = [self.B(f"glT{i}") for i in range(2)]
        self.b_gp = [[self.B(f"gp{i}_{d}") for d in range(2)] for i in range(2)]
        self.etmp = self.sb("etmp", [P, 512], F32)
        self.b_etmp = self.B("etmp")
        self.e3 = self.sb("e3", [P, 512], F32)
        self.b_e3 = self.B("e3")
        self.k3 = self.sb("k3", [P, 512], BF16)
        self.b_k3 = self.B("k3")
        self.dec = self.sb("dec", [P, 4], F32)
        self.b_dec = self.B("dec")

    def gate_gp(self, i, d, zbank=4):
        S = self.S
        cb = self.B("consts")
        ps = self.psum[zbank]
        S.op("pe", [lambda t: t.matmul(ps[:, :], lhsT=self.glT[i][:, :], rhs=self.wgu_sb[d][:, :], start=True, stop=False),
                    lambda t: t.matmul(ps[:, :], lhsT=self.c32["ones"][0:1, :], rhs=self.bg_sb[d][0:1, :], start=False, stop=True)],
             reads=[self.b_glT[i], cb, self.B("cst_chain")], writes=[self.pbuf[zbank]])
        S.op("act", lambda a: a.activation(out=self.etmp[:], in_=ps[:, :], func=AF.Exp, scale=-1.0),
             reads=[self.pbuf[zbank]], writes=[self.b_etmp])
        S.op("act", lambda a: a.activation(out=self.gp[i][d][:], in_=self.etmp[:], func=AF.Ln, bias=self.one_t[:, 0:1]),
             reads=[self.b_etmp], writes=[self.b_gp[i][d]])

    def chain_update(self, i, d, banks=(4, 5, 6, 7)):
        S = self.S
        cb = self.B("consts")
        m3 = self.c32["m3f" if d == 0 else "m3r"]
        r3b, totb, kvb0, kvb1 = banks
        ps = self.psum[r3b]
        S.op("pe", lambda t: t.matmul(ps[:, :], lhsT=m3[:, :], rhs=self.gp[i][d][:, :], start=True, stop=True),
             reads=[self.b_gp[i][d], cb], writes=[self.pbuf[r3b]])
        S.op("act", lambda a: a.activation(out=self.e3[:], in_=ps[:, :], func=AF.Exp), reads=[self.pbuf[r3b]], writes=[self.b_e3])
        S.op("dve", lambda v: v.tensor_tensor(out=self.k3[:], in0=self.ktok[i][:], in1=self.e3[:], op=ALU.mult),
             reads=[self.b_ktok[i], self.b_e3], writes=[self.b_k3])
        pt = self.psum[totb]
        S.op("pe", [lambda t, h=h: t.matmul(pt[:, h:h + 1], lhsT=self.gp[i][d][:, h * 128:(h + 1) * 128], rhs=self.c32["n16"][:, 0:1],
                                             start=True, stop=True) for h in range(4)],
             reads=[self.b_gp[i][d], cb], writes=[self.pbuf[totb]])
        S.op("act", lambda a: a.activation(out=self.dec[:], in_=pt[:, 0:4], func=AF.Exp), reads=[self.pbuf[totb]], writes=[self.b_dec])
        for hh in range(2):
            pk = self.psum[(kvb0, kvb1)[hh]]
            S.op("pe", [lambda t, h=h, pk=pk: t.matmul(pk[:, (h % 2) * 256:(h % 2 + 1) * 256], lhsT=self.k3[:, h * 128:(h + 1) * 128],
                                                      rhs=self.vtok[i][:, h * 256:(h + 1) * 256], start=True, stop=True)
                        for h in (2 * hh, 2 * hh + 1)],
                 reads=[self.b_k3, self.b_vtok[i]], writes=[self.pbuf[(kvb0, kvb1)[hh]]])
            for h in (2 * hh, 2 * hh + 1):
                S.op("dve", lambda v, h=h, pk=pk: v.scalar_tensor_tensor(
                    out=self.Sst[d][:, h * 256:(h + 1) * 256], in0=self.Sst[d][:, h * 256:(h + 1) * 256],
                    scalar=self.dec[:, h:h + 1], in1=pk[:, (h % 2) * 256:(h % 2 + 1) * 256], op0=ALU.mult, op1=ALU.add),
                    reads=[self.pbuf[(kvb0, kvb1)[hh]], self.b_dec, self.Sb[d]], writes=[self.Sb[d]])

    def proj_tile_B(self, i, hT, b_hT, want_kv=True, kaT=None, b_kaT=None, va=None, b_va=None, rope_cols=None):
        S = self.S
        wgt, wka, bw = self.wgt, self.wka, self.B("W_B")
        ps = self.psum[2]
        S.op("pe", [lambda t, k=k: t.matmul(ps[0:64, 0:128], lhsT=wgt[:, k, 0:64], rhs=hT[:, k, :], start=(k == 0), stop=(k == KC - 1))
                    for k in range(KC)], reads=[b_hT, bw], writes=[self.pbuf[2]])
        S.op("dve", lambda v: v.tensor_copy(out=self.glT[i][:], in_=ps[0:64, 0:128]), reads=[self.pbuf[2]], writes=[self.b_glT[i]])
        col0 = 64 + 256
        for n in range(3):
            pb_i = 3 if n % 2 == 0 else 2
            ps2 = self.psum[pb_i]
            S.op("pe", [lambda t, k=k, n=n, ps2=ps2: t.matmul(ps2[:, :], lhsT=hT[:, k, :], rhs=wgt[:, k, col0 + n * 512:col0 + (n + 1) * 512],
                                                           start=(k == 0), stop=(k == KC - 1)) for k in range(KC)],
                 reads=[b_hT, bw], writes=[self.pbuf[pb_i]])
            if n == 0:
                S.op("act", lambda a, ps2=ps2: a.copy(out=self.ktok[i][:], in_=ps2[:, :]), reads=[self.pbuf[pb_i]], writes=[self.b_ktok[i]])
            else:
                S.op("act", lambda a, ps2=ps2, n=n: a.copy(out=self.vtok[i][:, (n - 1) * 512:n * 512], in_=ps2[:, :]),
                     reads=[self.pbuf[pb_i]], writes=[self.b_vtok[i]])
        if va is not None:
            ps3 = self.psum[3]
            S.op("pe", [lambda t, k=k: t.matmul(ps3[:, 0:256], lhsT=hT[:, k, :], rhs=wgt[:, k, 64:64 + 256], start=(k == 0), stop=(k == KC - 1))
                        for k in range(KC)], reads=[b_hT, bw], writes=[self.pbuf[3]])
            S.op("act", lambda a: a.copy(out=va, in_=ps3[:, 0:256]), reads=[self.pbuf[3]], writes=[b_va])
        if kaT is not None:
            for blk in range(2):
                ps4 = self.psum[2]
                S.op("pe", [lambda t, k=k, blk=blk: t.matmul(ps4[:, 0:128], lhsT=wka[:, k, blk * 128:(blk + 1) * 128], rhs=hT[:, k, :],
                                                             start=(k == 0), stop=(k == KC - 1)) for k in range(KC)],
                     reads=[b_hT, bw], writes=[self.pbuf[2]])
                if rope_cols is None:
                    S.op("act", lambda a, blk=blk: a.copy(out=kaT[:, blk, :], in_=ps4[:, 0:128]), reads=[self.pbuf[2]], writes=[b_kaT])
                else:
                    self.rope(ps4[:, 0:128], self.pbuf[2], kaT[:, blk, :], b_kaT, rope_cols, 128, rotbank=3)

    def rope(self, src_ps, b_src, out_bf, b_out, col0, n, rotbank):
        S = self.S
        cb = self.B("consts")
        qs, t1, t2 = self.rp_qs, self.rp_t1, self.rp_t2
        S.op("act", lambda a: a.copy(out=qs[:, 0:n], in_=src_ps), reads=[b_src], writes=[self.B("rp_qs")])
        S.op("act", lambda a: a.copy(out=t1[:, 0:n], in_=src_ps), reads=[b_src], writes=[self.B("rp_t1")])
        pr = self.psum[rotbank]
        S.op("pe", lambda t: t.matmul(pr[:, 0:n], lhsT=self.c16["rotm"][:, :], rhs=qs[:, 0:n], start=True, stop=True),
             reads=[self.B("rp_qs"), cb], writes=[self.pbuf[rotbank]])
        S.op("act", lambda a: a.copy(out=t2[:, 0:n], in_=pr[:, 0:n]), reads=[self.pbuf[rotbank]], writes=[self.B("rp_t2")])
        S.op("dve", lambda v: v.tensor_tensor(out=t1[:, 0:n], in0=t1[:, 0:n], in1=self.cos_sb[:, col0:col0 + n], op=ALU.mult),
             reads=[self.B("rp_t1"), self.B("cst_rope")], writes=[self.B("rp_t1")])
        S.op("dve", lambda v: v.tensor_tensor(out=t2[:, 0:n], in0=t2[:, 0:n], in1=self.sin_sb[:, col0:col0 + n], op=ALU.mult),
             reads=[self.B("rp_t2"), self.B("cst_rope")], writes=[self.B("rp_t2")])
        S.op("dve", lambda g: g.tensor_tensor(out=out_bf, in0=t1[:, 0:n], in1=t2[:, 0:n], op=ALU.add),
             reads=[self.B("rp_t1"), self.B("rp_t2")], writes=[b_out])

    def phase1(self):
        S = self.S
        cb = self.B("consts")
        self.mix_setup()
        self.push_scope()
        self.chain_setup()
        self.kaT_c = self.sb("kaT_c", [P, 2, 256], BF16)
        self.va_c = self.sb("va_c", [P, 2, 256], BF16)
        self.kaT_h = self.sb("kaT_h", [P, 2, P], BF16)
        self.va_h = self.sb("va_h", [P, 256], BF16)
        self.push_scope()
        self.cos_sb = self.sb("cos_sb", [HD, NOWN + 128], F32)
        self.sin_sb = self.sb("sin_sb", [HD, NOWN + 128], F32)
        S.dma("sp", lambda q: q.dma_start(out=self.cos_sb[:], in_=self.cosT), "cstr", writes=[self.B("cst_rope")])
        S.dma("sp", lambda q: q.dma_start(out=self.sin_sb[:], in_=self.sinT), "cstr", writes=[self.B("cst_rope")])
        self.rp_qs = self.sb("rp_qs", [P, 512], BF16)
        self.rp_t1 = self.sb("rp_t1", [P, 512], F32)
        self.rp_t2 = self.sb("rp_t2", [P, 512], F32)
        self.push_scope()
        wv = self.w_in.rearrange("(k p) n -> p k n", p=P)
        self.wgt = self.sb("wgt", [P, KC, 1856], BF16)
        self.wka = self.sb("wka", [P, KC, 256], BF16)
        bw = self.B("W_B")
        S.dma("pool", lambda q: q.dma_start(out=self.wka[:], in_=wv[:, :, FM_KA * 128:FM_KA * 128 + 256]), "W_B", writes=[bw])
        for c in range(4):
            S.dma("pool", lambda q, c=c: q.dma_start(out=self.wgt[:, :, c * 464:(c + 1) * 464],
                                                      in_=wv[:, :, GB_OFF + c * 464:GB_OFF + (c + 1) * 464]), "W_B", writes=[bw])
        hT = [self.sb(f"hT{i}", [P, KC, P], BF16) for i in range(2)]
        b_hT = [self.B(f"hT{i}") for i in range(2)]
        kaT_tmp = self.sb("kaT_tmp", [P, 2, P], BF16)
        for ti in range(2):
            self.ln_norm(self.ctxl[ti * P:(ti + 1) * P, :], self.xn[ti][:], self.xnb[ti])
            self.transpose_mod(self.xn[ti][:], self.xnb[ti], hT[ti], b_hT[ti], 4, 5)
            self.proj_tile_B(ti, hT[ti], b_hT[ti], kaT=kaT_tmp, b_kaT=self.B("kaT_tmp"),
                             va=self.va_c[:, ti, :], b_va=self.B("va_c"))
            for blk in range(2):
                S.op("pool", lambda g, blk=blk, ti=ti: g.tensor_copy(out=self.kaT_c[:, blk, ti * P:(ti + 1) * P], in_=kaT_tmp[:, blk, :]),
                     reads=[self.B("kaT_tmp")], writes=[self.B("kaT_c")])
            for d in range(2):
                self.gate_gp(ti, d)
        for ti in (0, 1):
            self.chain_update(ti, 0)
        for ti in (1, 0):
            self.chain_update(ti, 1)
        self.dbg_out("S_ctx_F", self.Sst[0][:], [P, 1024], F32, [self.Sb[0]])
        self.dbg_out("S_ctx_R", self.Sst[1][:], [P, 1024], F32, [self.Sb[1]])
        self.dbg_out("kaT_c", self.kaT_c[:], [P, 2, 256], BF16, [self.B("kaT_c")])
        if self.n_other > 0:
            for idx, ti in enumerate(range(NT - 1, NT - 1 - self.n_other, -1)):
                i = idx % 2
                self.ln_norm(self.x_oth[ti * P:(ti + 1) * P, :], self.xn[i][:], self.xnb[i])
                self.transpose_mod(self.xn[i][:], self.xnb[i], hT[i], b_hT[i], 0, 1)
                if ti == 0:
                    self.proj_tile_B(i, hT[i], b_hT[i], kaT=self.kaT_h, b_kaT=self.B("kaT_h"), va=self.va_h[:], b_va=self.B("va_h"),
                                     rope_cols=NOWN)
                else:
                    self.proj_tile_B(i, hT[i], b_hT[i])
                self.gate_gp(i, 1)
                self.chain_update(i, 1)
        self.dbg_out("S_bnd_R", self.Sst[1][:], [P, 1024], F32, [self.Sb[1]])
        self.dbg_out("kaT_h", self.kaT_h[:], [P, 2, P], BF16, [self.B("kaT_h")])
        self.pop_scope()

    def phase2(self):
        S = self.S
        cb = self.B("consts")
        self.pfm_d = self.dscr("pfm_d", [N_FM, P, NOWN], BF16)
        self.pgl_d = self.dscr("pgl_d", [64, NOWN], F32)
        self.ptm_d = self.dscr("ptm_d", [NOWN, TM_COLS], BF16)
        b_pfm, b_pgl, b_ptm = self.B("pfm_d"), self.B("pgl_d"), self.B("ptm_d")
        self.push_scope()
        hT = self.sb("hT_own", [P, KC, NOWN], BF16)
        b_hT = self.B("hT_own")
        for ti in range(NT):
            i = ti % 2
            self.ln_norm(self.x_own[ti * P:(ti + 1) * P, :], self.xn[i][:], self.xnb[i])
            self.transpose_mod(self.xn[i][:], self.xnb[i], hT[:, :, ti * P:(ti + 1) * P], b_hT, 0, 1)
        import os
        stop = os.environ.get("P2_STOP", "")
        if stop == "ln":
            self.dbg_out("hT", hT[:, 0, :], [P, NOWN], BF16, [b_hT])
            self.pop_scope()
            return
        wv = self.w_in.rearrange("(k p) n -> p k n", p=P)
        NB = 2
        wbuf = [self.sb(f"wst{i}", [P, KC, 512], BF16) for i in range(NB)]
        wbb = [self.B(f"wst{i}") for i in range(NB)]
        stg = [self.sb(f"stg{i}", [P, NOWN], BF16) for i in range(2)]
        stgb = [self.B(f"stg{i}") for i in range(2)]
        stg32 = [self.sb(f"stg32_{i}", [64, 512], F32) for i in range(2)]
        stt = [self.sb(f"stt{i}", [P, 512], BF16) for i in range(3)]
        sttb = [self.B(f"stt{i}") for i in range(3)]
        gi = 0
        ev = 0
        pbank = 0
        nstg = 0
        fm_groups = [(g * 512, 512) for g in range(6)] + [(3072, 320)]
        if stop.startswith("fm"):
            fm_groups = fm_groups[int(stop[2:4]):int(stop[4:6])]
        if stop.startswith("tm"):
            fm_groups = []
        for (c0, ncol) in fm_groups:
            w = gi % NB
            gi += 1
            for hh in range(2):
                h0, h1 = hh * (ncol // 2), (hh + 1) * (ncol // 2)
                S.dma("pool", lambda q, w=w, c0=c0, h0=h0, h1=h1: q.dma_start(out=wbuf[w][:, :, h0:h1], in_=wv[:, :, c0 + h0:c0 + h1]),
                      f"wst{w}", writes=[wbb[w]])
            nblk = (ncol + 127) // 128
            for bl in range(nblk):
                blk = c0 // 128 + bl
                M = min(128, ncol - bl * 128)
                is_gb = (M == 64)
                si = nstg % 2
                if not is_gb:
                    nstg += 1
                for ch in range(4):
                    pb_i = pbank % 4
                    pbank += 1
                    ps = self.psum[pb_i]
                    S.op("pe", [lambda t, k=k, w=w, bl=bl, M=M, ch=ch, ps=ps: t.matmul(
                        ps[0:M, :], lhsT=wbuf[w][:, k, bl * 128:bl * 128 + M], rhs=hT[:, k, ch * 512:(ch + 1) * 512],
                        start=(k == 0), stop=(k == KC - 1)) for k in range(KC)],
                        reads=[b_hT, wbb[w]], writes=[self.pbuf[pb_i]])
                    if is_gb:
                        S.op("dve", lambda v, ps=ps, ch=ch: v.tensor_copy(out=stg32[ch % 2][:, :], in_=ps[0:64, :]),
                             reads=[self.pbuf[pb_i]], writes=[self.B(f"stg32_{ch % 2}")])
                        S.dma("sp", lambda q, ch=ch: q.dma_start(out=self.pgl_d[:, ch * 512:(ch + 1) * 512], in_=stg32[ch % 2][:, :]),
                              f"pgl{ch % 2}", reads=[self.B(f"stg32_{ch % 2}")], writes=[b_pgl])
                    elif blk < FM_QB:
                        self.rope(ps[:, :], self.pbuf[pb_i], stg[si][:, ch * 512:(ch + 1) * 512], stgb[si], ch * 512, 512,
                                  rotbank=4 + (ch % 2))
                    else:
                        eng = "act" if ev % 2 == 0 else "dve"
                        ev += 1
                        if eng == "act":
                            S.op("act", lambda a, ps=ps, ch=ch, si=si: a.copy(out=stg[si][:, ch * 512:(ch + 1) * 512], in_=ps[:, :]),
                                 reads=[self.pbuf[pb_i]], writes=[stgb[si]])
                        else:
                            S.op("dve", lambda v, ps=ps, ch=ch, si=si: v.tensor_copy(out=stg[si][:, ch * 512:(ch + 1) * 512], in_=ps[:, :]),
                                 reads=[self.pbuf[pb_i]], writes=[stgb[si]])
                if not is_gb:
                    S.dma("sp", lambda q, blk=blk, si=si: q.dma_start(out=self.pfm_d[blk], in_=stg[si][:]), f"stg{si}",
                          reads=[stgb[si]], writes=[b_pfm])
        tm_groups = [(0, 512), (512, 512), (1024, 512), (1536, 256)]
        if stop.startswith("fm"):
            tm_groups = []
        if stop.startswith("tm"):
            tm_groups = tm_groups[int(stop[2:4]):int(stop[4:6])]
        nt_ = 0
        for (c0, ncol) in tm_groups:
            w = gi % NB
            gi += 1
            for hh in range(2):
                h0, h1 = hh * (ncol // 2), (hh + 1) * (ncol // 2)
                S.dma("pool", lambda q, w=w, c0=c0, h0=h0, h1=h1: q.dma_start(out=wbuf[w][:, :, h0:h1], in_=wv[:, :, TM_OFF + c0 + h0:TM_OFF + c0 + h1]),
                      f"wst{w}", writes=[wbb[w]])
            for ti in range(NT):
                pb_i = pbank % 4
                pbank += 1
                ps = self.psum[pb_i]
                S.op("pe", [lambda t, k=k, w=w, ti=ti, ncol=ncol, ps=ps: t.matmul(
                    ps[:, 0:ncol], lhsT=hT[:, k, ti * P:(ti + 1) * P], rhs=wbuf[w][:, k, 0:ncol],
                    start=(k == 0), stop=(k == KC - 1)) for k in range(KC)],
                    reads=[b_hT, wbb[w]], writes=[self.pbuf[pb_i]])
                j = nt_ % 3
                nt_ += 1
                eng = "act" if ev % 2 == 0 else "dve"
                ev += 1
                if eng == "act":
                    S.op("act", lambda a, ps=ps, j=j, ncol=ncol: a.copy(out=stt[j][:, 0:ncol], in_=ps[:, 0:ncol]),
                         reads=[self.pbuf[pb_i]], writes=[sttb[j]])
                else:
                    S.op("dve", lambda v, ps=ps, j=j, ncol=ncol: v.tensor_copy(out=stt[j][:, 0:ncol], in_=ps[:, 0:ncol]),
                         reads=[self.pbuf[pb_i]], writes=[sttb[j]])
                S.dma("sp", lambda q, ti=ti, c0=c0, ncol=ncol, j=j: q.dma_start(
                    out=self.ptm_d[ti * P:(ti + 1) * P, c0:c0 + ncol], in_=stt[j][:, 0:ncol]), f"stt{j}",
                    reads=[sttb[j]], writes=[b_ptm])
        if "pfm" in self.debug:
            o = self.dout("dbg_pfm", [N_FM, P, NOWN], BF16)
            for blk in range(N_FM):
                self.final_toks.append(S.dma("sp", lambda q, o=o, blk=blk: q.dma_start(out=o[blk], in_=self.pfm_d[blk]), "dbg", reads=[b_pfm]))
        if "pgl" in self.debug:
            o2 = self.dout("dbg_pgl", [64, NOWN], F32)
            self.final_toks.append(S.dma("sp", lambda q: q.dma_start(out=o2, in_=self.pgl_d), "dbg", reads=[b_pgl]))
        if "ptm" in self.debug:
            o3 = self.dout("dbg_ptm", [NOWN, TM_COLS], BF16)
            for ti in range(NT):
                self.final_toks.append(S.dma("sp", lambda q, ti=ti: q.dma_start(out=o3[ti * P:(ti + 1) * P, :], in_=self.ptm_d[ti * P:(ti + 1) * P, :]),
                                             "dbg", reads=[b_ptm]))
        self.pop_scope()
        self.pop_scope()

    def load_chain_tile(self, i, ti):
        S = self.S
        S.dma("sp", lambda q: q.dma_start(out=self.ktok[i][:], in_=self.ptm_d[ti * P:(ti + 1) * P, TM_KB:TM_KB + 512]), f"ktok{i}",
              reads=[self.B("ptm_d")], writes=[self.b_ktok[i]])
        S.dma("sp", lambda q: q.dma_start(out=self.vtok[i][:], in_=self.ptm_d[ti * P:(ti + 1) * P, TM_VB:TM_VB + 1024]), f"vtok{i}",
              reads=[self.B("ptm_d")], writes=[self.b_vtok[i]])
        S.dma("sp", lambda q: q.dma_start(out=self.glT[i][:], in_=self.pgl_d[:, ti * P:(ti + 1) * P]), f"glT{i}",
              reads=[self.B("pgl_d")], writes=[self.b_glT[i]])

    def phase3(self):
        S = self.S
        self.SR_d = self.dscr("SR_d", [NT, P, 1024], BF16)
        b_SR = self.B("SR_d")
        self.push_scope()
        sbf = [self.sb(f"sbf{i}", [P, 1024], BF16) for i in range(2)]
        sbfb = [self.B(f"sbf{i}") for i in range(2)]
        for idx, ti in enumerate(range(NT - 1, -1, -1)):
            i = idx % 2
            self.load_chain_tile(i, ti)
            self.gate_gp(i, 1)
            S.op("act", lambda a, i=i: a.copy(out=sbf[i][:], in_=self.Sst[1][:]), reads=[self.Sb[1]], writes=[sbfb[i]])
            S.dma("sp", lambda q, i=i, ti=ti: q.dma_start(out=self.SR_d[ti], in_=sbf[i][:]), f"sbf{i}", reads=[sbfb[i]], writes=[b_SR])
            self.chain_update(i, 1)
        self.pop_scope()

    def phase4(self):
        S = self.S
        cb = self.B("consts")
        self.cat_d = self.dscr("cat_d", [NT, P, KC * P], BF16)
        b_cat = self.B("cat_d")
        b_pfm, b_ptm = self.B("pfm_d"), self.B("ptm_d")
        self.push_scope()
        SCALE = float(HD) ** -0.5
        kaT = self.sb("kaT_all", [P, 2, NOWN], BF16)
        va = self.sb("va_all", [P, NT, 256], BF16)
        b_ka, b_va = self.B("kaT_all"), self.B("va_all")
        for h in range(2):
            S.dma("sp", lambda q, h=h: q.dma_start(out=kaT[:, h, :], in_=self.pfm_d[FM_KA + h]), "kaT_all", reads=[b_pfm], writes=[b_ka])
        for t4 in range(4):
            S.dma("sp", lambda q, t4=t4: q.dma_start(out=va[:, t4 * 4:(t4 + 1) * 4, :],
                                                    in_=self.ptm_d[t4 * 512:(t4 + 1) * 512, TM_VA:TM_VA + 256].rearrange("(t p) n -> p t n", p=P)),
                  "va_all", reads=[b_ptm], writes=[b_va])
        esink = self.sb("esink", [P, 8], F32)
        b_es = self.B("esink")
        S.dma("sp", lambda q: q.dma_start(out=esink[:], in_=self.sink.partition_broadcast(P)), "esink", writes=[b_es])
        S.op("act", lambda a: a.activation(out=esink[:], in_=esink[:], func=AF.Exp), reads=[b_es], writes=[b_es])
        normw = self.sb("normw_sb", [P, 8], F32)
        S.dma("sp", lambda q: q.dma_start(out=normw[:], in_=self.normw), "normw", writes=[self.B("normw")])
        qaT = self.sb("qaT", [P, 8, P], BF16)
        qbT = self.sb("qbT", [P, 4, P], BF16)
        kbT = self.sb("kbT", [P, 4, P], BF16)
        rbT = self.sb("rbT", [P, 8, P], BF16)
        SRb = self.sb("SRb", [P, 1024], BF16)
        SFb = self.sb("SFb", [P, 1024], BF16)
        pT = [self.sb(f"pT{i}", [P, 512], BF16) for i in range(2)]
        pTb = [self.B(f"pT{i}") for i in range(2)]
        dsb = self.sb("dsb", [P, 512], F32)
        osb = self.sb("osb", [P, 512], F32)
        catT = self.sb("catT", [P, KC, P], BF16)
        b_catT = self.B("catT")
        E1 = self.sb("E1", [P, 4, P], F32)
        E2 = self.sb("E2", [P, 4, P], F32)
        EB = self.sb("EB", [P, 4, P], F32)
        qm = [self.sb(f"qm{d}", [P, 4, P], BF16) for d in range(2)]
        km = [self.sb(f"km{d}", [P, 4, P], BF16) for d in range(2)]
        qe = [self.sb(f"qe{d}", [P, 4, P], BF16) for d in range(2)]
        ATm = [self.sb(f"ATm{d}", [P, 512], BF16) for d in range(2)]
        attmp = self.sb("attmp", [P, 512], F32)
        osq = self.sb("osq", [P, 1024], F32)
        of = self.sb("of", [P, 1024], F32)
        rinv = self.sb("rinv", [P, 512], F32)
        srb = self.sb("srb", [P, 8, P], F32)
        otmp = self.sb("otmp", [P, P], F32)
        Bn = self.B
        nps = 0
        for ti in range(NT):
            c0 = ti * P
            S.dma("sp", lambda q, c0=c0: q.dma_start(out=qaT[:], in_=self.pfm_d[FM_QA:FM_QA + 8, :, c0:c0 + P].rearrange("b p n -> p b n")),
                  "qaT", reads=[b_pfm], writes=[Bn("qaT")])
            S.dma("sp", lambda q, c0=c0: q.dma_start(out=qbT[:], in_=self.pfm_d[FM_QB:FM_QB + 4, :, c0:c0 + P].rearrange("b p n -> p b n")),
                  "qbT", reads=[b_pfm], writes=[Bn("qbT")])
            S.dma("sp", lambda q, c0=c0: q.dma_start(out=kbT[:], in_=self.pfm_d[FM_KB:FM_KB + 4, :, c0:c0 + P].rearrange("b p n -> p b n")),
                  "kbT", reads=[b_pfm], writes=[Bn("kbT")])
            S.dma("sp", lambda q, c0=c0: q.dma_start(out=rbT[:], in_=self.pfm_d[FM_RB:FM_RB + 8, :, c0:c0 + P].rearrange("b p n -> p b n")),
                  "rbT", reads=[b_pfm], writes=[Bn("rbT")])
            S.dma("sp", lambda q, ti=ti: q.dma_start(out=SRb[:], in_=self.SR_d[ti]), "SRb", reads=[Bn("SR_d")], writes=[Bn("SRb")])
            i = ti % 2
            self.load_chain_tile(i, ti)
            for h in range(2):
                blocks = [("c", 0), ("c", 1)]
                if ti > 0:
                    blocks.append(("p", ti - 1))
                blocks.append(("o", ti))
                blocks.append(("n", ti + 1))
                nb = len(blocks)
                for bi, (kind, idx) in enumerate(blocks):
                    if kind == "c":
                        kT, rk = self.kaT_c[:, h, idx * P:(idx + 1) * P], Bn("kaT_c")
                        vv, rv = self.va_c[:, idx, h * P:(h + 1) * P], Bn("va_c")
                    elif kind == "n" and idx == NT:
                        kT, rk = self.kaT_h[:, h, :], Bn("kaT_h")
                        vv, rv = self.va_h[:, h * P:(h + 1) * P], Bn("va_h")
                    else:
                        kT, rk = kaT[:, h, idx * P:(idx + 1) * P], b_ka
                        vv, rv = va[:, idx, h * P:(h + 1) * P], b_va
                    sb_i = nps % 2
                    nps += 1
                    ps = self.psum[sb_i]
                    S.op("pe", lambda t, kT=kT, h=h, ps=ps: t.matmul(ps[:, :], lhsT=kT, rhs=qaT[:, 4 * h:4 * h + 4, :].rearrange("p a b -> p (a b)"),
                                                                     start=True, stop=True),
                         reads=[rk, Bn("qaT")], writes=[self.pbuf[sb_i]])
                    S.op("act", lambda a, ps=ps, sb_i=sb_i: a.activation(out=pT[sb_i][:], in_=ps[:, :], func=AF.Exp, scale=SCALE),
                         reads=[self.pbuf[sb_i]], writes=[pTb[sb_i]])
                    if kind in ("p", "n"):
                        mk = self.c16["amprev" if kind == "p" else "amnext"]
                        S.op("dve", lambda v, sb_i=sb_i, mk=mk: v.tensor_tensor(out=pT[sb_i][:], in0=pT[sb_i][:], in1=mk[:, :], op=ALU.mult),
                             reads=[pTb[sb_i], cb], writes=[pTb[sb_i]])
                    S.op("pe", [lambda t, vv=vv, sb_i=sb_i, bi=bi, nb=nb: t.matmul(self.psum[2][:, :], lhsT=vv, rhs=pT[sb_i][:], start=(bi == 0), stop=(bi == nb - 1)),
                                lambda t, sb_i=sb_i, bi=bi, nb=nb: t.matmul(self.psum[3][:, :], lhsT=self.c16["ones"][:, :], rhs=pT[sb_i][:], start=(bi == 0), stop=(bi == nb - 1))],
                         reads=[rv, pTb[sb_i], cb], writes=[self.pbuf[2], self.pbuf[3]])
                S.op("act", lambda a: a.copy(out=dsb[:], in_=self.psum[3][:, :]), reads=[self.pbuf[3]], writes=[Bn("dsb")])
                S.op("act", lambda a: a.copy(out=osb[:], in_=self.psum[2][:, :]), reads=[self.pbuf[2]], writes=[Bn("osb")])
                S.op("dve", [lambda v, g=g, h=h: v.tensor_scalar(out=dsb[:, g * P:(g + 1) * P], in0=dsb[:, g * P:(g + 1) * P],
                                                                 scalar1=esink[:, 4 * h + g:4 * h + g + 1], scalar2=None, op0=ALU.add)
                             for g in range(4)], reads=[Bn("dsb"), b_es], writes=[Bn("dsb")])
                S.op("dve", lambda v: v.reciprocal(out=dsb[:], in_=dsb[:]), reads=[Bn("dsb")], writes=[Bn("dsb")])
                S.op("dve", lambda v, h=h: v.tensor_tensor(out=catT[:, 4 * h:4 * h + 4, :].rearrange("p a b -> p (a b)"), in0=osb[:], in1=dsb[:], op=ALU.mult),
                     reads=[Bn("osb"), Bn("dsb")], writes=[b_catT])
            for d in range(2):
                self.gate_gp(i, d)
            S.op("act", lambda a: a.copy(out=SFb[:], in_=self.Sst[0][:]), reads=[self.Sb[0]], writes=[Bn("SFb")])
            for d in range(2):
                m12 = self.c32["m12f" if d == 0 else "m12r"]
                for hb in range(2):
                    S.op("pe", [lambda t, h=h, hb=hb, d=d, m12=m12, i=i: t.matmul(self.psum[6 + hb][:, (h % 2) * 256:(h % 2 + 1) * 256],
                                                                           lhsT=self.gp[i][d][:, h * P:(h + 1) * P], rhs=m12[:, :], start=True, stop=True)
                                for h in (2 * hb, 2 * hb + 1)], reads=[self.b_gp[i][d], cb], writes=[self.pbuf[6 + hb]])
                    src = self.psum[6 + hb][:, :].rearrange("p (h c) -> p h c", h=2)
                    S.op("act", [lambda a, hb=hb, src=src: a.activation(out=E1[:, 2 * hb:2 * hb + 2, :], in_=src[:, :, 0:P], func=AF.Exp),
                                 lambda a, hb=hb, src=src: a.activation(out=E2[:, 2 * hb:2 * hb + 2, :], in_=src[:, :, 0:P], func=AF.Exp, scale=-1.0),
                                 lambda a, hb=hb, src=src: a.activation(out=EB[:, 2 * hb:2 * hb + 2, :], in_=src[:, :, P:2 * P], func=AF.Exp)],
                         reads=[self.pbuf[6 + hb]], writes=[Bn("E123")])
                fl = lambda t_: t_[:].rearrange("p a b -> p (a b)")
                S.op("dve", lambda v, d=d: v.scalar_tensor_tensor(out=fl(qm[d]), in0=fl(qbT), scalar=SCALE, in1=fl(E1), op0=ALU.mult, op1=ALU.mult),
                     reads=[Bn("qbT"), Bn("E123")], writes=[Bn(f"qm{d}")])
                S.op("dve", lambda v, d=d: v.tensor_tensor(out=fl(km[d]), in0=fl(kbT), in1=fl(E2), op=ALU.mult),
                     reads=[Bn("kbT"), Bn("E123")], writes=[Bn(f"km{d}")])
                S.op("dve", lambda v, d=d: v.scalar_tensor_tensor(out=fl(qe[d]), in0=fl(qbT), scalar=SCALE, in1=fl(EB), op0=ALU.mult, op1=ALU.mult),
                     reads=[Bn("qbT"), Bn("E123")], writes=[Bn(f"qe{d}")])
                S.op("pe", [lambda t, h=h, d=d: t.matmul(self.psum[4][:, h * P:(h + 1) * P], lhsT=km[d][:, h, :], rhs=qm[d][:, h, :], start=True, stop=True)
                            for h in range(4)], reads=[Bn(f"qm{d}"), Bn(f"km{d}")], writes=[self.pbuf[4]])
                S.op("act", lambda a: a.copy(out=attmp[:], in_=self.psum[4][:, :]), reads=[self.pbuf[4]], writes=[Bn("attmp")])
                gm = self.c16["gmaskf" if d == 0 else "gmaskr"]
                S.op("dve", lambda v, d=d, gm=gm: v.tensor_tensor(out=ATm[d][:], in0=attmp[:], in1=gm[:, :], op=ALU.mult),
                     reads=[Bn("attmp"), cb], writes=[Bn(f"ATm{d}")])
            for hb in range(2):
                fns = []
                for r in range(4 * hb, 4 * hb + 4):
                    h, c = r // 2, r % 2
                    vs = slice(h * 256 + c * P, h * 256 + (c + 1) * P)
                    dst = self.psum[6 + hb][:, (r % 4) * P:(r % 4 + 1) * P]
                    fns += [lambda t, vs=vs, dst=dst, h=h, i=i: t.matmul(dst, lhsT=self.vtok[i][:, vs], rhs=ATm[0][:, h * P:(h + 1) * P], start=True, stop=False),
                            lambda t, vs=vs, dst=dst, h=h, i=i: t.matmul(dst, lhsT=self.vtok[i][:, vs], rhs=ATm[1][:, h * P:(h + 1) * P], start=False, stop=False),
                            lambda t, vs=vs, dst=dst, h=h: t.matmul(dst, lhsT=SFb[:, vs], rhs=qe[0][:, h, :], start=False, stop=False),
                            lambda t, vs=vs, dst=dst, h=h: t.matmul(dst, lhsT=SRb[:, vs], rhs=qe[1][:, h, :], start=False, stop=True)]
                S.op("pe", fns, reads=[self.b_vtok[i], Bn("ATm0"), Bn("ATm1"), Bn("SFb"), Bn("SRb"), Bn("qe0"), Bn("qe1")], writes=[self.pbuf[6 + hb]])
                S.op("act", [lambda a, hb=hb: a.activation(out=osq[:, hb * 512:(hb + 1) * 512], in_=self.psum[6 + hb][:, :], func=AF.Square),
                             lambda a, hb=hb: a.copy(out=of[:, hb * 512:(hb + 1) * 512], in_=self.psum[6 + hb][:, :])],
                     reads=[self.pbuf[6 + hb]], writes=[Bn("osq_of")])
            S.op("pe", [lambda t, h=h, c=c: t.matmul(self.psum[5][:, h * P:(h + 1) * P], lhsT=self.c32["ones"][:, :],
                                                   rhs=osq[:, (2 * h + c) * P:(2 * h + c + 1) * P], start=(c == 0), stop=(c == 1))
                        for h in range(4) for c in range(2)], reads=[Bn("osq_of"), cb], writes=[self.pbuf[5]])
            S.op("act", lambda a: a.activation(out=rinv[:], in_=self.psum[5][:, :], func=AF.Ln, scale=1.0 / 256.0, bias=self.eps_t[:, 0:1]),
                 reads=[self.pbuf[5], cb], writes=[Bn("rinv")])
            S.op("act", lambda a: a.activation(out=rinv[:], in_=rinv[:], func=AF.Exp, scale=-0.5), reads=[Bn("rinv")], writes=[Bn("rinv")])
            S.op("act", lambda a: a.activation(out=srb[:].rearrange("p a b -> p (a b)"), in_=rbT[:].rearrange("p a b -> p (a b)"), func=AF.Silu),
                 reads=[Bn("rbT")], writes=[Bn("srb")])
            for r in range(8):
                h = r // 2
                S.op("dve", lambda v, r=r, h=h: v.tensor_tensor(out=otmp[:], in0=of[:, r * P:(r + 1) * P], in1=rinv[:, h * P:(h + 1) * P], op=ALU.mult),
                     reads=[Bn("osq_of"), Bn("rinv")], writes=[Bn("otmp")])
                S.op("dve", lambda v, r=r: v.scalar_tensor_tensor(out=catT[:, 8 + r, :], in0=otmp[:], scalar=normw[:, r:r + 1], in1=srb[:, r, :],
                                                                  op0=ALU.mult, op1=ALU.mult),
                     reads=[Bn("otmp"), Bn("srb"), Bn("normw")], writes=[b_catT])
            self.chain_update(i, 0)
            S.dma("sp", lambda q, ti=ti: q.dma_start(out=self.cat_d[ti], in_=catT[:].rearrange("p a b -> p (a b)")), "catT",
                  reads=[b_catT], writes=[b_cat])
        if "cat" in self.debug:
            o = self.dout("dbg_cat", [NT, P, KC * P], BF16)
            for ti in range(NT):
                self.final_toks.append(S.dma("sp", lambda q, ti=ti: q.dma_start(out=o[ti], in_=self.cat_d[ti]), "dbg", reads=[b_cat]))
        self.pop_scope()

    def ln_stats(self, src, b_src):
        S = self.S
        S.op("dve", [lambda v, c=c: v.bn_stats(out=self.stats[:, c, :], in_=src[:, c * 512:(c + 1) * 512]) for c in range(4)],
             reads=[b_src], writes=[self.b_stats])
        S.op("dve", lambda v: v.bn_aggr(out=self.mv[:], in_=self.stats[:].rearrange("p a b -> p (a b)")),
             reads=[self.b_stats], writes=[self.b_mv])
        S.op("act", lambda a: a.activation(out=self.rstd[:], in_=self.mv[:, 1:2], func=AF.Ln, bias=self.eps_t[:, 0:1]),
             reads=[self.b_mv], writes=[self.b_rstd])
        S.op("act", lambda a: a.activation(out=self.rstd[:], in_=self.rstd[:], func=AF.Exp, scale=-0.5),
             reads=[self.b_rstd], writes=[self.b_rstd])

    def phase5(self):
        S = self.S
        cb = self.B("consts")
        Bn = self.B
        self.x1_d = self.dscr("x1_d", [NOWN, D], F32)
        self.xbuf_d = self.dscr("xbuf_d", [N_EXP * CAPS, D], BF16)
        b_x1, b_xbuf = Bn("x1_d"), Bn("xbuf_d")
        self.idx_all = self.sb("idx_all", [P, NT, 2], I32)
        self.w_all = self.sb("w_all", [P, NT, 2], F32)
        self.cnt_bc = self.sb("cnt_bc", [P, N_EXP], F32)
        self.cnt_i = self.sb("cnt_i", [1, N_EXP], I32)
        b_idx, b_w, b_cnt = Bn("idx_all"), Bn("w_all"), Bn("cnt_bc")
        S.op("dve", lambda v: v.memset(self.cnt_bc[:], 0.0), writes=[b_cnt])
        self.push_scope()
        wout = self.sb("wout", [P, KC, D], BF16)
        b_wout = Bn("wout")
        wov = self.w_out.rearrange("(k p) n -> p k n", p=P)
        for c in range(8):
            S.dma("pool", lambda q, c=c: q.dma_start(out=wout[:, :, c * 256:(c + 1) * 256], in_=wov[:, :, c * 256:(c + 1) * 256]),
                  "wout", writes=[b_wout])
        wr = self.sb("wr", [P, KC, 36], BF16)
        S.dma("pool", lambda q: q.dma_start(out=wr[:], in_=self.w_r.rearrange("(k p) n -> p k n", p=P)), "wr", writes=[Bn("wr")])
        brb = self.sb("brb", [P, 36], F32)
        S.dma("sp", lambda q: q.dma_start(out=brb[:], in_=self.b_r.partition_broadcast(P)), "brb", writes=[Bn("brb")])
        bc = [self.sb(f"bc{i}", [P, D], F32) for i in range(3)]
        bcb = [Bn(f"bc{i}") for i in range(3)]
        S.dma("sp", lambda q: q.dma_start(out=bc[0][:], in_=self.mod_d[0, 2 * D:3 * D].partition_broadcast(P)), "bc0",
              reads=[Bn("mod_d")], writes=[bcb[0]])
        S.dma("sp", lambda q: q.dma_start(out=bc[1][:], in_=self.ln1g.partition_broadcast(P)), "bc1", writes=[bcb[1]])
        S.dma("sp", lambda q: q.dma_start(out=bc[2][:], in_=self.ln1b.partition_broadcast(P)), "bc2", writes=[bcb[2]])
        bc2 = [self.sb(f"bcm{i}", [P, D], F32) for i in range(2)]
        bc2b = [Bn(f"bcm{i}") for i in range(2)]
        S.dma("sp", lambda q: q.dma_start(out=bc2[0][:], in_=self.mod_d[0, 3 * D:4 * D].partition_broadcast(P)), "bcm0",
              reads=[Bn("mod_d")], writes=[bc2b[0]])
        S.dma("sp", lambda q: q.dma_start(out=bc2[1][:], in_=self.mod_d[0, 4 * D:5 * D].partition_broadcast(P)), "bcm1",
              reads=[Bn("mod_d")], writes=[bc2b[1]])
        S.op("pool", lambda g: g.tensor_scalar(out=bc2[1][:], in0=bc2[1][:], scalar1=1.0, scalar2=None, op0=ALU.add), reads=[bc2b[1]], writes=[bc2b[1]])
        catT = [self.sb(f"catT{i}", [P, KC, P], BF16) for i in range(2)]
        catb = [Bn(f"catTb{i}") for i in range(2)]
        ysb = self.sb("ysb", [P, D], F32)
        b_ysb = Bn("ysb")
        h2T = self.sb("h2T", [P, KC, P], BF16)
        b_h2T = Bn("h2T")
        lg = self.sb("lg", [P, 36], F32)
        sm = self.sb("rsm", [P, 16], F32)
        gm4 = self.sb("gm4", [P, 4], F32)
        pen = self.sb("pen", [P, 4], F32)
        ge4 = self.sb("ge4", [P, 4], F32)
        elm = self.sb("elm", [P, N_EXP], F32)
        m8 = self.sb("m8", [P, 8], F32)
        A1 = self.sb("A1", [P, N_EXP], F32)
        A2 = self.sb("A2", [P, N_EXP], F32)
        A12 = self.sb("A12", [P, N_EXP], F32)
        posc = self.sb("posc", [P, 64], F32)
        tpos = self.sb("tpos", [P, N_EXP], F32)
        tsl = self.sb("tsl", [P, N_EXP], F32)
        tov = self.sb("tov", [P, N_EXP], F32)
        idxf = self.sb("idxf", [P, 2], F32)
        b_r_ = Bn("route_tmp")
        AX = mybir.AxisListType.X
        for ti in range(NT):
            i = ti % 2
            xin, xb = self.xin[i], self.xinb[i]
            S.dma("sp", lambda q, i=i, ti=ti: q.dma_start(out=catT[i][:].rearrange("p a b -> p (a b)"), in_=self.cat_d[ti]), f"catT{i}",
                  reads=[Bn("cat_d")], writes=[catb[i]])
            S.dma("sp", lambda q, xin=xin, ti=ti: q.dma_start(out=xin[:], in_=self.x_own[ti * P:(ti + 1) * P, :]), f"xin{i}", writes=[xb])
            for n in range(4):
                S.op("pe", [lambda t, k=k, n=n, i=i: t.matmul(self.psum[n][:, :], lhsT=catT[i][:, k, :], rhs=wout[:, k, n * 512:(n + 1) * 512],
                                                             start=(k == 0), stop=(k == KC - 1)) for k in range(KC)],
                     reads=[catb[i], b_wout], writes=[self.pbuf[n]])
                S.op("act", lambda a, n=n: a.copy(out=ysb[:, n * 512:(n + 1) * 512], in_=self.psum[n][:, :]), reads=[self.pbuf[n]], writes=[b_ysb])
            S.op("dve", lambda v: v.tensor_tensor(out=ysb[:], in0=ysb[:], in1=bc[0][:], op=ALU.mult), reads=[b_ysb, bcb[0]], writes=[b_ysb])
            S.op("dve", lambda v, xin=xin: v.scalar_tensor_tensor(out=xin[:], in0=xin[:], scalar=float(ALPHA), in1=ysb[:], op0=ALU.mult, op1=ALU.add),
                 reads=[xb, b_ysb], writes=[xb])
            self.ln_stats(xin, xb)
            S.op("dve", lambda v, xin=xin: v.tensor_scalar(out=ysb[:], in0=xin[:], scalar1=self.mv[:, 0:1], scalar2=self.rstd[:, 0:1],
                                                           op0=ALU.subtract, op1=ALU.mult), reads=[xb, self.b_mv, self.b_rstd], writes=[b_ysb])
            S.op("dve", lambda v: v.tensor_tensor(out=ysb[:], in0=ysb[:], in1=bc[1][:], op=ALU.mult), reads=[b_ysb, bcb[1]], writes=[b_ysb])
            S.op("pool", lambda g: g.tensor_tensor(out=ysb[:], in0=ysb[:], in1=bc[2][:], op=ALU.add), reads=[b_ysb, bcb[2]], writes=[b_ysb])
            S.dma("sp", lambda q, ti=ti: q.dma_start(out=self.x1_d[ti * P:(ti + 1) * P, :], in_=ysb[:]), "x1st", reads=[b_ysb], writes=[b_x1])
            self.ln_stats(ysb, b_ysb)
            S.op("dve", lambda v: v.tensor_scalar(out=ysb[:], in0=ysb[:], scalar1=self.mv[:, 0:1], scalar2=self.rstd[:, 0:1],
                                                  op0=ALU.subtract, op1=ALU.mult), reads=[b_ysb, self.b_mv, self.b_rstd], writes=[b_ysb])
            S.op("pool", lambda g: g.tensor_tensor(out=ysb[:], in0=ysb[:], in1=bc2[1][:], op=ALU.mult), reads=[b_ysb, bc2b[1]], writes=[b_ysb])
            S.op("dve", lambda v, i=i: v.tensor_tensor(out=self.xn[i][:], in0=ysb[:], in1=bc2[0][:], op=ALU.add), reads=[b_ysb, bc2b[0]], writes=[self.xnb[i]])
            self.transpose_mod(self.xn[i][:], self.xnb[i], h2T, b_h2T, 2, 3, banks=(4, 5), plain=True)
            S.op("pe", [lambda t, k=k: t.matmul(self.psum[6][:, 0:36], lhsT=h2T[:, k, :], rhs=wr[:, k, :], start=(k == 0), stop=(k == KC - 1))
                        for k in range(KC)], reads=[b_h2T, Bn("wr")], writes=[self.pbuf[6]])
            S.op("act", lambda a: a.copy(out=lg[:], in_=self.psum[6][:, 0:36]), reads=[self.pbuf[6]], writes=[b_r_])
            R = lambda fn, extra_r=(), extra_w=(): S.op("dve", fn, reads=[b_r_] + list(extra_r), writes=[b_r_] + list(extra_w))
            R(lambda v: v.tensor_tensor(out=lg[:], in0=lg[:], in1=brb[:], op=ALU.add), extra_r=[Bn("brb")])
            R(lambda v: v.tensor_reduce(out=sm[:, 0:1], in_=lg[:, 0:4], axis=AX, op=ALU.max))
            R(lambda v: v.tensor_scalar(out=sm[:, 1:2], in0=sm[:, 0:1], scalar1=-1.0, scalar2=None, op0=ALU.mult))
            S.op("act", lambda a: a.activation(out=ge4[:], in_=lg[:, 0:4], func=AF.Exp, bias=sm[:, 1:2], accum_out=sm[:, 2:3]),
                 reads=[b_r_], writes=[b_r_])
            R(lambda v: v.reciprocal(out=sm[:, 3:4], in_=sm[:, 2:3]))
            R(lambda v: v.tensor_scalar(out=gm4[:], in0=lg[:, 0:4], scalar1=sm[:, 0:1], scalar2=None, op0=ALU.is_ge))
            R(lambda v: v.tensor_scalar(out=pen[:], in0=gm4[:], scalar1=1.0, scalar2=1e30, op0=ALU.subtract, op1=ALU.mult))
            R([lambda v, g=g: v.tensor_scalar(out=elm[:, g * 8:(g + 1) * 8], in0=lg[:, 4 + g * 8:4 + (g + 1) * 8], scalar1=pen[:, g:g + 1],
                                              scalar2=None, op0=ALU.add) for g in range(4)])
            R(lambda v: v.max(out=m8[:], in_=elm[:]))
            R(lambda v: v.tensor_scalar(out=A1[:], in0=elm[:], scalar1=m8[:, 0:1], scalar2=None, op0=ALU.is_equal))
            R(lambda v: v.tensor_scalar(out=A2[:], in0=elm[:], scalar1=m8[:, 1:2], scalar2=None, op0=ALU.is_equal))
            R(lambda v: v.tensor_tensor(out=sm[:, 4:5], in0=m8[:, 1:2], in1=m8[:, 0:1], op=ALU.subtract))
            S.op("act", lambda a: a.activation(out=sm[:, 5:6], in_=sm[:, 4:5], func=AF.Exp), reads=[b_r_], writes=[b_r_])
            R(lambda v: v.tensor_scalar(out=sm[:, 5:6], in0=sm[:, 5:6], scalar1=1.0, scalar2=None, op0=ALU.add))
            R(lambda v: v.reciprocal(out=sm[:, 6:7], in_=sm[:, 5:6]))
            R(lambda v, ti=ti: v.tensor_tensor(out=self.w_all[:, ti, 0:1], in0=sm[:, 3:4], in1=sm[:, 6:7], op=ALU.mult), extra_w=[b_w])
            R(lambda v, ti=ti: v.tensor_tensor(out=self.w_all[:, ti, 1:2], in0=sm[:, 3:4], in1=self.w_all[:, ti, 0:1], op=ALU.subtract),
              extra_r=[b_w], extra_w=[b_w])
            R(lambda v: v.tensor_tensor(out=A12[:], in0=A1[:], in1=A2[:], op=ALU.add))
            S.op("pe", [lambda t: t.matmul(self.psum[7][:, 0:32], lhsT=self.c32["lstrict"][:, :], rhs=A12[:], start=True, stop=True),
                        lambda t: t.matmul(self.psum[7][:, 32:64], lhsT=self.c32["ones"][:, :], rhs=A12[:], start=True, stop=True)],
                 reads=[b_r_, cb], writes=[self.pbuf[7]])
            S.op("act", lambda a: a.copy(out=posc[:], in_=self.psum[7][:, 0:64]), reads=[self.pbuf[7]], writes=[b_r_])
            R(lambda v: v.tensor_tensor(out=tpos[:], in0=posc[:, 0:32], in1=self.cnt_bc[:], op=ALU.add), extra_r=[b_cnt])
            R(lambda v: v.tensor_tensor(out=self.cnt_bc[:], in0=self.cnt_bc[:], in1=posc[:, 32:64], op=ALU.add), extra_r=[b_cnt], extra_w=[b_cnt])
            R(lambda v: v.tensor_scalar(out=tov[:], in0=tpos[:], scalar1=float(CAPS), scalar2=40000.0, op0=ALU.is_ge, op1=ALU.mult))
            R(lambda v: v.tensor_tensor(out=tsl[:], in0=tpos[:], in1=self.c32["slotbase"][:], op=ALU.add), extra_r=[cb])
            R(lambda v: v.tensor_tensor(out=tsl[:], in0=tsl[:], in1=tov[:], op=ALU.add))
            R(lambda v: v.tensor_tensor(out=A1[:], in0=A1[:], in1=tsl[:], op=ALU.mult))
            R(lambda v: v.tensor_tensor(out=A2[:], in0=A2[:], in1=tsl[:], op=ALU.mult))
            R(lambda v: v.tensor_reduce(out=idxf[:, 0:1], in_=A1[:], axis=AX, op=ALU.add))
            R(lambda v: v.tensor_reduce(out=idxf[:, 1:2], in_=A2[:], axis=AX, op=ALU.add))
            R(lambda v, ti=ti: v.tensor_copy(out=self.idx_all[:, ti, :], in_=idxf[:]), extra_w=[b_idx])
            for j in range(2):
                S.dma("pool", lambda g, ti=ti, j=j, i=i: g.indirect_dma_start(
                    out=self.xbuf_d[:, :], out_offset=bass.IndirectOffsetOnAxis(ap=self.idx_all[:, ti, j:j + 1], axis=0),
                    in_=self.xn[i][:, :], in_offset=None, bounds_check=S.rt["bnd"], oob_is_err=False),
                    f"scat{i}", reads=[self.xnb[i], b_idx], writes=[Bn(f"xbuf_sc{i}")])
            if ti == 0 and "lg" in self.debug:
                self.dbg_out("lg", lg[:], [P, 36], F32, [b_r_])
        S.op("dve", lambda v: v.tensor_copy(out=self.cnt_i[:], in_=self.cnt_bc[0:1, :]), reads=[b_cnt], writes=[Bn("cnt_i")])
        zt = self.sb("zt", [P, D], BF16)
        S.op("pool", lambda g: g.memset(zt[:], 0.0), writes=[Bn("zt")])
        ci = self.sb("ci", [P, N_EXP], I32)
        rf = self.sb("rf", [P, N_EXP], F32)
        zi = self.sb("zi", [P, N_EXP], I32)
        bz = Bn("ztmp")
        Z = lambda fn, extra_r=(): S.op("dve", fn, reads=[bz, b_cnt, cb] + list(extra_r), writes=[bz])
        Z(lambda v: v.tensor_copy(out=ci[:], in_=self.cnt_bc[:]))
        Z(lambda v: v.tensor_single_scalar(out=ci[:], in_=ci[:], scalar=127, op=ALU.bitwise_and))
        Z(lambda v: v.tensor_copy(out=rf[:], in_=ci[:]))
        Z(lambda v: v.tensor_scalar(out=rf[:], in0=rf[:], scalar1=self.c32["iotap"][:, 0:1], scalar2=128.0, op0=ALU.add, op1=ALU.is_ge))
        Z(lambda v: v.tensor_scalar(out=rf[:], in0=rf[:], scalar1=40000.0, scalar2=self.c32["iotap"][:, 0:1], op0=ALU.mult, op1=ALU.add))
        Z(lambda v: v.tensor_tensor(out=rf[:], in0=rf[:], in1=self.cnt_bc[:], op=ALU.add))
        Z(lambda v: v.tensor_tensor(out=rf[:], in0=rf[:], in1=self.c32["slotbase"][:], op=ALU.add))
        Z(lambda v: v.tensor_copy(out=zi[:], in_=rf[:]))
        import os
        for e in range(int(os.environ.get("NZS", N_EXP))):
            S.dma("pool", lambda g, e=e: g.indirect_dma_start(
                out=self.xbuf_d[:, :], out_offset=bass.IndirectOffsetOnAxis(ap=zi[:, e:e + 1], axis=0),
                in_=zt[:, :], in_offset=None, bounds_check=S.rt["bnd"], oob_is_err=False),
                "scatz", reads=[Bn("zt"), bz], writes=[b_xbuf])
        self.dbg_out("zi", zi[:], [P, N_EXP], I32, [bz])
        self.dbg_out("rf", rf[:], [P, N_EXP], F32, [bz])
        for nm, t_, shp, dt, bb in (("idx_all", self.idx_all, [P, NT, 2], I32, b_idx), ("w_all", self.w_all, [P, NT, 2], F32, b_w),
                                    ("cnt", self.cnt_bc, [P, N_EXP], F32, b_cnt)):
            self.dbg_out(nm, t_[:], shp, dt, [bb])
        if "x1" in self.debug:
            o = self.dout("dbg_x1", [NOWN, D], F32)
            for ti in range(NT):
                self.final_toks.append(S.dma("sp", lambda q, ti=ti, o=o: q.dma_start(out=o[ti * P:(ti + 1) * P, :], in_=self.x1_d[ti * P:(ti + 1) * P, :]),
                                             "dbg", reads=[b_x1]))
        self.pop_scope()

    def phase6(self):
        S = self.S
        Bn = self.B
        cb = Bn("consts")
        self.obuf_d = self.dscr("obuf_d", [N_EXP * CAPS, D], F32)
        b_ob = Bn("obuf_d")
        b_xbuf = Bn("xbuf_d")
        self.push_scope()
        NWB = 3
        wg = [self.sb(f"wg{i}", [P, KC, 512], BF16) for i in range(NWB)]
        wu = [self.sb(f"wu{i}", [P, KC, 512], BF16) for i in range(NWB)]
        wd = [self.sb(f"wd{i}", [P, 4, D], BF16) for i in range(NWB)]
        wsb = [Bn(f"wset{i}") for i in range(NWB)]
        Gs = [self.sb(f"G{i}", [P, D], BF16) for i in range(2)]
        Gb = [Bn(f"G{i}") for i in range(2)]
        hxT = self.sb("hxT", [P, KC, P], BF16)
        sg = [self.sb(f"sg{i}", [P, 512], F32) for i in range(2)]
        su = [self.sb(f"su{i}", [P, 512], F32) for i in range(2)]
        hid = self.sb("hid", [P, 1024], BF16)
        hidT = self.sb("hidT", [P, 8, P], BF16)
        ost = self.xin[0]
        idn = self.c16["ident"]
        dm = self.dummy
        dummies = {
            "pe": lambda t, k: t.matmul(self.psum[0][0:1, k:k + 1], lhsT=idn[:, 0:1], rhs=idn[:, 0:1], start=True, stop=True),
            "act": lambda a, k: a.copy(out=dm[0:1, k:k + 1], in_=dm[0:1, 7:8]),
            "dve": lambda v, k: v.memset(dm[32:33, k:k + 1], 0.0),
            "sp": lambda q, k: q.dma_start(out=dm[64:65, k:k + 1], in_=dm[64:65, 7:8]),
        }
        nsec = 0
        import os
        n_exp = int(os.environ.get("MOE_NEXP", N_EXP))
        dbank = (6, 7, 2, 3)
        for e in range(n_exp):
            gv = self.weg[e].rearrange("(k p) n -> p k n", p=P)
            uv = self.weu[e].rearrange("(k p) n -> p k n", p=P)
            wb_i = [(2 * e + half) % NWB for half in range(2)]
            for half in range(2):
                s_ = wb_i[half]
                dv = self.wed[e, half * 512:(half + 1) * 512, :].rearrange("(k p) n -> p k n", p=P)
                for c in range(2):
                    S.dma("pool", lambda q, s_=s_, gv=gv, half=half, c=c: q.dma_start(
                        out=wg[s_][:, :, c * 256:(c + 1) * 256], in_=gv[:, :, half * 512 + c * 256:half * 512 + (c + 1) * 256]),
                        f"wset{s_}", writes=[wsb[s_]])
                    S.dma("pool", lambda q, s_=s_, uv=uv, half=half, c=c: q.dma_start(
                        out=wu[s_][:, :, c * 256:(c + 1) * 256], in_=uv[:, :, half * 512 + c * 256:half * 512 + (c + 1) * 256]),
                        f"wset{s_}", writes=[wsb[s_]])
                for c in range(WD_SPLIT):
                    S.dma("pool", lambda q, s_=s_, dv=dv, c=c: q.dma_start(out=wd[s_][:, c:c + 1, :], in_=dv[:, c:c + 1, :]),
                          f"wset{s_}", writes=[wsb[s_]])
            S.reg_load("cnt", self.cnt_i[0:1, e:e + 1], Bn("cnt_i"))
            for j in range(int(os.environ.get("MOE_CAPB", CAPB))):
                r0 = e * CAPS + j * P
                S.begin_if("cnt", j * P)
                gi_ = nsec % 2
                nsec += 1
                G = Gs[gi_]
                S.dma("sp", lambda q, r0=r0, G=G: q.dma_start(out=G[:], in_=self.xbuf_d[r0:r0 + P, :]), f"G{gi_}", reads=[b_xbuf], writes=[Gb[gi_]])
                self.transpose_mod(G[:], Gb[gi_], hxT, Bn("hxT"), 2, 3, banks=(0, 1), plain=True)
                for half in range(2):
                    s_ = wb_i[half]
                    bg, bu = (2, 3) if half == 0 else (4, 5)
                    S.op("pe", [lambda t, k=k, s_=s_, bg=bg: t.matmul(self.psum[bg][:, :], lhsT=hxT[:, k, :], rhs=wg[s_][:, k, :], start=(k == 0), stop=(k == KC - 1))
                                for k in range(KC)] +
                               [lambda t, k=k, s_=s_, bu=bu: t.matmul(self.psum[bu][:, :], lhsT=hxT[:, k, :], rhs=wu[s_][:, k, :], start=(k == 0), stop=(k == KC - 1))
                                for k in range(KC)], reads=[Bn("hxT"), wsb[s_]], writes=[self.pbuf[bg], self.pbuf[bu]])
                    S.op("act", [lambda a, half=half, bg=bg: a.activation(out=sg[half][:], in_=self.psum[bg][:, :], func=AF.Silu),
                                 lambda a, half=half, bu=bu: a.copy(out=su[half][:], in_=self.psum[bu][:, :])],
                         reads=[self.pbuf[bg], self.pbuf[bu]], writes=[Bn(f"sgsu{half}")])
                    S.op("dve", lambda v, half=half: v.tensor_tensor(out=hid[:, half * 512:(half + 1) * 512], in0=sg[half][:], in1=su[half][:], op=ALU.mult),
                         reads=[Bn(f"sgsu{half}")], writes=[Bn(f"hid{half}")])
                pb0 = self.psum[0][:].bitcast(BF16)
                for half in range(2):
                    S.op("pe", [lambda t, c=c, pb0=pb0: t.transpose(out=pb0[:, c * P:(c + 1) * P], in_=hid[:, c * P:(c + 1) * P], identity=idn[:])
                                for c in range(4 * half, 4 * half + 4)], reads=[Bn(f"hid{half}"), cb], writes=[self.pbuf[0]])
                S.op("act", lambda a, pb0=pb0: a.copy(out=hidT[:].rearrange("p a b -> p (a b)"), in_=pb0[:, :]), reads=[self.pbuf[0]], writes=[Bn("hidT")])
                for n in range(4):
                    db = dbank[n]
                    S.op("pe", [lambda t, c=c, n=n, db=db, wb_i=tuple(wb_i): t.matmul(self.psum[db][:, :], lhsT=hidT[:, c, :], rhs=wd[wb_i[c // 4]][:, c % 4, n * 512:(n + 1) * 512],
                                                                    start=(c == 0), stop=(c == 7)) for c in range(8)],
                         reads=[Bn("hidT"), wsb[wb_i[0]], wsb[wb_i[1]]], writes=[self.pbuf[db]])
                    if n % 2 == 0:
                        S.op("act", lambda a, n=n, db=db: a.copy(out=ost[:, n * 512:(n + 1) * 512], in_=self.psum[db][:, :]), reads=[self.pbuf[db]], writes=[self.xinb[0]])
                    else:
                        S.op("dve", lambda v, n=n, db=db: v.tensor_copy(out=ost[:, n * 512:(n + 1) * 512], in_=self.psum[db][:, :]), reads=[self.pbuf[db]], writes=[self.xinb[0]])
                S.dma("act", lambda q, r0=r0: q.dma_start(out=self.obuf_d[r0:r0 + P, :], in_=ost[:]), "ost", reads=[self.xinb[0]], writes=[b_ob])
                S.end_if(dummies)
        self.pop_scope()

    def phase7(self):
        S = self.S
        Bn = self.B
        self.push_scope()
        bc = [self.sb(f"bcf{i}", [P, D], F32) for i in range(3)]
        bcb = [Bn(f"bcf{i}") for i in range(3)]
        S.dma("sp", lambda q: q.dma_start(out=bc[0][:], in_=self.mod_d[0, 5 * D:6 * D].partition_broadcast(P)), "bcf0",
              reads=[Bn("mod_d")], writes=[bcb[0]])
        S.dma("sp", lambda q: q.dma_start(out=bc[1][:], in_=self.ln2g.partition_broadcast(P)), "bcf1", writes=[bcb[1]])
        S.dma("sp", lambda q: q.dma_start(out=bc[2][:], in_=self.ln2b.partition_broadcast(P)), "bcf2", writes=[bcb[2]])
        og = [[self.sb(f"og{i}{j}", [P, D], F32) for j in range(2)] for i in range(2)]
        ogb = [[Bn(f"og{i}{j}") for j in range(2)] for i in range(2)]
        fsb = [self.sb(f"fsb{i}", [P, D], F32) for i in range(2)]
        fb = [Bn(f"fsb{i}") for i in range(2)]
        b_ob = Bn("obuf_d")
        for ti in range(NT):
            i = ti % 2
            xin, xb = self.xin[i], self.xinb[i]
            f_, b_f = fsb[i], fb[i]
            S.dma("sp", lambda q, xin=xin, ti=ti: q.dma_start(out=xin[:], in_=self.x1_d[ti * P:(ti + 1) * P, :]), f"xin{i}",
                  reads=[Bn("x1_d")], writes=[xb])
            for j in range(2):
                S.dma("pool", lambda g, i=i, j=j, ti=ti: g.indirect_dma_start(
                    out=og[i][j][:, :], out_offset=None, in_=self.obuf_d[:, :],
                    in_offset=bass.IndirectOffsetOnAxis(ap=self.idx_all[:, ti, j:j + 1], axis=0),
                    bounds_check=S.rt["bnd"], oob_is_err=False), f"og{i}{j}", reads=[b_ob, Bn("idx_all")], writes=[ogb[i][j]])
            S.op("dve", lambda v, ti=ti, i=i, f_=f_: v.tensor_scalar(out=f_[:], in0=og[i][0][:], scalar1=self.w_all[:, ti, 0:1], scalar2=None, op0=ALU.mult),
                 reads=[ogb[i][0], Bn("w_all")], writes=[b_f])
            S.op("dve", lambda v, ti=ti, i=i, f_=f_: v.scalar_tensor_tensor(out=f_[:], in0=og[i][1][:], scalar=self.w_all[:, ti, 1:2], in1=f_[:],
                                                                          op0=ALU.mult, op1=ALU.add), reads=[ogb[i][1], Bn("w_all"), b_f], writes=[b_f])
            S.op("pool", lambda g, f_=f_: g.tensor_tensor(out=f_[:], in0=f_[:], in1=bc[0][:], op=ALU.mult), reads=[b_f, bcb[0]], writes=[b_f])
            S.op("dve", lambda v, xin=xin, f_=f_: v.scalar_tensor_tensor(out=xin[:], in0=xin[:], scalar=float(ALPHA), in1=f_[:], op0=ALU.mult, op1=ALU.add),
                 reads=[xb, b_f], writes=[xb])
            self.ln_stats(xin, xb)
            S.op("dve", lambda v, xin=xin, f_=f_: v.tensor_scalar(out=f_[:], in0=xin[:], scalar1=self.mv[:, 0:1], scalar2=self.rstd[:, 0:1],
                                                                op0=ALU.subtract, op1=ALU.mult), reads=[xb, self.b_mv, self.b_rstd], writes=[b_f])
            S.op("pool", lambda g, f_=f_: g.tensor_tensor(out=f_[:], in0=f_[:], in1=bc[1][:], op=ALU.mult), reads=[b_f, bcb[1]], writes=[b_f])
            S.op("pool", lambda g, f_=f_: g.tensor_tensor(out=f_[:], in0=f_[:], in1=bc[2][:], op=ALU.add), reads=[b_f, bcb[2]], writes=[b_f])
            self.final_toks.append(S.dma("sp", lambda q, ti=ti, f_=f_: q.dma_start(out=self.y[ti * P:(ti + 1) * P, :], in_=f_[:]), f"yout{i}",
                                         reads=[b_f], writes=[Bn("y")]))
        self.pop_scope()

    def finish(self):
        S = self.S
        S.final_wait("sp", self.final_toks)
        with self.nc.Block() as block:
            S.emit(block)
        while self.scopes:
            self.scopes.pop().close()
        self.es.close()
        return self.nc


def build_program(debug=(), upto="all", n_other=NT):
    b = Builder(debug)
    b.n_other = n_other
    b.declare_inputs(with_experts=(upto in ("all", "moe")))
    b.load_consts()
    b.phase0()
    if upto == "p0":
        return b.finish(), b
    b.phase1()
    if upto == "p1":
        return b.finish(), b
    b.phase2()
    if upto == "p2":
        return b.finish(), b
    b.phase3()
    b.phase4()
    b.pop_scope()
    if upto == "p4":
        return b.finish(), b
    b.phase5()
    if upto == "p5":
        return b.finish(), b
    b.phase6()
    b.phase7()
    return b.finish(), b


def _win_layout(w_in, half):
    qa, ka, va, qb, kb, vb, rb, gb = 0, 1024, 1280, 1536, 2048, 2560, 3584, 4608
    out = np.zeros((D, WIN_COLS), np.float32)
    out[:, FM_QA * 128:FM_QA * 128 + 1024] = w_in[:, qa:qa + 1024]
    out[:, FM_KA * 128:FM_KA * 128 + 256] = w_in[:, ka:ka + 256]
    out[:, FM_QB * 128:FM_QB * 128 + 512] = w_in[:, qb:qb + 512]
    out[:, FM_KB * 128:FM_KB * 128 + 512] = w_in[:, kb:kb + 512]
    out[:, FM_RB * 128:FM_RB * 128 + 1024] = w_in[:, rb:rb + 1024]
    gF, gR = (gb, gb + 16) if half == 0 else (gb + 16, gb)
    out[:, GB_OFF:GB_OFF + 16] = w_in[:, gF:gF + 16]
    out[:, GB_OFF + 32:GB_OFF + 48] = w_in[:, gR:gR + 16]
    out[:, TM_OFF + TM_VA:TM_OFF + TM_VA + 256] = w_in[:, va:va + 256]
    out[:, TM_OFF + TM_KB:TM_OFF + TM_KB + 512] = w_in[:, kb:kb + 512]
    out[:, TM_OFF + TM_VB:TM_OFF + TM_VB + 1024] = w_in[:, vb:vb + 1024]
    return out


def prep_inputs(inp, cores=range(8)):
    f = lambda a: np.ascontiguousarray(np.asarray(a, dtype=np.float32))
    x, c, ctx, c_ctx = f(inp["x"]), f(inp["c"]), f(inp["ctx"]), f(inp["c_ctx"])
    w_ada, b_ada = f(inp["w_ada"])[0], f(inp["b_ada"])[0]
    w_in = f(inp["w_in"])[0]
    wgu_in, bg_in = f(inp["w_gate_up"])[0], f(inp["b_gate"])[0]
    consts = _consts()
    shared = {
        "w_ada": w_ada, "b_ada": b_ada, "sink": f(inp["attn_sink"])[0],
        "normw": np.ascontiguousarray(f(inp["gla_norm_w"])[0].reshape(8, P).T),
        "w_out": f(inp["w_out"])[0], "ln1g": f(inp["ln1_g"])[0], "ln1b": f(inp["ln1_b"])[0],
        "ln2g": f(inp["ln2_g"])[0], "ln2b": f(inp["ln2_b"])[0],
        "w_r": np.ascontiguousarray(np.concatenate([f(inp["w_router_group"])[0], f(inp["w_router_expert"])[0]], axis=1)),
        "b_r": np.ascontiguousarray(np.concatenate([f(inp["b_router_group"])[0], f(inp["b_router_expert"])[0]])),
        "weg": f(inp["w_exp_gate"])[0], "weu": f(inp["w_exp_up"])[0], "wed": f(inp["w_exp_down"])[0],
    }
    for k, v in consts.items():
        shared["c_" + k] = v
    per_half = {}
    for half in (0, 1):
        cosT, sinT = _rope_tables(half)
        wgu = np.zeros((2, 64, 512), np.float32)
        sF, sR = (0, 1) if half == 0 else (1, 0)
        wgu[0, 0:16] = wgu_in[sF]
        wgu[1, 32:48] = wgu_in[sR]
        bgate = np.ascontiguousarray(np.stack([bg_in[sF], bg_in[sR]]))
        per_half[half] = {"w_in": _win_layout(w_in, half), "wgu": wgu, "bgate": bgate, "cosT": cosT, "sinT": sinT}
    maps = []
    for core in cores:
        b, half = core // 2, core % 2
        xl = x[b] if half == 0 else x[b][::-1]
        cl = ctx[b] if half == 0 else ctx[b][::-1]
        ccv = np.stack([c[b].reshape(KC, P).T, c_ctx.reshape(KC, P).T], axis=2).reshape(P, 32)
        m = {"x_own": np.ascontiguousarray(xl[:NOWN]), "x_oth": np.ascontiguousarray(xl[NOWN:]),
             "ctxl": np.ascontiguousarray(cl), "cc": np.ascontiguousarray(ccv)}
        m.update(per_half[half])
        m.update(shared)
        maps.append(m)
    return maps


def kernel(**inputs):
    nc, b = build_program()
    maps = prep_inputs(inputs)
    res = run_bass_kernel_spmd(nc, maps, core_ids=list(range(8)))
    out = np.zeros((4, SEQ, D), np.float32)
    for core in range(8):
        bb, half = core // 2, core % 2
        yc = np.asarray(res.results[core]["y"])
        if half == 0:
            out[bb, :NOWN] = yc
        else:
            out[bb, NOWN:] = yc[::-1]
    return out
```

```python
import numpy as np
from contextlib import ExitStack
import concourse.bass as bass
import concourse.mybir as mybir
from concourse.bass_utils import run_bass_kernel_spmd

F32 = mybir.dt.float32
BF16 = mybir.dt.bfloat16
I32 = mybir.dt.int32
AF = mybir.ActivationFunctionType
ALU = mybir.AluOpType

D = 2048
KC = 16
SEQ = 4096
NOWN = 2048
NT = 16
P = 128
LN_EPS = 1e-6
ALPHA = 2.0 ** 0.25
HD = 128
N_EXP = 32
HID = 1024
CAPB = 8
CAPS = CAPB * 128
WD_SPLIT = 4

FM_QA, FM_KA, FM_QB, FM_KB, FM_RB = 0, 8, 10, 14, 18
N_FM = 26
FM_COLS = N_FM * 128
GB_OFF = FM_COLS
TM_OFF = GB_OFF + 64
TM_VA, TM_KB, TM_VB = 0, 256, 768
TM_COLS = 1792
WIN_COLS = TM_OFF + TM_COLS


class Buf:
    __slots__ = ("name", "w", "r")

    def __init__(self, name):
        self.name = name
        self.w = None
        self.r = []


class Sched:
    ENG = ("pe", "act", "dve", "pool", "sp")

    def __init__(self, nc, es):
        self.nc = nc
        self.es = es
        self.eng = {"pe": nc.tensor, "act": nc.scalar, "dve": nc.vector, "pool": nc.gpsimd, "sp": nc.sync}
        self.items = {e: [] for e in self.ENG}
        self.sems = {}
        self.cnt = {}
        self.waited = {e: {} for e in self.ENG}
        for e in self.ENG:
            self._sem("E_" + e)
        self.in_if = None
        self.rt = {}
        self.if_keys = {}

    def _sem(self, key):
        if key not in self.sems:
            self.sems[key] = self.es.enter_context(self.nc.semaphore("s_" + key))
            self.cnt[key] = 0
        return self.sems[key]

    def _need(self, engine, tok):
        if tok is None:
            return
        key, val = tok
        if self.waited[engine].get(key, 0) >= val:
            return
        self.waited[engine][key] = val
        self.items[engine].append(("wait", key, val))

    def _need_all(self, engine, toks):
        best = {}
        for t in toks:
            if t is not None and best.get(t[0], 0) < t[1]:
                best[t[0]] = t[1]
        for k, v in best.items():
            self._need(engine, (k, v))

    def _deps(self, engine, reads, writes):
        toks = [b.w for b in reads]
        for b in writes:
            toks.append(b.w)
            toks.extend(b.r)
        self._need_all(engine, toks)

    def _commit(self, tok, reads, writes):
        for b in reads:
            b.r.append(tok)
        for b in writes:
            b.w = tok
            b.r = []

    def op(self, engine, fns, reads=(), writes=()):
        if not isinstance(fns, (list, tuple)):
            fns = [fns]
        self._deps(engine, reads, writes)
        key = "E_" + engine
        self.cnt[key] += 1
        tok = (key, self.cnt[key])
        self.items[engine].append(("ops", list(fns), key, 1))
        if engine == "pe":
            self.waited[engine][key] = tok[1]
        self._commit(tok, reads, writes)
        if self.in_if is not None:
            self.in_if["incs"].setdefault(engine, {}).setdefault(key, 0)
            self.in_if["incs"][engine][key] += 1
        return tok

    def dma(self, engine, fn, key, reads=(), writes=()):
        key = "D_" + key + "_" + engine
        self._sem(key)
        toks = [b.w for b in reads]
        for b in writes:
            if not (b.w is not None and b.w[0] == key):
                toks.append(b.w)
            toks.extend(b.r)
        self._need_all(engine, toks)
        self.cnt[key] += 16
        tok = (key, self.cnt[key])
        self.items[engine].append(("ops", [fn], key, 16))
        self._commit(tok, reads, writes)
        if self.in_if is not None:
            self.in_if["incs"].setdefault(engine, {}).setdefault(key, 0)
            self.in_if["incs"][engine][key] += 16
        return tok

    IF_ENG = ("pe", "act", "dve", "sp")

    def reg_load(self, regname, ap, buf):
        for e in self.IF_ENG:
            self._need(e, buf.w)
            self.items[e].append(("regload", regname, ap))

    def begin_if(self, regname, thr):
        assert self.in_if is None
        self.in_if = {"incs": {}, "start": {e: len(self.items[e]) for e in self.ENG},
                      "waited": {e: dict(self.waited[e]) for e in self.ENG},
                      "cnt0": dict(self.cnt)}
        for e in self.IF_ENG:
            self.items[e].append(("if", regname, thr))

    def end_if(self, dummies):
        st = self.in_if
        self.in_if = None
        for e in self.ENG:
            incs = st["incs"].get(e, {})
            if e not in self.IF_ENG:
                assert not incs, f"engine {e} cannot be used inside a dynamic section"
                del self.items[e][st["start"][e]:]
            else:
                self.if_keys.setdefault(e, set()).update(incs.keys())
                wk = set(self.if_keys[e]) if e == "sp" else (set("E_" + x for x in ("pe", "act", "dve")) | set(self.if_keys[e]))
                self.items[e].append(("else", dict(incs), dummies[e], {k: st["cnt0"].get(k, 0) for k in wk}))
                self.items[e].append(("endif",))
            self.waited[e] = st["waited"][e]

    def barrier(self):
        for e in self.ENG:
            for key, c in self.cnt.items():
                if c > 0:
                    self._need(e, (key, c))

    def final_wait(self, engine, toks):
        for t in toks:
            self._need(engine, t)

    def emit(self, block):
        nc = self.nc
        deco = {"pe": block.tensor, "act": block.scalar, "dve": block.vector, "pool": block.gpsimd, "sp": block.sync}
        for e in self.ENG:
            items = self.items[e]

            def body(eng, items=items, e=e):
                regs = {}
                stack = []
                with ExitStack() as rs:
                    if e == "pool":
                        self.rt["bnd"] = rs.enter_context(eng.register("bnd_reg"))
                        eng.reg_mov(self.rt["bnd"], N_EXP * CAPS - 1)
                    for it in items:
                        k = it[0]
                        if k == "wait":
                            eng.wait_ge(self.sems[it[1]], it[2])
                        elif k == "ops":
                            ins = None
                            for fn in it[1]:
                                ins = fn(eng)
                            ins.then_inc(self.sems[it[2]], it[3])
                        elif k == "regload":
                            if it[1] not in regs:
                                regs[it[1]] = rs.enter_context(eng.register(it[1] + "_" + e))
                            eng.reg_load(regs[it[1]], it[2])
                        elif k == "if":
                            g = eng.If_cmp(regs[it[1]], it[2], "IS_GT")
                            g.__enter__()
                            stack.append(g)
                        elif k == "else":
                            g = stack.pop()
                            g.__exit__(None, None, None)
                            g2 = eng.Else()
                            g2.__enter__()
                            for key, val in it[3].items():
                                if val > 0:
                                    eng.wait_ge(self.sems[key], val)
                            for kk, (key, inc) in enumerate(it[1].items()):
                                it[2](eng, kk).then_inc(self.sems[key], inc)
                            stack.append(g2)
                        elif k == "endif":
                            g = stack.pop()
                            g.__exit__(None, None, None)
            deco[e](body)


def _rope_tables(half):
    n_freq = HD // 4
    inv_freq = (10000.0 ** (-np.arange(n_freq, dtype=np.float32) / n_freq)).astype(np.float32)
    j = np.arange(NOWN + 128)
    g = j if half == 0 else (SEQ - 1 - j)
    row = (g // 64).astype(np.float32)
    col = (g % 64).astype(np.float32)
    cos = np.zeros((HD, NOWN + 128), np.float32)
    sin = np.zeros((HD, NOWN + 128), np.float32)
    for d in range(HD):
        seg, f = d // 32, d % 32
        ang = (row if seg < 2 else col) * inv_freq[f]
        cos[d] = np.cos(ang.astype(np.float32))
        s = np.sin(ang.astype(np.float32))
        sin[d] = -s if seg in (0, 2) else s
    return cos, sin


def _consts():
    c = {}
    c["ident"] = np.eye(P, dtype=np.float32)
    rm = np.zeros((P, P), np.float32)
    for m in range(P):
        seg = m // 32
        partner = m + 32 if seg in (0, 2) else m - 32
        rm[partner, m] = 1.0
    c["rotm"] = rm
    s = np.arange(P)[:, None]
    t = np.arange(P)[None, :]
    le = (s <= t).astype(np.float32)
    ge = (s >= t).astype(np.float32)
    m1f = -(le - (s <= 63).astype(np.float32)) / 16.0
    m2f = -le / 16.0
    m1r = -(ge - (s >= 64).astype(np.float32)) / 16.0
    m2r = -ge / 16.0
    c["m12f"] = np.concatenate([m1f, m2f], axis=1).astype(np.float32)
    c["m12r"] = np.concatenate([m1r, m2r], axis=1).astype(np.float32)
    c["m3f"] = (-(s > t).astype(np.float32) / 16.0)
    c["m3r"] = (-(s < t).astype(np.float32) / 16.0)
    c["gmaskf"] = np.tile(le, (1, 4))
    c["gmaskr"] = np.tile(ge, (1, 4))
    c["amprev"] = np.tile(ge, (1, 4))
    c["amnext"] = np.tile(le, (1, 4))
    c["ones"] = np.ones((P, P), np.float32)
    c["n16"] = np.full((P, 1), -1.0 / 16.0, np.float32)
    c["lstrict"] = (s < t).astype(np.float32)
    c["slotbase"] = np.tile((np.arange(N_EXP, dtype=np.float32) * CAPS)[None, :], (P, 1))
    c["iotap"] = np.arange(P, dtype=np.float32)[:, None].copy()
    return c


CONST_SHAPES = {"ident": (P, P), "rotm": (P, P), "m12f": (P, 256), "m12r": (P, 256), "m3f": (P, P), "m3r": (P, P),
                "gmaskf": (P, 512), "gmaskr": (P, 512), "amprev": (P, 512), "amnext": (P, 512), "ones": (P, P),
                "n16": (P, 1), "lstrict": (P, P), "slotbase": (P, N_EXP), "iotap": (P, 1)}


class Builder:
    def __init__(self, debug=()):
        self.debug = set(debug)
        self.nc = bass.Bass("TRN2", target_bir_lowering=False)
        self.es = ExitStack()
        self.S = Sched(self.nc, self.es)
        self.bufs = {}
        self.outs = []
        self.final_toks = []
        self.scopes = []
        self.nalloc = 0
        self.alloc_log = []

    def sb(self, name, shape, dt):
        stack = self.scopes[-1] if self.scopes else self.es
        self.nalloc += 1
        nb = int(np.prod(shape[1:])) * (4 if dt in (F32, I32) else 2)
        self.alloc_log.append((name, nb, len(self.scopes)))
        return stack.enter_context(self.nc.sbuf_tensor(f"{name}_{self.nalloc}", list(shape), dt))

    def push_scope(self):
        self.scopes.append(ExitStack())

    def pop_scope(self):
        self.S.barrier()
        self.scopes.pop().close()

    def din(self, name, shape, dt=F32):
        return self.nc.dram_tensor(name, list(shape), dt, kind="ExternalInput").ap()

    def dout(self, name, shape, dt=F32):
        self.outs.append(name)
        return self.nc.dram_tensor(name, list(shape), dt, kind="ExternalOutput").ap()

    def dscr(self, name, shape, dt):
        return self.nc.dram_tensor(name, list(shape), dt, kind="Internal").ap()

    def B(self, name):
        if name not in self.bufs:
            self.bufs[name] = Buf(name)
        return self.bufs[name]

    def dbg_out(self, name, src_ap, shape, dt, reads, eng="sp"):
        if name not in self.debug:
            return
        o = self.dout("dbg_" + name, shape, dt)
        tok = self.S.dma(eng, lambda q, o=o, s=src_ap: q.dma_start(out=o, in_=s), "dbg", reads=reads)
        self.final_toks.append(tok)

    def declare_inputs(self, with_experts=True):
        self.x_own = self.din("x_own", [NOWN, D])
        self.x_oth = self.din("x_oth", [NOWN, D])
        self.ctxl = self.din("ctxl", [256, D])
        self.cc = self.din("cc", [P, 32])
        self.w_ada = self.din("w_ada", [D, 6 * D])
        self.b_ada = self.din("b_ada", [6 * D])
        self.w_in = self.din("w_in", [D, WIN_COLS])
        self.wgu = self.din("wgu", [2, 64, 512])
        self.bgate = self.din("bgate", [2, 512])
        self.sink = self.din("sink", [8])
        self.normw = self.din("normw", [P, 8])
        self.w_out = self.din("w_out", [D, D])
        self.ln1g = self.din("ln1g", [D])
        self.ln1b = self.din("ln1b", [D])
        self.ln2g = self.din("ln2g", [D])
        self.ln2b = self.din("ln2b", [D])
        self.w_r = self.din("w_r", [D, 36])
        self.b_r = self.din("b_r", [36])
        if with_experts:
            self.weg = self.din("weg", [N_EXP, D, HID])
            self.weu = self.din("weu", [N_EXP, D, HID])
            self.wed = self.din("wed", [N_EXP, HID, D])
        self.cosT = self.din("cosT", [HD, NOWN + 128])
        self.sinT = self.din("sinT", [HD, NOWN + 128])
        self.cin = {k: self.din("c_" + k, list(s)) for k, s in CONST_SHAPES.items()}
        self.y = self.dout("y", [NOWN, D])

    def load_consts(self):
        S = self.S
        self.c32 = {}
        self.c16 = {}
        cb = self.B("consts")
        for k in ("m12f", "m12r", "m3f", "m3r", "ones", "n16", "ident", "lstrict", "slotbase", "iotap"):
            t = self.sb("c32_" + k, CONST_SHAPES[k], F32)
            S.dma("sp", lambda q, t=t, k=k: q.dma_start(out=t[:], in_=self.cin[k]), "consts", writes=[cb])
            self.c32[k] = t
        for k in ("ident", "rotm", "gmaskf", "gmaskr", "amprev", "amnext", "ones"):
            t = self.sb("c16_" + k, CONST_SHAPES[k], BF16)
            S.dma("pool", lambda q, t=t, k=k: q.dma_start(out=t[:], in_=self.cin[k]), "consts", writes=[cb])
            self.c16[k] = t
        self.psum = [self.es.enter_context(self.nc.psum_tensor(f"pb{i}", [P, 512], F32)) for i in range(8)]
        self.pbuf = [self.B(f"psum{i}") for i in range(8)]
        self.dummy = self.sb("dummy_t", [P, 8], F32)
        self.eps_t = self.sb("eps_t", [P, 1], F32)
        S.op("dve", lambda v: v.memset(self.eps_t[:], LN_EPS), writes=[cb])
        self.one_t = self.sb("one_t", [P, 1], F32)
        S.op("dve", lambda v: v.memset(self.one_t[:], 1.0), writes=[cb])
        S.op("pool", lambda g: g.memset(self.dummy[:], 0.0), writes=[self.B("dummy")])

    def phase0(self):
        S, nc = self.S, self.nc
        self.mod_d = self.dscr("mod_d", [2, 6 * D], F32)
        b_modd = self.B("mod_d")
        self.vecs = self.sb("vecs", [P, 6, KC], F32)
        b_vecs = self.B("vecs")
        self.push_scope()
        cc_sb = self.sb("cc_sb", [P, 32], F32)
        sc_bf = self.sb("sc_bf", [P, 32], BF16)
        b_cc, b_sc = self.B("cc"), self.B("sc_bf")
        S.dma("sp", lambda q: q.dma_start(out=cc_sb[:], in_=self.cc), "p0a", writes=[b_cc])
        S.op("act", lambda a: a.activation(out=sc_bf[:], in_=cc_sb[:], func=AF.Silu), reads=[b_cc], writes=[b_sc])
        wv = self.w_ada.rearrange("(k p) n -> p k n", p=P)
        NB = 3
        wbufs = [self.sb(f"wada{i}", [P, KC, 1024], BF16) for i in range(NB)]
        wb = [self.B(f"wada{i}") for i in range(NB)]
        bad = [self.sb(f"bada{i}", [2, 1024], F32) for i in range(2)]
        badb = [self.B(f"bada{i}") for i in range(2)]
        mods = [self.sb(f"mods{i}", [2, 1024], F32) for i in range(2)]
        modb = [self.B(f"mods{i}") for i in range(2)]
        for cbk in range(12):
            i = cbk % NB
            j = cbk % 2
            S.dma("pool", lambda q, i=i, cbk=cbk: q.dma_start(out=wbufs[i][:], in_=wv[:, :, cbk * 1024:(cbk + 1) * 1024]),
                  f"wada{i}", writes=[wb[i]])
            S.dma("sp", lambda q, j=j, cbk=cbk: q.dma_start(
                out=bad[j][:], in_=self.b_ada[cbk * 1024:(cbk + 1) * 1024].partition_broadcast(2)),
                f"bada{j}", writes=[badb[j]])
            for n in range(2):
                pi = n
                ps = self.psum[pi]
                S.op("pe", [lambda t, k=k, i=i, n=n, ps=ps: t.matmul(ps[0:2, :], lhsT=sc_bf[:, 2 * k:2 * k + 2],
                                                                    rhs=wbufs[i][:, k, n * 512:(n + 1) * 512],
                                                                    start=(k == 0), stop=(k == KC - 1)) for k in range(KC)],
                     reads=[b_sc, wb[i]], writes=[self.pbuf[pi]])
                S.op("dve", lambda v, ps=ps, n=n, j=j: v.tensor_tensor(out=mods[j][0:2, n * 512:(n + 1) * 512], in0=ps[0:2, :],
                                                                     in1=bad[j][0:2, n * 512:(n + 1) * 512], op=ALU.add),
                     reads=[self.pbuf[pi], badb[j]], writes=[modb[j]])
            S.dma("sp", lambda q, j=j, cbk=cbk: q.dma_start(out=self.mod_d[:, cbk * 1024:(cbk + 1) * 1024], in_=mods[j][:]),
                  f"p0b{j}", reads=[modb[j]], writes=[self.B(f"mod_d_st{j}")])
        srcs = [(0, 0), (0, D), (0, 3 * D), (0, 4 * D), (1, 0), (1, D)]
        for i, (r, off) in enumerate(srcs):
            S.dma("sp", lambda q, i=i, r=r, off=off: q.dma_start(
                out=self.vecs[:, i, :], in_=self.mod_d[r, off:off + D].rearrange("(k p) -> p k", p=P),
                allow_slow_non_contiguous=True), "p0c", reads=[self.B("mod_d_st0"), self.B("mod_d_st1")], writes=[b_vecs])
        for i in (1, 3, 5):
            S.op("dve", lambda v, i=i: v.tensor_scalar(out=self.vecs[:, i, :], in0=self.vecs[:, i, :], scalar1=1.0,
                                                       scalar2=None, op0=ALU.add), reads=[b_vecs], writes=[b_vecs])
        self.dbg_out("vecs", self.vecs[:], [P, 6, KC], F32, [b_vecs])
        if "mod" in self.debug:
            o = self.dout("dbg_mod", [2, 6 * D], F32)
            self.final_toks.append(S.dma("sp", lambda q: q.dma_start(out=o, in_=self.mod_d), "dbg", reads=[self.B("mod_d_st0"), self.B("mod_d_st1")]))
        self.pop_scope()

    def mix_setup(self):
        S = self.S
        self.xin = [self.sb(f"xin{i}", [P, D], F32) for i in range(2)]
        self.xinb = [self.B(f"xin{i}") for i in range(2)]
        self.xn = [self.sb(f"xn{i}", [P, D], BF16) for i in range(2)]
        self.xnb = [self.B(f"xn{i}") for i in range(2)]
        self.stats = self.sb("stats", [P, 4, 6], F32)
        self.mv = self.sb("mv", [P, 2], F32)
        self.rstd = self.sb("rstd", [P, 1], F32)
        self.b_stats, self.b_mv, self.b_rstd = self.B("stats"), self.B("mv"), self.B("rstd")
        self.ln_i = 0

    def ln_norm(self, src_ap, out_bf, b_out, src_buf=None, load=True, xin_idx=None):
        S = self.S
        i = self.ln_i % 2 if xin_idx is None else xin_idx
        self.ln_i += 1
        xin, xb = self.xin[i], self.xinb[i]
        if load:
            S.dma("sp", lambda q: q.dma_start(out=xin[:], in_=src_ap), f"xin{i}", writes=[xb])
        S.op("dve", [lambda v, c=c: v.bn_stats(out=self.stats[:, c, :], in_=xin[:, c * 512:(c + 1) * 512]) for c in range(4)],
             reads=[xb], writes=[self.b_stats])
        S.op("dve", lambda v: v.bn_aggr(out=self.mv[:], in_=self.stats[:].rearrange("p a b -> p (a b)")),
             reads=[self.b_stats, xb], writes=[self.b_mv])
        S.op("act", lambda a: a.activation(out=self.rstd[:], in_=self.mv[:, 1:2], func=AF.Ln, bias=self.eps_t[:, 0:1]),
             reads=[self.b_mv], writes=[self.b_rstd])
        S.op("act", lambda a: a.activation(out=self.rstd[:], in_=self.rstd[:], func=AF.Exp, scale=-0.5),
             reads=[self.b_rstd], writes=[self.b_rstd])
        S.op("dve", lambda v: v.tensor_scalar(out=out_bf, in0=xin[:], scalar1=self.mv[:, 0:1], scalar2=self.rstd[:, 0:1],
                                              op0=ALU.subtract, op1=ALU.mult),
             reads=[xb, self.b_mv, self.b_rstd], writes=[b_out])
        return i

    def transpose_mod(self, xn_bf, b_xn, hT, b_hT, vi_shift, vi_scale, banks=(0, 1), plain=False):
        S = self.S
        idn = self.c16["ident"]
        for half in range(2):
            pb = self.psum[banks[half]][:].bitcast(BF16)
            S.op("pe", [lambda t, k=k, pb=pb: t.transpose(out=pb[:, (k % 8) * 128:(k % 8 + 1) * 128],
                                                        in_=xn_bf[:, k * 128:(k + 1) * 128], identity=idn[:])
                        for k in range(half * 8, half * 8 + 8)],
                 reads=[b_xn, self.B("consts")], writes=[self.pbuf[banks[half]]])
            if plain:
                eng = "act" if half == 0 else "dve"
                if eng == "act":
                    S.op("act", lambda a, pb=pb, half=half: a.copy(out=hT[:, half * 8:half * 8 + 8, :].rearrange("p a b -> p (a b)"), in_=pb[:, :]),
                         reads=[self.pbuf[banks[half]]], writes=[b_hT])
                else:
                    S.op("dve", lambda v, pb=pb, half=half: v.tensor_copy(out=hT[:, half * 8:half * 8 + 8, :].rearrange("p a b -> p (a b)"), in_=pb[:, :]),
                         reads=[self.pbuf[banks[half]]], writes=[b_hT])
                continue
            S.op("act", [lambda a, k=k, pb=pb: a.activation(out=hT[:, k, :], in_=pb[:, (k % 8) * 128:(k % 8 + 1) * 128],
                                                          func=AF.Identity, scale=self.vecs[:, vi_scale, k:k + 1],
                                                          bias=self.vecs[:, vi_shift, k:k + 1])
                         for k in range(half * 8, half * 8 + 8)],
                 reads=[self.pbuf[banks[half]], self.B("vecs")], writes=[b_hT])

    def chain_setup(self):
        S = self.S
        cb = self.B("consts")
        self.Sst = [self.sb(f"Sst{d}", [P, 1024], F32) for d in range(2)]
        self.Sb = [self.B(f"Sst{d}") for d in range(2)]
        for d in range(2):
            S.op("dve", lambda v, d=d: v.memset(self.Sst[d][:], 0.0), writes=[self.Sb[d]])
        self.wgu_sb = [self.sb(f"wgu{d}", [64, 512], F32) for d in range(2)]
        self.bg_sb = [self.sb(f"bg{d}", [1, 512], F32) for d in range(2)]
        for d in range(2):
            S.dma("sp", lambda q, d=d: q.dma_start(out=self.wgu_sb[d][:], in_=self.wgu[d]), "cstc", writes=[self.B("cst_chain")])
            S.dma("sp", lambda q, d=d: q.dma_start(out=self.bg_sb[d][:], in_=self.bgate[d:d + 1, :]), "cstc", writes=[self.B("cst_chain")])
        self.ktok = [self.sb(f"ktok{i}", [P, 512], BF16) for i in range(2)]
        self.vtok = [self.sb(f"vtok{i}", [P, 1024], BF16) for i in range(2)]
        self.glT = [self.sb(f"glT{i}", [64, P], F32) for i in range(2)]
        self.gp = [[self.sb(f"gp{i}_{d}", [P, 512], F32) for d in range(2)] for i in range(2)]
        self.b_ktok = [self.B(f"ktok{i}") for i in range(2)]
        self.b_vtok = [self.B(f"vtok{i}") for i in range(2)]
        self.b_glT = [self.B(f"glT{i}") for i in range(2)]
        self.b_gp = [[self.B(f"gp{i}_{d}") for d in range(2)] for i in range(2)]
        self.etmp = self.sb("etmp", [P, 512], F32)
        self.b_etmp = self.B("etmp")
        self.e3 = self.sb("e3", [P, 512], F32)
        self.b_e3 = self.B("e3")
        self.k3 = self.sb("k3", [P, 512], BF16)
        self.b_k3 = self.B("k3")
        self.dec = self.sb("dec", [P, 4], F32)
        self.b_dec = self.B("dec")

    def gate_gp(self, i, d, zbank=4):
        S = self.S
        cb = self.B("consts")
        ps = self.psum[zbank]
        S.op("pe", [lambda t: t.matmul(ps[:, :], lhsT=self.glT[i][:, :], rhs=self.wgu_sb[d][:, :], start=True, stop=False),
                    lambda t: t.matmul(ps[:, :], lhsT=self.c32["ones"][0:1, :], rhs=self.bg_sb[d][0:1, :], start=False, stop=True)],
             reads=[self.b_glT[i], cb, self.B("cst_chain")], writes=[self.pbuf[zbank]])
        S.op("act", lambda a: a.activation(out=self.etmp[:], in_=ps[:, :], func=AF.Exp, scale=-1.0),
             reads=[self.pbuf[zbank]], writes=[self.b_etmp])
        S.op("act", lambda a: a.activation(out=self.gp[i][d][:], in_=self.etmp[:], func=AF.Ln, bias=self.one_t[:, 0:1]),
             reads=[self.b_etmp], writes=[self.b_gp[i][d]])

    def chain_update(self, i, d, banks=(4, 5, 6, 7)):
        S = self.S
        cb = self.B("consts")
        m3 = self.c32["m3f" if d == 0 else "m3r"]
        r3b, totb, kvb0, kvb1 = banks
        ps = self.psum[r3b]
        S.op("pe", lambda t: t.matmul(ps[:, :], lhsT=m3[:, :], rhs=self.gp[i][d][:, :], start=True, stop=True),
             reads=[self.b_gp[i][d], cb], writes=[self.pbuf[r3b]])
        S.op("act", lambda a: a.activation(out=self.e3[:], in_=ps[:, :], func=AF.Exp), reads=[self.pbuf[r3b]], writes=[self.b_e3])
        S.op("dve", lambda v: v.tensor_tensor(out=self.k3[:], in0=self.ktok[i][:], in1=self.e3[:], op=ALU.mult),
             reads=[self.b_ktok[i], self.b_e3], writes=[self.b_k3])
        pt = self.psum[totb]
        S.op("pe", [lambda t, h=h: t.matmul(pt[:, h:h + 1], lhsT=self.gp[i][d][:, h * 128:(h + 1) * 128], rhs=self.c32["n16"][:, 0:1],
                                             start=True, stop=True) for h in range(4)],
             reads=[self.b_gp[i][d], cb], writes=[self.pbuf[totb]])
        S.op("act", lambda a: a.activation(out=self.dec[:], in_=pt[:, 0:4], func=AF.Exp), reads=[self.pbuf[totb]], writes=[self.b_dec])
        for hh in range(2):
            pk = self.psum[(kvb0, kvb1)[hh]]
            S.op("pe", [lambda t, h=h, pk=pk: t.matmul(pk[:, (h % 2) * 256:(h % 2 + 1) * 256], lhsT=self.k3[:, h * 128:(h + 1) * 128],
                                                      rhs=self.vtok[i][:, h * 256:(h + 1) * 256], start=True, stop=True)
                        for h in (2 * hh, 2 * hh + 1)],
                 reads=[self.b_k3, self.b_vtok[i]], writes=[self.pbuf[(kvb0, kvb1)[hh]]])
            for h in (2 * hh, 2 * hh + 1):
                S.op("dve", lambda v, h=h, pk=pk: v.scalar_tensor_tensor(
                    out=self.Sst[d][:, h * 256:(h + 1) * 256], in0=self.Sst[d][:, h * 256:(h + 1) * 256],
                    scalar=self.dec[:, h:h + 1], in1=pk[:, (h % 2) * 256:(h % 2 + 1) * 256], op0=ALU.mult, op1=ALU.add),
                    reads=[self.pbuf[(kvb0, kvb1)[hh]], self.b_dec, self.Sb[d]], writes=[self.Sb[d]])

    def proj_tile_B(self, i, hT, b_hT, want_kv=True, kaT=None, b_kaT=None, va=None, b_va=None, rope_cols=None):
        S = self.S
        wgt, wka, bw = self.wgt, self.wka, self.B("W_B")
        ps = self.psum[2]
        S.op("pe", [lambda t, k=k: t.matmul(ps[0:64, 0:128], lhsT=wgt[:, k, 0:64], rhs=hT[:, k, :], start=(k == 0), stop=(k == KC - 1))
                    for k in range(KC)], reads=[b_hT, bw], writes=[self.pbuf[2]])
        S.op("dve", lambda v: v.tensor_copy(out=self.glT[i][:], in_=ps[0:64, 0:128]), reads=[self.pbuf[2]], writes=[self.b_glT[i]])
        col0 = 64 + 256
        for n in range(3):
            pb_i = 3 if n % 2 == 0 else 2
            ps2 = self.psum[pb_i]
            S.op("pe", [lambda t, k=k, n=n, ps2=ps2: t.matmul(ps2[:, :], lhsT=hT[:, k, :], rhs=wgt[:, k, col0 + n * 512:col0 + (n + 1) * 512],
                                                           start=(k == 0), stop=(k == KC - 1)) for k in range(KC)],
                 reads=[b_hT, bw], writes=[self.pbuf[pb_i]])
            if n == 0:
                S.op("act", lambda a, ps2=ps2: a.copy(out=self.ktok[i][:], in_=ps2[:, :]), reads=[self.pbuf[pb_i]], writes=[self.b_ktok[i]])
            else:
                S.op("act", lambda a, ps2=ps2, n=n: a.copy(out=self.vtok[i][:, (n - 1) * 512:n * 512], in_=ps2[:, :]),
                     reads=[self.pbuf[pb_i]], writes=[self.b_vtok[i]])
        if va is not None:
            ps3 = self.psum[3]
            S.op("pe", [lambda t, k=k: t.matmul(ps3[:, 0:256], lhsT=hT[:, k, :], rhs=wgt[:, k, 64:64 + 256], start=(k == 0), stop=(k == KC - 1))
                        for k in range(KC)], reads=[b_hT, bw], writes=[self.pbuf[3]])
            S.op("act", lambda a: a.copy(out=va, in_=ps3[:, 0:256]), reads=[self.pbuf[3]], writes=[b_va])
        if kaT is not None:
            for blk in range(2):
                ps4 = self.psum[2]
                S.op("pe", [lambda t, k=k, blk=blk: t.matmul(ps4[:, 0:128], lhsT=wka[:, k, blk * 128:(blk + 1) * 128], rhs=hT[:, k, :],
                                                             start=(k == 0), stop=(k == KC - 1)) for k in range(KC)],
                     reads=[b_hT, bw], writes=[self.pbuf[2]])
                if rope_cols is None:
                    S.op("act", lambda a, blk=blk: a.copy(out=kaT[:, blk, :], in_=ps4[:, 0:128]), reads=[self.pbuf[2]], writes=[b_kaT])
                else:
                    self.rope(ps4[:, 0:128], self.pbuf[2], kaT[:, blk, :], b_kaT, rope_cols, 128, rotbank=3)

    def rope(self, src_ps, b_src, out_bf, b_out, col0, n, rotbank):
        S = self.S
        cb = self.B("consts")
        qs, t1, t2 = self.rp_qs, self.rp_t1, self.rp_t2
        S.op("act", lambda a: a.copy(out=qs[:, 0:n], in_=src_ps), reads=[b_src], writes=[self.B("rp_qs")])
        S.op("act", lambda a: a.copy(out=t1[:, 0:n], in_=src_ps), reads=[b_src], writes=[self.B("rp_t1")])
        pr = self.psum[rotbank]
        S.op("pe", lambda t: t.matmul(pr[:, 0:n], lhsT=self.c16["rotm"][:, :], rhs=qs[:, 0:n], start=True, stop=True),
             reads=[self.B("rp_qs"), cb], writes=[self.pbuf[rotbank]])
        S.op("act", lambda a: a.copy(out=t2[:, 0:n], in_=pr[:, 0:n]), reads=[self.pbuf[rotbank]], writes=[self.B("rp_t2")])
        S.op("dve", lambda v: v.tensor_tensor(out=t1[:, 0:n], in0=t1[:, 0:n], in1=self.cos_sb[:, col0:col0 + n], op=ALU.mult),
             reads=[self.B("rp_t1"), self.B("cst_rope")], writes=[self.B("rp_t1")])
        S.op("dve", lambda v: v.tensor_tensor(out=t2[:, 0:n], in0=t2[:, 0:n], in1=self.sin_sb[:, col0:col0 + n], op=ALU.mult),
             reads=[self.B("rp_t2"), self.B("cst_rope")], writes=[self.B("rp_t2")])
        S.op("dve", lambda g: g.tensor_tensor(out=out_bf, in0=t1[:, 0:n], in1=t2[:, 0:n], op=ALU.add),
             reads=[self.B("rp_t1"), self.B("rp_t2")], writes=[b_out])

    def phase1(self):
        S = self.S
        cb = self.B("consts")
        self.mix_setup()
        self.push_scope()
        self.chain_setup()
        self.kaT_c = self.sb("kaT_c", [P, 2, 256], BF16)
        self.va_c = self.sb("va_c", [P, 2, 256], BF16)
        self.kaT_h = self.sb("kaT_h", [P, 2, P], BF16)
        self.va_h = self.sb("va_h", [P, 256], BF16)
        self.push_scope()
        self.cos_sb = self.sb("cos_sb", [HD, NOWN + 128], F32)
        self.sin_sb = self.sb("sin_sb", [HD, NOWN + 128], F32)
        S.dma("sp", lambda q: q.dma_start(out=self.cos_sb[:], in_=self.cosT), "cstr", writes=[self.B("cst_rope")])
        S.dma("sp", lambda q: q.dma_start(out=self.sin_sb[:], in_=self.sinT), "cstr", writes=[self.B("cst_rope")])
        self.rp_qs = self.sb("rp_qs", [P, 512], BF16)
        self.rp_t1 = self.sb("rp_t1", [P, 512], F32)
        self.rp_t2 = self.sb("rp_t2", [P, 512], F32)
        self.push_scope()
        wv = self.w_in.rearrange("(k p) n -> p k n", p=P)
        self.wgt = self.sb("wgt", [P, KC, 1856], BF16)
        self.wka = self.sb("wka", [P, KC, 256], BF16)
        bw = self.B("W_B")
        S.dma("pool", lambda q: q.dma_start(out=self.wka[:], in_=wv[:, :, FM_KA * 128:FM_KA * 128 + 256]), "W_B", writes=[bw])
        for c in range(4):
            S.dma("pool", lambda q, c=c: q.dma_start(out=self.wgt[:, :, c * 464:(c + 1) * 464],
                                                      in_=wv[:, :, GB_OFF + c * 464:GB_OFF + (c + 1) * 464]), "W_B", writes=[bw])
        hT = [self.sb(f"hT{i}", [P, KC, P], BF16) for i in range(2)]
        b_hT = [self.B(f"hT{i}") for i in range(2)]
        kaT_tmp = self.sb("kaT_tmp", [P, 2, P], BF16)
        for ti in range(2):
            self.ln_norm(self.ctxl[ti * P:(ti + 1) * P, :], self.xn[ti][:], self.xnb[ti])
            self.transpose_mod(self.xn[ti][:], self.xnb[ti], hT[ti], b_hT[ti], 4, 5)
            self.proj_tile_B(ti, hT[ti], b_hT[ti], kaT=kaT_tmp, b_kaT=self.B("kaT_tmp"),
                             va=self.va_c[:, ti, :], b_va=self.B("va_c"))
            for blk in range(2):
                S.op("pool", lambda g, blk=blk, ti=ti: g.tensor_copy(out=self.kaT_c[:, blk, ti * P:(ti + 1) * P], in_=kaT_tmp[:, blk, :]),
                     reads=[self.B("kaT_tmp")], writes=[self.B("kaT_c")])
            for d in range(2):
                self.gate_gp(ti, d)
        for ti in (0, 1):
            self.chain_update(ti, 0)
        for ti in (1, 0):
            self.chain_update(ti, 1)
        self.dbg_out("S_ctx_F", self.Sst[0][:], [P, 1024], F32, [self.Sb[0]])
        self.dbg_out("S_ctx_R", self.Sst[1][:], [P, 1024], F32, [self.Sb[1]])
        self.dbg_out("kaT_c", self.kaT_c[:], [P, 2, 256], BF16, [self.B("kaT_c")])
        if self.n_other > 0:
            for idx, ti in enumerate(range(NT - 1, NT - 1 - self.n_other, -1)):
                i = idx % 2
                self.ln_norm(self.x_oth[ti * P:(ti + 1) * P, :], self.xn[i][:], self.xnb[i])
                self.transpose_mod(self.xn[i][:], self.xnb[i], hT[i], b_hT[i], 0, 1)
                if ti == 0:
                    self.proj_tile_B(i, hT[i], b_hT[i], kaT=self.kaT_h, b_kaT=self.B("kaT_h"), va=self.va_h[:], b_va=self.B("va_h"),
                                     rope_cols=NOWN)
                else:
                    self.proj_tile_B(i, hT[i], b_hT[i])
                self.gate_gp(i, 1)
                self.chain_update(i, 1)
        self.dbg_out("S_bnd_R", self.Sst[1][:], [P, 1024], F32, [self.Sb[1]])
        self.dbg_out("kaT_h", self.kaT_h[:], [P, 2, P], BF16, [self.B("kaT_h")])
        self.pop_scope()

    def phase2(self):
        S = self.S
        cb = self.B("consts")
        self.pfm_d = self.dscr("pfm_d", [N_FM, P, NOWN], BF16)
        self.pgl_d = self.dscr("pgl_d", [64, NOWN], F32)
        self.ptm_d = self.dscr("ptm_d", [NOWN, TM_COLS], BF16)
        b_pfm, b_pgl, b_ptm = self.B("pfm_d"), self.B("pgl_d"), self.B("ptm_d")
        self.push_scope()
        hT = self.sb("hT_own", [P, KC, NOWN], BF16)
        b_hT = self.B("hT_own")
        for ti in range(NT):
            i = ti % 2
            self.ln_norm(self.x_own[ti * P:(ti + 1) * P, :], self.xn[i][:], self.xnb[i])
            self.transpose_mod(self.xn[i][:], self.xnb[i], hT[:, :, ti * P:(ti + 1) * P], b_hT, 0, 1)
        import os
        stop = os.environ.get("P2_STOP", "")
        if stop == "ln":
            self.dbg_out("hT", hT[:, 0, :], [P, NOWN], BF16, [b_hT])
            self.pop_scope()
            return
        wv = self.w_in.rearrange("(k p) n -> p k n", p=P)
        NB = 2
        wbuf = [self.sb(f"wst{i}", [P, KC, 512], BF16) for i in range(NB)]
        wbb = [self.B(f"wst{i}") for i in range(NB)]
        stg = [self.sb(f"stg{i}", [P, NOWN], BF16) for i in range(2)]
        stgb = [self.B(f"stg{i}") for i in range(2)]
        stg32 = [self.sb(f"stg32_{i}", [64, 512], F32) for i in range(2)]
        stt = [self.sb(f"stt{i}", [P, 512], BF16) for i in range(3)]
        sttb = [self.B(f"stt{i}") for i in range(3)]
        gi = 0
        ev = 0
        pbank = 0
        nstg = 0
        fm_groups = [(g * 512, 512) for g in range(6)] + [(3072, 320)]
        if stop.startswith("fm"):
            fm_groups = fm_groups[int(stop[2:4]):int(stop[4:6])]
        if stop.startswith("tm"):
            fm_groups = []
        for (c0, ncol) in fm_groups:
            w = gi % NB
            gi += 1
            for hh in range(2):
                h0, h1 = hh * (ncol // 2), (hh + 1) * (ncol // 2)
                S.dma("pool", lambda q, w=w, c0=c0, h0=h0, h1=h1: q.dma_start(out=wbuf[w][:, :, h0:h1], in_=wv[:, :, c0 + h0:c0 + h1]),
                      f"wst{w}", writes=[wbb[w]])
            nblk = (ncol + 127) // 128
            for bl in range(nblk):
                blk = c0 // 128 + bl
                M = min(128, ncol - bl * 128)
                is_gb = (M == 64)
                si = nstg % 2
                if not is_gb:
                    nstg += 1
                for ch in range(4):
                    pb_i = pbank % 4
                    pbank += 1
                    ps = self.psum[pb_i]
                    S.op("pe", [lambda t, k=k, w=w, bl=bl, M=M, ch=ch, ps=ps: t.matmul(
                        ps[0:M, :], lhsT=wbuf[w][:, k, bl * 128:bl * 128 + M], rhs=hT[:, k, ch * 512:(ch + 1) * 512],
                        start=(k == 0), stop=(k == KC - 1)) for k in range(KC)],
                        reads=[b_hT, wbb[w]], writes=[self.pbuf[pb_i]])
                    if is_gb:
                        S.op("dve", lambda v, ps=ps, ch=ch: v.tensor_copy(out=stg32[ch % 2][:, :], in_=ps[0:64, :]),
                             reads=[self.pbuf[pb_i]], writes=[self.B(f"stg32_{ch % 2}")])
                        S.dma("sp", lambda q, ch=ch: q.dma_start(out=self.pgl_d[:, ch * 512:(ch + 1) * 512], in_=stg32[ch % 2][:, :]),
                              f"pgl{ch % 2}", reads=[self.B(f"stg32_{ch % 2}")], writes=[b_pgl])
                    elif blk < FM_QB:
                        self.rope(ps[:, :], self.pbuf[pb_i], stg[si][:, ch * 512:(ch + 1) * 512], stgb[si], ch * 512, 512,
                                  rotbank=4 + (ch % 2))
                    else:
                        eng = "act" if ev % 2 == 0 else "dve"
                        ev += 1
                        if eng == "act":
                            S.op("act", lambda a, ps=ps, ch=ch, si=si: a.copy(out=stg[si][:, ch * 512:(ch + 1) * 512], in_=ps[:, :]),
                                 reads=[self.pbuf[pb_i]], writes=[stgb[si]])
                        else:
                            S.op("dve", lambda v, ps=ps, ch=ch, si=si: v.tensor_copy(out=stg[si][:, ch * 512:(ch + 1) * 512], in_=ps[:, :]),
                                 reads=[self.pbuf[pb_i]], writes=[stgb[si]])
                if not is_gb:
                    S.dma("sp", lambda q, blk=blk, si=si: q.dma_start(out=self.pfm_d[blk], in_=stg[si][:]), f"stg{si}",
                          reads=[stgb[si]], writes=[b_pfm])
        tm_groups = [(0, 512), (512, 512), (1024, 512), (1536, 256)]
        if stop.startswith("fm"):
            tm_groups = []
        if stop.startswith("tm"):
            tm_groups = tm_groups[int(stop[2:4]):int(stop[4:6])]
        nt_ = 0
        for (c0, ncol) in tm_groups:
            w = gi % NB
            gi += 1
            for hh in range(2):
                h0, h1 = hh * (ncol // 2), (hh + 1) * (ncol // 2)
                S.dma("pool", lambda q, w=w, c0=c0, h0=h0, h1=h1: q.dma_start(out=wbuf[w][:, :, h0:h1], in_=wv[:, :, TM_OFF + c0 + h0:TM_OFF + c0 + h1]),
                      f"wst{w}", writes=[wbb[w]])
            for ti in range(NT):
                pb_i = pbank % 4
                pbank += 1
                ps = self.psum[pb_i]
                S.op("pe", [lambda t, k=k, w=w, ti=ti, ncol=ncol, ps=ps: t.matmul(
                    ps[:, 0:ncol], lhsT=hT[:, k, ti * P:(ti + 1) * P], rhs=wbuf[w][:, k, 0:ncol],
                    start=(k == 0), stop=(k == KC - 1)) for k in range(KC)],
                    reads=[b_hT, wbb[w]], writes=[self.pbuf[pb_i]])
                j = nt_ % 3
                nt_ += 1
                eng = "act" if ev % 2 == 0 else "dve"
                ev += 1
                if eng == "act":
                    S.op("act", lambda a, ps=ps, j=j, ncol=ncol: a.copy(out=stt[j][:, 0:ncol], in_=ps[:, 0:ncol]),
                         reads=[self.pbuf[pb_i]], writes=[sttb[j]])
                else:
                    S.op("dve", lambda v, ps=ps, j=j, ncol=ncol: v.tensor_copy(out=stt[j][:, 0:ncol], in_=ps[:, 0:ncol]),
                         reads=[self.pbuf[pb_i]], writes=[sttb[j]])
                S.dma("sp", lambda q, ti=ti, c0=c0, ncol=ncol, j=j: q.dma_start(
                    out=self.ptm_d[ti * P:(ti + 1) * P, c0:c0 + ncol], in_=stt[j][:, 0:ncol]), f"stt{j}",
                    reads=[sttb[j]], writes=[b_ptm])
        if "pfm" in self.debug:
            o = self.dout("dbg_pfm", [N_FM, P, NOWN], BF16)
            for blk in range(N_FM):
                self.final_toks.append(S.dma("sp", lambda q, o=o, blk=blk: q.dma_start(out=o[blk], in_=self.pfm_d[blk]), "dbg", reads=[b_pfm]))
        if "pgl" in self.debug:
            o2 = self.dout("dbg_pgl", [64, NOWN], F32)
            self.final_toks.append(S.dma("sp", lambda q: q.dma_start(out=o2, in_=self.pgl_d), "dbg", reads=[b_pgl]))
        if "ptm" in self.debug:
            o3 = self.dout("dbg_ptm", [NOWN, TM_COLS], BF16)
            for ti in range(NT):
                self.final_toks.append(S.dma("sp", lambda q, ti=ti: q.dma_start(out=o3[ti * P:(ti + 1) * P, :], in_=self.ptm_d[ti * P:(ti + 1) * P, :]),
                                             "dbg", reads=[b_ptm]))
        self.pop_scope()
        self.pop_scope()

    def load_chain_tile(self, i, ti):
        S = self.S
        S.dma("sp", lambda q: q.dma_start(out=self.ktok[i][:], in_=self.ptm_d[ti * P:(ti + 1) * P, TM_KB:TM_KB + 512]), f"ktok{i}",
              reads=[self.B("ptm_d")], writes=[self.b_ktok[i]])
        S.dma("sp", lambda q: q.dma_start(out=self.vtok[i][:], in_=self.ptm_d[ti * P:(ti + 1) * P, TM_VB:TM_VB + 1024]), f"vtok{i}",
              reads=[self.B("ptm_d")], writes=[self.b_vtok[i]])
        S.dma("sp", lambda q: q.dma_start(out=self.glT[i][:], in_=self.pgl_d[:, ti * P:(ti + 1) * P]), f"glT{i}",
              reads=[self.B("pgl_d")], writes=[self.b_glT[i]])

    def phase3(self):
        S = self.S
        self.SR_d = self.dscr("SR_d", [NT, P, 1024], BF16)
        b_SR = self.B("SR_d")
        self.push_scope()
        sbf = [self.sb(f"sbf{i}", [P, 1024], BF16) for i in range(2)]
        sbfb = [self.B(f"sbf{i}") for i in range(2)]
        for idx, ti in enumerate(range(NT - 1, -1, -1)):
            i = idx % 2
            self.load_chain_tile(i, ti)
            self.gate_gp(i, 1)
            S.op("act", lambda a, i=i: a.copy(out=sbf[i][:], in_=self.Sst[1][:]), reads=[self.Sb[1]], writes=[sbfb[i]])
            S.dma("sp", lambda q, i=i, ti=ti: q.dma_start(out=self.SR_d[ti], in_=sbf[i][:]), f"sbf{i}", reads=[sbfb[i]], writes=[b_SR])
            self.chain_update(i, 1)
        self.pop_scope()

    def phase4(self):
        S = self.S
        cb = self.B("consts")
        self.cat_d = self.dscr("cat_d", [NT, P, KC * P], BF16)
        b_cat = self.B("cat_d")
        b_pfm, b_ptm = self.B("pfm_d"), self.B("ptm_d")
        self.push_scope()
        SCALE = float(HD) ** -0.5
        kaT = self.sb("kaT_all", [P, 2, NOWN], BF16)
        va = self.sb("va_all", [P, NT, 256], BF16)
        b_ka, b_va = self.B("kaT_all"), self.B("va_all")
        for h in range(2):
            S.dma("sp", lambda q, h=h: q.dma_start(out=kaT[:, h, :], in_=self.pfm_d[FM_KA + h]), "kaT_all", reads=[b_pfm], writes=[b_ka])
        for t4 in range(4):
            S.dma("sp", lambda q, t4=t4: q.dma_start(out=va[:, t4 * 4:(t4 + 1) * 4, :],
                                                    in_=self.ptm_d[t4 * 512:(t4 + 1) * 512, TM_VA:TM_VA + 256].rearrange("(t p) n -> p t n", p=P)),
                  "va_all", reads=[b_ptm], writes=[b_va])
        esink = self.sb("esink", [P, 8], F32)
        b_es = self.B("esink")
        S.dma("sp", lambda q: q.dma_start(out=esink[:], in_=self.sink.partition_broadcast(P)), "esink", writes=[b_es])
        S.op("act", lambda a: a.activation(out=esink[:], in_=esink[:], func=AF.Exp), reads=[b_es], writes=[b_es])
        normw = self.sb("normw_sb", [P, 8], F32)
        S.dma("sp", lambda q: q.dma_start(out=normw[:], in_=self.normw), "normw", writes=[self.B("normw")])
        qaT = self.sb("qaT", [P, 8, P], BF16)
        qbT = self.sb("qbT", [P, 4, P], BF16)
        kbT = self.sb("kbT", [P, 4, P], BF16)
        rbT = self.sb("rbT", [P, 8, P], BF16)
        SRb = self.sb("SRb", [P, 1024], BF16)
        SFb = self.sb("SFb", [P, 1024], BF16)
        pT = [self.sb(f"pT{i}", [P, 512], BF16) for i in range(2)]
        pTb = [self.B(f"pT{i}") for i in range(2)]
        dsb = self.sb("dsb", [P, 512], F32)
        osb = self.sb("osb", [P, 512], F32)
        catT = self.sb("catT", [P, KC, P], BF16)
        b_catT = self.B("catT")
        E1 = self.sb("E1", [P, 4, P], F32)
        E2 = self.sb("E2", [P, 4, P], F32)
        EB = self.sb("EB", [P, 4, P], F32)
        qm = [self.sb(f"qm{d}", [P, 4, P], BF16) for d in range(2)]
        km = [self.sb(f"km{d}", [P, 4, P], BF16) for d in range(2)]
        qe = [self.sb(f"qe{d}", [P, 4, P], BF16) for d in range(2)]
        ATm = [self.sb(f"ATm{d}", [P, 512], BF16) for d in range(2)]
        attmp = self.sb("attmp", [P, 512], F32)
        osq = self.sb("osq", [P, 1024], F32)
        of = self.sb("of", [P, 1024], F32)
        rinv = self.sb("rinv", [P, 512], F32)
        srb = self.sb("srb", [P, 8, P], F32)
        otmp = self.sb("otmp", [P, P], F32)
        Bn = self.B
        nps = 0
        for ti in range(NT):
            c0 = ti * P
            S.dma("sp", lambda q, c0=c0: q.dma_start(out=qaT[:], in_=self.pfm_d[FM_QA:FM_QA + 8, :, c0:c0 + P].rearrange("b p n -> p b n")),
                  "qaT", reads=[b_pfm], writes=[Bn("qaT")])
            S.dma("sp", lambda q, c0=c0: q.dma_start(out=qbT[:], in_=self.pfm_d[FM_QB:FM_QB + 4, :, c0:c0 + P].rearrange("b p n -> p b n")),
                  "qbT", reads=[b_pfm], writes=[Bn("qbT")])
            S.dma("sp", lambda q, c0=c0: q.dma_start(out=kbT[:], in_=self.pfm_d[FM_KB:FM_KB + 4, :, c0:c0 + P].rearrange("b p n -> p b n")),
                  "kbT", reads=[b_pfm], writes=[Bn("kbT")])
            S.dma("sp", lambda q, c0=c0: q.dma_start(out=rbT[:], in_=self.pfm_d[FM_RB:FM_RB + 8, :, c0:c0 + P].rearrange("b p n -> p b n")),
                  "rbT", reads=[b_pfm], writes=[Bn("rbT")])
            S.dma("sp", lambda q, ti=ti: q.dma_start(out=SRb[:], in_=self.SR_d[ti]), "SRb", reads=[Bn("SR_d")], writes=[Bn("SRb")])
            i = ti % 2
            self.load_chain_tile(i, ti)
            for h in range(2):
                blocks = [("c", 0), ("c", 1)]
                if ti > 0:
                    blocks.append(("p", ti - 1))
                blocks.append(("o", ti))
                blocks.append(("n", ti + 1))
                nb = len(blocks)
                for bi, (kind, idx) in enumerate(blocks):
                    if kind == "c":
                        kT, rk = self.kaT_c[:, h, idx * P:(idx + 1) * P], Bn("kaT_c")
                        vv, rv = self.va_c[:, idx, h * P:(h + 1) * P], Bn("va_c")
                    elif kind == "n" and idx == NT:
                        kT, rk = self.kaT_h[:, h, :], Bn("kaT_h")
                        vv, rv = self.va_h[:, h * P:(h + 1) * P], Bn("va_h")
                    else:
                        kT, rk = kaT[:, h, idx * P:(idx + 1) * P], b_ka
                        vv, rv = va[:, idx, h * P:(h + 1) * P], b_va
                    sb_i = nps % 2
                    nps += 1
                    ps = self.psum[sb_i]
                    S.op("pe", lambda t, kT=kT, h=h, ps=ps: t.matmul(ps[:, :], lhsT=kT, rhs=qaT[:, 4 * h:4 * h + 4, :].rearrange("p a b -> p (a b)"),
                                                                     start=True, stop=True),
                         reads=[rk, Bn("qaT")], writes=[self.pbuf[sb_i]])
                    S.op("act", lambda a, ps=ps, sb_i=sb_i: a.activation(out=pT[sb_i][:], in_=ps[:, :], func=AF.Exp, scale=SCALE),
                         reads=[self.pbuf[sb_i]], writes=[pTb[sb_i]])
                    if kind in ("p", "n"):
                        mk = self.c16["amprev" if kind == "p" else "amnext"]
                        S.op("dve", lambda v, sb_i=sb_i, mk=mk: v.tensor_tensor(out=pT[sb_i][:], in0=pT[sb_i][:], in1=mk[:, :], op=ALU.mult),
                             reads=[pTb[sb_i], cb], writes=[pTb[sb_i]])
                    S.op("pe", [lambda t, vv=vv, sb_i=sb_i, bi=bi, nb=nb: t.matmul(self.psum[2][:, :], lhsT=vv, rhs=pT[sb_i][:], start=(bi == 0), stop=(bi == nb - 1)),
                                lambda t, sb_i=sb_i, bi=bi, nb=nb: t.matmul(self.psum[3][:, :], lhsT=self.c16["ones"][:, :], rhs=pT[sb_i][:], start=(bi == 0), stop=(bi == nb - 1))],
                         reads=[rv, pTb[sb_i], cb], writes=[self.pbuf[2], self.pbuf[3]])
                S.op("act", lambda a: a.copy(out=dsb[:], in_=self.psum[3][:, :]), reads=[self.pbuf[3]], writes=[Bn("dsb")])
                S.op("act", lambda a: a.copy(out=osb[:], in_=self.psum[2][:, :]), reads=[self.pbuf[2]], writes=[Bn("osb")])
                S.op("dve", [lambda v, g=g, h=h: v.tensor_scalar(out=dsb[:, g * P:(g + 1) * P], in0=dsb[:, g * P:(g + 1) * P],
                                                                 scalar1=esink[:, 4 * h + g:4 * h + g + 1], scalar2=None, op0=ALU.add)
                             for g in range(4)], reads=[Bn("dsb"), b_es], writes=[Bn("dsb")])
                S.op("dve", lambda v: v.reciprocal(out=dsb[:], in_=dsb[:]), reads=[Bn("dsb")], writes=[Bn("dsb")])
                S.op("dve", lambda v, h=h: v.tensor_tensor(out=catT[:, 4 * h:4 * h + 4, :].rearrange("p a b -> p (a b)"), in0=osb[:], in1=dsb[:], op=ALU.mult),
                     reads=[Bn("osb"), Bn("dsb")], writes=[b_catT])
            for d in range(2):
                self.gate_gp(i, d)
            S.op("act", lambda a: a.copy(out=SFb[:], in_=self.Sst[0][:]), reads=[self.Sb[0]], writes=[Bn("SFb")])
            for d in range(2):
                m12 = self.c32["m12f" if d == 0 else "m12r"]
                for hb in range(2):
                    S.op("pe", [lambda t, h=h, hb=hb, d=d, m12=m12, i=i: t.matmul(self.psum[6 + hb][:, (h % 2) * 256:(h % 2 + 1) * 256],
                                                                           lhsT=self.gp[i][d][:, h * P:(h + 1) * P], rhs=m12[:, :], start=True, stop=True)
                                for h in (2 * hb, 2 * hb + 1)], reads=[self.b_gp[i][d], cb], writes=[self.pbuf[6 + hb]])
                    src = self.psum[6 + hb][:, :].rearrange("p (h c) -> p h c", h=2)
                    S.op("act", [lambda a, hb=hb, src=src: a.activation(out=E1[:, 2 * hb:2 * hb + 2, :], in_=src[:, :, 0:P], func=AF.Exp),
                                 lambda a, hb=hb, src=src: a.activation(out=E2[:, 2 * hb:2 * hb + 2, :], in_=src[:, :, 0:P], func=AF.Exp, scale=-1.0),
                                 lambda a, hb=hb, src=src: a.activation(out=EB[:, 2 * hb:2 * hb + 2, :], in_=src[:, :, P:2 * P], func=AF.Exp)],
                         reads=[self.pbuf[6 + hb]], writes=[Bn("E123")])
                fl = lambda t_: t_[:].rearrange("p a b -> p (a b)")
                S.op("dve", lambda v, d=d: v.scalar_tensor_tensor(out=fl(qm[d]), in0=fl(qbT), scalar=SCALE, in1=fl(E1), op0=ALU.mult, op1=ALU.mult),
                     reads=[Bn("qbT"), Bn("E123")], writes=[Bn(f"qm{d}")])
                S.op("dve", lambda v, d=d: v.tensor_tensor(out=fl(km[d]), in0=fl(kbT), in1=fl(E2), op=ALU.mult),
                     reads=[Bn("kbT"), Bn("E123")], writes=[Bn(f"km{d}")])
                S.op("dve", lambda v, d=d: v.scalar_tensor_tensor(out=fl(qe[d]), in0=fl(qbT), scalar=SCALE, in1=fl(EB), op0=ALU.mult, op1=ALU.mult),
                     reads=[Bn("qbT"), Bn("E123")], writes=[Bn(f"qe{d}")])
                S.op("pe", [lambda t, h=h, d=d: t.matmul(self.psum[4][:, h * P:(h + 1) * P], lhsT=km[d][:, h, :], rhs=qm[d][:, h, :], start=True, stop=True)
                            for h in range(4)], reads=[Bn(f"qm{d}"), Bn(f"km{d}")], writes=[self.pbuf[4]])
                S.op("act", lambda a: a.copy(out=attmp[:], in_=self.psum[4][:, :]), reads=[self.pbuf[4]], writes=[Bn("attmp")])
                gm = self.c16["gmaskf" if d == 0 else "gmaskr"]
                S.op("dve", lambda v, d=d, gm=gm: v.tensor_tensor(out=ATm[d][:], in0=attmp[:], in1=gm[:, :], op=ALU.mult),
                     reads=[Bn("attmp"), cb], writes=[Bn(f"ATm{d}")])
            for hb in range(2):
                fns = []
                for r in range(4 * hb, 4 * hb + 4):
                    h, c = r // 2, r % 2
                    vs = slice(h * 256 + c * P, h * 256 + (c + 1) * P)
                    dst = self.psum[6 + hb][:, (r % 4) * P:(r % 4 + 1) * P]
                    fns += [lambda t, vs=vs, dst=dst, h=h, i=i: t.matmul(dst, lhsT=self.vtok[i][:, vs], rhs=ATm[0][:, h * P:(h + 1) * P], start=True, stop=False),
                            lambda t, vs=vs, dst=dst, h=h, i=i: t.matmul(dst, lhsT=self.vtok[i][:, vs], rhs=ATm[1][:, h * P:(h + 1) * P], start=False, stop=False),
                            lambda t, vs=vs, dst=dst, h=h: t.matmul(dst, lhsT=SFb[:, vs], rhs=qe[0][:, h, :], start=False, stop=False),
                            lambda t, vs=vs, dst=dst, h=h: t.matmul(dst, lhsT=SRb[:, vs], rhs=qe[1][:, h, :], start=False, stop=True)]
                S.op("pe", fns, reads=[self.b_vtok[i], Bn("ATm0"), Bn("ATm1"), Bn("SFb"), Bn("SRb"), Bn("qe0"), Bn("qe1")], writes=[self.pbuf[6 + hb]])
                S.op("act", [lambda a, hb=hb: a.activation(out=osq[:, hb * 512:(hb + 1) * 512], in_=self.psum[6 + hb][:, :], func=AF.Square),
                             lambda a, hb=hb: a.copy(out=of[:, hb * 512:(hb + 1) * 512], in_=self.psum[6 + hb][:, :])],
                     reads=[self.pbuf[6 + hb]], writes=[Bn("osq_of")])
            S.op("pe", [lambda t, h=h, c=c: t.matmul(self.psum[5][:, h * P:(h + 1) * P], lhsT=self.c32["ones"][:, :],
                                                   rhs=osq[:, (2 * h + c) * P:(2 * h + c + 1) * P], start=(c == 0), stop=(c == 1))
                        for h in range(4) for c in range(2)], reads=[Bn("osq_of"), cb], writes=[self.pbuf[5]])
            S.op("act", lambda a: a.activation(out=rinv[:], in_=self.psum[5][:, :], func=AF.Ln, scale=1.0 / 256.0, bias=self.eps_t[:, 0:1]),
                 reads=[self.pbuf[5], cb], writes=[Bn("rinv")])
            S.op("act", lambda a: a.activation(out=rinv[:], in_=rinv[:], func=AF.Exp, scale=-0.5), reads=[Bn("rinv")], writes=[Bn("rinv")])
            S.op("act", lambda a: a.activation(out=srb[:].rearrange("p a b -> p (a b)"), in_=rbT[:].rearrange("p a b -> p (a b)"), func=AF.Silu),
                 reads=[Bn("rbT")], writes=[Bn("srb")])
            for r in range(8):
                h = r // 2
                S.op("dve", lambda v, r=r, h=h: v.tensor_tensor(out=otmp[:], in0=of[:, r * P:(r + 1) * P], in1=rinv[:, h * P:(h + 1) * P], op=ALU.mult),
                     reads=[Bn("osq_of"), Bn("rinv")], writes=[Bn("otmp")])
                S.op("dve", lambda v, r=r: v.scalar_tensor_tensor(out=catT[:, 8 + r, :], in0=otmp[:], scalar=normw[:, r:r + 1], in1=srb[:, r, :],
                                                                  op0=ALU.mult, op1=ALU.mult),
                     reads=[Bn("otmp"), Bn("srb"), Bn("normw")], writes=[b_catT])
            self.chain_update(i, 0)
            S.dma("sp", lambda q, ti=ti: q.dma_start(out=self.cat_d[ti], in_=catT[:].rearrange("p a b -> p (a b)")), "catT",
                  reads=[b_catT], writes=[b_cat])
        if "cat" in self.debug:
            o = self.dout("dbg_cat", [NT, P, KC * P], BF16)
            for ti in range(NT):
                self.final_toks.append(S.dma("sp", lambda q, ti=ti: q.dma_start(out=o[ti], in_=self.cat_d[ti]), "dbg", reads=[b_cat]))
        self.pop_scope()

    def ln_stats(self, src, b_src):
        S = self.S
        S.op("dve", [lambda v, c=c: v.bn_stats(out=self.stats[:, c, :], in_=src[:, c * 512:(c + 1) * 512]) for c in range(4)],
             reads=[b_src], writes=[self.b_stats])
        S.op("dve", lambda v: v.bn_aggr(out=self.mv[:], in_=self.stats[:].rearrange("p a b -> p (a b)")),
             reads=[self.b_stats], writes=[self.b_mv])
        S.op("act", lambda a: a.activation(out=self.rstd[:], in_=self.mv[:, 1:2], func=AF.Ln, bias=self.eps_t[:, 0:1]),
             reads=[self.b_mv], writes=[self.b_rstd])
        S.op("act", lambda a: a.activation(out=self.rstd[:], in_=self.rstd[:], func=AF.Exp, scale=-0.5),
             reads=[self.b_rstd], writes=[self.b_rstd])

    def phase5(self):
        S = self.S
        cb = self.B("consts")
        Bn = self.B
        self.x1_d = self.dscr("x1_d", [NOWN, D], F32)
        self.xbuf_d = self.dscr("xbuf_d", [N_EXP * CAPS, D], BF16)
        b_x1, b_xbuf = Bn("x1_d"), Bn("xbuf_d")
        self.idx_all = self.sb("idx_all", [P, NT, 2], I32)
        self.w_all = self.sb("w_all", [P, NT, 2], F32)
        self.cnt_bc = self.sb("cnt_bc", [P, N_EXP], F32)
        self.cnt_i = self.sb("cnt_i", [1, N_EXP], I32)
        b_idx, b_w, b_cnt = Bn("idx_all"), Bn("w_all"), Bn("cnt_bc")
        S.op("dve", lambda v: v.memset(self.cnt_bc[:], 0.0), writes=[b_cnt])
        self.push_scope()
        wout = self.sb("wout", [P, KC, D], BF16)
        b_wout = Bn("wout")
        wov = self.w_out.rearrange("(k p) n -> p k n", p=P)
        for c in range(8):
            S.dma("pool", lambda q, c=c: q.dma_start(out=wout[:, :, c * 256:(c + 1) * 256], in_=wov[:, :, c * 256:(c + 1) * 256]),
                  "wout", writes=[b_wout])
        wr = self.sb("wr", [P, KC, 36], BF16)
        S.dma("pool", lambda q: q.dma_start(out=wr[:], in_=self.w_r.rearrange("(k p) n -> p k n", p=P)), "wr", writes=[Bn("wr")])
        brb = self.sb("brb", [P, 36], F32)
        S.dma("sp", lambda q: q.dma_start(out=brb[:], in_=self.b_r.partition_broadcast(P)), "brb", writes=[Bn("brb")])
        bc = [self.sb(f"bc{i}", [P, D], F32) for i in range(3)]
        bcb = [Bn(f"bc{i}") for i in range(3)]
        S.dma("sp", lambda q: q.dma_start(out=bc[0][:], in_=self.mod_d[0, 2 * D:3 * D].partition_broadcast(P)), "bc0",
              reads=[Bn("mod_d")], writes=[bcb[0]])
        S.dma("sp", lambda q: q.dma_start(out=bc[1][:], in_=self.ln1g.partition_broadcast(P)), "bc1", writes=[bcb[1]])
        S.dma("sp", lambda q: q.dma_start(out=bc[2][:], in_=self.ln1b.partition_broadcast(P)), "bc2", writes=[bcb[2]])
        bc2 = [self.sb(f"bcm{i}", [P, D], F32) for i in range(2)]
        bc2b = [Bn(f"bcm{i}") for i in range(2)]
        S.dma("sp", lambda q: q.dma_start(out=bc2[0][:], in_=self.mod_d[0, 3 * D:4 * D].partition_broadcast(P)), "bcm0",
              reads=[Bn("mod_d")], writes=[bc2b[0]])
        S.dma("sp", lambda q: q.dma_start(out=bc2[1][:], in_=self.mod_d[0, 4 * D:5 * D].partition_broadcast(P)), "bcm1",
              reads=[Bn("mod_d")], writes=[bc2b[1]])
        S.op("pool", lambda g: g.tensor_scalar(out=bc2[1][:], in0=bc2[1][:], scalar1=1.0, scalar2=None, op0=ALU.add), reads=[bc2b[1]], writes=[bc2b[1]])
        catT = [self.sb(f"catT{i}", [P, KC, P], BF16) for i in range(2)]
        catb = [Bn(f"catTb{i}") for i in range(2)]
        ysb = self.sb("ysb", [P, D], F32)
        b_ysb = Bn("ysb")
        h2T = self.sb("h2T", [P, KC, P], BF16)
        b_h2T = Bn("h2T")
        lg = self.sb("lg", [P, 36], F32)
        sm = self.sb("rsm", [P, 16], F32)
        gm4 = self.sb("gm4", [P, 4], F32)
        pen = self.sb("pen", [P, 4], F32)
        ge4 = self.sb("ge4", [P, 4], F32)
        elm = self.sb("elm", [P, N_EXP], F32)
        m8 = self.sb("m8", [P, 8], F32)
        A1 = self.sb("A1", [P, N_EXP], F32)
        A2 = self.sb("A2", [P, N_EXP], F32)
        A12 = self.sb("A12", [P, N_EXP], F32)
        posc = self.sb("posc", [P, 64], F32)
        tpos = self.sb("tpos", [P, N_EXP], F32)
        tsl = self.sb("tsl", [P, N_EXP], F32)
        tov = self.sb("tov", [P, N_EXP], F32)
        idxf = self.sb("idxf", [P, 2], F32)
        b_r_ = Bn("route_tmp")
        AX = mybir.AxisListType.X
        for ti in range(NT):
            i = ti % 2
            xin, xb = self.xin[i], self.xinb[i]
            S.dma("sp", lambda q, i=i, ti=ti: q.dma_start(out=catT[i][:].rearrange("p a b -> p (a b)"), in_=self.cat_d[ti]), f"catT{i}",
                  reads=[Bn("cat_d")], writes=[catb[i]])
            S.dma("sp", lambda q, xin=xin, ti=ti: q.dma_start(out=xin[:], in_=self.x_own[ti * P:(ti + 1) * P, :]), f"xin{i}", writes=[xb])
            for n in range(4):
                S.op("pe", [lambda t, k=k, n=n, i=i: t.matmul(self.psum[n][:, :], lhsT=catT[i][:, k, :], rhs=wout[:, k, n * 512:(n + 1) * 512],
                                                             start=(k == 0), stop=(k == KC - 1)) for k in range(KC)],
                     reads=[catb[i], b_wout], writes=[self.pbuf[n]])
                S.op("act", lambda a, n=n: a.copy(out=ysb[:, n * 512:(n + 1) * 512], in_=self.psum[n][:, :]), reads=[self.pbuf[n]], writes=[b_ysb])
            S.op("dve", lambda v: v.tensor_tensor(out=ysb[:], in0=ysb[:], in1=bc[0][:], op=ALU.mult), reads=[b_ysb, bcb[0]], writes=[b_ysb])
            S.op("dve", lambda v, xin=xin: v.scalar_tensor_tensor(out=xin[:], in0=xin[:], scalar=float(ALPHA), in1=ysb[:], op0=ALU.mult, op1=ALU.add),
                 reads=[xb, b_ysb], writes=[xb])
            self.ln_stats(xin, xb)
            S.op("dve", lambda v, xin=xin: v.tensor_scalar(out=ysb[:], in0=xin[:], scalar1=self.mv[:, 0:1], scalar2=self.rstd[:, 0:1],
                                                           op0=ALU.subtract, op1=ALU.mult), reads=[xb, self.b_mv, self.b_rstd], writes=[b_ysb])
            S.op("dve", lambda v: v.tensor_tensor(out=ysb[:], in0=ysb[:], in1=bc[1][:], op=ALU.mult), reads=[b_ysb, bcb[1]], writes=[b_ysb])
            S.op("pool", lambda g: g.tensor_tensor(out=ysb[:], in0=ysb[:], in1=bc[2][:], op=ALU.add), reads=[b_ysb, bcb[2]], writes=[b_ysb])
            S.dma("sp", lambda q, ti=ti: q.dma_start(out=self.x1_d[ti * P:(ti + 1) * P, :], in_=ysb[:]), "x1st", reads=[b_ysb], writes=[b_x1])
            self.ln_stats(ysb, b_ysb)
            S.op("dve", lambda v: v.tensor_scalar(out=ysb[:], in0=ysb[:], scalar1=self.mv[:, 0:1], scalar2=self.rstd[:, 0:1],
                                                  op0=ALU.subtract, op1=ALU.mult), reads=[b_ysb, self.b_mv, self.b_rstd], writes=[b_ysb])
            S.op("pool", lambda g: g.tensor_tensor(out=ysb[:], in0=ysb[:], in1=bc2[1][:], op=ALU.mult), reads=[b_ysb, bc2b[1]], writes=[b_ysb])
            S.op("dve", lambda v, i=i: v.tensor_tensor(out=self.xn[i][:], in0=ysb[:], in1=bc2[0][:], op=ALU.add), reads=[b_ysb, bc2b[0]], writes=[self.xnb[i]])
            self.transpose_mod(self.xn[i][:], self.xnb[i], h2T, b_h2T, 2, 3, banks=(4, 5), plain=True)
            S.op("pe", [lambda t, k=k: t.matmul(self.psum[6][:, 0:36], lhsT=h2T[:, k, :], rhs=wr[:, k, :], start=(k == 0), stop=(k == KC - 1))
                        for k in range(KC)], reads=[b_h2T, Bn("wr")], writes=[self.pbuf[6]])
            S.op("act", lambda a: a.copy(out=lg[:], in_=self.psum[6][:, 0:36]), reads=[self.pbuf[6]], writes=[b_r_])
            R = lambda fn, extra_r=(), extra_w=(): S.op("dve", fn, reads=[b_r_] + list(extra_r), writes=[b_r_] + list(extra_w))
            R(lambda v: v.tensor_tensor(out=lg[:], in0=lg[:], in1=brb[:], op=ALU.add), extra_r=[Bn("brb")])
            R(lambda v: v.tensor_reduce(out=sm[:, 0:1], in_=lg[:, 0:4], axis=AX, op=ALU.max))
            R(lambda v: v.tensor_scalar(out=sm[:, 1:2], in0=sm[:, 0:1], scalar1=-1.0, scalar2=None, op0=ALU.mult))
            S.op("act", lambda a: a.activation(out=ge4[:], in_=lg[:, 0:4], func=AF.Exp, bias=sm[:, 1:2], accum_out=sm[:, 2:3]),
                 reads=[b_r_], writes=[b_r_])
            R(lambda v: v.reciprocal(out=sm[:, 3:4], in_=sm[:, 2:3]))
            R(lambda v: v.tensor_scalar(out=gm4[:], in0=lg[:, 0:4], scalar1=sm[:, 0:1], scalar2=None, op0=ALU.is_ge))
            R(lambda v: v.tensor_scalar(out=pen[:], in0=gm4[:], scalar1=1.0, scalar2=1e30, op0=ALU.subtract, op1=ALU.mult))
            R([lambda v, g=g: v.tensor_scalar(out=elm[:, g * 8:(g + 1) * 8], in0=lg[:, 4 + g * 8:4 + (g + 1) * 8], scalar1=pen[:, g:g + 1],
                                              scalar2=None, op0=ALU.add) for g in range(4)])
            R(lambda v: v.max(out=m8[:], in_=elm[:]))
            R(lambda v: v.tensor_scalar(out=A1[:], in0=elm[:], scalar1=m8[:, 0:1], scalar2=None, op0=ALU.is_equal))
            R(lambda v: v.tensor_scalar(out=A2[:], in0=elm[:], scalar1=m8[:, 1:2], scalar2=None, op0=ALU.is_equal))
            R(lambda v: v.tensor_tensor(out=sm[:, 4:5], in0=m8[:, 1:2], in1=m8[:, 0:1], op=ALU.subtract))
            S.op("act", lambda a: a.activation(out=sm[:, 5:6], in_=sm[:, 4:5], func=AF.Exp), reads=[b_r_], writes=[b_r_])
            R(lambda v: v.tensor_scalar(out=sm[:, 5:6], in0=sm[:, 5:6], scalar1=1.0, scalar2=None, op0=ALU.add))
            R(lambda v: v.reciprocal(out=sm[:, 6:7], in_=sm[:, 5:6]))
            R(lambda v, ti=ti: v.tensor_tensor(out=self.w_all[:, ti, 0:1], in0=sm[:, 3:4], in1=sm[:, 6:7], op=ALU.mult), extra_w=[b_w])
            R(lambda v, ti=ti: v.tensor_tensor(out=self.w_all[:, ti, 1:2], in0=sm[:, 3:4], in1=self.w_all[:, ti, 0:1], op=ALU.subtract),
              extra_r=[b_w], extra_w=[b_w])
            R(lambda v: v.tensor_tensor(out=A12[:], in0=A1[:], in1=A2[:], op=ALU.add))
            S.op("pe", [lambda t: t.matmul(self.psum[7][:, 0:32], lhsT=self.c32["lstrict"][:, :], rhs=A12[:], start=True, stop=True),
                        lambda t: t.matmul(self.psum[7][:, 32:64], lhsT=self.c32["ones"][:, :], rhs=A12[:], start=True, stop=True)],
                 reads=[b_r_, cb], writes=[self.pbuf[7]])
            S.op("act", lambda a: a.copy(out=posc[:], in_=self.psum[7][:, 0:64]), reads=[self.pbuf[7]], writes=[b_r_])
            R(lambda v: v.tensor_tensor(out=tpos[:], in0=posc[:, 0:32], in1=self.cnt_bc[:], op=ALU.add), extra_r=[b_cnt])
            R(lambda v: v.tensor_tensor(out=self.cnt_bc[:], in0=self.cnt_bc[:], in1=posc[:, 32:64], op=ALU.add), extra_r=[b_cnt], extra_w=[b_cnt])
            R(lambda v: v.tensor_scalar(out=tov[:], in0=tpos[:], scalar1=float(CAPS), scalar2=40000.0, op0=ALU.is_ge, op1=ALU.mult))
            R(lambda v: v.tensor_tensor(out=tsl[:], in0=tpos[:], in1=self.c32["slotbase"][:], op=ALU.add), extra_r=[cb])
            R(lambda v: v.tensor_tensor(out=tsl[:], in0=tsl[:], in1=tov[:], op=ALU.add))
            R(lambda v: v.tensor_tensor(out=A1[:], in0=A1[:], in1=tsl[:], op=ALU.mult))
            R(lambda v: v.tensor_tensor(out=A2[:], in0=A2[:], in1=tsl[:], op=ALU.mult))
            R(lambda v: v.tensor_reduce(out=idxf[:, 0:1], in_=A1[:], axis=AX, op=ALU.add))
            R(lambda v: v.tensor_reduce(out=idxf[:, 1:2], in_=A2[:], axis=AX, op=ALU.add))
            R(lambda v, ti=ti: v.tensor_copy(out=self.idx_all[:, ti, :], in_=idxf[:]), extra_w=[b_idx])
            for j in range(2):
                S.dma("pool", lambda g, ti=ti, j=j, i=i: g.indirect_dma_start(
                    out=self.xbuf_d[:, :], out_offset=bass.IndirectOffsetOnAxis(ap=self.idx_all[:, ti, j:j + 1], axis=0),
                    in_=self.xn[i][:, :], in_offset=None, bounds_check=S.rt["bnd"], oob_is_err=False),
                    f"scat{i}", reads=[self.xnb[i], b_idx], writes=[Bn(f"xbuf_sc{i}")])
            if ti == 0 and "lg" in self.debug:
                self.dbg_out("lg", lg[:], [P, 36], F32, [b_r_])
        S.op("dve", lambda v: v.tensor_copy(out=self.cnt_i[:], in_=self.cnt_bc[0:1, :]), reads=[b_cnt], writes=[Bn("cnt_i")])
        zt = self.sb("zt", [P, D], BF16)
        S.op("pool", lambda g: g.memset(zt[:], 0.0), writes=[Bn("zt")])
        ci = self.sb("ci", [P, N_EXP], I32)
        rf = self.sb("rf", [P, N_EXP], F32)
        zi = self.sb("zi", [P, N_EXP], I32)
        bz = Bn("ztmp")
        Z = lambda fn, extra_r=(): S.op("dve", fn, reads=[bz, b_cnt, cb] + list(extra_r), writes=[bz])
        Z(lambda v: v.tensor_copy(out=ci[:], in_=self.cnt_bc[:]))
        Z(lambda v: v.tensor_single_scalar(out=ci[:], in_=ci[:], scalar=127, op=ALU.bitwise_and))
        Z(lambda v: v.tensor_copy(out=rf[:], in_=ci[:]))
        Z(lambda v: v.tensor_scalar(out=rf[:], in0=rf[:], scalar1=self.c32["iotap"][:, 0:1], scalar2=128.0, op0=ALU.add, op1=ALU.is_ge))
        Z(lambda v: v.tensor_scalar(out=rf[:], in0=rf[:], scalar1=40000.0, scalar2=self.c32["iotap"][:, 0:1], op0=ALU.mult, op1=ALU.add))
        Z(lambda v: v.tensor_tensor(out=rf[:], in0=rf[:], in1=self.cnt_bc[:], op=ALU.add))
        Z(lambda v: v.tensor_tensor(out=rf[:], in0=rf[:], in1=self.c32["slotbase"][:], op=ALU.add))
        Z(lambda v: v.tensor_copy(out=zi[:], in_=rf[:]))
        import os
        for e in range(int(os.environ.get("NZS", N_EXP))):
            S.dma("pool", lambda g, e=e: g.indirect_dma_start(
                out=self.xbuf_d[:, :], out_offset=bass.IndirectOffsetOnAxis(ap=zi[:, e:e + 1], axis=0),
                in_=zt[:, :], in_offset=None, bounds_check=S.rt["bnd"], oob_is_err=False),
                "scatz", reads=[Bn("zt"), bz], writes=[b_xbuf])
        self.dbg_out("zi", zi[:], [P, N_EXP], I32, [bz])
        self.dbg_out("rf", rf[:], [P, N_EXP], F32, [bz])
        for nm, t_, shp, dt, bb in (("idx_all", self.idx_all, [P, NT, 2], I32, b_idx), ("w_all", self.w_all, [P, NT, 2], F32, b_w),
                                    ("cnt", self.cnt_bc, [P, N_EXP], F32, b_cnt)):
            self.dbg_out(nm, t_[:], shp, dt, [bb])
        if "x1" in self.debug:
            o = self.dout("dbg_x1", [NOWN, D], F32)
            for ti in range(NT):
                self.final_toks.append(S.dma("sp", lambda q, ti=ti, o=o: q.dma_start(out=o[ti * P:(ti + 1) * P, :], in_=self.x1_d[ti * P:(ti + 1) * P, :]),
                                             "dbg", reads=[b_x1]))
        self.pop_scope()

    def phase6(self):
        S = self.S
        Bn = self.B
        cb = Bn("consts")
        self.obuf_d = self.dscr("obuf_d", [N_EXP * CAPS, D], F32)
        b_ob = Bn("obuf_d")
        b_xbuf = Bn("xbuf_d")
        self.push_scope()
        NWB = 3
        wg = [self.sb(f"wg{i}", [P, KC, 512], BF16) for i in range(NWB)]
        wu = [self.sb(f"wu{i}", [P, KC, 512], BF16) for i in range(NWB)]
        wd = [self.sb(f"wd{i}", [P, 4, D], BF16) for i in range(NWB)]
        wsb = [Bn(f"wset{i}") for i in range(NWB)]
        wdb = [Bn(f"wdset{i}") for i in range(NWB)]
        Gs = [self.sb(f"G{i}", [P, D], BF16) for i in range(2)]
        Gb = [Bn(f"G{i}") for i in range(2)]
        hxT = self.sb("hxT", [P, KC, P], BF16)
        sg = [self.sb(f"sg{i}", [P, 512], F32) for i in range(2)]
        su = [self.sb(f"su{i}", [P, 512], F32) for i in range(2)]
        hid = self.sb("hid", [P, 1024], BF16)
        hidT = self.sb("hidT", [P, 8, P], BF16)
        ost = self.xin[0]
        idn = self.c16["ident"]
        dm = self.dummy
        dummies = {
            "pe": lambda t, k: t.matmul(self.psum[0][0:1, k:k + 1], lhsT=idn[:, 0:1], rhs=idn[:, 0:1], start=True, stop=True),
            "act": lambda a, k: a.copy(out=dm[0:1, k:k + 1], in_=dm[0:1, 7:8]),
            "dve": lambda v, k: v.memset(dm[32:33, k:k + 1], 0.0),
            "sp": lambda q, k: q.dma_start(out=dm[64:65, k:k + 1], in_=dm[64:65, 7:8]),
        }
        nsec = 0
        import os
        n_exp = int(os.environ.get("MOE_NEXP", N_EXP))
        dbank = (6, 7, 2, 3)
        for e in range(n_exp):
            gv = self.weg[e].rearrange("(k p) n -> p k n", p=P)
            uv = self.weu[e].rearrange("(k p) n -> p k n", p=P)
            wb_i = [(2 * e + half) % NWB for half in range(2)]
            for half in range(2):
                s_ = wb_i[half]
                for c in range(2):
                    S.dma("pool", lambda q, s_=s_, gv=gv, half=half, c=c: q.dma_start(
                        out=wg[s_][:, :, c * 256:(c + 1) * 256], in_=gv[:, :, half * 512 + c * 256:half * 512 + (c + 1) * 256]),
                        f"wset{s_}", writes=[wsb[s_]])
                    S.dma("pool", lambda q, s_=s_, uv=uv, half=half, c=c: q.dma_start(
                        out=wu[s_][:, :, c * 256:(c + 1) * 256], in_=uv[:, :, half * 512 + c * 256:half * 512 + (c + 1) * 256]),
                        f"wset{s_}", writes=[wsb[s_]])
            for half in range(2):
                s_ = wb_i[half]
                dv = self.wed[e, half * 512:(half + 1) * 512, :].rearrange("(k p) n -> p k n", p=P)
                for c in range(WD_SPLIT):
                    S.dma("pool", lambda q, s_=s_, dv=dv, c=c: q.dma_start(out=wd[s_][:, c:c + 1, :], in_=dv[:, c:c + 1, :]),
                          f"wdset{s_}", writes=[wdb[s_]])
            S.reg_load("cnt", self.cnt_i[0:1, e:e + 1], Bn("cnt_i"))
            for j in range(int(os.environ.get("MOE_CAPB", CAPB))):
                r0 = e * CAPS + j * P
                S.begin_if("cnt", j * P)
                gi_ = nsec % 2
                nsec += 1
                G = Gs[gi_]
                S.dma("sp", lambda q, r0=r0, G=G: q.dma_start(out=G[:], in_=self.xbuf_d[r0:r0 + P, :]), f"G{gi_}", reads=[b_xbuf], writes=[Gb[gi_]])
                self.transpose_mod(G[:], Gb[gi_], hxT, Bn("hxT"), 2, 3, banks=(0, 1), plain=True)
                for half in range(2):
                    s_ = wb_i[half]
                    bg, bu = (2, 3) if half == 0 else (4, 5)
                    S.op("pe", [lambda t, k=k, s_=s_, bg=bg: t.matmul(self.psum[bg][:, :], lhsT=hxT[:, k, :], rhs=wg[s_][:, k, :], start=(k == 0), stop=(k == KC - 1))
                                for k in range(KC)] +
                               [lambda t, k=k, s_=s_, bu=bu: t.matmul(self.psum[bu][:, :], lhsT=hxT[:, k, :], rhs=wu[s_][:, k, :], start=(k == 0), stop=(k == KC - 1))
                                for k in range(KC)], reads=[Bn("hxT"), wsb[s_]], writes=[self.pbuf[bg], self.pbuf[bu]])
                    S.op("act", [lambda a, half=half, bg=bg: a.activation(out=sg[half][:], in_=self.psum[bg][:, :], func=AF.Silu),
                                 lambda a, half=half, bu=bu: a.copy(out=su[half][:], in_=self.psum[bu][:, :])],
                         reads=[self.pbuf[bg], self.pbuf[bu]], writes=[Bn(f"sgsu{half}")])
                    S.op("dve", lambda v, half=half: v.tensor_tensor(out=hid[:, half * 512:(half + 1) * 512], in0=sg[half][:], in1=su[half][:], op=ALU.mult),
                         reads=[Bn(f"sgsu{half}")], writes=[Bn(f"hid{half}")])
                pb0 = self.psum[0][:].bitcast(BF16)
                for half in range(2):
                    S.op("pe", [lambda t, c=c, pb0=pb0: t.transpose(out=pb0[:, c * P:(c + 1) * P], in_=hid[:, c * P:(c + 1) * P], identity=idn[:])
                                for c in range(4 * half, 4 * half + 4)], reads=[Bn(f"hid{half}"), cb], writes=[self.pbuf[0]])
                S.op("act", lambda a, pb0=pb0: a.copy(out=hidT[:].rearrange("p a b -> p (a b)"), in_=pb0[:, :]), reads=[self.pbuf[0]], writes=[Bn("hidT")])
                for n in range(4):
                    db = dbank[n]
                    S.op("pe", [lambda t, c=c, n=n, db=db, wb_i=tuple(wb_i): t.matmul(self.psum[db][:, :], lhsT=hidT[:, c, :], rhs=wd[wb_i[c // 4]][:, c % 4, n * 512:(n + 1) * 512],
                                                                    start=(c == 0), stop=(c == 7)) for c in range(8)],
                         reads=[Bn("hidT"), wdb[wb_i[0]], wdb[wb_i[1]]], writes=[self.pbuf[db]])
                    if n % 2 == 0:
                        S.op("act", lambda a, n=n, db=db: a.copy(out=ost[:, n * 512:(n + 1) * 512], in_=self.psum[db][:, :]), reads=[self.pbuf[db]], writes=[self.xinb[0]])
                    else:
                        S.op("dve", lambda v, n=n, db=db: v.tensor_copy(out=ost[:, n * 512:(n + 1) * 512], in_=self.psum[db][:, :]), reads=[self.pbuf[db]], writes=[self.xinb[0]])
                S.dma("act", lambda q, r0=r0: q.dma_start(out=self.obuf_d[r0:r0 + P, :], in_=ost[:]), "ost", reads=[self.xinb[0]], writes=[b_ob])
                S.end_if(dummies)
        self.pop_scope()

    def phase7(self):
        S = self.S
        Bn = self.B
        self.push_scope()
        bc = [self.sb(f"bcf{i}", [P, D], F32) for i in range(3)]
        bcb = [Bn(f"bcf{i}") for i in range(3)]
        S.dma("sp", lambda q: q.dma_start(out=bc[0][:], in_=self.mod_d[0, 5 * D:6 * D].partition_broadcast(P)), "bcf0",
              reads=[Bn("mod_d")], writes=[bcb[0]])
        S.dma("sp", lambda q: q.dma_start(out=bc[1][:], in_=self.ln2g.partition_broadcast(P)), "bcf1", writes=[bcb[1]])
        S.dma("sp", lambda q: q.dma_start(out=bc[2][:], in_=self.ln2b.partition_broadcast(P)), "bcf2", writes=[bcb[2]])
        og = [[self.sb(f"og{i}{j}", [P, D], F32) for j in range(2)] for i in range(2)]
        ogb = [[Bn(f"og{i}{j}") for j in range(2)] for i in range(2)]
        fsb = [self.sb(f"fsb{i}", [P, D], F32) for i in range(2)]
        fb = [Bn(f"fsb{i}") for i in range(2)]
        b_ob = Bn("obuf_d")
        for ti in range(NT):
            i = ti % 2
            xin, xb = self.xin[i], self.xinb[i]
            f_, b_f = fsb[i], fb[i]
            S.dma("sp", lambda q, xin=xin, ti=ti: q.dma_start(out=xin[:], in_=self.x1_d[ti * P:(ti + 1) * P, :]), f"xin{i}",
                  reads=[Bn("x1_d")], writes=[xb])
            for j in range(2):
                S.dma("pool", lambda g, i=i, j=j, ti=ti: g.indirect_dma_start(
                    out=og[i][j][:, :], out_offset=None, in_=self.obuf_d[:, :],
                    in_offset=bass.IndirectOffsetOnAxis(ap=self.idx_all[:, ti, j:j + 1], axis=0),
                    bounds_check=S.rt["bnd"], oob_is_err=False), f"og{i}{j}", reads=[b_ob, Bn("idx_all")], writes=[ogb[i][j]])
            S.op("dve", lambda v, ti=ti, i=i, f_=f_: v.tensor_scalar(out=f_[:], in0=og[i][0][:], scalar1=self.w_all[:, ti, 0:1], scalar2=None, op0=ALU.mult),
                 reads=[ogb[i][0], Bn("w_all")], writes=[b_f])
            S.op("dve", lambda v, ti=ti, i=i, f_=f_: v.scalar_tensor_tensor(out=f_[:], in0=og[i][1][:], scalar=self.w_all[:, ti, 1:2], in1=f_[:],
                                                                          op0=ALU.mult, op1=ALU.add), reads=[ogb[i][1], Bn("w_all"), b_f], writes=[b_f])
            S.op("pool", lambda g, f_=f_: g.tensor_tensor(out=f_[:], in0=f_[:], in1=bc[0][:], op=ALU.mult), reads=[b_f, bcb[0]], writes=[b_f])
            S.op("dve", lambda v, xin=xin, f_=f_: v.scalar_tensor_tensor(out=xin[:], in0=xin[:], scalar=float(ALPHA), in1=f_[:], op0=ALU.mult, op1=ALU.add),
                 reads=[xb, b_f], writes=[xb])
            self.ln_stats(xin, xb)
            S.op("dve", lambda v, xin=xin, f_=f_: v.tensor_scalar(out=f_[:], in0=xin[:], scalar1=self.mv[:, 0:1], scalar2=self.rstd[:, 0:1],
                                                                op0=ALU.subtract, op1=ALU.mult), reads=[xb, self.b_mv, self.b_rstd], writes=[b_f])
            S.op("pool", lambda g, f_=f_: g.tensor_tensor(out=f_[:], in0=f_[:], in1=bc[1][:], op=ALU.mult), reads=[b_f, bcb[1]], writes=[b_f])
            S.op("pool", lambda g, f_=f_: g.tensor_tensor(out=f_[:], in0=f_[:], in1=bc[2][:], op=ALU.add), reads=[b_f, bcb[2]], writes=[b_f])
            self.final_toks.append(S.dma("sp", lambda q, ti=ti, f_=f_: q.dma_start(out=self.y[ti * P:(ti + 1) * P, :], in_=f_[:]), f"yout{i}",
                                         reads=[b_f], writes=[Bn("y")]))
        self.pop_scope()

    def finish(self):
        S = self.S
        S.final_wait("sp", self.final_toks)
        with self.nc.Block() as block:
            S.emit(block)
        while self.scopes:
            self.scopes.pop().close()
        self.es.close()
        return self.nc


def build_program(debug=(), upto="all", n_other=NT):
    b = Builder(debug)
    b.n_other = n_other
    b.declare_inputs(with_experts=(upto in ("all", "moe")))
    b.load_consts()
    b.phase0()
    if upto == "p0":
        return b.finish(), b
    b.phase1()
    if upto == "p1":
        return b.finish(), b
    b.phase2()
    if upto == "p2":
        return b.finish(), b
    b.phase3()
    b.phase4()
    b.pop_scope()
    if upto == "p4":
        return b.finish(), b
    b.phase5()
    if upto == "p5":
        return b.finish(), b
    b.phase6()
    b.phase7()
    return b.finish(), b


def _win_layout(w_in, half):
    qa, ka, va, qb, kb, vb, rb, gb = 0, 1024, 1280, 1536, 2048, 2560, 3584, 4608
    out = np.zeros((D, WIN_COLS), np.float32)
    out[:, FM_QA * 128:FM_QA * 128 + 1024] = w_in[:, qa:qa + 1024]
    out[:, FM_KA * 128:FM_KA * 128 + 256] = w_in[:, ka:ka + 256]
    out[:, FM_QB * 128:FM_QB * 128 + 512] = w_in[:, qb:qb + 512]
    out[:, FM_KB * 128:FM_KB * 128 + 512] = w_in[:, kb:kb + 512]
    out[:, FM_RB * 128:FM_RB * 128 + 1024] = w_in[:, rb:rb + 1024]
    gF, gR = (gb, gb + 16) if half == 0 else (gb + 16, gb)
    out[:, GB_OFF:GB_OFF + 16] = w_in[:, gF:gF + 16]
    out[:, GB_OFF + 32:GB_OFF + 48] = w_in[:, gR:gR + 16]
    out[:, TM_OFF + TM_VA:TM_OFF + TM_VA + 256] = w_in[:, va:va + 256]
    out[:, TM_OFF + TM_KB:TM_OFF + TM_KB + 512] = w_in[:, kb:kb + 512]
    out[:, TM_OFF + TM_VB:TM_OFF + TM_VB + 1024] = w_in[:, vb:vb + 1024]
    return out


def prep_inputs(inp, cores=range(8)):
    f = lambda a: np.ascontiguousarray(np.asarray(a, dtype=np.float32))
    x, c, ctx, c_ctx = f(inp["x"]), f(inp["c"]), f(inp["ctx"]), f(inp["c_ctx"])
    w_ada, b_ada = f(inp["w_ada"])[0], f(inp["b_ada"])[0]
    w_in = f(inp["w_in"])[0]
    wgu_in, bg_in = f(inp["w_gate_up"])[0], f(inp["b_gate"])[0]
    consts = _consts()
    shared = {
        "w_ada": w_ada, "b_ada": b_ada, "sink": f(inp["attn_sink"])[0],
        "normw": np.ascontiguousarray(f(inp["gla_norm_w"])[0].reshape(8, P).T),
        "w_out": f(inp["w_out"])[0], "ln1g": f(inp["ln1_g"])[0], "ln1b": f(inp["ln1_b"])[0],
        "ln2g": f(inp["ln2_g"])[0], "ln2b": f(inp["ln2_b"])[0],
        "w_r": np.ascontiguousarray(np.concatenate([f(inp["w_router_group"])[0], f(inp["w_router_expert"])[0]], axis=1)),
        "b_r": np.ascontiguousarray(np.concatenate([f(inp["b_router_group"])[0], f(inp["b_router_expert"])[0]])),
        "weg": f(inp["w_exp_gate"])[0], "weu": f(inp["w_exp_up"])[0], "wed": f(inp["w_exp_down"])[0],
    }
    for k, v in consts.items():
        shared["c_" + k] = v
    per_half = {}
    for half in (0, 1):
        cosT, sinT = _rope_tables(half)
        wgu = np.zeros((2, 64, 512), np.float32)
        sF, sR = (0, 1) if half == 0 else (1, 0)
        wgu[0, 0:16] = wgu_in[sF]
        wgu[1, 32:48] = wgu_in[sR]
        bgate = np.ascontiguousarray(np.stack([bg_in[sF], bg_in[sR]]))
        per_half[half] = {"w_in": _win_layout(w_in, half), "wgu": wgu, "bgate": bgate, "cosT": cosT, "sinT": sinT}
    maps = []
    for core in cores:
        b, half = core // 2, core % 2
        xl = x[b] if half == 0 else x[b][::-1]
        cl = ctx[b] if half == 0 else ctx[b][::-1]
        ccv = np.stack([c[b].reshape(KC, P).T, c_ctx.reshape(KC, P).T], axis=2).reshape(P, 32)
        m = {"x_own": np.ascontiguousarray(xl[:NOWN]), "x_oth": np.ascontiguousarray(xl[NOWN:]),
             "ctxl": np.ascontiguousarray(cl), "cc": np.ascontiguousarray(ccv)}
        m.update(per_half[half])
        m.update(shared)
        maps.append(m)
    return maps


def kernel(**inputs):
    nc, b = build_program()
    maps = prep_inputs(inputs)
    res = run_bass_kernel_spmd(nc, maps, core_ids=list(range(8)))
    out = np.zeros((4, SEQ, D), np.float32)
    for core in range(8):
        bb, half = core // 2, core % 2
        yc = np.asarray(res.results[core]["y"])
        if half == 0:
            out[bb, :NOWN] = yc
        else:
            out[bb, NOWN:] = yc[::-1]
    return out
```

```python
import numpy as np
from contextlib import ExitStack
import concourse.bass as bass
import concourse.mybir as mybir
from concourse.bass_utils import run_bass_kernel_spmd

F32 = mybir.dt.float32
BF16 = mybir.dt.bfloat16
I32 = mybir.dt.int32
AF = mybir.ActivationFunctionType
ALU = mybir.AluOpType

D = 2048
KC = 16
SEQ = 4096
NOWN = 2048
NT = 16
P = 128
LN_EPS = 1e-6
ALPHA = 2.0 ** 0.25
HD = 128
N_EXP = 32
HID = 1024
CAPB = 8
CAPS = CAPB * 128
WD_SPLIT = 4

FM_QA, FM_KA, FM_QB, FM_KB, FM_RB = 0, 8, 10, 14, 18
N_FM = 26
FM_COLS = N_FM * 128
GB_OFF = FM_COLS
TM_OFF = GB_OFF + 64
TM_VA, TM_KB, TM_VB = 0, 256, 768
TM_COLS = 1792
WIN_COLS = TM_OFF + TM_COLS


class Buf:
    __slots__ = ("name", "w", "r")

    def __init__(self, name):
        self.name = name
        self.w = None
        self.r = []


class Sched:
    ENG = ("pe", "act", "dve", "pool", "sp")

    def __init__(self, nc, es):
        self.nc = nc
        self.es = es
        self.eng = {"pe": nc.tensor, "act": nc.scalar, "dve": nc.vector, "pool": nc.gpsimd, "sp": nc.sync}
        self.items = {e: [] for e in self.ENG}
        self.sems = {}
        self.cnt = {}
        self.waited = {e: {} for e in self.ENG}
        for e in self.ENG:
            self._sem("E_" + e)
        self.in_if = None
        self.rt = {}
        self.if_keys = {}

    def _sem(self, key):
        if key not in self.sems:
            self.sems[key] = self.es.enter_context(self.nc.semaphore("s_" + key))
            self.cnt[key] = 0
        return self.sems[key]

    def _need(self, engine, tok):
        if tok is None:
            return
        key, val = tok
        if self.waited[engine].get(key, 0) >= val:
            return
        self.waited[engine][key] = val
        self.items[engine].append(("wait", key, val))

    def _need_all(self, engine, toks):
        best = {}
        for t in toks:
            if t is not None and best.get(t[0], 0) < t[1]:
                best[t[0]] = t[1]
        for k, v in best.items():
            self._need(engine, (k, v))

    def _deps(self, engine, reads, writes):
        toks = [b.w for b in reads]
        for b in writes:
            toks.append(b.w)
            toks.extend(b.r)
        self._need_all(engine, toks)

    def _commit(self, tok, reads, writes):
        for b in reads:
            b.r.append(tok)
        for b in writes:
            b.w = tok
            b.r = []

    def op(self, engine, fns, reads=(), writes=()):
        if not isinstance(fns, (list, tuple)):
            fns = [fns]
        self._deps(engine, reads, writes)
        key = "E_" + engine
        self.cnt[key] += 1
        tok = (key, self.cnt[key])
        self.items[engine].append(("ops", list(fns), key, 1))
        if engine == "pe":
            self.waited[engine][key] = tok[1]
        self._commit(tok, reads, writes)
        if self.in_if is not None:
            self.in_if["incs"].setdefault(engine, {}).setdefault(key, 0)
            self.in_if["incs"][engine][key] += 1
        return tok

    def dma(self, engine, fn, key, reads=(), writes=()):
        key = "D_" + key + "_" + engine
        self._sem(key)
        toks = [b.w for b in reads]
        for b in writes:
            if not (b.w is not None and b.w[0] == key):
                toks.append(b.w)
            toks.extend(b.r)
        self._need_all(engine, toks)
        self.cnt[key] += 16
        tok = (key, self.cnt[key])
        self.items[engine].append(("ops", [fn], key, 16))
        self._commit(tok, reads, writes)
        if self.in_if is not None:
            self.in_if["incs"].setdefault(engine, {}).setdefault(key, 0)
            self.in_if["incs"][engine][key] += 16
        return tok

    IF_ENG = ("pe", "act", "dve", "sp")

    def reg_load(self, regname, ap, buf):
        for e in self.IF_ENG:
            self._need(e, buf.w)
            self.items[e].append(("regload", regname, ap))

    def begin_if(self, regname, thr):
        assert self.in_if is None
        self.in_if = {"incs": {}, "start": {e: len(self.items[e]) for e in self.ENG},
                      "waited": {e: dict(self.waited[e]) for e in self.ENG},
                      "cnt0": dict(self.cnt)}
        for e in self.IF_ENG:
            self.items[e].append(("if", regname, thr))

    def end_if(self, dummies):
        st = self.in_if
        self.in_if = None
        for e in self.ENG:
            incs = st["incs"].get(e, {})
            if e not in self.IF_ENG:
                assert not incs, f"engine {e} cannot be used inside a dynamic section"
                del self.items[e][st["start"][e]:]
            else:
                self.if_keys.setdefault(e, set()).update(incs.keys())
                wk = set(self.if_keys[e]) if e == "sp" else (set("E_" + x for x in ("pe", "act", "dve")) | set(self.if_keys[e]))
                self.items[e].append(("else", dict(incs), dummies[e], {k: st["cnt0"].get(k, 0) for k in wk}))
                self.items[e].append(("endif",))
            self.waited[e] = st["waited"][e]

    def barrier(self):
        for e in self.ENG:
            for key, c in self.cnt.items():
                if c > 0:
                    self._need(e, (key, c))

    def final_wait(self, engine, toks):
        for t in toks:
            self._need(engine, t)

    def emit(self, block):
        nc = self.nc
        deco = {"pe": block.tensor, "act": block.scalar, "dve": block.vector, "pool": block.gpsimd, "sp": block.sync}
        for e in self.ENG:
            items = self.items[e]

            def body(eng, items=items, e=e):
                regs = {}
                stack = []
                with ExitStack() as rs:
                    if e == "pool":
                        self.rt["bnd"] = rs.enter_context(eng.register("bnd_reg"))
                        eng.reg_mov(self.rt["bnd"], N_EXP * CAPS - 1)
                    for it in items:
                        k = it[0]
                        if k == "wait":
                            eng.wait_ge(self.sems[it[1]], it[2])
                        elif k == "ops":
                            ins = None
                            for fn in it[1]:
                                ins = fn(eng)
                            ins.then_inc(self.sems[it[2]], it[3])
                        elif k == "regload":
                            if it[1] not in regs:
                                regs[it[1]] = rs.enter_context(eng.register(it[1] + "_" + e))
                            eng.reg_load(regs[it[1]], it[2])
                        elif k == "if":
                            g = eng.If_cmp(regs[it[1]], it[2], "IS_GT")
                            g.__enter__()
                            stack.append(g)
                        elif k == "else":
                            g = stack.pop()
                            g.__exit__(None, None, None)
                            g2 = eng.Else()
                            g2.__enter__()
                            for key, val in it[3].items():
                                if val > 0:
                                    eng.wait_ge(self.sems[key], val)
                            for kk, (key, inc) in enumerate(it[1].items()):
                                it[2](eng, kk).then_inc(self.sems[key], inc)
                            stack.append(g2)
                        elif k == "endif":
                            g = stack.pop()
                            g.__exit__(None, None, None)
            deco[e](body)


def _rope_tables(half):
    n_freq = HD // 4
    inv_freq = (10000.0 ** (-np.arange(n_freq, dtype=np.float32) / n_freq)).astype(np.float32)
    j = np.arange(NOWN + 128)
    g = j if half == 0 else (SEQ - 1 - j)
    row = (g // 64).astype(np.float32)
    col = (g % 64).astype(np.float32)
    cos = np.zeros((HD, NOWN + 128), np.float32)
    sin = np.zeros((HD, NOWN + 128), np.float32)
    for d in range(HD):
        seg, f = d // 32, d % 32
        ang = (row if seg < 2 else col) * inv_freq[f]
        cos[d] = np.cos(ang.astype(np.float32))
        s = np.sin(ang.astype(np.float32))
        sin[d] = -s if seg in (0, 2) else s
    return cos, sin


def _consts():
    c = {}
    c["ident"] = np.eye(P, dtype=np.float32)
    rm = np.zeros((P, P), np.float32)
    for m in range(P):
        seg = m // 32
        partner = m + 32 if seg in (0, 2) else m - 32
        rm[partner, m] = 1.0
    c["rotm"] = rm
    s = np.arange(P)[:, None]
    t = np.arange(P)[None, :]
    le = (s <= t).astype(np.float32)
    ge = (s >= t).astype(np.float32)
    m1f = -(le - (s <= 63).astype(np.float32)) / 16.0
    m2f = -le / 16.0
    m1r = -(ge - (s >= 64).astype(np.float32)) / 16.0
    m2r = -ge / 16.0
    c["m12f"] = np.concatenate([m1f, m2f], axis=1).astype(np.float32)
    c["m12r"] = np.concatenate([m1r, m2r], axis=1).astype(np.float32)
    c["m3f"] = (-(s > t).astype(np.float32) / 16.0)
    c["m3r"] = (-(s < t).astype(np.float32) / 16.0)
    c["gmaskf"] = np.tile(le, (1, 4))
    c["gmaskr"] = np.tile(ge, (1, 4))
    c["amprev"] = np.tile(ge, (1, 4))
    c["amnext"] = np.tile(le, (1, 4))
    c["ones"] = np.ones((P, P), np.float32)
    c["n16"] = np.full((P, 1), -1.0 / 16.0, np.float32)
    c["lstrict"] = (s < t).astype(np.float32)
    c["slotbase"] = np.tile((np.arange(N_EXP, dtype=np.float32) * CAPS)[None, :], (P, 1))
    c["iotap"] = np.arange(P, dtype=np.float32)[:, None].copy()
    return c


CONST_SHAPES = {"ident": (P, P), "rotm": (P, P), "m12f": (P, 256), "m12r": (P, 256), "m3f": (P, P), "m3r": (P, P),
                "gmaskf": (P, 512), "gmaskr": (P, 512), "amprev": (P, 512), "amnext": (P, 512), "ones": (P, P),
                "n16": (P, 1), "lstrict": (P, P), "slotbase": (P, N_EXP), "iotap": (P, 1)}


class Builder:
    def __init__(self, debug=()):
        self.debug = set(debug)
        self.nc = bass.Bass("TRN2", target_bir_lowering=False)
        self.es = ExitStack()
        self.S = Sched(self.nc, self.es)
        self.bufs = {}
        self.outs = []
        self.final_toks = []
        self.scopes = []
        self.nalloc = 0
        self.alloc_log = []

    def sb(self, name, shape, dt):
        stack = self.scopes[-1] if self.scopes else self.es
        self.nalloc += 1
        nb = int(np.prod(shape[1:])) * (4 if dt in (F32, I32) else 2)
        self.alloc_log.append((name, nb, len(self.scopes)))
        return stack.enter_context(self.nc.sbuf_tensor(f"{name}_{self.nalloc}", list(shape), dt))

    def push_scope(self):
        self.scopes.append(ExitStack())

    def pop_scope(self):
        self.S.barrier()
        self.scopes.pop().close()

    def din(self, name, shape, dt=F32):
        return self.nc.dram_tensor(name, list(shape), dt, kind="ExternalInput").ap()

    def dout(self, name, shape, dt=F32):
        self.outs.append(name)
        return self.nc.dram_tensor(name, list(shape), dt, kind="ExternalOutput").ap()

    def dscr(self, name, shape, dt):
        return self.nc.dram_tensor(name, list(shape), dt, kind="Internal").ap()

    def B(self, name):
        if name not in self.bufs:
            self.bufs[name] = Buf(name)
        return self.bufs[name]

    def dbg_out(self, name, src_ap, shape, dt, reads, eng="sp"):
        if name not in self.debug:
            return
        o = self.dout("dbg_" + name, shape, dt)
        tok = self.S.dma(eng, lambda q, o=o, s=src_ap: q.dma_start(out=o, in_=s), "dbg", reads=reads)
        self.final_toks.append(tok)

    def declare_inputs(self, with_experts=True):
        self.x_own = self.din("x_own", [NOWN, D])
        self.x_oth = self.din("x_oth", [NOWN, D])
        self.ctxl = self.din("ctxl", [256, D])
        self.cc = self.din("cc", [P, 32])
        self.w_ada = self.din("w_ada", [D, 6 * D])
        self.b_ada = self.din("b_ada", [6 * D])
        self.w_in = self.din("w_in", [D, WIN_COLS])
        self.wgu = self.din("wgu", [2, 64, 512])
        self.bgate = self.din("bgate", [2, 512])
        self.sink = self.din("sink", [8])
        self.normw = self.din("normw", [P, 8])
        self.w_out = self.din("w_out", [D, D])
        self.ln1g = self.din("ln1g", [D])
        self.ln1b = self.din("ln1b", [D])
        self.ln2g = self.din("ln2g", [D])
        self.ln2b = self.din("ln2b", [D])
        self.w_r = self.din("w_r", [D, 36])
        self.b_r = self.din("b_r", [36])
        if with_experts:
            self.weg = self.din("weg", [N_EXP, D, HID])
            self.weu = self.din("weu", [N_EXP, D, HID])
            self.wed = self.din("wed", [N_EXP, HID, D])
        self.cosT = self.din("cosT", [HD, NOWN + 128])
        self.sinT = self.din("sinT", [HD, NOWN + 128])
        self.cin = {k: self.din("c_" + k, list(s)) for k, s in CONST_SHAPES.items()}
        self.y = self.dout("y", [NOWN, D])

    def load_consts(self):
        S = self.S
        self.c32 = {}
        self.c16 = {}
        cb = self.B("consts")
        for k in ("m12f", "m12r", "m3f", "m3r", "ones", "n16", "ident", "lstrict", "slotbase", "iotap"):
            t = self.sb("c32_" + k, CONST_SHAPES[k], F32)
            S.dma("sp", lambda q, t=t, k=k: q.dma_start(out=t[:], in_=self.cin[k]), "consts", writes=[cb])
            self.c32[k] = t
        for k in ("ident", "rotm", "gmaskf", "gmaskr", "amprev", "amnext", "ones"):
            t = self.sb("c16_" + k, CONST_SHAPES[k], BF16)
            S.dma("pool", lambda q, t=t, k=k: q.dma_start(out=t[:], in_=self.cin[k]), "consts", writes=[cb])
            self.c16[k] = t
        self.psum = [self.es.enter_context(self.nc.psum_tensor(f"pb{i}", [P, 512], F32)) for i in range(8)]
        self.pbuf = [self.B(f"psum{i}") for i in range(8)]
        self.dummy = self.sb("dummy_t", [P, 8], F32)
        self.eps_t = self.sb("eps_t", [P, 1], F32)
        S.op("dve", lambda v: v.memset(self.eps_t[:], LN_EPS), writes=[cb])
        self.one_t = self.sb("one_t", [P, 1], F32)
        S.op("dve", lambda v: v.memset(self.one_t[:], 1.0), writes=[cb])
        S.op("pool", lambda g: g.memset(self.dummy[:], 0.0), writes=[self.B("dummy")])

    def phase0(self):
        S, nc = self.S, self.nc
        self.mod_d = self.dscr("mod_d", [2, 6 * D], F32)
        b_modd = self.B("mod_d")
        self.vecs = self.sb("vecs", [P, 6, KC], F32)
        b_vecs = self.B("vecs")
        self.push_scope()
        cc_sb = self.sb("cc_sb", [P, 32], F32)
        sc_bf = self.sb("sc_bf", [P, 32], BF16)
        b_cc, b_sc = self.B("cc"), self.B("sc_bf")
        S.dma("sp", lambda q: q.dma_start(out=cc_sb[:], in_=self.cc), "p0a", writes=[b_cc])
        S.op("act", lambda a: a.activation(out=sc_bf[:], in_=cc_sb[:], func=AF.Silu), reads=[b_cc], writes=[b_sc])
        wv = self.w_ada.rearrange("(k p) n -> p k n", p=P)
        NB = 3
        wbufs = [self.sb(f"wada{i}", [P, KC, 1024], BF16) for i in range(NB)]
        wb = [self.B(f"wada{i}") for i in range(NB)]
        bad = [self.sb(f"bada{i}", [2, 1024], F32) for i in range(2)]
        badb = [self.B(f"bada{i}") for i in range(2)]
        mods = [self.sb(f"mods{i}", [2, 1024], F32) for i in range(2)]
        modb = [self.B(f"mods{i}") for i in range(2)]
        for cbk in range(12):
            i = cbk % NB
            j = cbk % 2
            S.dma("pool", lambda q, i=i, cbk=cbk: q.dma_start(out=wbufs[i][:], in_=wv[:, :, cbk * 1024:(cbk + 1) * 1024]),
                  f"wada{i}", writes=[wb[i]])
            S.dma("sp", lambda q, j=j, cbk=cbk: q.dma_start(
                out=bad[j][:], in_=self.b_ada[cbk * 1024:(cbk + 1) * 1024].partition_broadcast(2)),
                f"bada{j}", writes=[badb[j]])
            for n in range(2):
                pi = n
                ps = self.psum[pi]
                S.op("pe", [lambda t, k=k, i=i, n=n, ps=ps: t.matmul(ps[0:2, :], lhsT=sc_bf[:, 2 * k:2 * k + 2],
                                                                    rhs=wbufs[i][:, k, n * 512:(n + 1) * 512],
                                                                    start=(k == 0), stop=(k == KC - 1)) for k in range(KC)],
                     reads=[b_sc, wb[i]], writes=[self.pbuf[pi]])
                S.op("dve", lambda v, ps=ps, n=n, j=j: v.tensor_tensor(out=mods[j][0:2, n * 512:(n + 1) * 512], in0=ps[0:2, :],
                                                                     in1=bad[j][0:2, n * 512:(n + 1) * 512], op=ALU.add),
                     reads=[self.pbuf[pi], badb[j]], writes=[modb[j]])
            S.dma("sp", lambda q, j=j, cbk=cbk: q.dma_start(out=self.mod_d[:, cbk * 1024:(cbk + 1) * 1024], in_=mods[j][:]),
                  f"p0b{j}", reads=[modb[j]], writes=[self.B(f"mod_d_st{j}")])
        srcs = [(0, 0), (0, D), (0, 3 * D), (0, 4 * D), (1, 0), (1, D)]
        for i, (r, off) in enumerate(srcs):
            S.dma("sp", lambda q, i=i, r=r, off=off: q.dma_start(
                out=self.vecs[:, i, :], in_=self.mod_d[r, off:off + D].rearrange("(k p) -> p k", p=P),
                allow_slow_non_contiguous=True), "p0c", reads=[self.B("mod_d_st0"), self.B("mod_d_st1")], writes=[b_vecs])
        for i in (1, 3, 5):
            S.op("dve", lambda v, i=i: v.tensor_scalar(out=self.vecs[:, i, :], in0=self.vecs[:, i, :], scalar1=1.0,
                                                       scalar2=None, op0=ALU.add), reads=[b_vecs], writes=[b_vecs])
        self.dbg_out("vecs", self.vecs[:], [P, 6, KC], F32, [b_vecs])
        if "mod" in self.debug:
            o = self.dout("dbg_mod", [2, 6 * D], F32)
            self.final_toks.append(S.dma("sp", lambda q: q.dma_start(out=o, in_=self.mod_d), "dbg", reads=[self.B("mod_d_st0"), self.B("mod_d_st1")]))
        self.pop_scope()

    def mix_setup(self):
        S = self.S
        self.xin = [self.sb(f"xin{i}", [P, D], F32) for i in range(2)]
        self.xinb = [self.B(f"xin{i}") for i in range(2)]
        self.xn = [self.sb(f"xn{i}", [P, D], BF16) for i in range(2)]
        self.xnb = [self.B(f"xn{i}") for i in range(2)]
        self.stats = self.sb("stats", [P, 4, 6], F32)
        self.mv = self.sb("mv", [P, 2], F32)
        self.rstd = self.sb("rstd", [P, 1], F32)
        self.b_stats, self.b_mv, self.b_rstd = self.B("stats"), self.B("mv"), self.B("rstd")
        self.ln_i = 0

    def ln_norm(self, src_ap, out_bf, b_out, src_buf=None, load=True, xin_idx=None):
        S = self.S
        i = self.ln_i % 2 if xin_idx is None else xin_idx
        self.ln_i += 1
        xin, xb = self.xin[i], self.xinb[i]
        if load:
            S.dma("sp", lambda q: q.dma_start(out=xin[:], in_=src_ap), f"xin{i}", writes=[xb])
        S.op("dve", [lambda v, c=c: v.bn_stats(out=self.stats[:, c, :], in_=xin[:, c * 512:(c + 1) * 512]) for c in range(4)],
             reads=[xb], writes=[self.b_stats])
        S.op("dve", lambda v: v.bn_aggr(out=self.mv[:], in_=self.stats[:].rearrange("p a b -> p (a b)")),
             reads=[self.b_stats, xb], writes=[self.b_mv])
        S.op("act", lambda a: a.activation(out=self.rstd[:], in_=self.mv[:, 1:2], func=AF.Ln, bias=self.eps_t[:, 0:1]),
             reads=[self.b_mv], writes=[self.b_rstd])
        S.op("act", lambda a: a.activation(out=self.rstd[:], in_=self.rstd[:], func=AF.Exp, scale=-0.5),
             reads=[self.b_rstd], writes=[self.b_rstd])
        S.op("dve", lambda v: v.tensor_scalar(out=out_bf, in0=xin[:], scalar1=self.mv[:, 0:1], scalar2=self.rstd[:, 0:1],
                                              op0=ALU.subtract, op1=ALU.mult),
             reads=[xb, self.b_mv, self.b_rstd], writes=[b_out])
        return i

    def transpose_mod(self, xn_bf, b_xn, hT, b_hT, vi_shift, vi_scale, banks=(0, 1), plain=False):
        S = self.S
        idn = self.c16["ident"]
        for half in range(2):
            pb = self.psum[banks[half]][:].bitcast(BF16)
            S.op("pe", [lambda t, k=k, pb=pb: t.transpose(out=pb[:, (k % 8) * 128:(k % 8 + 1) * 128],
                                                        in_=xn_bf[:, k * 128:(k + 1) * 128], identity=idn[:])
                        for k in range(half * 8, half * 8 + 8)],
                 reads=[b_xn, self.B("consts")], writes=[self.pbuf[banks[half]]])
            if plain:
                eng = "act" if half == 0 else "dve"
                if eng == "act":
                    S.op("act", lambda a, pb=pb, half=half: a.copy(out=hT[:, half * 8:half * 8 + 8, :].rearrange("p a b -> p (a b)"), in_=pb[:, :]),
                         reads=[self.pbuf[banks[half]]], writes=[b_hT])
                else:
                    S.op("dve", lambda v, pb=pb, half=half: v.tensor_copy(out=hT[:, half * 8:half * 8 + 8, :].rearrange("p a b -> p (a b)"), in_=pb[:, :]),
                         reads=[self.pbuf[banks[half]]], writes=[b_hT])
                continue
            S.op("act", [lambda a, k=k, pb=pb: a.activation(out=hT[:, k, :], in_=pb[:, (k % 8) * 128:(k % 8 + 1) * 128],
                                                          func=AF.Identity, scale=self.vecs[:, vi_scale, k:k + 1],
                                                          bias=self.vecs[:, vi_shift, k:k + 1])
                         for k in range(half * 8, half * 8 + 8)],
                 reads=[self.pbuf[banks[half]], self.B("vecs")], writes=[b_hT])

    def chain_setup(self):
        S = self.S
        cb = self.B("consts")
        self.Sst = [self.sb(f"Sst{d}", [P, 1024], F32) for d in range(2)]
        self.Sb = [self.B(f"Sst{d}") for d in range(2)]
        for d in range(2):
            S.op("dve", lambda v, d=d: v.memset(self.Sst[d][:], 0.0), writes=[self.Sb[d]])
        self.wgu_sb = [self.sb(f"wgu{d}", [64, 512], F32) for d in range(2)]
        self.bg_sb = [self.sb(f"bg{d}", [1, 512], F32) for d in range(2)]
        for d in range(2):
            S.dma("sp", lambda q, d=d: q.dma_start(out=self.wgu_sb[d][:], in_=self.wgu[d]), "cstc", writes=[self.B("cst_chain")])
            S.dma("sp", lambda q, d=d: q.dma_start(out=self.bg_sb[d][:], in_=self.bgate[d:d + 1, :]), "cstc", writes=[self.B("cst_chain")])
        self.ktok = [self.sb(f"ktok{i}", [P, 512], BF16) for i in range(2)]
        self.vtok = [self.sb(f"vtok{i}", [P, 1024], BF16) for i in range(2)]
        self.glT = [self.sb(f"glT{i}", [64, P], F32) for i in range(2)]
        self.gp = [[self.sb(f"gp{i}_{d}", [P, 512], F32) for d in range(2)] for i in range(2)]
        self.b_ktok = [self.B(f"ktok{i}") for i in range(2)]
        self.b_vtok = [self.B(f"vtok{i}") for i in range(2)]
        self.b_glT = [self.B(f"glT{i}") for i in range(2)]
        self.b_gp = [[self.B(f"gp{i}_{d}") for d in range(2)] for i in range(2)]
        self.etmp = self.sb("etmp", [P, 512], F32)
        self.b_etmp = self.B("etmp")
        self.e3 = self.sb("e3", [P, 512], F32)
        self.b_e3 = self.B("e3")
        self.k3 = self.sb("k3", [P, 512], BF16)
        self.b_k3 = self.B("k3")
        self.dec = self.sb("dec", [P, 4], F32)
        self.b_dec = self.B("dec")

    def gate_gp(self, i, d, zbank=4):
        S = self.S
        cb = self.B("consts")
        ps = self.psum[zbank]
        S.op("pe", [lambda t: t.matmul(ps[:, :], lhsT=self.glT[i][:, :], rhs=self.wgu_sb[d][:, :], start=True, stop=False),
                    lambda t: t.matmul(ps[:, :], lhsT=self.c32["ones"][0:1, :], rhs=self.bg_sb[d][0:1, :], start=False, stop=True)],
             reads=[self.b_glT[i], cb, self.B("cst_chain")], writes=[self.pbuf[zbank]])
        S.op("act", lambda a: a.activation(out=self.etmp[:], in_=ps[:, :], func=AF.Exp, scale=-1.0),
             reads=[self.pbuf[zbank]], writes=[self.b_etmp])
        S.op("act", lambda a: a.activation(out=self.gp[i][d][:], in_=self.etmp[:], func=AF.Ln, bias=self.one_t[:, 0:1]),
             reads=[self.b_etmp], writes=[self.b_gp[i][d]])

    def chain_update(self, i, d, banks=(4, 5, 6, 7)):
        S = self.S
        cb = self.B("consts")
        m3 = self.c32["m3f" if d == 0 else "m3r"]
        r3b, totb, kvb0, kvb1 = banks
        ps = self.psum[r3b]
        S.op("pe", lambda t: t.matmul(ps[:, :], lhsT=m3[:, :], rhs=self.gp[i][d][:, :], start=True, stop=True),
             reads=[self.b_gp[i][d], cb], writes=[self.pbuf[r3b]])
        S.op("act", lambda a: a.activation(out=self.e3[:], in_=ps[:, :], func=AF.Exp), reads=[self.pbuf[r3b]], writes=[self.b_e3])
        S.op("dve", lambda v: v.tensor_tensor(out=self.k3[:], in0=self.ktok[i][:], in1=self.e3[:], op=ALU.mult),
             reads=[self.b_ktok[i], self.b_e3], writes=[self.b_k3])
        pt = self.psum[totb]
        S.op("pe", [lambda t, h=h: t.matmul(pt[:, h:h + 1], lhsT=self.gp[i][d][:, h * 128:(h + 1) * 128], rhs=self.c32["n16"][:, 0:1],
                                             start=True, stop=True) for h in range(4)],
             reads=[self.b_gp[i][d], cb], writes=[self.pbuf[totb]])
        S.op("act", lambda a: a.activation(out=self.dec[:], in_=pt[:, 0:4], func=AF.Exp), reads=[self.pbuf[totb]], writes=[self.b_dec])
        for hh in range(2):
            pk = self.psum[(kvb0, kvb1)[hh]]
            S.op("pe", [lambda t, h=h, pk=pk: t.matmul(pk[:, (h % 2) * 256:(h % 2 + 1) * 256], lhsT=self.k3[:, h * 128:(h + 1) * 128],
                                                      rhs=self.vtok[i][:, h * 256:(h + 1) * 256], start=True, stop=True)
                        for h in (2 * hh, 2 * hh + 1)],
                 reads=[self.b_k3, self.b_vtok[i]], writes=[self.pbuf[(kvb0, kvb1)[hh]]])
            for h in (2 * hh, 2 * hh + 1):
                S.op("dve", lambda v, h=h, pk=pk: v.scalar_tensor_tensor(
                    out=self.Sst[d][:, h * 256:(h + 1) * 256], in0=self.Sst[d][:, h * 256:(h + 1) * 256],
                    scalar=self.dec[:, h:h + 1], in1=pk[:, (h % 2) * 256:(h % 2 + 1) * 256], op0=ALU.mult, op1=ALU.add),
                    reads=[self.pbuf[(kvb0, kvb1)[hh]], self.b_dec, self.Sb[d]], writes=[self.Sb[d]])

    def proj_tile_B(self, i, hT, b_hT, want_kv=True, kaT=None, b_kaT=None, va=None, b_va=None, rope_cols=None):
        S = self.S
        wgt, wka, bw = self.wgt, self.wka, self.B("W_B")
        ps = self.psum[2]
        S.op("pe", [lambda t, k=k: t.matmul(ps[0:64, 0:128], lhsT=wgt[:, k, 0:64], rhs=hT[:, k, :], start=(k == 0), stop=(k == KC - 1))
                    for k in range(KC)], reads=[b_hT, bw], writes=[self.pbuf[2]])
        S.op("dve", lambda v: v.tensor_copy(out=self.glT[i][:], in_=ps[0:64, 0:128]), reads=[self.pbuf[2]], writes=[self.b_glT[i]])
        col0 = 64 + 256
        for n in range(3):
            pb_i = 3 if n % 2 == 0 else 2
            ps2 = self.psum[pb_i]
            S.op("pe", [lambda t, k=k, n=n, ps2=ps2: t.matmul(ps2[:, :], lhsT=hT[:, k, :], rhs=wgt[:, k, col0 + n * 512:col0 + (n + 1) * 512],
                                                           start=(k == 0), stop=(k == KC - 1)) for k in range(KC)],
                 reads=[b_hT, bw], writes=[self.pbuf[pb_i]])
            if n == 0:
                S.op("act", lambda a, ps2=ps2: a.copy(out=self.ktok[i][:], in_=ps2[:, :]), reads=[self.pbuf[pb_i]], writes=[self.b_ktok[i]])
            else:
                S.op("act", lambda a, ps2=ps2, n=n: a.copy(out=self.vtok[i][:, (n - 1) * 512:n * 512], in_=ps2[:, :]),
                     reads=[self.pbuf[pb_i]], writes=[self.b_vtok[i]])
        if va is not None:
            ps3 = self.psum[3]
            S.op("pe", [lambda t, k=k: t.matmul(ps3[:, 0:256], lhsT=hT[:, k, :], rhs=wgt[:, k, 64:64 + 256], start=(k == 0), stop=(k == KC - 1))
                        for k in range(KC)], reads=[b_hT, bw], writes=[self.pbuf[3]])
            S.op("act", lambda a: a.copy(out=va, in_=ps3[:, 0:256]), reads=[self.pbuf[3]], writes=[b_va])
        if kaT is not None:
            for blk in range(2):
                ps4 = self.psum[2]
                S.op("pe", [lambda t, k=k, blk=blk: t.matmul(ps4[:, 0:128], lhsT=wka[:, k, blk * 128:(blk + 1) * 128], rhs=hT[:, k, :],
                                                             start=(k == 0), stop=(k == KC - 1)) for k in range(KC)],
                     reads=[b_hT, bw], writes=[self.pbuf[2]])
                if rope_cols is None:
                    S.op("act", lambda a, blk=blk: a.copy(out=kaT[:, blk, :], in_=ps4[:, 0:128]), reads=[self.pbuf[2]], writes=[b_kaT])
                else:
                    self.rope(ps4[:, 0:128], self.pbuf[2], kaT[:, blk, :], b_kaT, rope_cols, 128, rotbank=3)

    def rope(self, src_ps, b_src, out_bf, b_out, col0, n, rotbank):
        S = self.S
        cb = self.B("consts")
        qs, t1, t2 = self.rp_qs, self.rp_t1, self.rp_t2
        S.op("act", lambda a: a.copy(out=qs[:, 0:n], in_=src_ps), reads=[b_src], writes=[self.B("rp_qs")])
        S.op("act", lambda a: a.copy(out=t1[:, 0:n], in_=src_ps), reads=[b_src], writes=[self.B("rp_t1")])
        pr = self.psum[rotbank]
        S.op("pe", lambda t: t.matmul(pr[:, 0:n], lhsT=self.c16["rotm"][:, :], rhs=qs[:, 0:n], start=True, stop=True),
             reads=[self.B("rp_qs"), cb], writes=[self.pbuf[rotbank]])
        S.op("act", lambda a: a.copy(out=t2[:, 0:n], in_=pr[:, 0:n]), reads=[self.pbuf[rotbank]], writes=[self.B("rp_t2")])
        S.op("dve", lambda v: v.tensor_tensor(out=t1[:, 0:n], in0=t1[:, 0:n], in1=self.cos_sb[:, col0:col0 + n], op=ALU.mult),
             reads=[self.B("rp_t1"), self.B("cst_rope")], writes=[self.B("rp_t1")])
        S.op("dve", lambda v: v.tensor_tensor(out=t2[:, 0:n], in0=t2[:, 0:n], in1=self.sin_sb[:, col0:col0 + n], op=ALU.mult),
             reads=[self.B("rp_t2"), self.B("cst_rope")], writes=[self.B("rp_t2")])
        S.op("dve", lambda g: g.tensor_tensor(out=out_bf, in0=t1[:, 0:n], in1=t2[:, 0:n], op=ALU.add),
             reads=[self.B("rp_t1"), self.B("rp_t2")], writes=[b_out])

    def phase1(self):
        S = self.S
        cb = self.B("consts")
        self.mix_setup()
        self.push_scope()
        self.chain_setup()
        self.kaT_c = self.sb("kaT_c", [P, 2, 256], BF16)
        self.va_c = self.sb("va_c", [P, 2, 256], BF16)
        self.kaT_h = self.sb("kaT_h", [P, 2, P], BF16)
        self.va_h = self.sb("va_h", [P, 256], BF16)
        self.push_scope()
        self.cos_sb = self.sb("cos_sb", [HD, NOWN + 128], F32)
        self.sin_sb = self.sb("sin_sb", [HD, NOWN + 128], F32)
        S.dma("sp", lambda q: q.dma_start(out=self.cos_sb[:], in_=self.cosT), "cstr", writes=[self.B("cst_rope")])
        S.dma("sp", lambda q: q.dma_start(out=self.sin_sb[:], in_=self.sinT), "cstr", writes=[self.B("cst_rope")])
        self.rp_qs = self.sb("rp_qs", [P, 512], BF16)
        self.rp_t1 = self.sb("rp_t1", [P, 512], F32)
        self.rp_t2 = self.sb("rp_t2", [P, 512], F32)
        self.push_scope()
        wv = self.w_in.rearrange("(k p) n -> p k n", p=P)
        self.wgt = self.sb("wgt", [P, KC, 1856], BF16)
        self.wka = self.sb("wka", [P, KC, 256], BF16)
        bw = self.B("W_B")
        S.dma("pool", lambda q: q.dma_start(out=self.wka[:], in_=wv[:, :, FM_KA * 128:FM_KA * 128 + 256]), "W_B", writes=[bw])
        for c in range(4):
            S.dma("pool", lambda q, c=c: q.dma_start(out=self.wgt[:, :, c * 464:(c + 1) * 464],
                                                      in_=wv[:, :, GB_OFF + c * 464:GB_OFF + (c + 1) * 464]), "W_B", writes=[bw])
        hT = [self.sb(f"hT{i}", [P, KC, P], BF16) for i in range(2)]
        b_hT = [self.B(f"hT{i}") for i in range(2)]
        kaT_tmp = self.sb("kaT_tmp", [P, 2, P], BF16)
        for ti in range(2):
            self.ln_norm(self.ctxl[ti * P:(ti + 1) * P, :], self.xn[ti][:], self.xnb[ti])
            self.transpose_mod(self.xn[ti][:], self.xnb[ti], hT[ti], b_hT[ti], 4, 5)
            self.proj_tile_B(ti, hT[ti], b_hT[ti], kaT=kaT_tmp, b_kaT=self.B("kaT_tmp"),
                             va=self.va_c[:, ti, :], b_va=self.B("va_c"))
            for blk in range(2):
                S.op("pool", lambda g, blk=blk, ti=ti: g.tensor_copy(out=self.kaT_c[:, blk, ti * P:(ti + 1) * P], in_=kaT_tmp[:, blk, :]),
                     reads=[self.B("kaT_tmp")], writes=[self.B("kaT_c")])
            for d in range(2):
                self.gate_gp(ti, d)
        for ti in (0, 1):
            self.chain_update(ti, 0)
        for ti in (1, 0):
            self.chain_update(ti, 1)
        self.dbg_out("S_ctx_F", self.Sst[0][:], [P, 1024], F32, [self.Sb[0]])
        self.dbg_out("S_ctx_R", self.Sst[1][:], [P, 1024], F32, [self.Sb[1]])
        self.dbg_out("kaT_c", self.kaT_c[:], [P, 2, 256], BF16, [self.B("kaT_c")])
        if self.n_other > 0:
            for idx, ti in enumerate(range(NT - 1, NT - 1 - self.n_other, -1)):
                i = idx % 2
                self.ln_norm(self.x_oth[ti * P:(ti + 1) * P, :], self.xn[i][:], self.xnb[i])
                self.transpose_mod(self.xn[i][:], self.xnb[i], hT[i], b_hT[i], 0, 1)
                if ti == 0:
                    self.proj_tile_B(i, hT[i], b_hT[i], kaT=self.kaT_h, b_kaT=self.B("kaT_h"), va=self.va_h[:], b_va=self.B("va_h"),
                                     rope_cols=NOWN)
                else:
                    self.proj_tile_B(i, hT[i], b_hT[i])
                self.gate_gp(i, 1)
                self.chain_update(i, 1)
        self.dbg_out("S_bnd_R", self.Sst[1][:], [P, 1024], F32, [self.Sb[1]])
        self.dbg_out("kaT_h", self.kaT_h[:], [P, 2, P], BF16, [self.B("kaT_h")])
        self.pop_scope()

    def phase2(self):
        S = self.S
        cb = self.B("consts")
        self.pfm_d = self.dscr("pfm_d", [N_FM, P, NOWN], BF16)
        self.pgl_d = self.dscr("pgl_d", [64, NOWN], F32)
        self.ptm_d = self.dscr("ptm_d", [NOWN, TM_COLS], BF16)
        b_pfm, b_pgl, b_ptm = self.B("pfm_d"), self.B("pgl_d"), self.B("ptm_d")
        self.push_scope()
        hT = self.sb("hT_own", [P, KC, NOWN], BF16)
        b_hT = self.B("hT_own")
        for ti in range(NT):
            i = ti % 2
            self.ln_norm(self.x_own[ti * P:(ti + 1) * P, :], self.xn[i][:], self.xnb[i])
            self.transpose_mod(self.xn[i][:], self.xnb[i], hT[:, :, ti * P:(ti + 1) * P], b_hT, 0, 1)
        import os
        stop = os.environ.get("P2_STOP", "")
        if stop == "ln":
            self.dbg_out("hT", hT[:, 0, :], [P, NOWN], BF16, [b_hT])
            self.pop_scope()
            return
        wv = self.w_in.rearrange("(k p) n -> p k n", p=P)
        NB = 2
        wbuf = [self.sb(f"wst{i}", [P, KC, 512], BF16) for i in range(NB)]
        wbb = [self.B(f"wst{i}") for i in range(NB)]
        stg = [self.sb(f"stg{i}", [P, NOWN], BF16) for i in range(2)]
        stgb = [self.B(f"stg{i}") for i in range(2)]
        stg32 = [self.sb(f"stg32_{i}", [64, 512], F32) for i in range(2)]
        stt = [self.sb(f"stt{i}", [P, 512], BF16) for i in range(3)]
        sttb = [self.B(f"stt{i}") for i in range(3)]
        gi = 0
        ev = 0
        pbank = 0
        nstg = 0
        fm_groups = [(g * 512, 512) for g in range(6)] + [(3072, 320)]
        if stop.startswith("fm"):
            fm_groups = fm_groups[int(stop[2:4]):int(stop[4:6])]
        if stop.startswith("tm"):
            fm_groups = []
        for (c0, ncol) in fm_groups:
            w = gi % NB
            gi += 1
            for hh in range(2):
                h0, h1 = hh * (ncol // 2), (hh + 1) * (ncol // 2)
                S.dma("pool", lambda q, w=w, c0=c0, h0=h0, h1=h1: q.dma_start(out=wbuf[w][:, :, h0:h1], in_=wv[:, :, c0 + h0:c0 + h1]),
                      f"wst{w}", writes=[wbb[w]])
            nblk = (ncol + 127) // 128
            for bl in range(nblk):
                blk = c0 // 128 + bl
                M = min(128, ncol - bl * 128)
                is_gb = (M == 64)
                si = nstg % 2
                if not is_gb:
                    nstg += 1
                for ch in range(4):
                    pb_i = pbank % 4
                    pbank += 1
                    ps = self.psum[pb_i]
                    S.op("pe", [lambda t, k=k, w=w, bl=bl, M=M, ch=ch, ps=ps: t.matmul(
                        ps[0:M, :], lhsT=wbuf[w][:, k, bl * 128:bl * 128 + M], rhs=hT[:, k, ch * 512:(ch + 1) * 512],
                        start=(k == 0), stop=(k == KC - 1)) for k in range(KC)],
                        reads=[b_hT, wbb[w]], writes=[self.pbuf[pb_i]])
                    if is_gb:
                        S.op("dve", lambda v, ps=ps, ch=ch: v.tensor_copy(out=stg32[ch % 2][:, :], in_=ps[0:64, :]),
                             reads=[self.pbuf[pb_i]], writes=[self.B(f"stg32_{ch % 2}")])
                        S.dma("sp", lambda q, ch=ch: q.dma_start(out=self.pgl_d[:, ch * 512:(ch + 1) * 512], in_=stg32[ch % 2][:, :]),
                              f"pgl{ch % 2}", reads=[self.B(f"stg32_{ch % 2}")], writes=[b_pgl])
                    elif blk < FM_QB:
                        self.rope(ps[:, :], self.pbuf[pb_i], stg[si][:, ch * 512:(ch + 1) * 512], stgb[si], ch * 512, 512,
                                  rotbank=4 + (ch % 2))
                    else:
                        eng = "act" if ev % 2 == 0 else "dve"
                        ev += 1
                        if eng == "act":
                            S.op("act", lambda a, ps=ps, ch=ch, si=si: a.copy(out=stg[si][:, ch * 512:(ch + 1) * 512], in_=ps[:, :]),
                                 reads=[self.pbuf[pb_i]], writes=[stgb[si]])
                        else:
                            S.op("dve", lambda v, ps=ps, ch=ch, si=si: v.tensor_copy(out=stg[si][:, ch * 512:(ch + 1) * 512], in_=ps[:, :]),
                                 reads=[self.pbuf[pb_i]], writes=[stgb[si]])
                if not is_gb:
                    S.dma("sp", lambda q, blk=blk, si=si: q.dma_start(out=self.pfm_d[blk], in_=stg[si][:]), f"stg{si}",
                          reads=[stgb[si]], writes=[b_pfm])
        tm_groups = [(0, 512), (512, 512), (1024, 512), (1536, 256)]
        if stop.startswith("fm"):
            tm_groups = []
        if stop.startswith("tm"):
            tm_groups = tm_groups[int(stop[2:4]):int(stop[4:6])]
        nt_ = 0
        for (c0, ncol) in tm_groups:
            w = gi % NB
            gi += 1
            for hh in range(2):
                h0, h1 = hh * (ncol // 2), (hh + 1) * (ncol // 2)
                S.dma("pool", lambda q, w=w, c0=c0, h0=h0, h1=h1: q.dma_start(out=wbuf[w][:, :, h0:h1], in_=wv[:, :, TM_OFF + c0 + h0:TM_OFF + c0 + h1]),
                      f"wst{w}", writes=[wbb[w]])
            for ti in range(NT):
                pb_i = pbank % 4
                pbank += 1
                ps = self.psum[pb_i]
                S.op("pe", [lambda t, k=k, w=w, ti=ti, ncol=ncol, ps=ps: t.matmul(
                    ps[:, 0:ncol], lhsT=hT[:, k, ti * P:(ti + 1) * P], rhs=wbuf[w][:, k, 0:ncol],
                    start=(k == 0), stop=(k == KC - 1)) for k in range(KC)],
                    reads=[b_hT, wbb[w]], writes=[self.pbuf[pb_i]])
                j = nt_ % 3
                nt_ += 1
                eng = "act" if ev % 2 == 0 else "dve"
                ev += 1
                if eng == "act":
                    S.op("act", lambda a, ps=ps, j=j, ncol=ncol: a.copy(out=stt[j][:, 0:ncol], in_=ps[:, 0:ncol]),
                         reads=[self.pbuf[pb_i]], writes=[sttb[j]])
                else:
                    S.op("dve", lambda v, ps=ps, j=j, ncol=ncol: v.tensor_copy(out=stt[j][:, 0:ncol], in_=ps[:, 0:ncol]),
                         reads=[self.pbuf[pb_i]], writes=[sttb[j]])
                S.dma("sp", lambda q, ti=ti, c0=c0, ncol=ncol, j=j: q.dma_start(
                    out=self.ptm_d[ti * P:(ti + 1) * P, c0:c0 + ncol], in_=stt[j][:, 0:ncol]), f"stt{j}",
                    reads=[sttb[j]], writes=[b_ptm])
        if "pfm" in self.debug:
            o = self.dout("dbg_pfm", [N_FM, P, NOWN], BF16)
            for blk in range(N_FM):
                self.final_toks.append(S.dma("sp", lambda q, o=o, blk=blk: q.dma_start(out=o[blk], in_=self.pfm_d[blk]), "dbg", reads=[b_pfm]))
        if "pgl" in self.debug:
            o2 = self.dout("dbg_pgl", [64, NOWN], F32)
            self.final_toks.append(S.dma("sp", lambda q: q.dma_start(out=o2, in_=self.pgl_d), "dbg", reads=[b_pgl]))
        if "ptm" in self.debug:
            o3 = self.dout("dbg_ptm", [NOWN, TM_COLS], BF16)
            for ti in range(NT):
                self.final_toks.append(S.dma("sp", lambda q, ti=ti: q.dma_start(out=o3[ti * P:(ti + 1) * P, :], in_=self.ptm_d[ti * P:(ti + 1) * P, :]),
                                             "dbg", reads=[b_ptm]))
        self.pop_scope()
        self.pop_scope()

    def load_chain_tile(self, i, ti):
        S = self.S
        S.dma("sp", lambda q: q.dma_start(out=self.ktok[i][:], in_=self.ptm_d[ti * P:(ti + 1) * P, TM_KB:TM_KB + 512]), f"ktok{i}",
              reads=[self.B("ptm_d")], writes=[self.b_ktok[i]])
        S.dma("sp", lambda q: q.dma_start(out=self.vtok[i][:], in_=self.ptm_d[ti * P:(ti + 1) * P, TM_VB:TM_VB + 1024]), f"vtok{i}",
              reads=[self.B("ptm_d")], writes=[self.b_vtok[i]])
        S.dma("sp", lambda q: q.dma_start(out=self.glT[i][:], in_=self.pgl_d[:, ti * P:(ti + 1) * P]), f"glT{i}",
              reads=[self.B("pgl_d")], writes=[self.b_glT[i]])

    def phase3(self):
        S = self.S
        self.SR_d = self.dscr("SR_d", [NT, P, 1024], BF16)
        b_SR = self.B("SR_d")
        self.push_scope()
        sbf = [self.sb(f"sbf{i}", [P, 1024], BF16) for i in range(2)]
        sbfb = [self.B(f"sbf{i}") for i in range(2)]
        for idx, ti in enumerate(range(NT - 1, -1, -1)):
            i = idx % 2
            self.load_chain_tile(i, ti)
            self.gate_gp(i, 1)
            S.op("act", lambda a, i=i: a.copy(out=sbf[i][:], in_=self.Sst[1][:]), reads=[self.Sb[1]], writes=[sbfb[i]])
            S.dma("pool", lambda q, i=i, ti=ti: q.dma_start(out=self.SR_d[ti], in_=sbf[i][:]), f"sbf{i}", reads=[sbfb[i]], writes=[b_SR])
            self.chain_update(i, 1)
        self.pop_scope()

    def phase4(self):
        S = self.S
        cb = self.B("consts")
        self.cat_d = self.dscr("cat_d", [NT, P, KC * P], BF16)
        b_cat = self.B("cat_d")
        b_pfm, b_ptm = self.B("pfm_d"), self.B("ptm_d")
        self.push_scope()
        SCALE = float(HD) ** -0.5
        kaT = self.sb("kaT_all", [P, 2, NOWN], BF16)
        va = self.sb("va_all", [P, NT, 256], BF16)
        b_ka, b_va = self.B("kaT_all"), self.B("va_all")
        for h in range(2):
            S.dma("sp", lambda q, h=h: q.dma_start(out=kaT[:, h, :], in_=self.pfm_d[FM_KA + h]), "kaT_all", reads=[b_pfm], writes=[b_ka])
        for t4 in range(4):
            S.dma("sp", lambda q, t4=t4: q.dma_start(out=va[:, t4 * 4:(t4 + 1) * 4, :],
                                                    in_=self.ptm_d[t4 * 512:(t4 + 1) * 512, TM_VA:TM_VA + 256].rearrange("(t p) n -> p t n", p=P)),
                  "va_all", reads=[b_ptm], writes=[b_va])
        esink = self.sb("esink", [P, 8], F32)
        b_es = self.B("esink")
        S.dma("sp", lambda q: q.dma_start(out=esink[:], in_=self.sink.partition_broadcast(P)), "esink", writes=[b_es])
        S.op("act", lambda a: a.activation(out=esink[:], in_=esink[:], func=AF.Exp), reads=[b_es], writes=[b_es])
        normw = self.sb("normw_sb", [P, 8], F32)
        S.dma("sp", lambda q: q.dma_start(out=normw[:], in_=self.normw), "normw", writes=[self.B("normw")])
        qaT = self.sb("qaT", [P, 8, P], BF16)
        qbT = self.sb("qbT", [P, 4, P], BF16)
        kbT = self.sb("kbT", [P, 4, P], BF16)
        rbT = self.sb("rbT", [P, 8, P], BF16)
        SRb = self.sb("SRb", [P, 1024], BF16)
        SFb = self.sb("SFb", [P, 1024], BF16)
        pT = [self.sb(f"pT{i}", [P, 512], BF16) for i in range(2)]
        pTb = [self.B(f"pT{i}") for i in range(2)]
        dsb = self.sb("dsb", [P, 512], F32)
        osb = self.sb("osb", [P, 512], F32)
        catT = self.sb("catT", [P, KC, P], BF16)
        b_catT = self.B("catT")
        E1 = self.sb("E1", [P, 4, P], F32)
        E2 = self.sb("E2", [P, 4, P], F32)
        EB = self.sb("EB", [P, 4, P], F32)
        qm = [self.sb(f"qm{d}", [P, 4, P], BF16) for d in range(2)]
        km = [self.sb(f"km{d}", [P, 4, P], BF16) for d in range(2)]
        qe = [self.sb(f"qe{d}", [P, 4, P], BF16) for d in range(2)]
        ATm = [self.sb(f"ATm{d}", [P, 512], BF16) for d in range(2)]
        attmp = self.sb("attmp", [P, 512], F32)
        osq = self.sb("osq", [P, 1024], F32)
        of = self.sb("of", [P, 1024], F32)
        rinv = self.sb("rinv", [P, 512], F32)
        srb = self.sb("srb", [P, 8, P], F32)
        otmp = self.sb("otmp", [P, P], F32)
        Bn = self.B
        nps = 0
        for ti in range(NT):
            c0 = ti * P
            S.dma("sp", lambda q, c0=c0: q.dma_start(out=qaT[:], in_=self.pfm_d[FM_QA:FM_QA + 8, :, c0:c0 + P].rearrange("b p n -> p b n")),
                  "qaT", reads=[b_pfm], writes=[Bn("qaT")])
            S.dma("sp", lambda q, c0=c0: q.dma_start(out=qbT[:], in_=self.pfm_d[FM_QB:FM_QB + 4, :, c0:c0 + P].rearrange("b p n -> p b n")),
                  "qbT", reads=[b_pfm], writes=[Bn("qbT")])
            S.dma("sp", lambda q, c0=c0: q.dma_start(out=kbT[:], in_=self.pfm_d[FM_KB:FM_KB + 4, :, c0:c0 + P].rearrange("b p n -> p b n")),
                  "kbT", reads=[b_pfm], writes=[Bn("kbT")])
            S.dma("sp", lambda q, c0=c0: q.dma_start(out=rbT[:], in_=self.pfm_d[FM_RB:FM_RB + 8, :, c0:c0 + P].rearrange("b p n -> p b n")),
                  "rbT", reads=[b_pfm], writes=[Bn("rbT")])
            S.dma("sp", lambda q, ti=ti: q.dma_start(out=SRb[:], in_=self.SR_d[ti]), "SRb", reads=[Bn("SR_d")], writes=[Bn("SRb")])
            i = ti % 2
            self.load_chain_tile(i, ti)
            for h in range(2):
                blocks = [("c", 0), ("c", 1)]
                if ti > 0:
                    blocks.append(("p", ti - 1))
                blocks.append(("o", ti))
                blocks.append(("n", ti + 1))
                nb = len(blocks)
                for bi, (kind, idx) in enumerate(blocks):
                    if kind == "c":
                        kT, rk = self.kaT_c[:, h, idx * P:(idx + 1) * P], Bn("kaT_c")
                        vv, rv = self.va_c[:, idx, h * P:(h + 1) * P], Bn("va_c")
                    elif kind == "n" and idx == NT:
                        kT, rk = self.kaT_h[:, h, :], Bn("kaT_h")
                        vv, rv = self.va_h[:, h * P:(h + 1) * P], Bn("va_h")
                    else:
                        kT, rk = kaT[:, h, idx * P:(idx + 1) * P], b_ka
                        vv, rv = va[:, idx, h * P:(h + 1) * P], b_va
                    sb_i = nps % 2
                    nps += 1
                    ps = self.psum[sb_i]
                    S.op("pe", lambda t, kT=kT, h=h, ps=ps: t.matmul(ps[:, :], lhsT=kT, rhs=qaT[:, 4 * h:4 * h + 4, :].rearrange("p a b -> p (a b)"),
                                                                     start=True, stop=True),
                         reads=[rk, Bn("qaT")], writes=[self.pbuf[sb_i]])
                    S.op("act", lambda a, ps=ps, sb_i=sb_i: a.activation(out=pT[sb_i][:], in_=ps[:, :], func=AF.Exp, scale=SCALE),
                         reads=[self.pbuf[sb_i]], writes=[pTb[sb_i]])
                    if kind in ("p", "n"):
                        mk = self.c16["amprev" if kind == "p" else "amnext"]
                        S.op("dve", lambda v, sb_i=sb_i, mk=mk: v.tensor_tensor(out=pT[sb_i][:], in0=pT[sb_i][:], in1=mk[:, :], op=ALU.mult),
                             reads=[pTb[sb_i], cb], writes=[pTb[sb_i]])
                    S.op("pe", [lambda t, vv=vv, sb_i=sb_i, bi=bi, nb=nb: t.matmul(self.psum[2][:, :], lhsT=vv, rhs=pT[sb_i][:], start=(bi == 0), stop=(bi == nb - 1)),
                                lambda t, sb_i=sb_i, bi=bi, nb=nb: t.matmul(self.psum[3][:, :], lhsT=self.c16["ones"][:, :], rhs=pT[sb_i][:], start=(bi == 0), stop=(bi == nb - 1))],
                         reads=[rv, pTb[sb_i], cb], writes=[self.pbuf[2], self.pbuf[3]])
                S.op("act", lambda a: a.copy(out=dsb[:], in_=self.psum[3][:, :]), reads=[self.pbuf[3]], writes=[Bn("dsb")])
                S.op("act", lambda a: a.copy(out=osb[:], in_=self.psum[2][:, :]), reads=[self.pbuf[2]], writes=[Bn("osb")])
                S.op("dve", [lambda v, g=g, h=h: v.tensor_scalar(out=dsb[:, g * P:(g + 1) * P], in0=dsb[:, g * P:(g + 1) * P],
                                                                 scalar1=esink[:, 4 * h + g:4 * h + g + 1], scalar2=None, op0=ALU.add)
                             for g in range(4)], reads=[Bn("dsb"), b_es], writes=[Bn("dsb")])
                S.op("dve", lambda v: v.reciprocal(out=dsb[:], in_=dsb[:]), reads=[Bn("dsb")], writes=[Bn("dsb")])
                S.op("dve", lambda v, h=h: v.tensor_tensor(out=catT[:, 4 * h:4 * h + 4, :].rearrange("p a b -> p (a b)"), in0=osb[:], in1=dsb[:], op=ALU.mult),
                     reads=[Bn("osb"), Bn("dsb")], writes=[b_catT])
            for d in range(2):
                self.gate_gp(i, d)
            S.op("act", lambda a: a.copy(out=SFb[:], in_=self.Sst[0][:]), reads=[self.Sb[0]], writes=[Bn("SFb")])
            for d in range(2):
                m12 = self.c32["m12f" if d == 0 else "m12r"]
                for hb in range(2):
                    S.op("pe", [lambda t, h=h, hb=hb, d=d, m12=m12, i=i: t.matmul(self.psum[6 + hb][:, (h % 2) * 256:(h % 2 + 1) * 256],
                                                                           lhsT=self.gp[i][d][:, h * P:(h + 1) * P], rhs=m12[:, :], start=True, stop=True)
                                for h in (2 * hb, 2 * hb + 1)], reads=[self.b_gp[i][d], cb], writes=[self.pbuf[6 + hb]])
                    src = self.psum[6 + hb][:, :].rearrange("p (h c) -> p h c", h=2)
                    S.op("act", [lambda a, hb=hb, src=src: a.activation(out=E1[:, 2 * hb:2 * hb + 2, :], in_=src[:, :, 0:P], func=AF.Exp),
                                 lambda a, hb=hb, src=src: a.activation(out=E2[:, 2 * hb:2 * hb + 2, :], in_=src[:, :, 0:P], func=AF.Exp, scale=-1.0),
                                 lambda a, hb=hb, src=src: a.activation(out=EB[:, 2 * hb:2 * hb + 2, :], in_=src[:, :, P:2 * P], func=AF.Exp)],
                         reads=[self.pbuf[6 + hb]], writes=[Bn("E123")])
                fl = lambda t_: t_[:].rearrange("p a b -> p (a b)")
                S.op("dve", lambda v, d=d: v.scalar_tensor_tensor(out=fl(qm[d]), in0=fl(qbT), scalar=SCALE, in1=fl(E1), op0=ALU.mult, op1=ALU.mult),
                     reads=[Bn("qbT"), Bn("E123")], writes=[Bn(f"qm{d}")])
                S.op("dve", lambda v, d=d: v.tensor_tensor(out=fl(km[d]), in0=fl(kbT), in1=fl(E2), op=ALU.mult),
                     reads=[Bn("kbT"), Bn("E123")], writes=[Bn(f"km{d}")])
                S.op("dve", lambda v, d=d: v.scalar_tensor_tensor(out=fl(qe[d]), in0=fl(qbT), scalar=SCALE, in1=fl(EB), op0=ALU.mult, op1=ALU.mult),
                     reads=[Bn("qbT"), Bn("E123")], writes=[Bn(f"qe{d}")])
                S.op("pe", [lambda t, h=h, d=d: t.matmul(self.psum[4][:, h * P:(h + 1) * P], lhsT=km[d][:, h, :], rhs=qm[d][:, h, :], start=True, stop=True)
                            for h in range(4)], reads=[Bn(f"qm{d}"), Bn(f"km{d}")], writes=[self.pbuf[4]])
                S.op("act", lambda a: a.copy(out=attmp[:], in_=self.psum[4][:, :]), reads=[self.pbuf[4]], writes=[Bn("attmp")])
                gm = self.c16["gmaskf" if d == 0 else "gmaskr"]
                S.op("dve", lambda v, d=d, gm=gm: v.tensor_tensor(out=ATm[d][:], in0=attmp[:], in1=gm[:, :], op=ALU.mult),
                     reads=[Bn("attmp"), cb], writes=[Bn(f"ATm{d}")])
            for hb in range(2):
                fns = []
                for r in range(4 * hb, 4 * hb + 4):
                    h, c = r // 2, r % 2
                    vs = slice(h * 256 + c * P, h * 256 + (c + 1) * P)
                    dst = self.psum[6 + hb][:, (r % 4) * P:(r % 4 + 1) * P]
                    fns += [lambda t, vs=vs, dst=dst, h=h, i=i: t.matmul(dst, lhsT=self.vtok[i][:, vs], rhs=ATm[0][:, h * P:(h + 1) * P], start=True, stop=False),
                            lambda t, vs=vs, dst=dst, h=h, i=i: t.matmul(dst, lhsT=self.vtok[i][:, vs], rhs=ATm[1][:, h * P:(h + 1) * P], start=False, stop=False),
                            lambda t, vs=vs, dst=dst, h=h: t.matmul(dst, lhsT=SFb[:, vs], rhs=qe[0][:, h, :], start=False, stop=False),
                            lambda t, vs=vs, dst=dst, h=h: t.matmul(dst, lhsT=SRb[:, vs], rhs=qe[1][:, h, :], start=False, stop=True)]
                S.op("pe", fns, reads=[self.b_vtok[i], Bn("ATm0"), Bn("ATm1"), Bn("SFb"), Bn("SRb"), Bn("qe0"), Bn("qe1")], writes=[self.pbuf[6 + hb]])
                S.op("act", [lambda a, hb=hb: a.activation(out=osq[:, hb * 512:(hb + 1) * 512], in_=self.psum[6 + hb][:, :], func=AF.Square),
                             lambda a, hb=hb: a.copy(out=of[:, hb * 512:(hb + 1) * 512], in_=self.psum[6 + hb][:, :])],
                     reads=[self.pbuf[6 + hb]], writes=[Bn("osq_of")])
            S.op("pe", [lambda t, h=h, c=c: t.matmul(self.psum[5][:, h * P:(h + 1) * P], lhsT=self.c32["ones"][:, :],
                                                   rhs=osq[:, (2 * h + c) * P:(2 * h + c + 1) * P], start=(c == 0), stop=(c == 1))
                        for h in range(4) for c in range(2)], reads=[Bn("osq_of"), cb], writes=[self.pbuf[5]])
            S.op("act", lambda a: a.activation(out=rinv[:], in_=self.psum[5][:, :], func=AF.Ln, scale=1.0 / 256.0, bias=self.eps_t[:, 0:1]),
                 reads=[self.pbuf[5], cb], writes=[Bn("rinv")])
            S.op("act", lambda a: a.activation(out=rinv[:], in_=rinv[:], func=AF.Exp, scale=-0.5), reads=[Bn("rinv")], writes=[Bn("rinv")])
            S.op("act", lambda a: a.activation(out=srb[:].rearrange("p a b -> p (a b)"), in_=rbT[:].rearrange("p a b -> p (a b)"), func=AF.Silu),
                 reads=[Bn("rbT")], writes=[Bn("srb")])
            for r in range(8):
                h = r // 2
                S.op("dve", lambda v, r=r, h=h: v.tensor_tensor(out=otmp[:], in0=of[:, r * P:(r + 1) * P], in1=rinv[:, h * P:(h + 1) * P], op=ALU.mult),
                     reads=[Bn("osq_of"), Bn("rinv")], writes=[Bn("otmp")])
                S.op("dve", lambda v, r=r: v.scalar_tensor_tensor(out=catT[:, 8 + r, :], in0=otmp[:], scalar=normw[:, r:r + 1], in1=srb[:, r, :],
                                                                  op0=ALU.mult, op1=ALU.mult),
                     reads=[Bn("otmp"), Bn("srb"), Bn("normw")], writes=[b_catT])
            self.chain_update(i, 0)
            S.dma("pool", lambda q, ti=ti: q.dma_start(out=self.cat_d[ti], in_=catT[:].rearrange("p a b -> p (a b)")), "catT",
                  reads=[b_catT], writes=[b_cat])
        if "cat" in self.debug:
            o = self.dout("dbg_cat", [NT, P, KC * P], BF16)
            for ti in range(NT):
                self.final_toks.append(S.dma("sp", lambda q, ti=ti: q.dma_start(out=o[ti], in_=self.cat_d[ti]), "dbg", reads=[b_cat]))
        self.pop_scope()

    def ln_stats(self, src, b_src):
        S = self.S
        S.op("dve", [lambda v, c=c: v.bn_stats(out=self.stats[:, c, :], in_=src[:, c * 512:(c + 1) * 512]) for c in range(4)],
             reads=[b_src], writes=[self.b_stats])
        S.op("dve", lambda v: v.bn_aggr(out=self.mv[:], in_=self.stats[:].rearrange("p a b -> p (a b)")),
             reads=[self.b_stats], writes=[self.b_mv])
        S.op("act", lambda a: a.activation(out=self.rstd[:], in_=self.mv[:, 1:2], func=AF.Ln, bias=self.eps_t[:, 0:1]),
             reads=[self.b_mv], writes=[self.b_rstd])
        S.op("act", lambda a: a.activation(out=self.rstd[:], in_=self.rstd[:], func=AF.Exp, scale=-0.5),
             reads=[self.b_rstd], writes=[self.b_rstd])

    def phase5(self):
        S = self.S
        cb = self.B("consts")
        Bn = self.B
        self.x1_d = self.dscr("x1_d", [NOWN, D], F32)
        self.xbuf_d = self.dscr("xbuf_d", [N_EXP * CAPS, D], BF16)
        b_x1, b_xbuf = Bn("x1_d"), Bn("xbuf_d")
        self.idx_all = self.sb("idx_all", [P, NT, 2], I32)
        self.w_all = self.sb("w_all", [P, NT, 2], F32)
        self.cnt_bc = self.sb("cnt_bc", [P, N_EXP], F32)
        self.cnt_i = self.sb("cnt_i", [1, N_EXP], I32)
        b_idx, b_w, b_cnt = Bn("idx_all"), Bn("w_all"), Bn("cnt_bc")
        S.op("dve", lambda v: v.memset(self.cnt_bc[:], 0.0), writes=[b_cnt])
        self.push_scope()
        wout = self.sb("wout", [P, KC, D], BF16)
        b_wout = Bn("wout")
        wov = self.w_out.rearrange("(k p) n -> p k n", p=P)
        for c in range(8):
            S.dma("pool", lambda q, c=c: q.dma_start(out=wout[:, :, c * 256:(c + 1) * 256], in_=wov[:, :, c * 256:(c + 1) * 256]),
                  "wout", writes=[b_wout])
        wr = self.sb("wr", [P, KC, 36], BF16)
        S.dma("pool", lambda q: q.dma_start(out=wr[:], in_=self.w_r.rearrange("(k p) n -> p k n", p=P)), "wr", writes=[Bn("wr")])
        brb = self.sb("brb", [P, 36], F32)
        S.dma("sp", lambda q: q.dma_start(out=brb[:], in_=self.b_r.partition_broadcast(P)), "brb", writes=[Bn("brb")])
        bc = [self.sb(f"bc{i}", [P, D], F32) for i in range(3)]
        bcb = [Bn(f"bc{i}") for i in range(3)]
        S.dma("sp", lambda q: q.dma_start(out=bc[0][:], in_=self.mod_d[0, 2 * D:3 * D].partition_broadcast(P)), "bc0",
              reads=[Bn("mod_d")], writes=[bcb[0]])
        S.dma("sp", lambda q: q.dma_start(out=bc[1][:], in_=self.ln1g.partition_broadcast(P)), "bc1", writes=[bcb[1]])
        S.dma("sp", lambda q: q.dma_start(out=bc[2][:], in_=self.ln1b.partition_broadcast(P)), "bc2", writes=[bcb[2]])
        bc2 = [self.sb(f"bcm{i}", [P, D], F32) for i in range(2)]
        bc2b = [Bn(f"bcm{i}") for i in range(2)]
        S.dma("sp", lambda q: q.dma_start(out=bc2[0][:], in_=self.mod_d[0, 3 * D:4 * D].partition_broadcast(P)), "bcm0",
              reads=[Bn("mod_d")], writes=[bc2b[0]])
        S.dma("sp", lambda q: q.dma_start(out=bc2[1][:], in_=self.mod_d[0, 4 * D:5 * D].partition_broadcast(P)), "bcm1",
              reads=[Bn("mod_d")], writes=[bc2b[1]])
        S.op("pool", lambda g: g.tensor_scalar(out=bc2[1][:], in0=bc2[1][:], scalar1=1.0, scalar2=None, op0=ALU.add), reads=[bc2b[1]], writes=[bc2b[1]])
        catT = [self.sb(f"catT{i}", [P, KC, P], BF16) for i in range(2)]
        catb = [Bn(f"catTb{i}") for i in range(2)]
        ysb = self.sb("ysb", [P, D], F32)
        b_ysb = Bn("ysb")
        h2T = self.sb("h2T", [P, KC, P], BF16)
        b_h2T = Bn("h2T")
        lg = self.sb("lg", [P, 36], F32)
        sm = self.sb("rsm", [P, 16], F32)
        gm4 = self.sb("gm4", [P, 4], F32)
        pen = self.sb("pen", [P, 4], F32)
        ge4 = self.sb("ge4", [P, 4], F32)
        elm = self.sb("elm", [P, N_EXP], F32)
        m8 = self.sb("m8", [P, 8], F32)
        A1 = self.sb("A1", [P, N_EXP], F32)
        A2 = self.sb("A2", [P, N_EXP], F32)
        A12 = self.sb("A12", [P, N_EXP], F32)
        posc = self.sb("posc", [P, 64], F32)
        tpos = self.sb("tpos", [P, N_EXP], F32)
        tsl = self.sb("tsl", [P, N_EXP], F32)
        tov = self.sb("tov", [P, N_EXP], F32)
        idxf = self.sb("idxf", [P, 2], F32)
        b_r_ = Bn("route_tmp")
        AX = mybir.AxisListType.X
        for ti in range(NT):
            i = ti % 2
            xin, xb = self.xin[i], self.xinb[i]
            S.dma("sp", lambda q, i=i, ti=ti: q.dma_start(out=catT[i][:].rearrange("p a b -> p (a b)"), in_=self.cat_d[ti]), f"catT{i}",
                  reads=[Bn("cat_d")], writes=[catb[i]])
            S.dma("sp", lambda q, xin=xin, ti=ti: q.dma_start(out=xin[:], in_=self.x_own[ti * P:(ti + 1) * P, :]), f"xin{i}", writes=[xb])
            for n in range(4):
                S.op("pe", [lambda t, k=k, n=n, i=i: t.matmul(self.psum[n][:, :], lhsT=catT[i][:, k, :], rhs=wout[:, k, n * 512:(n + 1) * 512],
                                                             start=(k == 0), stop=(k == KC - 1)) for k in range(KC)],
                     reads=[catb[i], b_wout], writes=[self.pbuf[n]])
                S.op("act", lambda a, n=n: a.copy(out=ysb[:, n * 512:(n + 1) * 512], in_=self.psum[n][:, :]), reads=[self.pbuf[n]], writes=[b_ysb])
            S.op("dve", lambda v: v.tensor_tensor(out=ysb[:], in0=ysb[:], in1=bc[0][:], op=ALU.mult), reads=[b_ysb, bcb[0]], writes=[b_ysb])
            S.op("dve", lambda v, xin=xin: v.scalar_tensor_tensor(out=xin[:], in0=xin[:], scalar=float(ALPHA), in1=ysb[:], op0=ALU.mult, op1=ALU.add),
                 reads=[xb, b_ysb], writes=[xb])
            self.ln_stats(xin, xb)
            S.op("dve", lambda v, xin=xin: v.tensor_scalar(out=ysb[:], in0=xin[:], scalar1=self.mv[:, 0:1], scalar2=self.rstd[:, 0:1],
                                                           op0=ALU.subtract, op1=ALU.mult), reads=[xb, self.b_mv, self.b_rstd], writes=[b_ysb])
            S.op("dve", lambda v: v.tensor_tensor(out=ysb[:], in0=ysb[:], in1=bc[1][:], op=ALU.mult), reads=[b_ysb, bcb[1]], writes=[b_ysb])
            S.op("pool", lambda g: g.tensor_tensor(out=ysb[:], in0=ysb[:], in1=bc[2][:], op=ALU.add), reads=[b_ysb, bcb[2]], writes=[b_ysb])
            S.dma("pool", lambda q, ti=ti: q.dma_start(out=self.x1_d[ti * P:(ti + 1) * P, :], in_=ysb[:]), "x1st", reads=[b_ysb], writes=[b_x1])
            self.ln_stats(ysb, b_ysb)
            S.op("dve", lambda v: v.tensor_scalar(out=ysb[:], in0=ysb[:], scalar1=self.mv[:, 0:1], scalar2=self.rstd[:, 0:1],
                                                  op0=ALU.subtract, op1=ALU.mult), reads=[b_ysb, self.b_mv, self.b_rstd], writes=[b_ysb])
            S.op("pool", lambda g: g.tensor_tensor(out=ysb[:], in0=ysb[:], in1=bc2[1][:], op=ALU.mult), reads=[b_ysb, bc2b[1]], writes=[b_ysb])
            S.op("dve", lambda v, i=i: v.tensor_tensor(out=self.xn[i][:], in0=ysb[:], in1=bc2[0][:], op=ALU.add), reads=[b_ysb, bc2b[0]], writes=[self.xnb[i]])
            self.transpose_mod(self.xn[i][:], self.xnb[i], h2T, b_h2T, 2, 3, banks=(4, 5), plain=True)
            S.op("pe", [lambda t, k=k: t.matmul(self.psum[6][:, 0:36], lhsT=h2T[:, k, :], rhs=wr[:, k, :], start=(k == 0), stop=(k == KC - 1))
                        for k in range(KC)], reads=[b_h2T, Bn("wr")], writes=[self.pbuf[6]])
            S.op("act", lambda a: a.copy(out=lg[:], in_=self.psum[6][:, 0:36]), reads=[self.pbuf[6]], writes=[b_r_])
            R = lambda fn, extra_r=(), extra_w=(): S.op("dve", fn, reads=[b_r_] + list(extra_r), writes=[b_r_] + list(extra_w))
            R(lambda v: v.tensor_tensor(out=lg[:], in0=lg[:], in1=brb[:], op=ALU.add), extra_r=[Bn("brb")])
            R(lambda v: v.tensor_reduce(out=sm[:, 0:1], in_=lg[:, 0:4], axis=AX, op=ALU.max))
            R(lambda v: v.tensor_scalar(out=sm[:, 1:2], in0=sm[:, 0:1], scalar1=-1.0, scalar2=None, op0=ALU.mult))
            S.op("act", lambda a: a.activation(out=ge4[:], in_=lg[:, 0:4], func=AF.Exp, bias=sm[:, 1:2], accum_out=sm[:, 2:3]),
                 reads=[b_r_], writes=[b_r_])
            R(lambda v: v.reciprocal(out=sm[:, 3:4], in_=sm[:, 2:3]))
            R(lambda v: v.tensor_scalar(out=gm4[:], in0=lg[:, 0:4], scalar1=sm[:, 0:1], scalar2=None, op0=ALU.is_ge))
            R(lambda v: v.tensor_scalar(out=pen[:], in0=gm4[:], scalar1=1.0, scalar2=1e30, op0=ALU.subtract, op1=ALU.mult))
            R([lambda v, g=g: v.tensor_scalar(out=elm[:, g * 8:(g + 1) * 8], in0=lg[:, 4 + g * 8:4 + (g + 1) * 8], scalar1=pen[:, g:g + 1],
                                              scalar2=None, op0=ALU.add) for g in range(4)])
            R(lambda v: v.max(out=m8[:], in_=elm[:]))
            R(lambda v: v.tensor_scalar(out=A1[:], in0=elm[:], scalar1=m8[:, 0:1], scalar2=None, op0=ALU.is_equal))
            R(lambda v: v.tensor_scalar(out=A2[:], in0=elm[:], scalar1=m8[:, 1:2], scalar2=None, op0=ALU.is_equal))
            R(lambda v: v.tensor_tensor(out=sm[:, 4:5], in0=m8[:, 1:2], in1=m8[:, 0:1], op=ALU.subtract))
            S.op("act", lambda a: a.activation(out=sm[:, 5:6], in_=sm[:, 4:5], func=AF.Exp), reads=[b_r_], writes=[b_r_])
            R(lambda v: v.tensor_scalar(out=sm[:, 5:6], in0=sm[:, 5:6], scalar1=1.0, scalar2=None, op0=ALU.add))
            R(lambda v: v.reciprocal(out=sm[:, 6:7], in_=sm[:, 5:6]))
            R(lambda v, ti=ti: v.tensor_tensor(out=self.w_all[:, ti, 0:1], in0=sm[:, 3:4], in1=sm[:, 6:7], op=ALU.mult), extra_w=[b_w])
            R(lambda v, ti=ti: v.tensor_tensor(out=self.w_all[:, ti, 1:2], in0=sm[:, 3:4], in1=self.w_all[:, ti, 0:1], op=ALU.subtract),
              extra_r=[b_w], extra_w=[b_w])
            R(lambda v: v.tensor_tensor(out=A12[:], in0=A1[:], in1=A2[:], op=ALU.add))
            S.op("pe", [lambda t: t.matmul(self.psum[7][:, 0:32], lhsT=self.c32["lstrict"][:, :], rhs=A12[:], start=True, stop=True),
                        lambda t: t.matmul(self.psum[7][:, 32:64], lhsT=self.c32["ones"][:, :], rhs=A12[:], start=True, stop=True)],
                 reads=[b_r_, cb], writes=[self.pbuf[7]])
            S.op("act", lambda a: a.copy(out=posc[:], in_=self.psum[7][:, 0:64]), reads=[self.pbuf[7]], writes=[b_r_])
            R(lambda v: v.tensor_tensor(out=tpos[:], in0=posc[:, 0:32], in1=self.cnt_bc[:], op=ALU.add), extra_r=[b_cnt])
            R(lambda v: v.tensor_tensor(out=self.cnt_bc[:], in0=self.cnt_bc[:], in1=posc[:, 32:64], op=ALU.add), extra_r=[b_cnt], extra_w=[b_cnt])
            R(lambda v: v.tensor_scalar(out=tov[:], in0=tpos[:], scalar1=float(CAPS), scalar2=40000.0, op0=ALU.is_ge, op1=ALU.mult))
            R(lambda v: v.tensor_tensor(out=tsl[:], in0=tpos[:], in1=self.c32["slotbase"][:], op=ALU.add), extra_r=[cb])
            R(lambda v: v.tensor_tensor(out=tsl[:], in0=tsl[:], in1=tov[:], op=ALU.add))
            R(lambda v: v.tensor_tensor(out=A1[:], in0=A1[:], in1=tsl[:], op=ALU.mult))
            R(lambda v: v.tensor_tensor(out=A2[:], in0=A2[:], in1=tsl[:], op=ALU.mult))
            R(lambda v: v.tensor_reduce(out=idxf[:, 0:1], in_=A1[:], axis=AX, op=ALU.add))
            R(lambda v: v.tensor_reduce(out=idxf[:, 1:2], in_=A2[:], axis=AX, op=ALU.add))
            R(lambda v, ti=ti: v.tensor_copy(out=self.idx_all[:, ti, :], in_=idxf[:]), extra_w=[b_idx])
            for j in range(2):
                S.dma("pool", lambda g, ti=ti, j=j, i=i: g.indirect_dma_start(
                    out=self.xbuf_d[:, :], out_offset=bass.IndirectOffsetOnAxis(ap=self.idx_all[:, ti, j:j + 1], axis=0),
                    in_=self.xn[i][:, :], in_offset=None, bounds_check=S.rt["bnd"], oob_is_err=False),
                    f"scat{i}", reads=[self.xnb[i], b_idx], writes=[Bn(f"xbuf_sc{i}")])
            if ti == 0 and "lg" in self.debug:
                self.dbg_out("lg", lg[:], [P, 36], F32, [b_r_])
        S.op("dve", lambda v: v.tensor_copy(out=self.cnt_i[:], in_=self.cnt_bc[0:1, :]), reads=[b_cnt], writes=[Bn("cnt_i")])
        zt = self.sb("zt", [P, D], BF16)
        S.op("pool", lambda g: g.memset(zt[:], 0.0), writes=[Bn("zt")])
        ci = self.sb("ci", [P, N_EXP], I32)
        rf = self.sb("rf", [P, N_EXP], F32)
        zi = self.sb("zi", [P, N_EXP], I32)
        bz = Bn("ztmp")
        Z = lambda fn, extra_r=(): S.op("dve", fn, reads=[bz, b_cnt, cb] + list(extra_r), writes=[bz])
        Z(lambda v: v.tensor_copy(out=ci[:], in_=self.cnt_bc[:]))
        Z(lambda v: v.tensor_single_scalar(out=ci[:], in_=ci[:], scalar=127, op=ALU.bitwise_and))
        Z(lambda v: v.tensor_copy(out=rf[:], in_=ci[:]))
        Z(lambda v: v.tensor_scalar(out=rf[:], in0=rf[:], scalar1=self.c32["iotap"][:, 0:1], scalar2=128.0, op0=ALU.add, op1=ALU.is_ge))
        Z(lambda v: v.tensor_scalar(out=rf[:], in0=rf[:], scalar1=40000.0, scalar2=self.c32["iotap"][:, 0:1], op0=ALU.mult, op1=ALU.add))
        Z(lambda v: v.tensor_tensor(out=rf[:], in0=rf[:], in1=self.cnt_bc[:], op=ALU.add))
        Z(lambda v: v.tensor_tensor(out=rf[:], in0=rf[:], in1=self.c32["slotbase"][:], op=ALU.add))
        Z(lambda v: v.tensor_copy(out=zi[:], in_=rf[:]))
        import os
        for e in range(int(os.environ.get("NZS", N_EXP))):
            S.dma("pool", lambda g, e=e: g.indirect_dma_start(
                out=self.xbuf_d[:, :], out_offset=bass.IndirectOffsetOnAxis(ap=zi[:, e:e + 1], axis=0),
                in_=zt[:, :], in_offset=None, bounds_check=S.rt["bnd"], oob_is_err=False),
                "scatz", reads=[Bn("zt"), bz], writes=[b_xbuf])
        self.dbg_out("zi", zi[:], [P, N_EXP], I32, [bz])
        self.dbg_out("rf", rf[:], [P, N_EXP], F32, [bz])
        for nm, t_, shp, dt, bb in (("idx_all", self.idx_all, [P, NT, 2], I32, b_idx), ("w_all", self.w_all, [P, NT, 2], F32, b_w),
                                    ("cnt", self.cnt_bc, [P, N_EXP], F32, b_cnt)):
            self.dbg_out(nm, t_[:], shp, dt, [bb])
        if "x1" in self.debug:
            o = self.dout("dbg_x1", [NOWN, D], F32)
            for ti in range(NT):
                self.final_toks.append(S.dma("sp", lambda q, ti=ti, o=o: q.dma_start(out=o[ti * P:(ti + 1) * P, :], in_=self.x1_d[ti * P:(ti + 1) * P, :]),
                                             "dbg", reads=[b_x1]))
        self.pop_scope()

    def phase6(self):
        S = self.S
        Bn = self.B
        cb = Bn("consts")
        self.obuf_d = self.dscr("obuf_d", [N_EXP * CAPS, D], F32)
        b_ob = Bn("obuf_d")
        b_xbuf = Bn("xbuf_d")
        self.push_scope()
        NWB = 3
        wg = [self.sb(f"wg{i}", [P, KC, 512], BF16) for i in range(NWB)]
        wu = [self.sb(f"wu{i}", [P, KC, 512], BF16) for i in range(NWB)]
        wd = [self.sb(f"wd{i}", [P, 4, D], BF16) for i in range(NWB)]
        wsb = [Bn(f"wset{i}") for i in range(NWB)]
        Gs = [self.sb(f"G{i}", [P, D], BF16) for i in range(2)]
        Gb = [Bn(f"G{i}") for i in range(2)]
        hxT = self.sb("hxT", [P, KC, P], BF16)
        sg = [self.sb(f"sg{i}", [P, 512], F32) for i in range(2)]
        su = [self.sb(f"su{i}", [P, 512], F32) for i in range(2)]
        hid = self.sb("hid", [P, 1024], BF16)
        hidT = self.sb("hidT", [P, 8, P], BF16)
        ost = self.xin[0]
        idn = self.c16["ident"]
        dm = self.dummy
        dummies = {
            "pe": lambda t, k: t.matmul(self.psum[0][0:1, k:k + 1], lhsT=idn[:, 0:1], rhs=idn[:, 0:1], start=True, stop=True),
            "act": lambda a, k: a.copy(out=dm[0:1, k:k + 1], in_=dm[0:1, 7:8]),
            "dve": lambda v, k: v.memset(dm[32:33, k:k + 1], 0.0),
            "sp": lambda q, k: q.dma_start(out=dm[64:65, k:k + 1], in_=dm[64:65, 7:8]),
        }
        nsec = 0
        import os
        n_exp = int(os.environ.get("MOE_NEXP", N_EXP))
        dbank = (6, 7, 2, 3)
        for e in range(n_exp):
            gv = self.weg[e].rearrange("(k p) n -> p k n", p=P)
            uv = self.weu[e].rearrange("(k p) n -> p k n", p=P)
            wb_i = [(2 * e + half) % NWB for half in range(2)]
            for half in range(2):
                s_ = wb_i[half]
                dv = self.wed[e, half * 512:(half + 1) * 512, :].rearrange("(k p) n -> p k n", p=P)
                for c in range(2):
                    S.dma("pool", lambda q, s_=s_, gv=gv, half=half, c=c: q.dma_start(
                        out=wg[s_][:, :, c * 256:(c + 1) * 256], in_=gv[:, :, half * 512 + c * 256:half * 512 + (c + 1) * 256]),
                        f"wset{s_}", writes=[wsb[s_]])
                    S.dma("pool", lambda q, s_=s_, uv=uv, half=half, c=c: q.dma_start(
                        out=wu[s_][:, :, c * 256:(c + 1) * 256], in_=uv[:, :, half * 512 + c * 256:half * 512 + (c + 1) * 256]),
                        f"wset{s_}", writes=[wsb[s_]])
                for c in range(WD_SPLIT):
                    S.dma("pool", lambda q, s_=s_, dv=dv, c=c: q.dma_start(out=wd[s_][:, c:c + 1, :], in_=dv[:, c:c + 1, :]),
                          f"wset{s_}", writes=[wsb[s_]])
            S.reg_load("cnt", self.cnt_i[0:1, e:e + 1], Bn("cnt_i"))
            for j in range(int(os.environ.get("MOE_CAPB", CAPB))):
                r0 = e * CAPS + j * P
                S.begin_if("cnt", j * P)
                gi_ = nsec % 2
                nsec += 1
                G = Gs[gi_]
                S.dma("sp", lambda q, r0=r0, G=G: q.dma_start(out=G[:], in_=self.xbuf_d[r0:r0 + P, :]), f"G{gi_}", reads=[b_xbuf], writes=[Gb[gi_]])
                self.transpose_mod(G[:], Gb[gi_], hxT, Bn("hxT"), 2, 3, banks=(0, 1), plain=True)
                for half in range(2):
                    s_ = wb_i[half]
                    bg, bu = (2, 3) if half == 0 else (4, 5)
                    S.op("pe", [lambda t, k=k, s_=s_, bg=bg: t.matmul(self.psum[bg][:, :], lhsT=hxT[:, k, :], rhs=wg[s_][:, k, :], start=(k == 0), stop=(k == KC - 1))
                                for k in range(KC)] +
                               [lambda t, k=k, s_=s_, bu=bu: t.matmul(self.psum[bu][:, :], lhsT=hxT[:, k, :], rhs=wu[s_][:, k, :], start=(k == 0), stop=(k == KC - 1))
                                for k in range(KC)], reads=[Bn("hxT"), wsb[s_]], writes=[self.pbuf[bg], self.pbuf[bu]])
                    S.op("act", [lambda a, half=half, bg=bg: a.activation(out=sg[half][:], in_=self.psum[bg][:, :], func=AF.Silu),
                                 lambda a, half=half, bu=bu: a.copy(out=su[half][:], in_=self.psum[bu][:, :])],
                         reads=[self.pbuf[bg], self.pbuf[bu]], writes=[Bn(f"sgsu{half}")])
                    S.op("dve", lambda v, half=half: v.tensor_tensor(out=hid[:, half * 512:(half + 1) * 512], in0=sg[half][:], in1=su[half][:], op=ALU.mult),
                         reads=[Bn(f"sgsu{half}")], writes=[Bn(f"hid{half}")])
                pb0 = self.psum[0][:].bitcast(BF16)
                for half in range(2):
                    S.op("pe", [lambda t, c=c, pb0=pb0: t.transpose(out=pb0[:, c * P:(c + 1) * P], in_=hid[:, c * P:(c + 1) * P], identity=idn[:])
                                for c in range(4 * half, 4 * half + 4)], reads=[Bn(f"hid{half}"), cb], writes=[self.pbuf[0]])
                S.op("act", lambda a, pb0=pb0: a.copy(out=hidT[:].rearrange("p a b -> p (a b)"), in_=pb0[:, :]), reads=[self.pbuf[0]], writes=[Bn("hidT")])
                for n in range(4):
                    db = dbank[n]
                    S.op("pe", [lambda t, c=c, n=n, db=db, wb_i=tuple(wb_i): t.matmul(self.psum[db][:, :], lhsT=hidT[:, c, :], rhs=wd[wb_i[c // 4]][:, c % 4, n * 512:(n + 1) * 512],
                                                                    start=(c == 0), stop=(c == 7)) for c in range(8)],
                         reads=[Bn("hidT"), wsb[wb_i[0]], wsb[wb_i[1]]], writes=[self.pbuf[db]])
                    if n % 2 == 0:
                        S.op("act", lambda a, n=n, db=db: a.copy(out=ost[:, n * 512:(n + 1) * 512], in_=self.psum[db][:, :]), reads=[self.pbuf[db]], writes=[self.xinb[0]])
                    else:
                        S.op("dve", lambda v, n=n, db=db: v.tensor_copy(out=ost[:, n * 512:(n + 1) * 512], in_=self.psum[db][:, :]), reads=[self.pbuf[db]], writes=[self.xinb[0]])
                S.dma("act", lambda q, r0=r0: q.dma_start(out=self.obuf_d[r0:r0 + P, :], in_=ost[:]), "ost", reads=[self.xinb[0]], writes=[b_ob])
                S.end_if(dummies)
        self.pop_scope()

    def phase7(self):
        S = self.S
        Bn = self.B
        self.push_scope()
        bc = [self.sb(f"bcf{i}", [P, D], F32) for i in range(3)]
        bcb = [Bn(f"bcf{i}") for i in range(3)]
        S.dma("sp", lambda q: q.dma_start(out=bc[0][:], in_=self.mod_d[0, 5 * D:6 * D].partition_broadcast(P)), "bcf0",
              reads=[Bn("mod_d")], writes=[bcb[0]])
        S.dma("sp", lambda q: q.dma_start(out=bc[1][:], in_=self.ln2g.partition_broadcast(P)), "bcf1", writes=[bcb[1]])
        S.dma("sp", lambda q: q.dma_start(out=bc[2][:], in_=self.ln2b.partition_broadcast(P)), "bcf2", writes=[bcb[2]])
        og = [[self.sb(f"og{i}{j}", [P, D], F32) for j in range(2)] for i in range(2)]
        ogb = [[Bn(f"og{i}{j}") for j in range(2)] for i in range(2)]
        fsb = [self.sb(f"fsb{i}", [P, D], F32) for i in range(2)]
        fb = [Bn(f"fsb{i}") for i in range(2)]
        b_ob = Bn("obuf_d")
        for ti in range(NT):
            i = ti % 2
            xin, xb = self.xin[i], self.xinb[i]
            f_, b_f = fsb[i], fb[i]
            S.dma("sp", lambda q, xin=xin, ti=ti: q.dma_start(out=xin[:], in_=self.x1_d[ti * P:(ti + 1) * P, :]), f"xin{i}",
                  reads=[Bn("x1_d")], writes=[xb])
            for j in range(2):
                S.dma("pool", lambda g, i=i, j=j, ti=ti: g.indirect_dma_start(
                    out=og[i][j][:, :], out_offset=None, in_=self.obuf_d[:, :],
                    in_offset=bass.IndirectOffsetOnAxis(ap=self.idx_all[:, ti, j:j + 1], axis=0),
                    bounds_check=S.rt["bnd"], oob_is_err=False), f"og{i}{j}", reads=[b_ob, Bn("idx_all")], writes=[ogb[i][j]])
            S.op("dve", lambda v, ti=ti, i=i, f_=f_: v.tensor_scalar(out=f_[:], in0=og[i][0][:], scalar1=self.w_all[:, ti, 0:1], scalar2=None, op0=ALU.mult),
                 reads=[ogb[i][0], Bn("w_all")], writes=[b_f])
            S.op("dve", lambda v, ti=ti, i=i, f_=f_: v.scalar_tensor_tensor(out=f_[:], in0=og[i][1][:], scalar=self.w_all[:, ti, 1:2], in1=f_[:],
                                                                          op0=ALU.mult, op1=ALU.add), reads=[ogb[i][1], Bn("w_all"), b_f], writes=[b_f])
            S.op("pool", lambda g, f_=f_: g.tensor_tensor(out=f_[:], in0=f_[:], in1=bc[0][:], op=ALU.mult), reads=[b_f, bcb[0]], writes=[b_f])
            S.op("dve", lambda v, xin=xin, f_=f_: v.scalar_tensor_tensor(out=xin[:], in0=xin[:], scalar=float(ALPHA), in1=f_[:], op0=ALU.mult, op1=ALU.add),
                 reads=[xb, b_f], writes=[xb])
            self.ln_stats(xin, xb)
            S.op("dve", lambda v, xin=xin, f_=f_: v.tensor_scalar(out=f_[:], in0=xin[:], scalar1=self.mv[:, 0:1], scalar2=self.rstd[:, 0:1],
                                                                op0=ALU.subtract, op1=ALU.mult), reads=[xb, self.b_mv, self.b_rstd], writes=[b_f])
            S.op("pool", lambda g, f_=f_: g.tensor_tensor(out=f_[:], in0=f_[:], in1=bc[1][:], op=ALU.mult), reads=[b_f, bcb[1]], writes=[b_f])
            S.op("pool", lambda g, f_=f_: g.tensor_tensor(out=f_[:], in0=f_[:], in1=bc[2][:], op=ALU.add), reads=[b_f, bcb[2]], writes=[b_f])
            self.final_toks.append(S.dma("pool", lambda q, ti=ti, f_=f_: q.dma_start(out=self.y[ti * P:(ti + 1) * P, :], in_=f_[:]), f"yout{i}",
                                         reads=[b_f], writes=[Bn("y")]))
        self.pop_scope()

    def finish(self):
        S = self.S
        S.final_wait("sp", self.final_toks)
        with self.nc.Block() as block:
            S.emit(block)
        while self.scopes:
            self.scopes.pop().close()
        self.es.close()
        return self.nc


def build_program(debug=(), upto="all", n_other=NT):
    b = Builder(debug)
    b.n_other = n_other
    b.declare_inputs(with_experts=(upto in ("all", "moe")))
    b.load_consts()
    b.phase0()
    if upto == "p0":
        return b.finish(), b
    b.phase1()
    if upto == "p1":
        return b.finish(), b
    b.phase2()
    if upto == "p2":
        return b.finish(), b
    b.phase3()
    b.phase4()
    b.pop_scope()
    if upto == "p4":
        return b.finish(), b
    b.phase5()
    if upto == "p5":
        return b.finish(), b
    b.phase6()
    b.phase7()
    return b.finish(), b


def _win_layout(w_in, half):
    qa, ka, va, qb, kb, vb, rb, gb = 0, 1024, 1280, 1536, 2048, 2560, 3584, 4608
    out = np.zeros((D, WIN_COLS), np.float32)
    out[:, FM_QA * 128:FM_QA * 128 + 1024] = w_in[:, qa:qa + 1024]
    out[:, FM_KA * 128:FM_KA * 128 + 256] = w_in[:, ka:ka + 256]
    out[:, FM_QB * 128:FM_QB * 128 + 512] = w_in[:, qb:qb + 512]
    out[:, FM_KB * 128:FM_KB * 128 + 512] = w_in[:, kb:kb + 512]
    out[:, FM_RB * 128:FM_RB * 128 + 1024] = w_in[:, rb:rb + 1024]
    gF, gR = (gb, gb + 16) if half == 0 else (gb + 16, gb)
    out[:, GB_OFF:GB_OFF + 16] = w_in[:, gF:gF + 16]
    out[:, GB_OFF + 32:GB_OFF + 48] = w_in[:, gR:gR + 16]
    out[:, TM_OFF + TM_VA:TM_OFF + TM_VA + 256] = w_in[:, va:va + 256]
    out[:, TM_OFF + TM_KB:TM_OFF + TM_KB + 512] = w_in[:, kb:kb + 512]
    out[:, TM_OFF + TM_VB:TM_OFF + TM_VB + 1024] = w_in[:, vb:vb + 1024]
    return out


def prep_inputs(inp, cores=range(8)):
    f = lambda a: np.ascontiguousarray(np.asarray(a, dtype=np.float32))
    x, c, ctx, c_ctx = f(inp["x"]), f(inp["c"]), f(inp["ctx"]), f(inp["c_ctx"])
    w_ada, b_ada = f(inp["w_ada"])[0], f(inp["b_ada"])[0]
    w_in = f(inp["w_in"])[0]
    wgu_in, bg_in = f(inp["w_gate_up"])[0], f(inp["b_gate"])[0]
    consts = _consts()
    shared = {
        "w_ada": w_ada, "b_ada": b_ada, "sink": f(inp["attn_sink"])[0],
        "normw": np.ascontiguousarray(f(inp["gla_norm_w"])[0].reshape(8, P).T),
        "w_out": f(inp["w_out"])[0], "ln1g": f(inp["ln1_g"])[0], "ln1b": f(inp["ln1_b"])[0],
        "ln2g": f(inp["ln2_g"])[0], "ln2b": f(inp["ln2_b"])[0],
        "w_r": np.ascontiguousarray(np.concatenate([f(inp["w_router_group"])[0], f(inp["w_router_expert"])[0]], axis=1)),
        "b_r": np.ascontiguousarray(np.concatenate([f(inp["b_router_group"])[0], f(inp["b_router_expert"])[0]])),
        "weg": f(inp["w_exp_gate"])[0], "weu": f(inp["w_exp_up"])[0], "wed": f(inp["w_exp_down"])[0],
    }
    for k, v in consts.items():
        shared["c_" + k] = v
    per_half = {}
    for half in (0, 1):
        cosT, sinT = _rope_tables(half)
        wgu = np.zeros((2, 64, 512), np.float32)
        sF, sR = (0, 1) if half == 0 else (1, 0)
        wgu[0, 0:16] = wgu_in[sF]
        wgu[1, 32:48] = wgu_in[sR]
        bgate = np.ascontiguousarray(np.stack([bg_in[sF], bg_in[sR]]))
        per_half[half] = {"w_in": _win_layout(w_in, half), "wgu": wgu, "bgate": bgate, "cosT": cosT, "sinT": sinT}
    maps = []
    for core in cores:
        b, half = core // 2, core % 2
        xl = x[b] if half == 0 else x[b][::-1]
        cl = ctx[b] if half == 0 else ctx[b][::-1]
        ccv = np.stack([c[b].reshape(KC, P).T, c_ctx.reshape(KC, P).T], axis=2).reshape(P, 32)
        m = {"x_own": np.ascontiguousarray(xl[:NOWN]), "x_oth": np.ascontiguousarray(xl[NOWN:]),
             "ctxl": np.ascontiguousarray(cl), "cc": np.ascontiguousarray(ccv)}
        m.update(per_half[half])
        m.update(shared)
        maps.append(m)
    return maps


def kernel(**inputs):
    nc, b = build_program()
    maps = prep_inputs(inputs)
    res = run_bass_kernel_spmd(nc, maps, core_ids=list(range(8)))
    out = np.zeros((4, SEQ, D), np.float32)
    for core in range(8):
        bb, half = core // 2, core % 2
        yc = np.asarray(res.results[core]["y"])
        if half == 0:
            out[bb, :NOWN] = yc
        else:
            out[bb, NOWN:] = yc[::-1]
    return out
```
